# Optimizing a Trainium2 kernel written in Bass

```python
import math, functools
import jax, jax.numpy as jnp
from jax import lax
import numpy as np

D_MODEL = 2048
BATCH = 1
SEQ = 16384
DEPTH = 1
DEC_BATCH = 32
DEC_SEQ = 8
PAST_LEN = 16384
PAGE_SIZE = 128

EPS = 1e-6
NEG_INF = -1e30
CHUNK = 128
SGU_WIDTH = D_MODEL // 2
SGU_GROUPS = 8
SGU_GROUP_DIM = SGU_WIDTH // SGU_GROUPS
HEAD_DIM = 128
KV_HEADS = 8
DIL_PAIRS = ((128, 1), (512, 4), (2048, 16))
N_DIL = 3
Q_HEADS = N_DIL * KV_HEADS
MAX_WINDOW = 2048
KV_WIDTH = KV_HEADS * HEAD_DIM
ATT_WIDTH = KV_HEADS * HEAD_DIM
REL_BUCKETS = 32
REL_MAX_DIST = 2048
IN_WIDTH = 2 * SGU_WIDTH + Q_HEADS * HEAD_DIM + 2 * KV_WIDTH + SGU_WIDTH + ATT_WIDTH
MEM_TOKENS = 256
MEM_HEADS = 4
MEM_HEAD_DIM = D_MODEL // MEM_HEADS
MEM_WIDTH = MEM_HEADS * MEM_HEAD_DIM
PEER_HEADS = 8
PEER_N_KEYS = 128
PEER_KEY_DIM = 128
PEER_TOPK = 16
PEER_BLOCK = 128

kernel_name = "gated_gmlp_dilated_attn_peer_step"


def _rmsnorm(x, g):
    xf = x.astype(jnp.float32)
    y = xf * lax.rsqrt(jnp.mean(xf * xf, axis=-1, keepdims=True) + EPS)
    return (y * g.astype(jnp.float32)).astype(x.dtype)


def _layernorm(x, g, b):
    xf = x.astype(jnp.float32)
    mu = jnp.mean(xf, axis=-1, keepdims=True)
    var = jnp.mean(jnp.square(xf - mu), axis=-1, keepdims=True)
    y = (xf - mu) * lax.rsqrt(var + EPS) * g.astype(jnp.float32) + b.astype(jnp.float32)
    return y.astype(x.dtype)


def _t5_bucket(dist):
    max_exact = REL_BUCKETS // 2
    d = np.maximum(dist, 1).astype(np.float64)
    large = max_exact + (np.log(d / max_exact) / math.log(REL_MAX_DIST / max_exact)
                         * (REL_BUCKETS - max_exact)).astype(np.int64)
    large = np.minimum(large, REL_BUCKETS - 1)
    return np.where(dist < max_exact, dist, large).astype(np.int32)


def _chunk_mix(v, w_s, b_s):
    n = v.shape[2]
    w = jnp.tril(w_s[:, :n, :n])
    return jnp.einsum('gij,bcjgd->bcigd', w, v) + b_s[:, :n].T[None, None, :, :, None]


def _dilated_prompt(q, k, v, bias, dil, steps):
    B, S, H, Dh = q.shape
    span = dil * steps
    L = -(-S // span) * span
    pad = ((0, 0), (0, L - S), (0, 0), (0, 0))
    q, k, v = jnp.pad(q, pad), jnp.pad(k, pad), jnp.pad(v, pad)
    nb = L // span
    qb = q.reshape(B, nb, steps, dil, H, Dh)

    def with_prev(t):
        t = t.reshape(B, nb, steps, dil, H, Dh)
        prev = jnp.pad(t[:, :-1], ((0, 0), (1, 0), (0, 0), (0, 0), (0, 0), (0, 0)))
        return jnp.concatenate([prev, t], axis=2)

    kc, vc = with_prev(k), with_prev(v)
    logits = jnp.einsum('bnirhd,bnjrhd->bnrhij', qb, kc,
                        preferred_element_type=jnp.float32) * (HEAD_DIM ** -0.5)
    i = np.arange(steps)[:, None]
    j = np.arange(2 * steps)[None, :]
    off = i + steps - j
    band = (off >= 0) & (off <= steps)
    blk = np.arange(nb)[:, None, None]
    valid = band[None] & ((blk > 0) | (j >= steps)[None])
    bias_b = bias.astype(jnp.float32)[:, np.clip(off, 0, steps)]
    logits = jnp.where(valid[None, :, None, None], logits + bias_b[None, None, None], NEG_INF)
    lse = jax.nn.logsumexp(logits, axis=-1)
    p = jnp.exp(logits - lse[..., None])
    o = jnp.einsum('bnrhij,bnjrhd->bnirhd', p.astype(v.dtype), vc)
    o = o.reshape(B, L, H, Dh)[:, :S]
    lse = lse.transpose(0, 1, 4, 2, 3).reshape(B, L, H)[:, :S]
    return o, lse


def _dilated_sample(q, kc, vc, bias, dil, steps, n_past):
    T = q.shape[1]
    idx = n_past + np.arange(T)[:, None] - dil * np.arange(steps + 1)[None, :]
    valid = idx >= 0
    idx = np.maximum(idx, 0)
    kg = kc[:, idx]
    vg = vc[:, idx]
    logits = jnp.einsum('bthd,btkhd->bhtk', q, kg, preferred_element_type=jnp.float32) * (HEAD_DIM ** -0.5)
    logits = jnp.where(valid[None, None], logits + bias.astype(jnp.float32)[:, None, :], NEG_INF)
    lse = jax.nn.logsumexp(logits, axis=-1)
    p = jnp.exp(logits - lse[..., None])
    o = jnp.einsum('bhtk,btkhd->bthd', p.astype(vc.dtype), vg)
    return o, lse.transpose(0, 2, 1)


def _dilated_attention(q, k, v, rel_bias, attend):
    outs, lses = [], []
    for g, (win, dil) in enumerate(DIL_PAIRS):
        steps = win // dil
        bucket = _t5_bucket(dil * np.arange(steps + 1))
        bias = rel_bias[bucket, g * KV_HEADS:(g + 1) * KV_HEADS].T
        o, l = attend(q[:, :, g], k, v, bias, dil, steps)
        outs.append(o)
        lses.append(l)
    w = jax.nn.softmax(jnp.stack(lses), axis=0)
    o = jnp.einsum('gbsh,gbshd->bshd', w.astype(v.dtype), jnp.stack(outs))
    B, S = q.shape[:2]
    return o.reshape(B, S, ATT_WIDTH)


def _memory_kv(mem, g, w_mk, w_mv):
    m = _rmsnorm(mem, g)
    B, M, _ = mem.shape
    mk = (m @ w_mk).reshape(B, M, MEM_HEADS, MEM_HEAD_DIM)
    mv = (m @ w_mv).reshape(B, M, MEM_HEADS, MEM_HEAD_DIM)
    return jnp.stack([mk, mv], axis=2)


def _memory_attention(h, mem_kv, w_mq, w_mo):
    B, S, _ = h.shape
    q = (h @ w_mq).reshape(B, S, MEM_HEADS, MEM_HEAD_DIM)
    logits = jnp.einsum('bshd,bmhd->bhsm', q, mem_kv[:, :, 0],
                        preferred_element_type=jnp.float32) * (MEM_HEAD_DIM ** -0.5)
    p = jax.nn.softmax(logits, axis=-1)
    o = jnp.einsum('bhsm,bmhd->bshd', p.astype(h.dtype), mem_kv[:, :, 1])
    return o.reshape(B, S, MEM_WIDTH) @ w_mo


def _peer(h, w_pq, keys1, keys2, u_tab, v_tab):
    B, S, D = h.shape
    n = B * S
    npad = -(-n // PEER_BLOCK) * PEER_BLOCK
    xb = jnp.pad(h.reshape(n, D), ((0, npad - n), (0, 0))).reshape(npad // PEER_BLOCK, PEER_BLOCK, D)

    def block(xt):
        q = (xt @ w_pq).reshape(PEER_BLOCK, PEER_HEADS, 2, PEER_KEY_DIM)
        s1 = jnp.einsum('thd,hkd->thk', q[:, :, 0], keys1, preferred_element_type=jnp.float32)
        s2 = jnp.einsum('thd,hkd->thk', q[:, :, 1], keys2, preferred_element_type=jnp.float32)
        v1, i1 = lax.top_k(s1, PEER_TOPK)
        v2, i2 = lax.top_k(s2, PEER_TOPK)
        cand = (v1[..., :, None] + v2[..., None, :]).reshape(PEER_BLOCK, PEER_HEADS, PEER_TOPK * PEER_TOPK)
        cidx = (i1[..., :, None] * PEER_N_KEYS + i2[..., None, :]).reshape(PEER_BLOCK, PEER_HEADS, PEER_TOPK * PEER_TOPK)
        best, pos = lax.top_k(cand, PEER_TOPK)
        eidx = jnp.take_along_axis(cidx, pos, axis=-1)
        gate = jax.nn.softmax(best, axis=-1)
        ue = u_tab[eidx]
        ve = v_tab[eidx]
        a = jax.nn.gelu(jnp.einsum('td,thkd->thk', xt, ue, preferred_element_type=jnp.float32))
        return jnp.einsum('thk,thkd->td', (gate * a).astype(xt.dtype), ve)

    y = lax.map(block, xb).reshape(npad, D)[:n]
    return y.reshape(B, S, D)


def _layer(x, mem_kv, cache_win, lw, rel_bias):
    B, S, _ = x.shape
    h = _rmsnorm(x, lw['norm_mix'])
    z = h @ lw['w_in']
    splits = np.cumsum([SGU_WIDTH, SGU_WIDTH, Q_HEADS * HEAD_DIM, KV_WIDTH, KV_WIDTH, SGU_WIDTH]).tolist()
    u, vs, q, k, v, ga, gb = jnp.split(z, splits, axis=-1)
    u = jax.nn.gelu(u)
    vs = _layernorm(jax.nn.gelu(vs), lw['sgu_ln_g'], lw['sgu_ln_b'])
    n = min(S, CHUNK)
    vch = vs.reshape(B, S // n, n, SGU_GROUPS, SGU_GROUP_DIM)
    ya = u * _chunk_mix(vch, lw['sgu_w'], lw['sgu_b']).reshape(B, S, SGU_WIDTH)
    q = q.reshape(B, S, N_DIL, KV_HEADS, HEAD_DIM)
    k = k.reshape(B, S, KV_HEADS, HEAD_DIM)
    v = v.reshape(B, S, KV_HEADS, HEAD_DIM)
    new_kv = jnp.stack([k, v], axis=2)
    if cache_win is None:
        yb = _dilated_attention(q, k, v, rel_bias, _dilated_prompt)
        win_state = new_kv[:, -min(MAX_WINDOW, S):]
    else:
        kc = jnp.concatenate([cache_win[:, :, 0], k], axis=1)
        vc = jnp.concatenate([cache_win[:, :, 1], v], axis=1)
        attend = functools.partial(_dilated_sample, n_past=cache_win.shape[1])
        yb = _dilated_attention(q, kc, vc, rel_bias, attend)
        win_state = new_kv
    merged = jnp.concatenate([jax.nn.sigmoid(ga) * ya, jax.nn.sigmoid(gb) * yb], axis=-1)
    x = x + merged @ lw['w_out']
    x = x + _memory_attention(_rmsnorm(x, lw['norm_mem']), mem_kv, lw['w_mq'], lw['w_mo'])
    h = _rmsnorm(x, lw['norm_peer'])
    x = x + _peer(h, lw['peer_wq'], lw['peer_keys1'], lw['peer_keys2'], lw['peer_u'], lw['peer_v'])
    return x, win_state, vs


def setup_inputs(seed: int = 0) -> dict:
    key = jax.random.key(seed)
    ks = jax.random.split(key, 32)

    def nrm(k, shape, scale):
        return jax.random.normal(k, shape, jnp.float32) * scale

    def gain(k, shape):
        return 1.0 + nrm(k, shape, 0.02)

    win_rows = min(MAX_WINDOW, PAST_LEN)
    return {
        'x_prompt': nrm(ks[0], (BATCH, SEQ, D_MODEL), 1.0),
        'x_sample': nrm(ks[1], (DEC_BATCH, DEC_SEQ, D_MODEL), 1.0),
        'mem_prompt': nrm(ks[2], (BATCH, MEM_TOKENS, D_MODEL), 1.0),
        'cache_win': nrm(ks[3], (DEPTH, DEC_BATCH, win_rows, 2, KV_HEADS, HEAD_DIM), 1.0),
        'cache_mem_kv': nrm(ks[4], (DEPTH, DEC_BATCH, MEM_TOKENS, 2, MEM_HEADS, MEM_HEAD_DIM), 1.0),
        'rel_bias': nrm(ks[5], (REL_BUCKETS, Q_HEADS), 0.5),
        'norm_mix': gain(ks[6], (DEPTH, D_MODEL)),
        'w_in': nrm(ks[7], (DEPTH, D_MODEL, IN_WIDTH), D_MODEL ** -0.5),
        'sgu_ln_g': gain(ks[8], (DEPTH, SGU_WIDTH)),
        'sgu_ln_b': nrm(ks[9], (DEPTH, SGU_WIDTH), 0.02),
        'sgu_w': nrm(ks[10], (DEPTH, SGU_GROUPS, CHUNK, CHUNK), CHUNK ** -0.5),
        'sgu_b': 1.0 + nrm(ks[11], (DEPTH, SGU_GROUPS, CHUNK), 0.02),
        'w_out': nrm(ks[12], (DEPTH, SGU_WIDTH + ATT_WIDTH, D_MODEL), (SGU_WIDTH + ATT_WIDTH) ** -0.5),
        'norm_mem': gain(ks[13], (DEPTH, D_MODEL)),
        'norm_memtok': gain(ks[14], (DEPTH, D_MODEL)),
        'w_mq': nrm(ks[15], (DEPTH, D_MODEL, MEM_WIDTH), D_MODEL ** -0.5),
        'w_mk': nrm(ks[16], (DEPTH, D_MODEL, MEM_WIDTH), D_MODEL ** -0.5),
        'w_mv': nrm(ks[17], (DEPTH, D_MODEL, MEM_WIDTH), D_MODEL ** -0.5),
        'w_mo': nrm(ks[18], (DEPTH, MEM_WIDTH, D_MODEL), MEM_WIDTH ** -0.5),
        'norm_peer': gain(ks[19], (DEPTH, D_MODEL)),
        'peer_wq': nrm(ks[20], (DEPTH, D_MODEL, PEER_HEADS * 2 * PEER_KEY_DIM), D_MODEL ** -0.5),
        'peer_keys1': nrm(ks[21], (DEPTH, PEER_HEADS, PEER_N_KEYS, PEER_KEY_DIM), PEER_KEY_DIM ** -0.5),
        'peer_keys2': nrm(ks[22], (DEPTH, PEER_HEADS, PEER_N_KEYS, PEER_KEY_DIM), PEER_KEY_DIM ** -0.5),
        'peer_u': nrm(ks[23], (DEPTH, PEER_N_KEYS * PEER_N_KEYS, D_MODEL), D_MODEL ** -0.5),
        'peer_v': nrm(ks[24], (DEPTH, PEER_N_KEYS * PEER_N_KEYS, D_MODEL), 0.3),
        'norm_final': gain(ks[25], (D_MODEL,)),
    }


def reference(x_prompt, x_sample, mem_prompt, cache_win, cache_mem_kv, rel_bias, norm_mix, w_in,
              sgu_ln_g, sgu_ln_b, sgu_w, sgu_b, w_out, norm_mem, norm_memtok, w_mq, w_mk, w_mv, w_mo,
              norm_peer, peer_wq, peer_keys1, peer_keys2, peer_u, peer_v, norm_final):
    yp, ys = x_prompt, x_sample
    win_p, mem_p, win_s, sgu_s = [], [], [], []
    for l in range(DEPTH):
        lw = dict(norm_mix=norm_mix[l], w_in=w_in[l], sgu_ln_g=sgu_ln_g[l], sgu_ln_b=sgu_ln_b[l],
                  sgu_w=sgu_w[l], sgu_b=sgu_b[l], w_out=w_out[l], norm_mem=norm_mem[l],
                  w_mq=w_mq[l], w_mo=w_mo[l], norm_peer=norm_peer[l], peer_wq=peer_wq[l],
                  peer_keys1=peer_keys1[l], peer_keys2=peer_keys2[l], peer_u=peer_u[l], peer_v=peer_v[l])
        mkv = _memory_kv(mem_prompt, norm_memtok[l], w_mk[l], w_mv[l])
        yp, wp, _ = _layer(yp, mkv, None, lw, rel_bias)
        ys, wsmp, vsmp = _layer(ys, cache_mem_kv[l], cache_win[l], lw, rel_bias)
        win_p.append(wp)
        mem_p.append(mkv)
        win_s.append(wsmp)
        sgu_s.append(vsmp)
    y_prompt = _rmsnorm(yp, norm_final)
    y_sample = _rmsnorm(ys, norm_final)
    return (y_prompt, y_sample, jnp.stack(win_p), jnp.stack(mem_p), jnp.stack(win_s), jnp.stack(sgu_s))
```

```python
import contextlib
import math
import numpy as np
import concourse.bass as bass
import concourse.mybir as mybir
from concourse.bass_utils import run_bass_kernel_spmd

F32 = mybir.dt.float32
BF16 = mybir.dt.bfloat16
I32 = mybir.dt.int32
U32 = mybir.dt.uint32
AF = mybir.ActivationFunctionType
ALU = mybir.AluOpType

NCORES = 8
D = 2048
TOK = 2048
NT = TOK // 128
SP = 32
EPS = 1e-6
DILS = (1, 4, 16)
REL_BUCKETS = 32
REL_MAX_DIST = 2048
ATT_SCALE = 128 ** -0.5
MEM_SCALE = 512 ** -0.5
NEG = -1e30


class Res:
    __slots__ = ("w", "r")

    def __init__(self):
        self.w = None
        self.r = {}


class Ctx:
    NDMA = 32

    def __init__(self, nc, es):
        self.nc = nc
        self.es = es
        self.eng = {"pe": nc.tensor, "act": nc.scalar, "dve": nc.vector, "pool": nc.gpsimd, "sp": nc.sync}
        self.sem = {}
        self.cnt = {}
        self.known = {e: {} for e in self.eng}
        for e in self.eng:
            self.sem[e] = es.enter_context(nc.semaphore("s_" + e))
            self.cnt[e] = 0
        self.dsem = [es.enter_context(nc.semaphore("d%d" % i)) for i in range(self.NDMA)]
        self.dtgt = [0] * self.NDMA
        self.drr = 0
        self.nop = 0
        self.muted = False
        self.NSW = 8
        self.swsem = [es.enter_context(nc.semaphore("w%d" % i)) for i in range(self.NSW)]
        self.swtgt = [0] * self.NSW
        self.swrr = 0

    def sb(self, es, name, shape, dt):
        return es.enter_context(self.nc.sbuf_tensor(name, list(shape), dt))

    def _semof(self, key):
        if isinstance(key, str):
            return self.sem[key]
        if isinstance(key, tuple):
            return self.swsem[key[1]]
        return self.dsem[key]

    def swdma(self, out, in_, reads=(), writes=(), indirect=None, **kw):
        if self.muted:
            return None
        i = self.swrr % self.NSW
        self.swrr += 1
        deps = self._deps(reads, writes)
        if self.swtgt[i]:
            deps.append((("w", i), self.swtgt[i]))
        self._wait("pool", deps)
        self.swtgt[i] += 16
        if indirect is not None:
            inst = self.eng["pool"].indirect_dma_start(out=out, out_offset=None, in_=in_, in_offset=indirect, **kw)
        else:
            inst = self.eng["pool"].dma_start(out=out, in_=in_, **kw)
        inst.then_inc(self.swsem[i], 16)
        tok = (("w", i), self.swtgt[i])
        self._mark(tok, reads, writes)
        self.nop += 1
        return tok

    def _wait(self, e, deps):
        need = {}
        for tok in deps:
            if tok is None:
                continue
            k, v = tok
            if self.known[e].get(k, 0) >= v:
                continue
            if need.get(k, 0) < v:
                need[k] = v
        for k, v in need.items():
            self.eng[e].wait_ge(self._semof(k), v)
            self.known[e][k] = v

    @staticmethod
    def _deps(reads, writes):
        deps = []
        for r in reads:
            deps.append(r.w)
        for r in writes:
            deps.append(r.w)
            for k, v in r.r.items():
                deps.append((k, v))
        return deps

    @staticmethod
    def _mark(tok, reads, writes):
        k, v = tok
        for r in reads:
            if r.r.get(k, 0) < v:
                r.r[k] = v
        for r in writes:
            r.w = tok
            r.r = {}

    def op(self, e, fn, reads=(), writes=()):
        if self.muted:
            return None
        self._wait(e, self._deps(reads, writes))
        inst = fn(self.eng[e])
        self.cnt[e] += 1
        inst.then_inc(self.sem[e], 1)
        tok = (e, self.cnt[e])
        self._mark(tok, reads, writes)
        self.nop += 1
        return tok

    def dma(self, q, out, in_, reads=(), writes=(), indirect=None, **kw):
        if self.muted:
            return None
        i = self.drr % self.NDMA
        self.drr += 1
        deps = self._deps(reads, writes)
        if self.dtgt[i]:
            deps.append((i, self.dtgt[i]))
        self._wait(q, deps)
        self.dtgt[i] += 16
        if indirect is not None:
            inst = self.eng[q].indirect_dma_start(out=out, out_offset=None, in_=in_, in_offset=indirect, **kw)
        else:
            inst = self.eng[q].dma_start(out=out, in_=in_, **kw)
        inst.then_inc(self.dsem[i], 16)
        tok = (i, self.dtgt[i])
        self._mark(tok, reads, writes)
        self.nop += 1
        return tok

    def _swtoks(self):
        return [(("w", i), t) for i, t in enumerate(self.swtgt) if t]

    def barrier(self):
        self.muted = False
        deps = [(i, t) for i, t in enumerate(self.dtgt) if t] + self._swtoks()
        deps += [(e, n) for e, n in self.cnt.items() if n]
        for e in self.eng:
            self._wait(e, [d for d in deps if d[0] != e])

    def finish(self):
        deps = [(i, t) for i, t in enumerate(self.dtgt) if t] + self._swtoks()
        deps += [(e, n) for e, n in self.cnt.items() if n and e != "sp"]
        self._wait("sp", deps)


def _t5_bucket(dist):
    max_exact = REL_BUCKETS // 2
    d = np.maximum(dist, 1).astype(np.float64)
    large = max_exact + (np.log(d / max_exact) / math.log(REL_MAX_DIST / max_exact)
                         * (REL_BUCKETS - max_exact)).astype(np.int64)
    large = np.minimum(large, REL_BUCKETS - 1)
    return np.where(dist < max_exact, dist, large).astype(np.int32)


class _SkipPhase(Exception):
    pass


_PH = {0, 1, 2, 3, 4, 5, 6}
_LIM = {"groups": None, "heads": None, "groups5": None}


def build_program():
    nc = bass.Bass("TRN2", target_bir_lowering=False)

    def din(name, shape, dt=F32):
        return nc.dram_tensor(name, list(shape), dt, kind="ExternalInput").ap()

    def dout(name, shape, dt=F32):
        return nc.dram_tensor(name, list(shape), dt, kind="ExternalOutput").ap()

    def dscr(name, shape, dt=BF16):
        return nc.dram_tensor(name, list(shape), dt, kind="Internal").ap()

    xo = din("xo", [TOK, D])
    xh = din("xh", [TOK, D])
    xs = din("xs", [SP, D])
    mem = din("mem", [256, D])
    cwin = din("cwin", [4, 2048, 2, 8, 128])
    cmem = din("cmem", [4, 256, 2, 2048])
    w_in = din("w_in", [D, 9216])
    wsq = {n: din(n, [D, D]) for n in ("w_out", "w_mq", "w_mk", "w_mv", "w_mo", "peer_wq")}
    peer_u = din("peer_u", [16384, D])
    peer_v = din("peer_v", [16384, D])
    keys1 = din("keys1", [8, 128, 128])
    keys2 = din("keys2", [8, 128, 128])
    gains = {n: din(n, [1, D]) for n in ("norm_mix", "norm_mem", "norm_memtok", "norm_peer", "norm_final")}
    sgu_ln_g = din("sgu_ln_g", [1, 1024])
    sgu_ln_b = din("sgu_ln_b", [1, 1024])
    sgu_wT = din("sgu_wT", [8, 128, 128])
    sgu_wTs = din("sgu_wTs", [8, 128, 128])
    sgu_bT = din("sgu_bT", [128, 8])
    sgu_bTs = din("sgu_bTs", [128, 8])
    biasT = din("biasT", [24, 128, 2, 128])
    bandmask = din("bandmask", [128, 2, 128])
    sbias = din("sbias", [8, 128, 16, 24])
    smask = din("smask", [128, 16, 24])
    sbias_n = din("sbias_n", [8, 32, 4, 24])
    smask_n = din("smask_n", [32, 4, 24])
    pflag = din("pflag", [128, 1])

    y_p = dout("y_p", [TOK, D])
    y_s = dout("y_s", [SP, D])
    win_p = dout("win_p", [TOK, 2, 1024])
    memkv = dout("memkv", [256, 2, 2048])
    win_s = dout("win_s", [SP, 2, 1024])
    sgu_s = dout("sgu_s", [SP, 1024])

    WBLK = {"w_in": 18, "w_out": 4, "w_mq": 4, "w_mk": 4, "w_mv": 4, "w_mo": 4, "peer_wq": 4}
    wscr = {n: dscr("wb_" + n, [k, 128, 16, 512]) for n, k in WBLK.items()}
    wscr_res = {n: [Res() for _ in range(k)] for n, k in WBLK.items()}
    qTs = dscr("qTs", [24, 128, TOK])
    kTs = dscr("kTs", [8, 128, 2 * TOK])
    vTs = dscr("vTs", [8, 128, 2 * TOK])
    gbTs = dscr("gbTs", [8, 128, TOK])
    mTs = dscr("mTs", [16, 128, TOK])
    r_qTs = [Res() for _ in range(24)]
    r_kTs = [Res() for _ in range(8)]
    r_vTs = [Res() for _ in range(8)]
    r_gbTs = [Res() for _ in range(8)]
    r_mTs = [Res() for _ in range(16)]

    with contextlib.ExitStack() as es:
        c = Ctx(nc, es)

        pb = [es.enter_context(nc.psum_tensor("pb%d" % i, [128, 512], F32)) for i in range(8)]
        rpb = [Res() for _ in range(8)]
        pbb = [p[:].bitcast(BF16) for p in pb]

        identf = c.sb(es, "identf", [128, 128], F32)
        ident = c.sb(es, "ident", [128, 128], BF16)
        ones = c.sb(es, "ones", [128, 128], BF16)
        r_const = Res()
        c.op("pool", lambda e: e.memset(identf[:], 0.0), writes=[r_const])
        c.op("pool", lambda e: e.affine_select(out=identf[:], in_=identf[:], pattern=[[-1, 128]],
                                               compare_op=ALU.not_equal, fill=1.0, base=0,
                                               channel_multiplier=1), reads=[r_const], writes=[r_const])
        c.op("dve", lambda e: e.tensor_copy(out=ident[:], in_=identf[:]), reads=[r_const], writes=[r_const])
        c.op("dve", lambda e: e.memset(ones[:], 1.0), writes=[r_const])

        def cut(k):
            if _LIM.get("cut") == k:
                c.muted = True

        wpool = {"buf": [], "res": [], "n": 0}
        wctr = [0]

        def make_wpool(es_, n):
            tag = "%d" % len(wpool.setdefault("gen", []))
            wpool["gen"].append(n)
            wpool["buf"] = [c.sb(es_, "wbuf%s_%d" % (tag, i), [128, 16, 512], BF16) for i in range(n)]
            wpool["res"] = [Res() for _ in range(n)]
            wpool["n"] = n
            wctr[0] = 0

        class WStream:
            def __init__(self, blocks):
                self.blocks = list(blocks)
                self.issued = 0
                self.pos = 0
                self.base = wctr[0]

            def _issue(self):
                name, blk = self.blocks[self.issued]
                i = (self.base + self.issued) % wpool["n"]
                c.dma("sp", wpool["buf"][i][:], wscr[name][blk], reads=[wscr_res[name][blk]],
                      writes=[wpool["res"][i]])
                self.issued += 1

            def get(self):
                while self.issued < len(self.blocks) and self.issued < self.pos + wpool["n"]:
                    self._issue()
                i = (self.base + self.pos) % wpool["n"]
                self.pos += 1
                wctr[0] = self.base + self.pos
                return wpool["buf"][i], wpool["res"][i]

        def load_gain(es_, name, ap, width=D):
            t = c.sb(es_, "g_" + name, [128, width], F32)
            r = Res()
            c.dma("sp", t[:], ap[0:1, :].partition_broadcast(128), writes=[r])
            return t, r

        bankctr = [0]

        def bank(lo, hi):
            n = hi - lo
            i = lo + bankctr[0] % n
            bankctr[0] += 1
            return i

        NSM = 4
        sm = [c.sb(es, "sm%d" % i, [128, 8], F32) for i in range(NSM)]
        r_sm = [Res() for _ in range(NSM)]
        smctr = [0]
        junk = c.sb(es, "junk", [128, D], BF16)
        r_junk = Res()

        def rms_h(x_t, r_x, P, gain_t, r_gain, h_t, r_h):
            i = smctr[0] % NSM
            smctr[0] += 1
            s, rs = sm[i], r_sm[i]
            c.op("act", lambda e: e.activation(out=junk[:P, :], in_=x_t[:P, :], func=AF.Square,
                                               accum_out=s[:P, 0:1]), reads=[r_x], writes=[r_junk, rs])
            c.op("act", lambda e: e.activation(out=s[:P, 1:2], in_=s[:P, 0:1], func=AF.Sqrt,
                                               scale=1.0 / D, bias=epsc[:P, 0:1]), reads=[rs, r_const], writes=[rs])
            c.op("dve", lambda e: e.reciprocal(out=s[:P, 2:3], in_=s[:P, 1:2]), reads=[rs], writes=[rs])
            c.op("dve", lambda e: e.scalar_tensor_tensor(out=h_t[:P, :], in0=x_t[:P, :], scalar=s[:P, 2:3],
                                                         in1=gain_t[:P, :], op0=ALU.mult, op1=ALU.mult),
                 reads=[r_x, rs, r_gain], writes=[r_h])

        epsc = c.sb(es, "epsc", [128, 1], F32)
        c.op("dve", lambda e: e.memset(epsc[:], EPS), writes=[r_const])

        def transpose_to(h_t, r_h, P, nchunk, dst_fn, r_dst, evac="act"):
            for c0 in range(0, nchunk, 8):
                c1 = min(nchunk, c0 + 8)
                b = bank(0, 2)
                pv = pbb[b][:, 0:(c1 - c0) * 128].rearrange("p (a t) -> p a t", t=128)

                def f(e, c0=c0, c1=c1, pv=pv):
                    inst = None
                    for k in range(c0, c1):
                        inst = e.transpose(out=pv[:, k - c0, 0:P], in_=h_t[:P, k * 128:(k + 1) * 128],
                                           identity=ident[:P, :P])
                    return inst
                c.op("pe", f, reads=[r_h, r_const], writes=[rpb[b]])
                dst = dst_fn(c0, c1)
                if evac == "act":
                    c.op("act", lambda e, dst=dst, pv=pv: e.activation(out=dst, in_=pv[:, :, 0:P], func=AF.Copy),
                         reads=[rpb[b]], writes=[r_dst])
                else:
                    c.op("dve", lambda e, dst=dst, pv=pv: e.tensor_copy(out=dst, in_=pv[:, :, 0:P]),
                         reads=[rpb[b]], writes=[r_dst])

        def mm_tok(hT, r_hT, t0, P, wt, r_w, b, ncol=512, c0=0):
            def f(e):
                inst = None
                for kc in range(16):
                    inst = e.matmul(pb[b][:P, 0:ncol], lhsT=hT[:, kc, t0:t0 + P], rhs=wt[:, kc, c0:c0 + ncol],
                                    start=(kc == 0), stop=(kc == 15))
                return inst
            c.op("pe", f, reads=[r_hT, r_w], writes=[rpb[b]])

        def mm_feat(hT, r_hT, t0, ntok, wt, r_w, ct, b):
            def f(e):
                inst = None
                for kc in range(16):
                    inst = e.matmul(pb[b][:, 0:ntok], lhsT=wt[:, kc, ct * 128:(ct + 1) * 128],
                                    rhs=hT[:, kc, t0:t0 + ntok], start=(kc == 0), stop=(kc == 15))
                return inst
            c.op("pe", f, reads=[r_hT, r_w], writes=[rpb[b]])

        with contextlib.suppress(_SkipPhase), contextlib.ExitStack() as es0:
            if 0 not in _PH:
                raise _SkipPhase()
            NST = 3
            stf = [c.sb(es0, "stf%d" % i, [128, 16, 512], F32) for i in range(NST)]
            stb = [c.sb(es0, "stb%d" % i, [128, 16, 512], BF16) for i in range(NST)]
            r_stf = [Res() for _ in range(NST)]
            r_stb = [Res() for _ in range(NST)]
            n = 0
            wlist = [("w_mk", wsq["w_mk"]), ("w_mv", wsq["w_mv"]), ("w_in", w_in), ("w_out", wsq["w_out"]),
                     ("w_mq", wsq["w_mq"]), ("w_mo", wsq["w_mo"]), ("peer_wq", wsq["peer_wq"])]
            for name, W in wlist:
                Wv = W.rearrange("(kc p) n -> p kc n", p=128)
                for blk in range(WBLK[name]):
                    i = n % NST
                    c.dma("sp", stf[i][:], Wv[:, :, blk * 512:(blk + 1) * 512], writes=[r_stf[i]])
                    if n % 2 == 0:
                        c.op("dve", lambda e, i=i: e.tensor_copy(out=stb[i][:], in_=stf[i][:]),
                             reads=[r_stf[i]], writes=[r_stb[i]])
                    else:
                        c.op("pool", lambda e, i=i: e.tensor_copy(out=stb[i][:], in_=stf[i][:]),
                             reads=[r_stf[i]], writes=[r_stb[i]])
                    c.dma("act", wscr[name][blk], stb[i][:], reads=[r_stb[i]], writes=[wscr_res[name][blk]])
                    n += 1
            c.barrier()

        mkT = c.sb(es, "mkT", [128, 16, 256], BF16)
        mv_bf = c.sb(es, "mv_bf", [128, 2, D], BF16)
        r_mkT = Res()
        r_mvbf = Res()

        with contextlib.suppress(_SkipPhase), contextlib.ExitStack() as es1:
            if 1 not in _PH:
                raise _SkipPhase()
            make_wpool(es1, 3)
            g_mt, r_gmt = load_gain(es1, "memtok", gains["norm_memtok"])
            mT = c.sb(es1, "mT", [128, 16, 256], BF16)
            r_mT = Res()
            xm = c.sb(es1, "xm", [128, D], F32)
            r_xm = Res()
            hm = c.sb(es1, "hm1", [128, D], BF16)
            r_hm = Res()
            mk_bf = c.sb(es1, "mk_bf", [128, 2, D], BF16)
            r_mkbf = Res()
            kvf = [c.sb(es1, "kvf%d" % i, [128, D], F32) for i in range(2)]
            r_kvf = [Res() for _ in range(2)]
            for mt in range(2):
                c.dma("sp", xm[:], mem[mt * 128:(mt + 1) * 128, :], writes=[r_xm])
                rms_h(xm, r_xm, 128, g_mt, r_gmt, hm, r_hm)
                transpose_to(hm, r_hm, 128, 16, lambda c0, c1, mt=mt: mT[:, c0:c1, mt * 128:(mt + 1) * 128], r_mT)
            n = 0
            for kv, name in enumerate(("w_mk", "w_mv")):
                bf = mk_bf if kv == 0 else mv_bf
                r_bf = r_mkbf if kv == 0 else r_mvbf
                for mt in range(2):
                    i = n % 2
                    n += 1
                    ws = WStream([(name, blk) for blk in range(4)])
                    for blk in range(4):
                        wt, r_w = ws.get()
                        b = bank(2, 6)
                        mm_tok(mT, r_mT, mt * 128, 128, wt, r_w, b)
                        c.op("act", lambda e, i=i, blk=blk, b=b: e.activation(
                            out=kvf[i][:, blk * 512:(blk + 1) * 512], in_=pb[b][:, :], func=AF.Copy),
                            reads=[rpb[b]], writes=[r_kvf[i]])
                    c.op("dve", lambda e, i=i, mt=mt, bf=bf: e.tensor_copy(out=bf[:, mt, :], in_=kvf[i][:]),
                         reads=[r_kvf[i]], writes=[r_bf])
                    c.dma("sp", memkv[mt * 128:(mt + 1) * 128, kv, :], kvf[i][:], reads=[r_kvf[i]])
            for mt in range(2):
                transpose_to(mk_bf[:, mt, :], r_mkbf, 128, 16,
                             lambda c0, c1, mt=mt: mkT[:, c0:c1, mt * 128:(mt + 1) * 128], r_mkT)
            c.barrier()

        G = 2
        GT = G * 128

        def gmlp_tile(es_, P, gv_j, r_gv, gu_j, r_gu, sga_j, r_sga, Wmix, r_W, bs, r_bs, lng, lnb, r_ln,
                      tmp, mA, r_mA, vln_out=None, r_vln=None):
            st, mvv, vt, vlnb, gs = tmp["st"], tmp["mvv"], tmp["vt"], tmp["vlnb"], tmp["gs"]
            r_t = tmp["r"]
            c.op("dve", lambda e: e.bn_stats(out=st[:P, 0, :], in_=gv_j[:P, 0:512]), reads=[r_gv], writes=[r_t])
            c.op("dve", lambda e: e.bn_stats(out=st[:P, 1, :], in_=gv_j[:P, 512:1024]), reads=[r_gv], writes=[r_t])
            c.op("dve", lambda e: e.bn_aggr(out=mvv[:P, 0:2], in_=st[:P].rearrange("p a b -> p (a b)")),
                 reads=[r_t], writes=[r_t])
            c.op("act", lambda e: e.activation(out=mvv[:P, 2:3], in_=mvv[:P, 1:2], func=AF.Sqrt,
                                               bias=epsc[:P, 0:1], scale=1.0), reads=[r_t, r_const], writes=[r_t])
            c.op("dve", lambda e: e.reciprocal(out=mvv[:P, 3:4], in_=mvv[:P, 2:3]), reads=[r_t], writes=[r_t])
            c.op("dve", lambda e: e.tensor_scalar(out=vt[:P, :], in0=gv_j[:P, :], scalar1=mvv[:P, 0:1],
                                                  scalar2=mvv[:P, 3:4], op0=ALU.subtract, op1=ALU.mult),
                 reads=[r_gv, r_t], writes=[r_t])
            c.op("dve", lambda e: e.tensor_tensor(out=vt[:P, :], in0=vt[:P, :], in1=lng[:P, :], op=ALU.mult),
                 reads=[r_t, r_ln], writes=[r_t])
            if vln_out is None:
                c.op("pool", lambda e: e.tensor_tensor(out=vlnb[:P, :], in0=vt[:P, :], in1=lnb[:P, :], op=ALU.add),
                     reads=[r_t, r_ln], writes=[r_t])
            else:
                c.op("pool", lambda e: e.tensor_tensor(out=vln_out[:P, :], in0=vt[:P, :], in1=lnb[:P, :],
                                                       op=ALU.add), reads=[r_t, r_ln], writes=[r_vln])
                c.op("act", lambda e: e.activation(out=vlnb[:P, :], in_=vln_out[:P, :], func=AF.Copy),
                     reads=[r_vln], writes=[r_t])

            def f(e):
                inst = None
                for g in range(8):
                    inst = e.matmul(pb[6 + g // 4][:P, (g % 4) * 128:(g % 4 + 1) * 128], lhsT=Wmix[:P, g, :P],
                                    rhs=vlnb[:P, g * 128:(g + 1) * 128], start=True, stop=True)
                return inst
            c.op("pe", f, reads=[r_t, r_W], writes=[rpb[6], rpb[7]])
            for half in range(2):
                c.op("dve", lambda e, half=half: e.tensor_tensor(
                    out=vt[:P, half * 512:(half + 1) * 512].rearrange("p (g d) -> p g d", d=128),
                    in0=pb[6 + half][:P, :].rearrange("p (g d) -> p g d", d=128),
                    in1=bs[:P, half * 4:half * 4 + 4].unsqueeze(2).to_broadcast([P, 4, 128]), op=ALU.add),
                    reads=[rpb[6 + half], r_bs], writes=[r_t])
            c.op("pool", lambda e: e.tensor_tensor(out=gs[:P, :], in0=gu_j[:P, :], in1=sga_j[:P, :], op=ALU.mult),
                 reads=[r_gu, r_sga], writes=[r_t])
            c.op("dve", lambda e: e.tensor_tensor(out=mA[:P, :], in0=vt[:P, :], in1=gs[:P, :], op=ALU.mult),
                 reads=[r_t], writes=[r_mA])

        def gmlp_tmp(es_, tag):
            return {"st": c.sb(es_, "g_st" + tag, [128, 2, 6], F32), "mvv": c.sb(es_, "g_mvv" + tag, [128, 4], F32),
                    "vt": c.sb(es_, "g_vt" + tag, [128, 1024], F32), "vlnb": c.sb(es_, "g_vlnb" + tag, [128, 1024], BF16),
                    "gs": c.sb(es_, "g_gs" + tag, [128, 1024], BF16), "r": Res()}

        with contextlib.suppress(_SkipPhase), contextlib.ExitStack() as es2:
            if 2 not in _PH:
                raise _SkipPhase()
            make_wpool(es2, 3)
            g_mix, r_gmix = load_gain(es2, "mix", gains["norm_mix"])
            lng, r_ln = load_gain(es2, "lng", sgu_ln_g, 1024)
            lnb = c.sb(es2, "g_lnb", [128, 1024], F32)
            c.dma("sp", lnb[:], sgu_ln_b[0:1, :].partition_broadcast(128), writes=[r_ln])
            wsf = c.sb(es2, "wsf", [128, 8, 128], F32)
            WsT = c.sb(es2, "WsT", [128, 8, 128], BF16)
            r_ws = Res()
            c.dma("sp", wsf[:], sgu_wT.rearrange("g j i -> j g i"), writes=[r_ws])
            c.op("pool", lambda e: e.affine_select(out=wsf[:], in_=wsf[:], pattern=[[0, 8], [1, 128]],
                                                   compare_op=ALU.is_ge, fill=0.0, base=0, channel_multiplier=-1),
                 reads=[r_ws], writes=[r_ws])
            c.op("dve", lambda e: e.tensor_copy(out=WsT[:], in_=wsf[:]), reads=[r_ws], writes=[r_ws])
            bsT = c.sb(es2, "bsT", [128, 8], F32)
            r_bs = Res()
            c.dma("sp", bsT[:], sgu_bT[:, :], writes=[r_bs])

            hT = [c.sb(es2, "hT%d" % i, [128, 16, GT], BF16) for i in range(2)]
            r_hT = [Res() for _ in range(2)]
            xt = [c.sb(es2, "xt%d" % i, [128, D], F32) for i in range(2)]
            r_xt = [Res() for _ in range(2)]
            hb = [c.sb(es2, "hb%d" % i, [128, D], BF16) for i in range(2)]
            r_hb = [Res() for _ in range(2)]
            gv = c.sb(es2, "gv", [128, G, 1024], F32)
            gu = c.sb(es2, "gu", [128, G, 1024], BF16)
            sga = c.sb(es2, "sga", [128, G, 1024], BF16)
            r_gv = [Res() for _ in range(G)]
            r_gu = [Res() for _ in range(G)]
            r_sga = [Res() for _ in range(G)]
            gtmp = gmlp_tmp(es2, "p")
            mA = c.sb(es2, "mA", [128, 1024], BF16)
            r_mA = Res()
            mst = [c.sb(es2, "mst%d" % i, [128, 8, 128], BF16) for i in range(2)]
            r_mst = [Res() for _ in range(2)]
            fst = [c.sb(es2, "fst%d" % i, [128, 4, GT], BF16) for i in range(2)]
            r_fst = [Res() for _ in range(2)]
            kvo = [c.sb(es2, "kvo%d" % i, [128, 512], F32) for i in range(2)]
            r_kvo = [Res() for _ in range(2)]
            ctr = {"x": 0, "f": 0, "k": 0, "m": 0}

            groups = [("h", g) for g in range(NT // G)][:_LIM["groups"]] + [("o", g) for g in range(NT // G)][:_LIM["groups"]]

            def prep_group(gidx):
                kind, g = groups[gidx]
                src = xh if kind == "h" else xo
                hbuf = gidx % 2
                for j in range(G):
                    i = ctr["x"] % 2
                    ctr["x"] += 1
                    t0 = g * GT + j * 128
                    c.dma("sp", xt[i][:], src[t0:t0 + 128, :], writes=[r_xt[i]])
                    rms_h(xt[i], r_xt[i], 128, g_mix, r_gmix, hb[i], r_hb[i])
                    transpose_to(hb[i], r_hb[i], 128, 16,
                                 lambda c0, c1, j=j, hbuf=hbuf: hT[hbuf][:, c0:c1, j * 128:(j + 1) * 128], r_hT[hbuf])

            def feat_block(ws, hbuf, dst, r_dst_list, tcol0, sigmoid=False):
                wt, r_w = ws.get()
                i = ctr["f"] % 2
                ctr["f"] += 1
                for ct in range(4):
                    b = bank(2, 6)
                    mm_feat(hT[hbuf], r_hT[hbuf], 0, GT, wt, r_w, ct, b)
                    if sigmoid:
                        c.op("act", lambda e, b=b, ct=ct, i=i: e.activation(out=fst[i][:, ct, :], in_=pb[b][:, 0:GT],
                                                                          func=AF.Sigmoid),
                             reads=[rpb[b]], writes=[r_fst[i]])
                    elif ct % 2 == 0:
                        c.op("act", lambda e, b=b, ct=ct, i=i: e.activation(out=fst[i][:, ct, :], in_=pb[b][:, 0:GT],
                                                                          func=AF.Copy),
                             reads=[rpb[b]], writes=[r_fst[i]])
                    else:
                        c.op("dve", lambda e, b=b, ct=ct, i=i: e.tensor_copy(out=fst[i][:, ct, :], in_=pb[b][:, 0:GT]),
                             reads=[rpb[b]], writes=[r_fst[i]])
                c.dma("act", dst[:, :, tcol0:tcol0 + GT].rearrange("g p t -> p g t"), fst[i][:],
                      reads=[r_fst[i]], writes=r_dst_list)

            prep_group(0)
            for gidx, (kind, g) in enumerate(groups):
                hbuf = gidx % 2
                if kind == "h":
                    ws = WStream([("w_in", b) for b in (10, 11, 12, 13)])
                    for bi, blk in enumerate((10, 11, 12, 13)):
                        dst = kTs if blk < 12 else vTs
                        rr = r_kTs if blk < 12 else r_vTs
                        h0 = (blk % 2) * 4
                        feat_block(ws, hbuf, dst[h0:h0 + 4], rr[h0:h0 + 4], g * GT)
                        if bi == 0 and gidx + 1 < len(groups):
                            prep_group(gidx + 1)
                    continue
                order = [2, 3, 0, 1, 14, 15, 10, 11, 12, 13, 4, 5, 6, 7, 8, 9, 10, 11, 12, 13, 16, 17]
                ws = WStream([("w_in", b) for b in order])
                for bi, blk in enumerate(order[:10]):
                    wt, r_w = ws.get()
                    for j in range(G):
                        b = bank(2, 6)
                        mm_tok(hT[hbuf], r_hT[hbuf], j * 128, 128, wt, r_w, b)
                        if blk in (2, 3):
                            c.op("act", lambda e, b=b, j=j, blk=blk: e.activation(
                                out=gv[:, j, (blk - 2) * 512:(blk - 1) * 512], in_=pb[b][:, :], func=AF.Gelu_apprx_tanh),
                                reads=[rpb[b]], writes=[r_gv[j]])
                        elif blk in (0, 1):
                            c.op("act", lambda e, b=b, j=j, blk=blk: e.activation(
                                out=gu[:, j, blk * 512:(blk + 1) * 512], in_=pb[b][:, :], func=AF.Gelu_apprx_tanh),
                                reads=[rpb[b]], writes=[r_gu[j]])
                        elif blk in (14, 15):
                            c.op("act", lambda e, b=b, j=j, blk=blk: e.activation(
                                out=sga[:, j, (blk - 14) * 512:(blk - 13) * 512], in_=pb[b][:, :], func=AF.Sigmoid),
                                reads=[rpb[b]], writes=[r_sga[j]])
                        else:
                            i = ctr["k"] % 2
                            ctr["k"] += 1
                            c.op("dve", lambda e, b=b, i=i: e.tensor_copy(out=kvo[i][:], in_=pb[b][:, :]),
                                 reads=[rpb[b]], writes=[r_kvo[i]])
                            t0 = g * GT + j * 128
                            kv = 0 if blk < 12 else 1
                            half = blk % 2
                            c.dma("act", win_p[t0:t0 + 128, kv, half * 512:(half + 1) * 512], kvo[i][:],
                                  reads=[r_kvo[i]])
                    if bi == 0 and gidx + 1 < len(groups):
                        prep_group(gidx + 1)
                    if bi == 5:
                        for j in range(G):
                            gmlp_tile(es2, 128, gv[:, j, :], r_gv[j], gu[:, j, :], r_gu[j], sga[:, j, :], r_sga[j],
                                      WsT, r_ws, bsT, r_bs, lng, lnb, r_ln, gtmp, mA, r_mA)
                            i = ctr["m"] % 2
                            ctr["m"] += 1
                            transpose_to(mA, r_mA, 128, 8, lambda c0, c1, i=i: mst[i][:, c0:c1, :], r_mst[i],
                                         evac="dve")
                            t0 = g * GT + j * 128
                            c.dma("act", mTs[0:8, :, t0:t0 + 128].rearrange("g p t -> p g t"), mst[i][:],
                                  reads=[r_mst[i]], writes=r_mTs[0:8])
                for blk in order[10:]:
                    if 4 <= blk <= 9:
                        gh0 = ((blk - 4) // 2) * 8 + ((blk - 4) % 2) * 4
                        feat_block(ws, hbuf, qTs[gh0:gh0 + 4], r_qTs[gh0:gh0 + 4], g * GT)
                    elif blk in (10, 11):
                        h0 = (blk % 2) * 4
                        feat_block(ws, hbuf, kTs[h0:h0 + 4], r_kTs[h0:h0 + 4], TOK + g * GT)
                    elif blk in (12, 13):
                        h0 = (blk % 2) * 4
                        feat_block(ws, hbuf, vTs[h0:h0 + 4], r_vTs[h0:h0 + 4], TOK + g * GT)
                    else:
                        h0 = (blk % 2) * 4
                        feat_block(ws, hbuf, gbTs[h0:h0 + 4], r_gbTs[h0:h0 + 4], g * GT, sigmoid=True)
            c.barrier()

        with contextlib.suppress(_SkipPhase), contextlib.ExitStack() as es3:
            if 3 not in _PH:
                raise _SkipPhase()
            EBT = c.sb(es3, "EBT", [128, 24, 256], BF16)
            r_EB = Res()
            bm = c.sb(es3, "bm", [128, 256], F32)
            r_bm = Res()
            c.dma("sp", bm[:], bandmask.rearrange("p a b -> p (a b)"), writes=[r_bm])
            btmp = [c.sb(es3, "btmp%d" % i, [128, 256], F32) for i in range(2)]
            r_btmp = [Res() for _ in range(2)]
            for gh in range(24):
                i = gh % 2
                c.dma("sp", btmp[i][:], biasT[gh].rearrange("p a b -> p (a b)"), writes=[r_btmp[i]])
                c.op("act", lambda e, i=i: e.activation(out=btmp[i][:], in_=btmp[i][:], func=AF.Exp),
                     reads=[r_btmp[i]], writes=[r_btmp[i]])
                c.op("dve", lambda e, i=i, gh=gh: e.tensor_tensor(out=EBT[:, gh, :], in0=btmp[i][:], in1=bm[:],
                                                                  op=ALU.mult),
                     reads=[r_btmp[i], r_bm], writes=[r_EB])
            pf = c.sb(es3, "pf", [128, 1], F32)
            r_pf = Res()
            c.dma("sp", pf[:], pflag[:, :], writes=[r_pf])

            kT = [c.sb(es3, "kT%d" % i, [128, 2 * TOK], BF16) for i in range(2)]
            vT = [c.sb(es3, "vT%d" % i, [128, 2 * TOK], BF16) for i in range(2)]
            qT3 = [c.sb(es3, "qT3%d" % i, [128, 3, TOK], BF16) for i in range(2)]
            gbT = [c.sb(es3, "gbT%d" % i, [128, TOK], BF16) for i in range(2)]
            r_hd = [Res() for _ in range(2)]
            acc = c.sb(es3, "acc", [128, 2, TOK], F32)
            r_acc = Res()
            ybT = c.sb(es3, "ybT", [128, TOK], BF16)
            r_ybT = Res()
            NVP = 4
            Vp = [c.sb(es3, "Vp%d" % i, [128, 128], BF16) for i in range(NVP)]
            r_Vp = [Res() for _ in range(NVP)]
            pTf = [c.sb(es3, "pTf%d" % i, [128, 256], BF16) for i in range(2)]
            pT = [c.sb(es3, "pT%d" % i, [128, 256], BF16) for i in range(2)]
            r_pTf = [Res() for _ in range(2)]
            r_pT = [Res() for _ in range(2)]
            qTs4 = qTs.rearrange("(g h) p t -> g h p t", h=8)
            actr = {"vp": 0, "u": 0}

            def load_head(h):
                i = h % 2
                c.dma("sp", kT[i][:], kTs[h], reads=[r_kTs[h]], writes=[r_hd[i]])
                c.dma("sp", vT[i][:], vTs[h], reads=[r_vTs[h]], writes=[r_hd[i]])
                c.dma("sp", qT3[i][:], qTs4[:, h].rearrange("g p t -> p g t"),
                      reads=[r_qTs[h], r_qTs[8 + h], r_qTs[16 + h]], writes=[r_hd[i]])
                c.dma("sp", gbT[i][:], gbTs[h], reads=[r_gbTs[h]], writes=[r_hd[i]])

            def make_vp(i, dil, a0, r):
                k = actr["vp"] % NVP
                actr["vp"] += 1
                b = bank(2, 4)
                vv = vT[i][:].rearrange("p (a b) -> p a b", b=dil)
                c.op("pe", lambda e: e.transpose(out=pbb[b][:, 0:128], in_=vv[:, a0:a0 + 128, r], identity=ident[:]),
                     reads=[r_hd[i], r_const], writes=[rpb[b]])
                c.op("dve", lambda e: e.tensor_copy(out=Vp[k][:], in_=pbb[b][:, 0:128]), reads=[rpb[b]],
                     writes=[r_Vp[k]])
                return k

            load_head(0)
            NH = 8 if _LIM["heads"] is None else _LIM["heads"]
            for h in range(NH):
                i = h % 2
                if h + 1 < NH:
                    load_head(h + 1)
                for gi, dil in enumerate(DILS):
                    span = 128 * dil
                    nbk = TOK // span
                    kv_ = kT[i][:].rearrange("p (a b) -> p a b", b=dil)
                    qv_ = qT3[i][:, gi, :].rearrange("p (a b) -> p a b", b=dil)
                    accv = acc[:].rearrange("p s (a b) -> p s a b", b=dil)
                    for r in range(dil):
                        kprev = make_vp(i, dil, (TOK - span) // dil, r)
                        for bb in range(nbk):
                            a_own = (TOK + bb * span) // dil
                            kcur = make_vp(i, dil, a_own, r)
                            u = actr["u"] % 2
                            actr["u"] += 1
                            b_s = bank(0, 2)
                            a_prev = a_own - 128

                            def fs(e, a_prev=a_prev, bb=bb, r=r, b_s=b_s, kv_=kv_, qv_=qv_):
                                inst = None
                                for jt in range(2):
                                    inst = e.matmul(pb[b_s][:, jt * 128:(jt + 1) * 128],
                                                    lhsT=kv_[:, a_prev + jt * 128:a_prev + (jt + 1) * 128, r],
                                                    rhs=qv_[:, bb * 128:(bb + 1) * 128, r], start=True, stop=True)
                                return inst
                            c.op("pe", fs, reads=[r_hd[i]], writes=[rpb[b_s]])
                            c.op("act", lambda e, u=u, b_s=b_s: e.activation(out=pTf[u][:], in_=pb[b_s][:, 0:256],
                                                                            func=AF.Exp, scale=ATT_SCALE),
                                 reads=[rpb[b_s]], writes=[r_pTf[u]])
                            c.op("pool", lambda e, u=u, gi=gi, h=h: e.tensor_tensor(
                                out=pT[u][:], in0=pTf[u][:], in1=EBT[:, gi * 8 + h, :], op=ALU.mult),
                                reads=[r_pTf[u], r_EB], writes=[r_pT[u]])
                            if bb == 0:
                                c.op("pool", lambda e, u=u: e.tensor_scalar(
                                    out=pT[u][:, 0:128], in0=pT[u][:, 0:128], scalar1=pf[:, 0:1], scalar2=None,
                                    op0=ALU.mult), reads=[r_pT[u], r_pf], writes=[r_pT[u]])
                            b_o = bank(4, 6)

                            def fo(e, u=u, b_o=b_o, kprev=kprev, kcur=kcur):
                                e.matmul(pb[b_o][:, 0:128], lhsT=Vp[kprev][:], rhs=pT[u][:, 0:128], start=True, stop=False)
                                e.matmul(pb[b_o][:, 0:128], lhsT=Vp[kcur][:], rhs=pT[u][:, 128:256], start=False, stop=True)
                                e.matmul(pb[b_o][:, 128:256], lhsT=ones[:], rhs=pT[u][:, 0:128], start=True, stop=False)
                                return e.matmul(pb[b_o][:, 128:256], lhsT=ones[:], rhs=pT[u][:, 128:256],
                                                start=False, stop=True)
                            c.op("pe", fo, reads=[r_pT[u], r_Vp[kprev], r_Vp[kcur], r_const], writes=[rpb[b_o]])
                            av = accv[:, :, bb * 128:(bb + 1) * 128, r]
                            pv = pb[b_o][:, 0:256].rearrange("p (s t) -> p s t", s=2)
                            if gi == 0:
                                c.op("dve", lambda e, av=av, pv=pv: e.tensor_copy(out=av, in_=pv),
                                     reads=[rpb[b_o]], writes=[r_acc])
                            else:
                                c.op("dve", lambda e, av=av, pv=pv: e.tensor_tensor(out=av, in0=pv, in1=av, op=ALU.add),
                                     reads=[rpb[b_o], r_acc], writes=[r_acc])
                            kprev = kcur
                c.op("dve", lambda e: e.reciprocal(out=acc[:, 1, :], in_=acc[:, 1, :]), reads=[r_acc], writes=[r_acc])
                c.op("dve", lambda e: e.tensor_tensor(out=acc[:, 0, :], in0=acc[:, 0, :], in1=acc[:, 1, :], op=ALU.mult),
                     reads=[r_acc], writes=[r_acc])
                c.op("pool", lambda e, i=i: e.tensor_tensor(out=ybT[:], in0=acc[:, 0, :], in1=gbT[i][:], op=ALU.mult),
                     reads=[r_acc, r_hd[i]], writes=[r_ybT])
                c.dma("sp", mTs[8 + h], ybT[:], reads=[r_ybT], writes=[r_mTs[8 + h]])
            c.barrier()


        PS = 128
        mTg_s = c.sb(es, "mTg_s", [128, 16, PS], BF16)
        r_mTgs = Res()
        c.op("pool", lambda e: e.memset(mTg_s[:], 0.0), writes=[r_mTgs])
        with contextlib.suppress(_SkipPhase), contextlib.ExitStack() as ess:
            if 4 not in _PH:
                raise _SkipPhase()
            make_wpool(ess, 2)
            cut(10)
            g_mix, r_gmix = load_gain(ess, "mix_s", gains["norm_mix"])
            lng, r_ln = load_gain(ess, "lng_s", sgu_ln_g, 1024)
            lnb = c.sb(ess, "g_lnb_s", [128, 1024], F32)
            c.dma("sp", lnb[:], sgu_ln_b[0:1, :].partition_broadcast(128), writes=[r_ln])
            wsf = c.sb(ess, "wsf_s", [128, 8, 128], F32)
            WsTs = c.sb(ess, "WsTs", [128, 8, 128], BF16)
            r_ws = Res()
            c.dma("sp", wsf[:], sgu_wTs.rearrange("g j i -> j g i"), writes=[r_ws])
            c.op("pool", lambda e: e.affine_select(out=wsf[:], in_=wsf[:], pattern=[[0, 8], [1, 128]],
                                                   compare_op=ALU.is_ge, fill=0.0, base=0, channel_multiplier=-1),
                 reads=[r_ws], writes=[r_ws])
            c.op("dve", lambda e: e.tensor_copy(out=WsTs[:], in_=wsf[:]), reads=[r_ws], writes=[r_ws])
            bsTs = c.sb(ess, "bsTs", [128, 8], F32)
            r_bs = Res()
            c.dma("sp", bsTs[:], sgu_bTs[:, :], writes=[r_bs])
            cut(11)

            xts = c.sb(ess, "xts", [128, D], F32)
            hbs = c.sb(ess, "hbs", [128, D], BF16)
            hTs = c.sb(ess, "hTs", [128, 16, PS], BF16)
            r_xts, r_hbs, r_hTs = Res(), Res(), Res()
            c.op("pool", lambda e: e.memset(xts[:], 0.0), writes=[r_xts])
            c.dma("sp", xts[:SP, :], xs[:, :], writes=[r_xts])
            cut(12)
            rms_h(xts, r_xts, PS, g_mix, r_gmix, hbs, r_hbs)
            cut(13)
            transpose_to(hbs, r_hbs, PS, 16, lambda c0, c1: hTs[:, c0:c1, :], r_hTs)
            cut(1)

            gv_s = c.sb(ess, "gv_s", [128, 1024], F32)
            gu_s = c.sb(ess, "gu_s", [128, 1024], BF16)
            sga_s = c.sb(ess, "sga_s", [128, 1024], BF16)
            sgb_s = c.sb(ess, "sgb_s", [128, 1024], BF16)
            q_s = c.sb(ess, "q_s", [128, 3072], BF16)
            kf_s = c.sb(ess, "kf_s", [128, 1024], F32)
            vf_s = c.sb(ess, "vf_s", [128, 1024], F32)
            k_sb = c.sb(ess, "k_sb", [128, 1024], BF16)
            vn = c.sb(ess, "vn", [128, 1024], BF16)
            r_gvs, r_gus, r_sgas, r_sgbs, r_qs, r_kfs, r_vfs, r_ksb, r_vn = (Res() for _ in range(9))
            ws = WStream([("w_in", b) for b in range(18)])
            for blk in range(18):
                cut(20 + blk)
                wt, r_w = ws.get()
                b = bank(2, 6)
                mm_tok(hTs, r_hTs, 0, PS, wt, r_w, b)
                src = pb[b][:, :]
                if blk < 2:
                    c.op("act", lambda e, blk=blk, src=src: e.activation(out=gu_s[:, blk * 512:(blk + 1) * 512], in_=src,
                                                                        func=AF.Gelu_apprx_tanh), reads=[rpb[b]], writes=[r_gus])
                elif blk < 4:
                    c.op("act", lambda e, blk=blk, src=src: e.activation(out=gv_s[:, (blk - 2) * 512:(blk - 1) * 512], in_=src,
                                                                        func=AF.Gelu_apprx_tanh), reads=[rpb[b]], writes=[r_gvs])
                elif blk < 10:
                    c.op("act", lambda e, blk=blk, src=src: e.activation(out=q_s[:, (blk - 4) * 512:(blk - 3) * 512], in_=src,
                                                                        func=AF.Copy), reads=[rpb[b]], writes=[r_qs])
                elif blk < 12:
                    c.op("act", lambda e, blk=blk, src=src: e.activation(out=kf_s[:, (blk - 10) * 512:(blk - 9) * 512], in_=src,
                                                                        func=AF.Copy), reads=[rpb[b]], writes=[r_kfs])
                    c.op("dve", lambda e, blk=blk: e.tensor_copy(out=k_sb[:, (blk - 10) * 512:(blk - 9) * 512],
                                                                 in_=kf_s[:, (blk - 10) * 512:(blk - 9) * 512]),
                         reads=[r_kfs], writes=[r_ksb])
                elif blk < 14:
                    c.op("act", lambda e, blk=blk, src=src: e.activation(out=vf_s[:, (blk - 12) * 512:(blk - 11) * 512], in_=src,
                                                                        func=AF.Copy), reads=[rpb[b]], writes=[r_vfs])
                    c.op("dve", lambda e, blk=blk: e.tensor_copy(out=vn[:, (blk - 12) * 512:(blk - 11) * 512],
                                                                 in_=vf_s[:, (blk - 12) * 512:(blk - 11) * 512]),
                         reads=[r_vfs], writes=[r_vn])
                elif blk < 16:
                    c.op("act", lambda e, blk=blk, src=src: e.activation(out=sga_s[:, (blk - 14) * 512:(blk - 13) * 512], in_=src,
                                                                        func=AF.Sigmoid), reads=[rpb[b]], writes=[r_sgas])
                else:
                    c.op("act", lambda e, blk=blk, src=src: e.activation(out=sgb_s[:, (blk - 16) * 512:(blk - 15) * 512], in_=src,
                                                                        func=AF.Sigmoid), reads=[rpb[b]], writes=[r_sgbs])
            cut(40)
            c.dma("sp", win_s[:, 0, :], kf_s[:SP, :], reads=[r_kfs])
            c.dma("sp", win_s[:, 1, :], vf_s[:SP, :], reads=[r_vfs])
            cut(2)
            gtmp_s = gmlp_tmp(ess, "s")
            vlnf_s = c.sb(ess, "vlnf_s", [128, 1024], F32)
            r_vlnf = Res()
            mA_s = c.sb(ess, "mA_s", [128, 1024], BF16)
            r_mAs = Res()
            gmlp_tile(ess, PS, gv_s, r_gvs, gu_s, r_gus, sga_s, r_sgas, WsTs, r_ws, bsTs, r_bs, lng, lnb, r_ln,
                      gtmp_s, mA_s, r_mAs, vln_out=vlnf_s, r_vln=r_vlnf)
            c.dma("sp", sgu_s[:, :], vlnf_s[:SP, :], reads=[r_vlnf])
            transpose_to(mA_s, r_mAs, PS, 8, lambda c0, c1: mTg_s[:, c0:c1, :], r_mTgs)
            cut(3)

            qTs_s = c.sb(ess, "qTs_s", [128, 24, PS], BF16)
            kTn = c.sb(ess, "kTn", [128, 8, PS], BF16)
            gbT_s = c.sb(ess, "gbT_s", [128, 8, PS], BF16)
            r_qTss, r_kTn, r_gbTs_ = Res(), Res(), Res()
            transpose_to(q_s, r_qs, PS, 24, lambda c0, c1: qTs_s[:, c0:c1, :], r_qTss)
            transpose_to(k_sb, r_ksb, PS, 8, lambda c0, c1: kTn[:, c0:c1, :], r_kTn)
            transpose_to(sgb_s, r_sgbs, PS, 8, lambda c0, c1: gbT_s[:, c0:c1, :], r_gbTs_)
            EBs = c.sb(ess, "EBs", [128, 8, 384], BF16)
            EBn = c.sb(ess, "EBn", [128, 8, 96], BF16)
            r_EBs = Res()
            c.op("pool", lambda e: e.memset(EBn[:], 0.0), writes=[r_EBs])
            smk = c.sb(ess, "smk", [128, 384], F32)
            smkn = c.sb(ess, "smkn", [128, 96], F32)
            r_smk = Res()
            c.dma("sp", smk[:], smask.rearrange("p a b -> p (a b)"), writes=[r_smk])
            c.dma("sp", smkn[:SP, :], smask_n.rearrange("p a b -> p (a b)"), writes=[r_smk])
            stmp = [c.sb(ess, "stmp%d" % i, [128, 480], F32) for i in range(2)]
            r_stmp = [Res() for _ in range(2)]
            for h in range(8):
                i = h % 2
                c.dma("sp", stmp[i][:, 0:384], sbias[h].rearrange("p a b -> p (a b)"), writes=[r_stmp[i]])
                c.dma("sp", stmp[i][:SP, 384:480], sbias_n[h].rearrange("p a b -> p (a b)"), writes=[r_stmp[i]])
                c.op("act", lambda e, i=i: e.activation(out=stmp[i][:, 0:384], in_=stmp[i][:, 0:384], func=AF.Exp),
                     reads=[r_stmp[i]], writes=[r_stmp[i]])
                c.op("act", lambda e, i=i: e.activation(out=stmp[i][:SP, 384:480], in_=stmp[i][:SP, 384:480], func=AF.Exp),
                     reads=[r_stmp[i]], writes=[r_stmp[i]])
                c.op("dve", lambda e, i=i, h=h: e.tensor_tensor(out=EBs[:, h, :], in0=stmp[i][:, 0:384], in1=smk[:],
                                                                op=ALU.mult), reads=[r_stmp[i], r_smk], writes=[r_EBs])
                c.op("dve", lambda e, i=i, h=h: e.tensor_tensor(out=EBn[:SP, h, :], in0=stmp[i][:SP, 384:480],
                                                                in1=smkn[:SP, :], op=ALU.mult),
                     reads=[r_stmp[i], r_smk], writes=[r_EBs])
            cut(4)
            Ppad = c.sb(ess, "Ppad", [128, 4, 17 * 96], BF16)
            r_Pp = Res()
            c.op("pool", lambda e: e.memset(Ppad[:], 0.0), writes=[r_Pp])
            Kc = c.sb(ess, "Kc0", [128, 16, 128], F32)
            Vc = c.sb(ess, "Vc0", [128, 16, 128], F32)
            Kcb = [c.sb(ess, "Kcb0", [128, 16, 128], BF16)] * 2
            Vcb = [c.sb(ess, "Vcb%d" % i, [128, 16, 128], BF16) for i in range(2)]
            KT = [c.sb(ess, "KT0", [128, 16, 128], BF16)] * 2
            r_Kc, r_Vc = Res(), Res()
            r_Kcb = [Res()] * 2
            r_Vcb = [Res() for _ in range(2)]
            r_KT = [Res()] * 2
            tmpE = c.sb(ess, "tmpE", [128, 408], BF16)
            r_tmpE = Res()
            qc = [c.sb(ess, "qc%d" % i, [128, 24], BF16) for i in range(2)]
            r_qc = [Res() for _ in range(2)]
            rec_s = c.sb(ess, "rec_s", [128, SP], F32)
            ot_s = c.sb(ess, "ot_s", [128, SP], F32)
            r_recs = Res()
            qv4 = qTs_s[:].rearrange("p (g h) t -> p g h t", h=8)
            tiles_g = ([15], [12, 13, 14, 15], list(range(16)))
            n = 0
            for h in range(8):
                b_o = 4 + h % 2
                b_sum = 6 + h % 2
                for b in range(4):
                    i = n % 2
                    n += 1
                    c.dma("sp", Kc[:], cwin[b, :, 0, h, :].rearrange("(rt p) d -> p rt d", p=128), writes=[r_Kc])
                    c.dma("sp", Vc[:], cwin[b, :, 1, h, :].rearrange("(rt p) d -> p rt d", p=128), writes=[r_Vc])
                    c.op("pool", lambda e, i=i: e.tensor_copy(out=Kcb[i][:], in_=Kc[:]), reads=[r_Kc], writes=[r_Kcb[i]])
                    c.op("act", lambda e, i=i: e.activation(out=Vcb[i][:], in_=Vc[:], func=AF.Copy),
                         reads=[r_Vc], writes=[r_Vcb[i]])
                    transpose_to(Kcb[i][:].rearrange("p a d -> p (a d)"), r_Kcb[i], 128, 16,
                                 lambda c0, c1, i=i: KT[i][:, c0:c1, :], r_KT[i], evac="dve")
                    c.op("dve", lambda e, i=i, h=h, b=b: e.tensor_copy(
                        out=qc[i][:].rearrange("p (g t) -> p g t", t=8), in_=qv4[:, :, h, b * 8:(b + 1) * 8]),
                        reads=[r_qTss], writes=[r_qc[i]])
                    b_s = bank(2, 4)

                    def fs(e, i=i, b_s=b_s, h=h):
                        for rt in range(16):
                            e.matmul(pb[b_s][:, rt * 24:(rt + 1) * 24], lhsT=KT[i][:, rt, :], rhs=qc[i][:],
                                     start=True, stop=True)
                        return e.matmul(pb[b_s][:, 384:408], lhsT=kTn[:, h, :], rhs=qc[i][:], start=True, stop=True)
                    c.op("pe", fs, reads=[r_KT[i], r_qc[i], r_kTn], writes=[rpb[b_s]])
                    c.op("act", lambda e, b_s=b_s: e.activation(out=tmpE[:], in_=pb[b_s][:, 0:408], func=AF.Exp,
                                                                scale=ATT_SCALE), reads=[rpb[b_s]], writes=[r_tmpE])
                    Pv = Ppad[:, b, :].rearrange("p (rt g m) -> p rt g m", g=3, m=32)
                    c.op("dve", lambda e, Pv=Pv, b=b, h=h: e.tensor_tensor(
                        out=Pv[:, 0:16, :, b * 8:(b + 1) * 8],
                        in0=tmpE[:, 0:384].rearrange("p (rt g t) -> p rt g t", g=3, t=8),
                        in1=EBs[:, h, :].rearrange("p (rt g t) -> p rt g t", g=3, t=8), op=ALU.mult),
                        reads=[r_tmpE, r_EBs], writes=[r_Pp])
                    c.op("dve", lambda e, Pv=Pv, b=b, h=h: e.tensor_tensor(
                        out=Pv[:, 16, :, b * 8:(b + 1) * 8],
                        in0=tmpE[:, 384:408].rearrange("p (g t) -> p g t", t=8),
                        in1=EBn[:, h, b * 24:(b + 1) * 24].rearrange("p (g t) -> p g t", t=8), op=ALU.mult),
                        reads=[r_tmpE, r_EBs], writes=[r_Pp])

                    def fo(e, i=i, b=b, h=h, b_o=b_o, b_sum=b_sum, Pv=Pv):
                        mms = []
                        for g in range(3):
                            for rt in tiles_g[g]:
                                mms.append((Vcb[i][:, rt, :], Pv[:, rt, g, :]))
                            mms.append((vn[:, h * 128:(h + 1) * 128], Pv[:, 16, g, :]))
                        inst = None
                        for k, (l, r) in enumerate(mms):
                            first = (b == 0 and k == 0)
                            last = (b == 3 and k == len(mms) - 1)
                            e.matmul(pb[b_o][:, 0:SP], lhsT=l, rhs=r, start=first, stop=last)
                            inst = e.matmul(pb[b_sum][:, 0:SP], lhsT=ones[:], rhs=r, start=first, stop=last)
                        return inst
                    c.op("pe", fo, reads=[r_Pp, r_Vcb[i], r_vn, r_const], writes=[rpb[b_o], rpb[b_sum]])
                c.op("dve", lambda e, b_sum=b_sum: e.reciprocal(out=rec_s[:], in_=pb[b_sum][:, 0:SP]),
                     reads=[rpb[b_sum]], writes=[r_recs])
                c.op("dve", lambda e, b_o=b_o: e.tensor_tensor(out=ot_s[:], in0=pb[b_o][:, 0:SP], in1=rec_s[:],
                                                               op=ALU.mult), reads=[rpb[b_o], r_recs], writes=[r_recs])
                c.op("dve", lambda e, h=h: e.tensor_tensor(out=mTg_s[:, 8 + h, 0:SP], in0=ot_s[:], in1=gbT_s[:, h, 0:SP],
                                                           op=ALU.mult), reads=[r_recs, r_gbTs_], writes=[r_mTgs])
            c.barrier()

        es4 = es.enter_context(contextlib.ExitStack())
        make_wpool(es4, 2)
        g_mem, r_gmem = load_gain(es4, "mem", gains["norm_mem"])
        g_peer, r_gpeer = load_gain(es4, "peer", gains["norm_peer"])
        g_fin, r_gfin = load_gain(es4, "fin", gains["norm_final"])
        keysT = c.sb(es4, "keysT", [128, 2, 8, 128], BF16)
        r_keysT = Res()
        with contextlib.ExitStack() as esk:
            kf = c.sb(esk, "kf", [128, 2, 8, 128], F32)
            kb = c.sb(esk, "kb", [128, 2, 8, 128], BF16)
            r_kf = Res()
            c.dma("sp", kf[:, 0, :, :], keys1.rearrange("h k d -> k h d"), writes=[r_kf])
            c.dma("sp", kf[:, 1, :, :], keys2.rearrange("h k d -> k h d"), writes=[r_kf])
            c.op("dve", lambda e: e.tensor_copy(out=kb[:], in_=kf[:]), reads=[r_kf], writes=[r_kf])
            transpose_to(kb[:].rearrange("p s h d -> p (s h d)"), r_kf, 128, 16,
                         lambda c0, c1: keysT[:].rearrange("p s h k -> p (s h) k")[:, c0:c1, :], r_keysT)
            c.barrier()
        iota_i = c.sb(es4, "iota_i", [128, 16], I32)
        iota16 = c.sb(es4, "iota16", [128, 16], F32)
        r_iota = Res()
        c.op("pool", lambda e: e.iota(iota_i[:], pattern=[[1, 16]], base=0, channel_multiplier=0), writes=[r_iota])
        c.op("dve", lambda e: e.tensor_copy(out=iota16[:], in_=iota_i[:]), reads=[r_iota], writes=[r_iota])

        x1 = [c.sb(es4, "x1_%d" % i, [128, D], F32) for i in range(G)]
        r_x1 = [Res() for _ in range(G)]
        h2 = [c.sb(es4, "h2_%d" % i, [128, D], BF16) for i in range(G)]
        r_h2 = [Res() for _ in range(G)]
        hb4 = c.sb(es4, "hb4", [128, D], BF16)
        r_hb4 = Res()
        bufA = c.sb(es4, "bufA", [128, 16, GT], BF16)
        bufB = c.sb(es4, "bufB", [128, 16, GT], BF16)
        r_bufA = Res()
        r_bufB = Res()
        pTm = c.sb(es4, "pTm", [128, 2, GT], BF16)
        r_pTm = Res()
        rsm = c.sb(es4, "rsm", [128, GT], F32)
        r_rsm = Res()
        sc = c.sb(es4, "sc", [128, 2048], F32)
        cand = c.sb(es4, "cand", [128, 2048], F32)
        r_sc = Res()
        r_cand = Res()
        wk = c.sb(es4, "wk", [128, 256], F32)
        r_wk = Res()
        tv = c.sb(es4, "tv", [128, 16, 16], F32)
        ti = c.sb(es4, "ti", [128, 16, 16], U32)
        tif = c.sb(es4, "tif", [128, 16, 16], F32)
        best = c.sb(es4, "best", [128, 8, 16], F32)
        pos = c.sb(es4, "pos", [128, 8, 16], U32)
        pa = c.sb(es4, "pa", [128, 128], U32)
        pbi = c.sb(es4, "pbi", [128, 128], U32)
        paf = c.sb(es4, "paf", [128, 128], F32)
        pbf = c.sb(es4, "pbf", [128, 128], F32)
        i1s = c.sb(es4, "i1s", [128, 128], F32)
        i2s = c.sb(es4, "i2s", [128, 128], F32)
        eidf = c.sb(es4, "eidf", [128, 128], F32)
        gate = c.sb(es4, "gate", [128, 8, 16], F32)
        gsm = c.sb(es4, "gsm", [128, 24], F32)
        r_pk = Res()
        idxT = c.sb(es4, "idxT", [128, 128], I32)
        GTt = c.sb(es4, "GTt", [128, 128], F32)
        apart = c.sb(es4, "apart", [128, 128, 4], F32)
        AT = c.sb(es4, "AT", [128, 128], F32)
        CT = c.sb(es4, "CT", [128, 128], BF16)
        r_idxT = Res()
        r_GTt = Res()
        r_apart = Res()
        r_CT = Res()
        NGB = 4
        gbuf = [c.sb(es4, "gbuf%d" % i, [128, D], BF16) for i in range(NGB)]
        r_gbuf = [Res() for _ in range(NGB)]
        NCB = 4
        cbuf = [c.sb(es4, "cbuf%d" % i, [128, 256], BF16) for i in range(NCB)]
        r_cbuf = [Res() for _ in range(NCB)]
        for i in range(NCB):
            c.op("pool", lambda e, i=i: e.memset(cbuf[i][:], 0.0), writes=[r_cbuf[i]])
        junk4 = c.sb(es4, "junk4", [128, 512], BF16)
        r_junk4 = Res()
        yout = cand
        r_yout = r_cand
        pctr = {"g": 0, "c": 0}

        def cross_attn(qT_, r_q, col0, n, mkT_, r_mk, mv_fn, r_mv, oT_, r_o):
            for h in range(4):
                for mt in range(2):
                    b = bank(2, 6)

                    def f(e, h=h, mt=mt, b=b):
                        inst = None
                        for dc in range(4):
                            inst = e.matmul(pb[b][:, 0:n], lhsT=mkT_[:, h * 4 + dc, mt * 128:(mt + 1) * 128],
                                            rhs=qT_[:, h * 4 + dc, col0:col0 + n], start=(dc == 0), stop=(dc == 3))
                        return inst
                    c.op("pe", f, reads=[r_q] + r_mk, writes=[rpb[b]])
                    c.op("act", lambda e, mt=mt, b=b: e.activation(out=pTm[:, mt, 0:n], in_=pb[b][:, 0:n],
                                                                  func=AF.Exp, scale=MEM_SCALE),
                         reads=[rpb[b]], writes=[r_pTm])
                b = bank(2, 6)

                def fs(e, b=b):
                    e.matmul(pb[b][:, 0:n], lhsT=ones[:], rhs=pTm[:, 0, 0:n], start=True, stop=False)
                    return e.matmul(pb[b][:, 0:n], lhsT=ones[:], rhs=pTm[:, 1, 0:n], start=False, stop=True)
                c.op("pe", fs, reads=[r_pTm, r_const], writes=[rpb[b]])
                c.op("dve", lambda e, b=b: e.reciprocal(out=rsm[:, 0:n], in_=pb[b][:, 0:n]), reads=[rpb[b]],
                     writes=[r_rsm])
                for dc in range(4):
                    b = bank(2, 6)

                    def fo(e, h=h, dc=dc, b=b):
                        c0 = h * 512 + dc * 128
                        e.matmul(pb[b][:, 0:n], lhsT=mv_fn(0, c0), rhs=pTm[:, 0, 0:n], start=True, stop=False)
                        return e.matmul(pb[b][:, 0:n], lhsT=mv_fn(1, c0), rhs=pTm[:, 1, 0:n],
                                        start=False, stop=True)
                    c.op("pe", fo, reads=[r_pTm] + r_mv, writes=[rpb[b]])
                    c.op("dve", lambda e, h=h, dc=dc, b=b: e.tensor_tensor(
                        out=oT_[:, h * 4 + dc, col0:col0 + n], in0=pb[b][:, 0:n], in1=rsm[:, 0:n], op=ALU.mult),
                        reads=[rpb[b], r_rsm], writes=[r_o])

        def top16(P, src_ap, n, dst_v, dst_i, rd, wr):
            c.op("dve", lambda e: e.max(out=dst_v[:, 0:8], in_=src_ap), reads=rd, writes=wr)
            c.op("dve", lambda e: e.max_index(out=dst_i[:, 0:8], in_max=dst_v[:, 0:8], in_values=src_ap),
                 reads=rd + wr, writes=wr)
            c.op("dve", lambda e: e.match_replace(out=wk[:P, 0:n], in_to_replace=dst_v[:, 0:8], in_values=src_ap,
                                                  imm_value=NEG), reads=rd + wr, writes=[r_wk])
            c.op("dve", lambda e: e.max(out=dst_v[:, 8:16], in_=wk[:P, 0:n]), reads=[r_wk], writes=wr)
            c.op("dve", lambda e: e.max_index(out=dst_i[:, 8:16], in_max=dst_v[:, 8:16], in_values=wk[:P, 0:n]),
                 reads=[r_wk] + wr, writes=wr)

        def peer_tile(P, nv, qpT, r_qp, col0, h2_t, r_h2t, x2_t, r_x2t, y_dst):
            def fsc(e):
                inst = None
                for hs in range(16):
                    inst = e.matmul(pb[4 + hs // 4][:P, (hs % 4) * 128:(hs % 4 + 1) * 128],
                                    lhsT=qpT[:, hs, col0:col0 + P], rhs=keysT[:, hs % 2, hs // 2, :],
                                    start=True, stop=True)
                return inst
            c.op("pe", fsc, reads=[r_qp, r_keysT], writes=[rpb[4], rpb[5], rpb[6], rpb[7]])
            for q in range(4):
                c.op("act", lambda e, q=q: e.activation(out=sc[:P, q * 512:(q + 1) * 512], in_=pb[4 + q][:P, :],
                                                        func=AF.Copy), reads=[rpb[4 + q]], writes=[r_sc])
            scv = sc[:P, :].rearrange("p (a k) -> p a k", k=128)
            for hs in range(16):
                top16(P, scv[:, hs, :], 128, tv[:P, hs, :], ti[:P, hs, :], [r_sc], [r_pk])
            tv4 = tv[:P].rearrange("p (h s) k -> p h s k", s=2)
            candv = cand[:P, :].rearrange("p (h a b) -> p h a b", a=16, b=16)
            c.op("dve", lambda e: e.tensor_tensor(out=candv, in0=tv4[:, :, 0, :].unsqueeze(3).to_broadcast([P, 8, 16, 16]),
                                                  in1=tv4[:, :, 1, :].unsqueeze(2).to_broadcast([P, 8, 16, 16]),
                                                  op=ALU.add), reads=[r_pk], writes=[r_cand])
            cand3 = cand[:P, :].rearrange("p (h c) -> p h c", c=256)
            for h in range(8):
                top16(P, cand3[:, h, :], 256, best[:P, h, :], pos[:P, h, :], [r_cand], [r_pk])
            c.op("dve", lambda e: e.tensor_scalar(out=gsm[:P, 0:8], in0=best[:P, :, 0], scalar1=-1.0, scalar2=None,
                                                  op0=ALU.mult), reads=[r_pk], writes=[r_pk])
            for h in range(8):
                c.op("act", lambda e, h=h: e.activation(out=gate[:P, h, :], in_=best[:P, h, :], func=AF.Exp,
                                                        bias=gsm[:P, h:h + 1], scale=1.0,
                                                        accum_out=gsm[:P, 8 + h:9 + h]), reads=[r_pk], writes=[r_pk])
            c.op("dve", lambda e: e.reciprocal(out=gsm[:P, 16:24], in_=gsm[:P, 8:16]), reads=[r_pk], writes=[r_pk])
            c.op("dve", lambda e: e.tensor_tensor(out=gate[:P], in0=gate[:P],
                                                  in1=gsm[:P, 16:24].unsqueeze(2).to_broadcast([P, 8, 16]),
                                                  op=ALU.mult), reads=[r_pk], writes=[r_pk])
            posf = pos[:P].rearrange("p h k -> p (h k)")
            c.op("dve", lambda e: e.tensor_copy(out=pbf[:P, :], in_=posf), reads=[r_pk], writes=[r_pk])
            c.op("dve", lambda e: e.tensor_scalar(out=paf[:P, :], in0=pbf[:P, :], scalar1=16.0, scalar2=None,
                                                  op0=ALU.is_ge), reads=[r_pk], writes=[r_pk])
            for m in range(2, 16):
                c.op("dve", lambda e, m=m: e.scalar_tensor_tensor(out=paf[:P, :], in0=pbf[:P, :], scalar=16.0 * m,
                                                                  in1=paf[:P, :], op0=ALU.is_ge, op1=ALU.add),
                     reads=[r_pk], writes=[r_pk])
            c.op("dve", lambda e: e.scalar_tensor_tensor(out=pbf[:P, :], in0=paf[:P, :], scalar=-16.0, in1=pbf[:P, :],
                                                         op0=ALU.mult, op1=ALU.add), reads=[r_pk], writes=[r_pk])
            c.op("dve", lambda e: e.tensor_copy(out=tif[:P], in_=ti[:P]), reads=[r_pk], writes=[r_pk])
            tif4 = tif[:P].rearrange("p (h s) k -> p h s k", s=2)
            ohv = sc[:P, :].rearrange("p (h a b) -> p h a b", a=16, b=16)
            io4 = iota16[:P, :].unsqueeze(1).unsqueeze(1).to_broadcast([P, 8, 16, 16])
            for side, (pf_, dsts) in enumerate(((paf, i1s), (pbf, i2s))):
                pv = pf_[:P, :].rearrange("p (h k) -> p h k", k=16).unsqueeze(3).to_broadcast([P, 8, 16, 16])
                c.op("dve", lambda e, pv=pv: e.tensor_tensor(out=ohv, in0=pv, in1=io4, op=ALU.is_equal),
                     reads=[r_pk, r_iota], writes=[r_sc])
                c.op("dve", lambda e, side=side: e.tensor_tensor(
                    out=ohv, in0=ohv, in1=tif4[:, :, side, :].unsqueeze(2).to_broadcast([P, 8, 16, 16]), op=ALU.mult),
                    reads=[r_sc, r_pk], writes=[r_sc])
                c.op("dve", lambda e, dsts=dsts: e.tensor_reduce(
                    out=dsts[:P, :].rearrange("p (h k) -> p h k", k=16), in_=ohv, axis=mybir.AxisListType.X,
                    op=ALU.add), reads=[r_sc], writes=[r_pk])
            c.op("dve", lambda e: e.scalar_tensor_tensor(out=eidf[:P, :], in0=i1s[:P, :], scalar=128.0, in1=i2s[:P, :],
                                                         op0=ALU.mult, op1=ALU.add), reads=[r_pk], writes=[r_pk])
            c.op("pe", lambda e: e.transpose(out=pb[0][:, 0:P], in_=eidf[:P, :], identity=identf[:P, :P]),
                 reads=[r_pk, r_const], writes=[rpb[0]])
            c.op("dve", lambda e: e.tensor_copy(out=idxT[:, 0:P], in_=pb[0][:, 0:P]), reads=[rpb[0]], writes=[r_idxT])
            c.op("pe", lambda e: e.transpose(out=pb[1][:, 0:P], in_=gate[:P].rearrange("p h k -> p (h k)"),
                                             identity=identf[:P, :P]), reads=[r_pk, r_const], writes=[rpb[1]])
            c.op("act", lambda e: e.activation(out=GTt[:, 0:P], in_=pb[1][:, 0:P], func=AF.Copy), reads=[rpb[1]],
                 writes=[r_GTt])
            for t in range(nv):
                k = pctr["g"] % NGB
                pctr["g"] += 1
                c.swdma(gbuf[k][:], peer_u[:, :], reads=[r_idxT], writes=[r_gbuf[k]],
                        indirect=bass.IndirectOffsetOnAxis(ap=idxT[:, t:t + 1], axis=0))
                s0 = (t % 2) * 4

                def fx(e, t=t, s0=s0):
                    inst = None
                    for q in range(4):
                        inst = e.matmul(pb[s0 + q][:, :], lhsT=ident[:P, t:t + 1].to_broadcast([P, 128]),
                                        rhs=h2_t[:P, q * 512:(q + 1) * 512], start=True, stop=True)
                    return inst
                c.op("pe", fx, reads=[r_h2t, r_const], writes=[rpb[s0 + q] for q in range(4)])
                for q in range(4):
                    c.op("dve", lambda e, t=t, q=q, k=k, s0=s0: e.scalar_tensor_tensor(
                        out=junk4[:], in0=gbuf[k][:, q * 512:(q + 1) * 512], scalar=1.0, in1=pb[s0 + q][:, :],
                        op0=ALU.mult, op1=ALU.mult, accum_out=apart[:, t, q:q + 1]),
                        reads=[r_gbuf[k], rpb[s0 + q]], writes=[r_junk4, r_apart])
            if nv < P:
                c.op("dve", lambda e: e.memset(apart[:, nv:P, :], 0.0), writes=[r_apart])
            c.op("dve", lambda e: e.tensor_reduce(out=AT[:, 0:P], in_=apart[:, 0:P, :], axis=mybir.AxisListType.X,
                                                  op=ALU.add), reads=[r_apart], writes=[r_apart])
            c.op("act", lambda e: e.activation(out=AT[:, 0:P], in_=AT[:, 0:P], func=AF.Gelu_apprx_tanh),
                 reads=[r_apart], writes=[r_apart])
            c.op("dve", lambda e: e.tensor_tensor(out=CT[:, 0:P], in0=AT[:, 0:P], in1=GTt[:, 0:P], op=ALU.mult),
                 reads=[r_apart, r_GTt], writes=[r_CT])
            for t in range(nv):
                k = pctr["g"] % NGB
                pctr["g"] += 1
                c.swdma(gbuf[k][:], peer_v[:, :], reads=[r_idxT], writes=[r_gbuf[k]],
                        indirect=bass.IndirectOffsetOnAxis(ap=idxT[:, t:t + 1], axis=0))
                kc = pctr["c"] % NCB
                pctr["c"] += 1
                c.op("act", lambda e, t=t, kc=kc: e.activation(out=cbuf[kc][:, 127:128], in_=CT[:, t:t + 1],
                                                               func=AF.Copy), reads=[r_CT], writes=[r_cbuf[kc]])

                def fy(e, t=t, k=k, kc=kc):
                    inst = None
                    for q in range(4):
                        inst = e.matmul(pb[q][:P, :], lhsT=cbuf[kc][:, 127 - t:127 - t + P],
                                        rhs=gbuf[k][:, q * 512:(q + 1) * 512], start=(t == 0), stop=(t == nv - 1))
                    return inst
                c.op("pe", fy, reads=[r_cbuf[kc], r_gbuf[k]], writes=[rpb[q] for q in range(4)])
            for q in range(4):
                c.op("dve", lambda e, q=q: e.tensor_tensor(out=x2_t[:P, q * 512:(q + 1) * 512], in0=pb[q][:P, :],
                                                           in1=x2_t[:P, q * 512:(q + 1) * 512], op=ALU.add),
                     reads=[rpb[q], r_x2t], writes=[r_x2t])
            rms_h(x2_t, r_x2t, P, g_fin, r_gfin, yout, r_yout)
            c.dma("sp", y_dst, yout[:nv, :], reads=[r_yout])

        def phase4_group(ntile, P, nv, x_src, mTg, r_mTg, cross_fn, y_dst_fn):
            ntok = ntile * P
            for j in range(ntile):
                if nv < P:
                    c.op("pool", lambda e, j=j: e.memset(x1[j][:], 0.0), writes=[r_x1[j]])
                c.dma("sp", x1[j][:nv, :], x_src(j), writes=[r_x1[j]])
            ws = WStream([(nm, b) for nm in ("w_out", "w_mq", "w_mo", "peer_wq") for b in range(4)])
            for blk in range(4):
                wt, r_w = ws.get()
                for j in range(ntile):
                    b = bank(2, 6)
                    mm_tok(mTg, r_mTg, j * P, P, wt, r_w, b)
                    c.op("dve", lambda e, j=j, blk=blk, b=b: e.tensor_tensor(
                        out=x1[j][:P, blk * 512:(blk + 1) * 512], in0=pb[b][:P, :],
                        in1=x1[j][:P, blk * 512:(blk + 1) * 512], op=ALU.add), reads=[rpb[b], r_x1[j]], writes=[r_x1[j]])
            for j in range(ntile):
                rms_h(x1[j], r_x1[j], P, g_mem, r_gmem, hb4, r_hb4)
                transpose_to(hb4, r_hb4, P, 16, lambda c0, c1, j=j: bufA[:, c0:c1, j * P:(j + 1) * P], r_bufA)
            for blk in range(4):
                wt, r_w = ws.get()
                for ct in range(4):
                    b = bank(2, 6)
                    mm_feat(bufA, r_bufA, 0, ntok, wt, r_w, ct, b)
                    c.op("act", lambda e, blk=blk, ct=ct, b=b: e.activation(
                        out=bufB[:, blk * 4 + ct, 0:ntok], in_=pb[b][:, 0:ntok], func=AF.Copy),
                        reads=[rpb[b]], writes=[r_bufB])
            cross_fn(bufB, r_bufB, bufA, r_bufA)
            for blk in range(4):
                wt, r_w = ws.get()
                for j in range(ntile):
                    b = bank(2, 6)
                    mm_tok(bufA, r_bufA, j * P, P, wt, r_w, b)
                    c.op("dve", lambda e, j=j, blk=blk, b=b: e.tensor_tensor(
                        out=x1[j][:P, blk * 512:(blk + 1) * 512], in0=pb[b][:P, :],
                        in1=x1[j][:P, blk * 512:(blk + 1) * 512], op=ALU.add), reads=[rpb[b], r_x1[j]], writes=[r_x1[j]])
            for j in range(ntile):
                rms_h(x1[j], r_x1[j], P, g_peer, r_gpeer, h2[j], r_h2[j])
                transpose_to(h2[j], r_h2[j], P, 16, lambda c0, c1, j=j: bufB[:, c0:c1, j * P:(j + 1) * P], r_bufB)
            for blk in range(4):
                wt, r_w = ws.get()
                for ct in range(4):
                    b = bank(2, 6)
                    mm_feat(bufB, r_bufB, 0, ntok, wt, r_w, ct, b)
                    c.op("act", lambda e, blk=blk, ct=ct, b=b: e.activation(
                        out=bufA[:, blk * 4 + ct, 0:ntok], in_=pb[b][:, 0:ntok], func=AF.Copy),
                        reads=[rpb[b]], writes=[r_bufA])
            for j in range(ntile):
                peer_tile(P, nv, bufA, r_bufA, j * P, h2[j], r_h2[j], x1[j], r_x1[j], y_dst_fn(j))

        mTg = c.sb(es4, "mTg", [128, 16, GT], BF16)
        r_mTg = Res()
        for g in range((NT // G) if 5 in _PH else 0)[:_LIM["groups5"]]:
            c.dma("sp", mTg[:], mTs[:, :, g * GT:(g + 1) * GT].rearrange("k p t -> p k t"), reads=r_mTs,
                  writes=[r_mTg])
            phase4_group(
                G, 128, 128, lambda j, g=g: xo[g * GT + j * 128:g * GT + (j + 1) * 128, :], mTg, r_mTg,
                lambda qT_, r_q, oT_, r_o: cross_attn(qT_, r_q, 0, GT, mkT, [r_mkT],
                                                      lambda mt, c0: mv_bf[:, mt, c0:c0 + 128], [r_mvbf], oT_, r_o),
                lambda j, g=g: y_p[g * GT + j * 128:g * GT + (j + 1) * 128, :])

        def cross_sample(qT_, r_q, oT_, r_o):
            stage = ((sc, r_sc), (cand, r_cand))
            n = 0
            for b in range(4):
                for kv in range(2):
                    for mt in range(2):
                        st, r_st = stage[n % 2]
                        n += 1
                        c.dma("sp", st[:], cmem[b, mt * 128:(mt + 1) * 128, kv, :], writes=[r_st])
                        k = kv * 2 + mt
                        c.op("pool", lambda e, st=st, k=k: e.tensor_copy(out=gbuf[k][:], in_=st[:]), reads=[r_st],
                             writes=[r_gbuf[k]])
                for mt in range(2):
                    transpose_to(gbuf[mt], r_gbuf[mt], 128, 16,
                                 lambda c0, c1, mt=mt: mTg[:, c0:c1, mt * 128:(mt + 1) * 128], r_mTg)
                cross_attn(qT_, r_q, b * 8, 8, mTg, [r_mTg], lambda mt, c0: gbuf[2 + mt][:, c0:c0 + 128],
                           [r_gbuf[2], r_gbuf[3]], oT_, r_o)

        if 6 in _PH:
            phase4_group(1, 128, SP, lambda j: xs[:, :], mTg_s, r_mTgs, cross_sample, lambda j: y_s[:, :])

        c.finish()
    return nc


def _host_tables(rel_bias):
    biasT = np.zeros((24, 128, 2, 128), np.float32)
    jl = np.arange(128)[:, None, None]
    jt = np.arange(2)[None, :, None]
    i = np.arange(128)[None, None, :]
    off = i + 128 - (jt * 128 + jl)
    band = ((off >= 0) & (off <= 128)).astype(np.float32)
    offc = np.clip(off, 0, 128)
    for g, dil in enumerate(DILS):
        bucket = _t5_bucket(dil * np.arange(129))
        for h in range(8):
            biasT[g * 8 + h] = rel_bias[bucket[offc], g * 8 + h]
    sbias = np.zeros((8, 128, 16, 24), np.float32)
    smask = np.zeros((128, 16, 24), np.float32)
    sbias_n = np.zeros((8, 32, 4, 24), np.float32)
    smask_n = np.zeros((32, 4, 24), np.float32)
    for g, dil in enumerate(DILS):
        bucket = _t5_bucket(dil * np.arange(129))
        for t in range(8):
            for j in range(129):
                row = 2048 + t - dil * j
                col = g * 8 + t
                if row < 2048:
                    smask[row % 128, row // 128, col] = 1.0
                    sbias[:, row % 128, row // 128, col] = rel_bias[bucket[j], g * 8:(g + 1) * 8]
                else:
                    tp = row - 2048
                    for b in range(4):
                        smask_n[b * 8 + tp, b, col] = 1.0
                        sbias_n[:, b * 8 + tp, b, col] = rel_bias[bucket[j], g * 8:(g + 1) * 8]
    return biasT, band, sbias, smask, sbias_n, smask_n


_NC_CACHE = {}


def _prepare(x_prompt, x_sample, mem_prompt, cache_win, cache_mem_kv, rel_bias, norm_mix, w_in,
             sgu_ln_g, sgu_ln_b, sgu_w, sgu_b, w_out, norm_mem, norm_memtok, w_mq, w_mk, w_mv, w_mo,
             norm_peer, peer_wq, peer_keys1, peer_keys2, peer_u, peer_v, norm_final):
    f = lambda a: np.ascontiguousarray(np.asarray(a, dtype=np.float32))
    xp = f(x_prompt)[0]
    xsm = f(x_sample)
    rel_bias = f(rel_bias)
    biasT, band, sbias, smask, sbias_n, smask_n = _host_tables(rel_bias)
    sgu_w0 = f(sgu_w)[0]
    sgu_wT = np.ascontiguousarray(sgu_w0.transpose(0, 2, 1))
    sgu_wTs = np.zeros((8, 128, 128), np.float32)
    for b in range(4):
        sgu_wTs[:, b * 8:(b + 1) * 8, b * 8:(b + 1) * 8] = sgu_wT[:, :8, :8]
    sgu_b0 = f(sgu_b)[0]
    sgu_bT = np.ascontiguousarray(sgu_b0.T)
    sgu_bTs = np.zeros((128, 8), np.float32)
    sgu_bTs[:32] = np.tile(sgu_b0[:, :8].T, (4, 1))
    shared = {
        "mem": f(mem_prompt)[0], "w_in": f(w_in)[0], "w_out": f(w_out)[0], "w_mq": f(w_mq)[0],
        "w_mk": f(w_mk)[0], "w_mv": f(w_mv)[0], "w_mo": f(w_mo)[0], "peer_wq": f(peer_wq)[0],
        "peer_u": f(peer_u)[0], "peer_v": f(peer_v)[0], "keys1": f(peer_keys1)[0], "keys2": f(peer_keys2)[0],
        "norm_mix": f(norm_mix), "norm_mem": f(norm_mem), "norm_memtok": f(norm_memtok),
        "norm_peer": f(norm_peer), "norm_final": f(norm_final).reshape(1, D),
        "sgu_ln_g": f(sgu_ln_g), "sgu_ln_b": f(sgu_ln_b), "sgu_wT": sgu_wT, "sgu_wTs": sgu_wTs,
        "sgu_bT": sgu_bT, "sgu_bTs": sgu_bTs, "biasT": biasT, "bandmask": band, "sbias": sbias,
        "smask": smask, "sbias_n": sbias_n, "smask_n": smask_n,
    }
    cw = f(cache_win)[0]
    cm = f(cache_mem_kv)[0].reshape(32, 256, 2, 2048)
    in_maps = []
    for cidx in range(NCORES):
        m = dict(shared)
        m["xo"] = xp[cidx * TOK:(cidx + 1) * TOK]
        m["xh"] = xp[(cidx - 1) * TOK:cidx * TOK] if cidx > 0 else np.zeros((TOK, D), np.float32)
        m["xs"] = np.ascontiguousarray(xsm[cidx * 4:(cidx + 1) * 4].reshape(SP, D))
        m["cwin"] = cw[cidx * 4:(cidx + 1) * 4]
        m["cmem"] = cm[cidx * 4:(cidx + 1) * 4]
        m["pflag"] = np.full((128, 1), 0.0 if cidx == 0 else 1.0, np.float32)
        in_maps.append(m)
    return in_maps


def kernel(**inputs):
    in_maps = _prepare(**inputs)
    if "nc" not in _NC_CACHE:
        _NC_CACHE["nc"] = build_program()
    ncr = _LIM.get("ncores") or NCORES
    res = run_bass_kernel_spmd(_NC_CACHE["nc"], in_maps[:ncr], core_ids=list(range(ncr)))
    R = list(res.results)
    while len(R) < NCORES:
        R.append(R[0])
    y_prompt = np.concatenate([R[i]["y_p"] for i in range(NCORES)], axis=0).reshape(1, NCORES * TOK, D)
    y_sample = np.concatenate([R[i]["y_s"] for i in range(NCORES)], axis=0).reshape(32, 8, D)
    win_p = R[NCORES - 1]["win_p"].reshape(1, 1, TOK, 2, 8, 128)
    memkv = R[0]["memkv"].reshape(1, 1, 256, 2, 4, 512)
    win_s = np.concatenate([R[i]["win_s"] for i in range(NCORES)], axis=0).reshape(1, 32, 8, 2, 8, 128)
    sgu_s = np.concatenate([R[i]["sgu_s"] for i in range(NCORES)], axis=0).reshape(1, 32, 8, 1024)
    return (y_prompt, y_sample, win_p, memkv, win_s, sgu_s)
```

```python
import contextlib
import math
import numpy as np
import concourse.bass as bass
import concourse.mybir as mybir
from concourse.bass_utils import run_bass_kernel_spmd

F32 = mybir.dt.float32
BF16 = mybir.dt.bfloat16
I32 = mybir.dt.int32
U32 = mybir.dt.uint32
AF = mybir.ActivationFunctionType
ALU = mybir.AluOpType

NCORES = 8
D = 2048
TOK = 2048
NT = TOK // 128
SP = 32
EPS = 1e-6
DILS = (1, 4, 16)
REL_BUCKETS = 32
REL_MAX_DIST = 2048
ATT_SCALE = 128 ** -0.5
MEM_SCALE = 512 ** -0.5
NEG = -1e30


class Res:
    __slots__ = ("w", "r")

    def __init__(self):
        self.w = None
        self.r = {}


class Ctx:
    NDMA = 32

    def __init__(self, nc, es):
        self.nc = nc
        self.es = es
        self.eng = {"pe": nc.tensor, "act": nc.scalar, "dve": nc.vector, "pool": nc.gpsimd, "sp": nc.sync}
        self.sem = {}
        self.cnt = {}
        self.known = {e: {} for e in self.eng}
        for e in self.eng:
            self.sem[e] = es.enter_context(nc.semaphore("s_" + e))
            self.cnt[e] = 0
        self.dsem = [es.enter_context(nc.semaphore("d%d" % i)) for i in range(self.NDMA)]
        self.dtgt = [0] * self.NDMA
        self.drr = 0
        self.nop = 0
        self.muted = False
        self.NSW = 8
        self.swsem = [es.enter_context(nc.semaphore("w%d" % i)) for i in range(self.NSW)]
        self.swtgt = [0] * self.NSW
        self.swrr = 0

    def sb(self, es, name, shape, dt):
        return es.enter_context(self.nc.sbuf_tensor(name, list(shape), dt))

    def _semof(self, key):
        if isinstance(key, str):
            return self.sem[key]
        if isinstance(key, tuple):
            return self.swsem[key[1]]
        return self.dsem[key]

    def swdma(self, out, in_, reads=(), writes=(), indirect=None, **kw):
        if self.muted:
            return None
        i = self.swrr % self.NSW
        self.swrr += 1
        deps = self._deps(reads, writes)
        if self.swtgt[i]:
            deps.append((("w", i), self.swtgt[i]))
        self._wait("pool", deps)
        self.swtgt[i] += 16
        if indirect is not None:
            inst = self.eng["pool"].indirect_dma_start(out=out, out_offset=None, in_=in_, in_offset=indirect, **kw)
        else:
            inst = self.eng["pool"].dma_start(out=out, in_=in_, **kw)
        inst.then_inc(self.swsem[i], 16)
        tok = (("w", i), self.swtgt[i])
        self._mark(tok, reads, writes)
        self.nop += 1
        return tok

    def _wait(self, e, deps):
        need = {}
        for tok in deps:
            if tok is None:
                continue
            k, v = tok
            if self.known[e].get(k, 0) >= v:
                continue
            if need.get(k, 0) < v:
                need[k] = v
        for k, v in need.items():
            self.eng[e].wait_ge(self._semof(k), v)
            self.known[e][k] = v

    @staticmethod
    def _deps(reads, writes):
        deps = []
        for r in reads:
            deps.append(r.w)
        for r in writes:
            deps.append(r.w)
            for k, v in r.r.items():
                deps.append((k, v))
        return deps

    @staticmethod
    def _mark(tok, reads, writes):
        k, v = tok
        for r in reads:
            if r.r.get(k, 0) < v:
                r.r[k] = v
        for r in writes:
            r.w = tok
            r.r = {}

    def op(self, e, fn, reads=(), writes=()):
        if self.muted:
            return None
        self._wait(e, self._deps(reads, writes))
        inst = fn(self.eng[e])
        self.cnt[e] += 1
        inst.then_inc(self.sem[e], 1)
        tok = (e, self.cnt[e])
        self._mark(tok, reads, writes)
        self.nop += 1
        return tok

    def dma(self, q, out, in_, reads=(), writes=(), indirect=None, **kw):
        if self.muted:
            return None
        i = self.drr % self.NDMA
        self.drr += 1
        deps = self._deps(reads, writes)
        if self.dtgt[i]:
            deps.append((i, self.dtgt[i]))
        self._wait(q, deps)
        self.dtgt[i] += 16
        if indirect is not None:
            inst = self.eng[q].indirect_dma_start(out=out, out_offset=None, in_=in_, in_offset=indirect, **kw)
        else:
            inst = self.eng[q].dma_start(out=out, in_=in_, **kw)
        inst.then_inc(self.dsem[i], 16)
        tok = (i, self.dtgt[i])
        self._mark(tok, reads, writes)
        self.nop += 1
        return tok

    def _swtoks(self):
        return [(("w", i), t) for i, t in enumerate(self.swtgt) if t]

    def barrier(self):
        self.muted = False
        deps = [(i, t) for i, t in enumerate(self.dtgt) if t] + self._swtoks()
        deps += [(e, n) for e, n in self.cnt.items() if n]
        for e in self.eng:
            self._wait(e, [d for d in deps if d[0] != e])

    def finish(self):
        deps = [(i, t) for i, t in enumerate(self.dtgt) if t] + self._swtoks()
        deps += [(e, n) for e, n in self.cnt.items() if n and e != "sp"]
        self._wait("sp", deps)


def _t5_bucket(dist):
    max_exact = REL_BUCKETS // 2
    d = np.maximum(dist, 1).astype(np.float64)
    large = max_exact + (np.log(d / max_exact) / math.log(REL_MAX_DIST / max_exact)
                         * (REL_BUCKETS - max_exact)).astype(np.int64)
    large = np.minimum(large, REL_BUCKETS - 1)
    return np.where(dist < max_exact, dist, large).astype(np.int32)


class _SkipPhase(Exception):
    pass


_PH = {0, 1, 2, 3, 4, 5, 6}
_LIM = {"groups": None, "heads": None, "groups5": None}


def build_program():
    nc = bass.Bass("TRN2", target_bir_lowering=False)

    def din(name, shape, dt=F32):
        return nc.dram_tensor(name, list(shape), dt, kind="ExternalInput").ap()

    def dout(name, shape, dt=F32):
        return nc.dram_tensor(name, list(shape), dt, kind="ExternalOutput").ap()

    def dscr(name, shape, dt=BF16):
        return nc.dram_tensor(name, list(shape), dt, kind="Internal").ap()

    xo = din("xo", [TOK, D])
    xh = din("xh", [TOK, D])
    xs = din("xs", [SP, D])
    mem = din("mem", [256, D])
    cwin = din("cwin", [4, 2048, 2, 8, 128])
    cmem = din("cmem", [4, 256, 2, 2048])
    w_in = din("w_in", [D, 9216])
    wsq = {n: din(n, [D, D]) for n in ("w_out", "w_mq", "w_mk", "w_mv", "w_mo", "peer_wq")}
    peer_u = din("peer_u", [16384, D])
    peer_v = din("peer_v", [16384, D])
    keys1 = din("keys1", [8, 128, 128])
    keys2 = din("keys2", [8, 128, 128])
    gains = {n: din(n, [1, D]) for n in ("norm_mix", "norm_mem", "norm_memtok", "norm_peer", "norm_final")}
    sgu_ln_g = din("sgu_ln_g", [1, 1024])
    sgu_ln_b = din("sgu_ln_b", [1, 1024])
    sgu_wT = din("sgu_wT", [8, 128, 128])
    sgu_wTs = din("sgu_wTs", [8, 128, 128])
    sgu_bT = din("sgu_bT", [128, 8])
    sgu_bTs = din("sgu_bTs", [128, 8])
    biasT = din("biasT", [24, 128, 2, 128])
    bandmask = din("bandmask", [128, 2, 128])
    sbias = din("sbias", [8, 128, 16, 24])
    smask = din("smask", [128, 16, 24])
    sbias_n = din("sbias_n", [8, 32, 4, 24])
    smask_n = din("smask_n", [32, 4, 24])
    pflag = din("pflag", [128, 1])

    y_p = dout("y_p", [TOK, D])
    y_s = dout("y_s", [SP, D])
    win_p = dout("win_p", [TOK, 2, 1024])
    memkv = dout("memkv", [256, 2, 2048])
    win_s = dout("win_s", [SP, 2, 1024])
    sgu_s = dout("sgu_s", [SP, 1024])

    WBLK = {"w_in": 18, "w_out": 4, "w_mq": 4, "w_mk": 4, "w_mv": 4, "w_mo": 4, "peer_wq": 4}
    wscr = {n: dscr("wb_" + n, [k, 128, 16, 512]) for n, k in WBLK.items()}
    wscr_res = {n: [Res() for _ in range(k)] for n, k in WBLK.items()}
    tabu = dscr("tabu", [16384, D])
    tabv = dscr("tabv", [16384, D])
    r_tab = Res()
    qTs = dscr("qTs", [24, 128, TOK])
    kTs = dscr("kTs", [8, 128, 2 * TOK])
    vTs = dscr("vTs", [8, 128, 2 * TOK])
    gbTs = dscr("gbTs", [8, 128, TOK])
    mTs = dscr("mTs", [16, 128, TOK])
    r_qTs = [Res() for _ in range(24)]
    r_kTs = [Res() for _ in range(8)]
    r_vTs = [Res() for _ in range(8)]
    r_gbTs = [Res() for _ in range(8)]
    r_mTs = [Res() for _ in range(16)]

    with contextlib.ExitStack() as es:
        c = Ctx(nc, es)

        pb = [es.enter_context(nc.psum_tensor("pb%d" % i, [128, 512], F32)) for i in range(8)]
        rpb = [Res() for _ in range(8)]
        pbb = [p[:].bitcast(BF16) for p in pb]

        identf = c.sb(es, "identf", [128, 128], F32)
        ident = c.sb(es, "ident", [128, 128], BF16)
        ones = c.sb(es, "ones", [128, 128], BF16)
        r_const = Res()
        c.op("pool", lambda e: e.memset(identf[:], 0.0), writes=[r_const])
        c.op("pool", lambda e: e.affine_select(out=identf[:], in_=identf[:], pattern=[[-1, 128]],
                                               compare_op=ALU.not_equal, fill=1.0, base=0,
                                               channel_multiplier=1), reads=[r_const], writes=[r_const])
        c.op("dve", lambda e: e.tensor_copy(out=ident[:], in_=identf[:]), reads=[r_const], writes=[r_const])
        c.op("dve", lambda e: e.memset(ones[:], 1.0), writes=[r_const])

        def cut(k):
            if _LIM.get("cut") == k:
                c.muted = True

        wpool = {"buf": [], "res": [], "n": 0}
        wctr = [0]

        def make_wpool(es_, n):
            tag = "%d" % len(wpool.setdefault("gen", []))
            wpool["gen"].append(n)
            wpool["buf"] = [c.sb(es_, "wbuf%s_%d" % (tag, i), [128, 16, 512], BF16) for i in range(n)]
            wpool["res"] = [Res() for _ in range(n)]
            wpool["n"] = n
            wctr[0] = 0

        class WStream:
            def __init__(self, blocks):
                self.blocks = list(blocks)
                self.issued = 0
                self.pos = 0
                self.base = wctr[0]

            def _issue(self):
                name, blk = self.blocks[self.issued]
                i = (self.base + self.issued) % wpool["n"]
                c.dma("sp", wpool["buf"][i][:], wscr[name][blk], reads=[wscr_res[name][blk]],
                      writes=[wpool["res"][i]])
                self.issued += 1

            def get(self):
                while self.issued < len(self.blocks) and self.issued < self.pos + wpool["n"]:
                    self._issue()
                i = (self.base + self.pos) % wpool["n"]
                self.pos += 1
                wctr[0] = self.base + self.pos
                return wpool["buf"][i], wpool["res"][i]

        def load_gain(es_, name, ap, width=D):
            t = c.sb(es_, "g_" + name, [128, width], F32)
            r = Res()
            c.dma("sp", t[:], ap[0:1, :].partition_broadcast(128), writes=[r])
            return t, r

        bankctr = [0]

        def bank(lo, hi):
            n = hi - lo
            i = lo + bankctr[0] % n
            bankctr[0] += 1
            return i

        NSM = 4
        sm = [c.sb(es, "sm%d" % i, [128, 8], F32) for i in range(NSM)]
        r_sm = [Res() for _ in range(NSM)]
        smctr = [0]
        junk = c.sb(es, "junk", [128, D], BF16)
        r_junk = Res()

        def rms_h(x_t, r_x, P, gain_t, r_gain, h_t, r_h):
            i = smctr[0] % NSM
            smctr[0] += 1
            s, rs = sm[i], r_sm[i]
            c.op("act", lambda e: e.activation(out=junk[:P, :], in_=x_t[:P, :], func=AF.Square,
                                               accum_out=s[:P, 0:1]), reads=[r_x], writes=[r_junk, rs])
            c.op("act", lambda e: e.activation(out=s[:P, 1:2], in_=s[:P, 0:1], func=AF.Sqrt,
                                               scale=1.0 / D, bias=epsc[:P, 0:1]), reads=[rs, r_const], writes=[rs])
            c.op("dve", lambda e: e.reciprocal(out=s[:P, 2:3], in_=s[:P, 1:2]), reads=[rs], writes=[rs])
            c.op("dve", lambda e: e.scalar_tensor_tensor(out=h_t[:P, :], in0=x_t[:P, :], scalar=s[:P, 2:3],
                                                         in1=gain_t[:P, :], op0=ALU.mult, op1=ALU.mult),
                 reads=[r_x, rs, r_gain], writes=[r_h])

        epsc = c.sb(es, "epsc", [128, 1], F32)
        c.op("dve", lambda e: e.memset(epsc[:], EPS), writes=[r_const])

        def transpose_to(h_t, r_h, P, nchunk, dst_fn, r_dst, evac="act"):
            for c0 in range(0, nchunk, 8):
                c1 = min(nchunk, c0 + 8)
                b = bank(0, 2)
                pv = pbb[b][:, 0:(c1 - c0) * 128].rearrange("p (a t) -> p a t", t=128)

                def f(e, c0=c0, c1=c1, pv=pv):
                    inst = None
                    for k in range(c0, c1):
                        inst = e.transpose(out=pv[:, k - c0, 0:P], in_=h_t[:P, k * 128:(k + 1) * 128],
                                           identity=ident[:P, :P])
                    return inst
                c.op("pe", f, reads=[r_h, r_const], writes=[rpb[b]])
                dst = dst_fn(c0, c1)
                if evac == "act":
                    c.op("act", lambda e, dst=dst, pv=pv: e.activation(out=dst, in_=pv[:, :, 0:P], func=AF.Copy),
                         reads=[rpb[b]], writes=[r_dst])
                else:
                    c.op("dve", lambda e, dst=dst, pv=pv: e.tensor_copy(out=dst, in_=pv[:, :, 0:P]),
                         reads=[rpb[b]], writes=[r_dst])

        def mm_tok(hT, r_hT, t0, P, wt, r_w, b, ncol=512, c0=0):
            def f(e):
                inst = None
                for kc in range(16):
                    inst = e.matmul(pb[b][:P, 0:ncol], lhsT=hT[:, kc, t0:t0 + P], rhs=wt[:, kc, c0:c0 + ncol],
                                    start=(kc == 0), stop=(kc == 15))
                return inst
            c.op("pe", f, reads=[r_hT, r_w], writes=[rpb[b]])

        def mm_feat(hT, r_hT, t0, ntok, wt, r_w, ct, b):
            def f(e):
                inst = None
                for kc in range(16):
                    inst = e.matmul(pb[b][:, 0:ntok], lhsT=wt[:, kc, ct * 128:(ct + 1) * 128],
                                    rhs=hT[:, kc, t0:t0 + ntok], start=(kc == 0), stop=(kc == 15))
                return inst
            c.op("pe", f, reads=[r_hT, r_w], writes=[rpb[b]])

        with contextlib.suppress(_SkipPhase), contextlib.ExitStack() as es0:
            if 0 not in _PH:
                raise _SkipPhase()
            NST = 3
            stf = [c.sb(es0, "stf%d" % i, [128, 16, 512], F32) for i in range(NST)]
            stb = [c.sb(es0, "stb%d" % i, [128, 16, 512], BF16) for i in range(NST)]
            r_stf = [Res() for _ in range(NST)]
            r_stb = [Res() for _ in range(NST)]
            n = 0
            wlist = [("w_mk", wsq["w_mk"]), ("w_mv", wsq["w_mv"]), ("w_in", w_in), ("w_out", wsq["w_out"]),
                     ("w_mq", wsq["w_mq"]), ("w_mo", wsq["w_mo"]), ("peer_wq", wsq["peer_wq"])]
            for name, W in wlist:
                Wv = W.rearrange("(kc p) n -> p kc n", p=128)
                for blk in range(WBLK[name]):
                    i = n % NST
                    c.dma("sp", stf[i][:], Wv[:, :, blk * 512:(blk + 1) * 512], writes=[r_stf[i]])
                    if n % 2 == 0:
                        c.op("dve", lambda e, i=i: e.tensor_copy(out=stb[i][:], in_=stf[i][:]),
                             reads=[r_stf[i]], writes=[r_stb[i]])
                    else:
                        c.op("pool", lambda e, i=i: e.tensor_copy(out=stb[i][:], in_=stf[i][:]),
                             reads=[r_stf[i]], writes=[r_stb[i]])
                    c.dma("act", wscr[name][blk], stb[i][:], reads=[r_stb[i]], writes=[wscr_res[name][blk]])
                    n += 1
            if 5 in _PH or 6 in _PH:
                for src, dst in ((peer_u, tabu), (peer_v, tabv)):
                    sv = src.rearrange("(c p r) d -> c p (r d)", p=128, r=4)
                    dv = dst.rearrange("(c p r) d -> c p (r d)", p=128, r=4)
                    for ci in range(32):
                        i = n % NST
                        c.dma("sp", stf[i][:].rearrange("p a b -> p (a b)"), sv[ci], writes=[r_stf[i]])
                        if n % 2 == 0:
                            c.op("dve", lambda e, i=i: e.tensor_copy(out=stb[i][:], in_=stf[i][:]),
                                 reads=[r_stf[i]], writes=[r_stb[i]])
                        else:
                            c.op("pool", lambda e, i=i: e.tensor_copy(out=stb[i][:], in_=stf[i][:]),
                                 reads=[r_stf[i]], writes=[r_stb[i]])
                        c.dma("act", dv[ci], stb[i][:].rearrange("p a b -> p (a b)"), reads=[r_stb[i]], writes=[r_tab])
                        n += 1
            c.barrier()

        mkT = c.sb(es, "mkT", [128, 16, 256], BF16)
        mv_bf = c.sb(es, "mv_bf", [128, 2, D], BF16)
        r_mkT = Res()
        r_mvbf = Res()

        with contextlib.suppress(_SkipPhase), contextlib.ExitStack() as es1:
            if 1 not in _PH:
                raise _SkipPhase()
            make_wpool(es1, 3)
            g_mt, r_gmt = load_gain(es1, "memtok", gains["norm_memtok"])
            mT = c.sb(es1, "mT", [128, 16, 256], BF16)
            r_mT = Res()
            xm = c.sb(es1, "xm", [128, D], F32)
            r_xm = Res()
            hm = c.sb(es1, "hm1", [128, D], BF16)
            r_hm = Res()
            mk_bf = c.sb(es1, "mk_bf", [128, 2, D], BF16)
            r_mkbf = Res()
            kvf = [c.sb(es1, "kvf%d" % i, [128, D], F32) for i in range(2)]
            r_kvf = [Res() for _ in range(2)]
            for mt in range(2):
                c.dma("sp", xm[:], mem[mt * 128:(mt + 1) * 128, :], writes=[r_xm])
                rms_h(xm, r_xm, 128, g_mt, r_gmt, hm, r_hm)
                transpose_to(hm, r_hm, 128, 16, lambda c0, c1, mt=mt: mT[:, c0:c1, mt * 128:(mt + 1) * 128], r_mT)
            n = 0
            for kv, name in enumerate(("w_mk", "w_mv")):
                bf = mk_bf if kv == 0 else mv_bf
                r_bf = r_mkbf if kv == 0 else r_mvbf
                for mt in range(2):
                    i = n % 2
                    n += 1
                    ws = WStream([(name, blk) for blk in range(4)])
                    for blk in range(4):
                        wt, r_w = ws.get()
                        b = bank(2, 6)
                        mm_tok(mT, r_mT, mt * 128, 128, wt, r_w, b)
                        c.op("act", lambda e, i=i, blk=blk, b=b: e.activation(
                            out=kvf[i][:, blk * 512:(blk + 1) * 512], in_=pb[b][:, :], func=AF.Copy),
                            reads=[rpb[b]], writes=[r_kvf[i]])
                    c.op("dve", lambda e, i=i, mt=mt, bf=bf: e.tensor_copy(out=bf[:, mt, :], in_=kvf[i][:]),
                         reads=[r_kvf[i]], writes=[r_bf])
                    c.dma("sp", memkv[mt * 128:(mt + 1) * 128, kv, :], kvf[i][:], reads=[r_kvf[i]])
            for mt in range(2):
                transpose_to(mk_bf[:, mt, :], r_mkbf, 128, 16,
                             lambda c0, c1, mt=mt: mkT[:, c0:c1, mt * 128:(mt + 1) * 128], r_mkT)
            c.barrier()

        G = 2
        GT = G * 128

        def gmlp_tile(es_, P, gv_j, r_gv, gu_j, r_gu, sga_j, r_sga, Wmix, r_W, bs, r_bs, lng, lnb, r_ln,
                      tmp, mA, r_mA, vln_out=None, r_vln=None):
            st, mvv, vt, vlnb, gs = tmp["st"], tmp["mvv"], tmp["vt"], tmp["vlnb"], tmp["gs"]
            r_t = tmp["r"]
            c.op("dve", lambda e: e.bn_stats(out=st[:P, 0, :], in_=gv_j[:P, 0:512]), reads=[r_gv], writes=[r_t])
            c.op("dve", lambda e: e.bn_stats(out=st[:P, 1, :], in_=gv_j[:P, 512:1024]), reads=[r_gv], writes=[r_t])
            c.op("dve", lambda e: e.bn_aggr(out=mvv[:P, 0:2], in_=st[:P].rearrange("p a b -> p (a b)")),
                 reads=[r_t], writes=[r_t])
            c.op("act", lambda e: e.activation(out=mvv[:P, 2:3], in_=mvv[:P, 1:2], func=AF.Sqrt,
                                               bias=epsc[:P, 0:1], scale=1.0), reads=[r_t, r_const], writes=[r_t])
            c.op("dve", lambda e: e.reciprocal(out=mvv[:P, 3:4], in_=mvv[:P, 2:3]), reads=[r_t], writes=[r_t])
            c.op("dve", lambda e: e.tensor_scalar(out=vt[:P, :], in0=gv_j[:P, :], scalar1=mvv[:P, 0:1],
                                                  scalar2=mvv[:P, 3:4], op0=ALU.subtract, op1=ALU.mult),
                 reads=[r_gv, r_t], writes=[r_t])
            c.op("dve", lambda e: e.tensor_tensor(out=vt[:P, :], in0=vt[:P, :], in1=lng[:P, :], op=ALU.mult),
                 reads=[r_t, r_ln], writes=[r_t])
            if vln_out is None:
                c.op("pool", lambda e: e.tensor_tensor(out=vlnb[:P, :], in0=vt[:P, :], in1=lnb[:P, :], op=ALU.add),
                     reads=[r_t, r_ln], writes=[r_t])
            else:
                c.op("pool", lambda e: e.tensor_tensor(out=vln_out[:P, :], in0=vt[:P, :], in1=lnb[:P, :],
                                                       op=ALU.add), reads=[r_t, r_ln], writes=[r_vln])
                c.op("act", lambda e: e.activation(out=vlnb[:P, :], in_=vln_out[:P, :], func=AF.Copy),
                     reads=[r_vln], writes=[r_t])

            def f(e):
                inst = None
                for g in range(8):
                    inst = e.matmul(pb[6 + g // 4][:P, (g % 4) * 128:(g % 4 + 1) * 128], lhsT=Wmix[:P, g, :P],
                                    rhs=vlnb[:P, g * 128:(g + 1) * 128], start=True, stop=True)
                return inst
            c.op("pe", f, reads=[r_t, r_W], writes=[rpb[6], rpb[7]])
            for half in range(2):
                c.op("dve", lambda e, half=half: e.tensor_tensor(
                    out=vt[:P, half * 512:(half + 1) * 512].rearrange("p (g d) -> p g d", d=128),
                    in0=pb[6 + half][:P, :].rearrange("p (g d) -> p g d", d=128),
                    in1=bs[:P, half * 4:half * 4 + 4].unsqueeze(2).to_broadcast([P, 4, 128]), op=ALU.add),
                    reads=[rpb[6 + half], r_bs], writes=[r_t])
            c.op("pool", lambda e: e.tensor_tensor(out=gs[:P, :], in0=gu_j[:P, :], in1=sga_j[:P, :], op=ALU.mult),
                 reads=[r_gu, r_sga], writes=[r_t])
            c.op("dve", lambda e: e.tensor_tensor(out=mA[:P, :], in0=vt[:P, :], in1=gs[:P, :], op=ALU.mult),
                 reads=[r_t], writes=[r_mA])

        def gmlp_tmp(es_, tag):
            return {"st": c.sb(es_, "g_st" + tag, [128, 2, 6], F32), "mvv": c.sb(es_, "g_mvv" + tag, [128, 4], F32),
                    "vt": c.sb(es_, "g_vt" + tag, [128, 1024], F32), "vlnb": c.sb(es_, "g_vlnb" + tag, [128, 1024], BF16),
                    "gs": c.sb(es_, "g_gs" + tag, [128, 1024], BF16), "r": Res()}

        with contextlib.suppress(_SkipPhase), contextlib.ExitStack() as es2:
            if 2 not in _PH:
                raise _SkipPhase()
            make_wpool(es2, 3)
            g_mix, r_gmix = load_gain(es2, "mix", gains["norm_mix"])
            lng, r_ln = load_gain(es2, "lng", sgu_ln_g, 1024)
            lnb = c.sb(es2, "g_lnb", [128, 1024], F32)
            c.dma("sp", lnb[:], sgu_ln_b[0:1, :].partition_broadcast(128), writes=[r_ln])
            wsf = c.sb(es2, "wsf", [128, 8, 128], F32)
            WsT = c.sb(es2, "WsT", [128, 8, 128], BF16)
            r_ws = Res()
            c.dma("sp", wsf[:], sgu_wT.rearrange("g j i -> j g i"), writes=[r_ws])
            c.op("pool", lambda e: e.affine_select(out=wsf[:], in_=wsf[:], pattern=[[0, 8], [1, 128]],
                                                   compare_op=ALU.is_ge, fill=0.0, base=0, channel_multiplier=-1),
                 reads=[r_ws], writes=[r_ws])
            c.op("dve", lambda e: e.tensor_copy(out=WsT[:], in_=wsf[:]), reads=[r_ws], writes=[r_ws])
            bsT = c.sb(es2, "bsT", [128, 8], F32)
            r_bs = Res()
            c.dma("sp", bsT[:], sgu_bT[:, :], writes=[r_bs])

            hT = [c.sb(es2, "hT%d" % i, [128, 16, GT], BF16) for i in range(2)]
            r_hT = [Res() for _ in range(2)]
            xt = [c.sb(es2, "xt%d" % i, [128, D], F32) for i in range(2)]
            r_xt = [Res() for _ in range(2)]
            hb = [c.sb(es2, "hb%d" % i, [128, D], BF16) for i in range(2)]
            r_hb = [Res() for _ in range(2)]
            gv = c.sb(es2, "gv", [128, G, 1024], F32)
            gu = c.sb(es2, "gu", [128, G, 1024], BF16)
            sga = c.sb(es2, "sga", [128, G, 1024], BF16)
            r_gv = [Res() for _ in range(G)]
            r_gu = [Res() for _ in range(G)]
            r_sga = [Res() for _ in range(G)]
            gtmp = gmlp_tmp(es2, "p")
            mA = c.sb(es2, "mA", [128, 1024], BF16)
            r_mA = Res()
            mst = [c.sb(es2, "mst%d" % i, [128, 8, 128], BF16) for i in range(2)]
            r_mst = [Res() for _ in range(2)]
            fst = [c.sb(es2, "fst%d" % i, [128, 4, GT], BF16) for i in range(2)]
            r_fst = [Res() for _ in range(2)]
            kvo = [c.sb(es2, "kvo%d" % i, [128, 512], F32) for i in range(2)]
            r_kvo = [Res() for _ in range(2)]
            ctr = {"x": 0, "f": 0, "k": 0, "m": 0}

            groups = [("h", g) for g in range(NT // G)][:_LIM["groups"]] + [("o", g) for g in range(NT // G)][:_LIM["groups"]]

            def prep_group(gidx):
                kind, g = groups[gidx]
                src = xh if kind == "h" else xo
                hbuf = gidx % 2
                for j in range(G):
                    i = ctr["x"] % 2
                    ctr["x"] += 1
                    t0 = g * GT + j * 128
                    c.dma("sp", xt[i][:], src[t0:t0 + 128, :], writes=[r_xt[i]])
                    rms_h(xt[i], r_xt[i], 128, g_mix, r_gmix, hb[i], r_hb[i])
                    transpose_to(hb[i], r_hb[i], 128, 16,
                                 lambda c0, c1, j=j, hbuf=hbuf: hT[hbuf][:, c0:c1, j * 128:(j + 1) * 128], r_hT[hbuf])

            def feat_block(ws, hbuf, dst, r_dst_list, tcol0, sigmoid=False):
                wt, r_w = ws.get()
                i = ctr["f"] % 2
                ctr["f"] += 1
                for ct in range(4):
                    b = bank(2, 6)
                    mm_feat(hT[hbuf], r_hT[hbuf], 0, GT, wt, r_w, ct, b)
                    if sigmoid:
                        c.op("act", lambda e, b=b, ct=ct, i=i: e.activation(out=fst[i][:, ct, :], in_=pb[b][:, 0:GT],
                                                                          func=AF.Sigmoid),
                             reads=[rpb[b]], writes=[r_fst[i]])
                    elif ct % 2 == 0:
                        c.op("act", lambda e, b=b, ct=ct, i=i: e.activation(out=fst[i][:, ct, :], in_=pb[b][:, 0:GT],
                                                                          func=AF.Copy),
                             reads=[rpb[b]], writes=[r_fst[i]])
                    else:
                        c.op("dve", lambda e, b=b, ct=ct, i=i: e.tensor_copy(out=fst[i][:, ct, :], in_=pb[b][:, 0:GT]),
                             reads=[rpb[b]], writes=[r_fst[i]])
                c.dma("act", dst[:, :, tcol0:tcol0 + GT].rearrange("g p t -> p g t"), fst[i][:],
                      reads=[r_fst[i]], writes=r_dst_list)

            prep_group(0)
            for gidx, (kind, g) in enumerate(groups):
                hbuf = gidx % 2
                if kind == "h":
                    ws = WStream([("w_in", b) for b in (10, 11, 12, 13)])
                    for bi, blk in enumerate((10, 11, 12, 13)):
                        dst = kTs if blk < 12 else vTs
                        rr = r_kTs if blk < 12 else r_vTs
                        h0 = (blk % 2) * 4
                        feat_block(ws, hbuf, dst[h0:h0 + 4], rr[h0:h0 + 4], g * GT)
                        if bi == 0 and gidx + 1 < len(groups):
                            prep_group(gidx + 1)
                    continue
                order = [2, 3, 0, 1, 14, 15, 10, 11, 12, 13, 4, 5, 6, 7, 8, 9, 10, 11, 12, 13, 16, 17]
                ws = WStream([("w_in", b) for b in order])
                for bi, blk in enumerate(order[:10]):
                    wt, r_w = ws.get()
                    for j in range(G):
                        b = bank(2, 6)
                        mm_tok(hT[hbuf], r_hT[hbuf], j * 128, 128, wt, r_w, b)
                        if blk in (2, 3):
                            c.op("act", lambda e, b=b, j=j, blk=blk: e.activation(
                                out=gv[:, j, (blk - 2) * 512:(blk - 1) * 512], in_=pb[b][:, :], func=AF.Gelu_apprx_tanh),
                                reads=[rpb[b]], writes=[r_gv[j]])
                        elif blk in (0, 1):
                            c.op("act", lambda e, b=b, j=j, blk=blk: e.activation(
                                out=gu[:, j, blk * 512:(blk + 1) * 512], in_=pb[b][:, :], func=AF.Gelu_apprx_tanh),
                                reads=[rpb[b]], writes=[r_gu[j]])
                        elif blk in (14, 15):
                            c.op("act", lambda e, b=b, j=j, blk=blk: e.activation(
                                out=sga[:, j, (blk - 14) * 512:(blk - 13) * 512], in_=pb[b][:, :], func=AF.Sigmoid),
                                reads=[rpb[b]], writes=[r_sga[j]])
                        else:
                            i = ctr["k"] % 2
                            ctr["k"] += 1
                            c.op("dve", lambda e, b=b, i=i: e.tensor_copy(out=kvo[i][:], in_=pb[b][:, :]),
                                 reads=[rpb[b]], writes=[r_kvo[i]])
                            t0 = g * GT + j * 128
                            kv = 0 if blk < 12 else 1
                            half = blk % 2
                            c.dma("act", win_p[t0:t0 + 128, kv, half * 512:(half + 1) * 512], kvo[i][:],
                                  reads=[r_kvo[i]])
                    if bi == 0 and gidx + 1 < len(groups):
                        prep_group(gidx + 1)
                    if bi == 5:
                        for j in range(G):
                            gmlp_tile(es2, 128, gv[:, j, :], r_gv[j], gu[:, j, :], r_gu[j], sga[:, j, :], r_sga[j],
                                      WsT, r_ws, bsT, r_bs, lng, lnb, r_ln, gtmp, mA, r_mA)
                            i = ctr["m"] % 2
                            ctr["m"] += 1
                            transpose_to(mA, r_mA, 128, 8, lambda c0, c1, i=i: mst[i][:, c0:c1, :], r_mst[i],
                                         evac="dve")
                            t0 = g * GT + j * 128
                            c.dma("act", mTs[0:8, :, t0:t0 + 128].rearrange("g p t -> p g t"), mst[i][:],
                                  reads=[r_mst[i]], writes=r_mTs[0:8])
                for blk in order[10:]:
                    if 4 <= blk <= 9:
                        gh0 = ((blk - 4) // 2) * 8 + ((blk - 4) % 2) * 4
                        feat_block(ws, hbuf, qTs[gh0:gh0 + 4], r_qTs[gh0:gh0 + 4], g * GT)
                    elif blk in (10, 11):
                        h0 = (blk % 2) * 4
                        feat_block(ws, hbuf, kTs[h0:h0 + 4], r_kTs[h0:h0 + 4], TOK + g * GT)
                    elif blk in (12, 13):
                        h0 = (blk % 2) * 4
                        feat_block(ws, hbuf, vTs[h0:h0 + 4], r_vTs[h0:h0 + 4], TOK + g * GT)
                    else:
                        h0 = (blk % 2) * 4
                        feat_block(ws, hbuf, gbTs[h0:h0 + 4], r_gbTs[h0:h0 + 4], g * GT, sigmoid=True)
            c.barrier()

        with contextlib.suppress(_SkipPhase), contextlib.ExitStack() as es3:
            if 3 not in _PH:
                raise _SkipPhase()
            EBT = c.sb(es3, "EBT", [128, 24, 256], BF16)
            r_EB = Res()
            bm = c.sb(es3, "bm", [128, 256], F32)
            r_bm = Res()
            c.dma("sp", bm[:], bandmask.rearrange("p a b -> p (a b)"), writes=[r_bm])
            btmp = [c.sb(es3, "btmp%d" % i, [128, 256], F32) for i in range(2)]
            r_btmp = [Res() for _ in range(2)]
            for gh in range(24):
                i = gh % 2
                c.dma("sp", btmp[i][:], biasT[gh].rearrange("p a b -> p (a b)"), writes=[r_btmp[i]])
                c.op("act", lambda e, i=i: e.activation(out=btmp[i][:], in_=btmp[i][:], func=AF.Exp),
                     reads=[r_btmp[i]], writes=[r_btmp[i]])
                c.op("dve", lambda e, i=i, gh=gh: e.tensor_tensor(out=EBT[:, gh, :], in0=btmp[i][:], in1=bm[:],
                                                                  op=ALU.mult),
                     reads=[r_btmp[i], r_bm], writes=[r_EB])
            pf = c.sb(es3, "pf", [128, 1], F32)
            r_pf = Res()
            c.dma("sp", pf[:], pflag[:, :], writes=[r_pf])

            kT = [c.sb(es3, "kT%d" % i, [128, 2 * TOK], BF16) for i in range(2)]
            vT = [c.sb(es3, "vT%d" % i, [128, 2 * TOK], BF16) for i in range(2)]
            qT3 = [c.sb(es3, "qT3%d" % i, [128, 3, TOK], BF16) for i in range(2)]
            gbT = [c.sb(es3, "gbT%d" % i, [128, TOK], BF16) for i in range(2)]
            r_hd = [Res() for _ in range(2)]
            acc = c.sb(es3, "acc", [128, 2, TOK], F32)
            r_acc = Res()
            ybT = c.sb(es3, "ybT", [128, TOK], BF16)
            r_ybT = Res()
            NVP = 4
            Vp = [c.sb(es3, "Vp%d" % i, [128, 128], BF16) for i in range(NVP)]
            r_Vp = [Res() for _ in range(NVP)]
            pTf = [c.sb(es3, "pTf%d" % i, [128, 256], BF16) for i in range(2)]
            pT = [c.sb(es3, "pT%d" % i, [128, 256], BF16) for i in range(2)]
            r_pTf = [Res() for _ in range(2)]
            r_pT = [Res() for _ in range(2)]
            qTs4 = qTs.rearrange("(g h) p t -> g h p t", h=8)
            actr = {"vp": 0, "u": 0}

            def load_head(h):
                i = h % 2
                c.dma("sp", kT[i][:], kTs[h], reads=[r_kTs[h]], writes=[r_hd[i]])
                c.dma("sp", vT[i][:], vTs[h], reads=[r_vTs[h]], writes=[r_hd[i]])
                c.dma("sp", qT3[i][:], qTs4[:, h].rearrange("g p t -> p g t"),
                      reads=[r_qTs[h], r_qTs[8 + h], r_qTs[16 + h]], writes=[r_hd[i]])
                c.dma("sp", gbT[i][:], gbTs[h], reads=[r_gbTs[h]], writes=[r_hd[i]])

            def make_vp(i, dil, a0, r):
                k = actr["vp"] % NVP
                actr["vp"] += 1
                b = bank(2, 4)
                vv = vT[i][:].rearrange("p (a b) -> p a b", b=dil)
                c.op("pe", lambda e: e.transpose(out=pbb[b][:, 0:128], in_=vv[:, a0:a0 + 128, r], identity=ident[:]),
                     reads=[r_hd[i], r_const], writes=[rpb[b]])
                c.op("dve", lambda e: e.tensor_copy(out=Vp[k][:], in_=pbb[b][:, 0:128]), reads=[rpb[b]],
                     writes=[r_Vp[k]])
                return k

            load_head(0)
            NH = 8 if _LIM["heads"] is None else _LIM["heads"]
            for h in range(NH):
                i = h % 2
                if h + 1 < NH:
                    load_head(h + 1)
                for gi, dil in enumerate(DILS):
                    span = 128 * dil
                    nbk = TOK // span
                    kv_ = kT[i][:].rearrange("p (a b) -> p a b", b=dil)
                    qv_ = qT3[i][:, gi, :].rearrange("p (a b) -> p a b", b=dil)
                    accv = acc[:].rearrange("p s (a b) -> p s a b", b=dil)
                    for r in range(dil):
                        kprev = make_vp(i, dil, (TOK - span) // dil, r)
                        for bb in range(nbk):
                            a_own = (TOK + bb * span) // dil
                            kcur = make_vp(i, dil, a_own, r)
                            u = actr["u"] % 2
                            actr["u"] += 1
                            b_s = bank(0, 2)
                            a_prev = a_own - 128

                            def fs(e, a_prev=a_prev, bb=bb, r=r, b_s=b_s, kv_=kv_, qv_=qv_):
                                inst = None
                                for jt in range(2):
                                    inst = e.matmul(pb[b_s][:, jt * 128:(jt + 1) * 128],
                                                    lhsT=kv_[:, a_prev + jt * 128:a_prev + (jt + 1) * 128, r],
                                                    rhs=qv_[:, bb * 128:(bb + 1) * 128, r], start=True, stop=True)
                                return inst
                            c.op("pe", fs, reads=[r_hd[i]], writes=[rpb[b_s]])
                            c.op("act", lambda e, u=u, b_s=b_s: e.activation(out=pTf[u][:], in_=pb[b_s][:, 0:256],
                                                                            func=AF.Exp, scale=ATT_SCALE),
                                 reads=[rpb[b_s]], writes=[r_pTf[u]])
                            c.op("pool", lambda e, u=u, gi=gi, h=h: e.tensor_tensor(
                                out=pT[u][:], in0=pTf[u][:], in1=EBT[:, gi * 8 + h, :], op=ALU.mult),
                                reads=[r_pTf[u], r_EB], writes=[r_pT[u]])
                            if bb == 0:
                                c.op("pool", lambda e, u=u: e.tensor_scalar(
                                    out=pT[u][:, 0:128], in0=pT[u][:, 0:128], scalar1=pf[:, 0:1], scalar2=None,
                                    op0=ALU.mult), reads=[r_pT[u], r_pf], writes=[r_pT[u]])
                            b_o = bank(4, 6)

                            def fo(e, u=u, b_o=b_o, kprev=kprev, kcur=kcur):
                                e.matmul(pb[b_o][:, 0:128], lhsT=Vp[kprev][:], rhs=pT[u][:, 0:128], start=True, stop=False)
                                e.matmul(pb[b_o][:, 0:128], lhsT=Vp[kcur][:], rhs=pT[u][:, 128:256], start=False, stop=True)
                                e.matmul(pb[b_o][:, 128:256], lhsT=ones[:], rhs=pT[u][:, 0:128], start=True, stop=False)
                                return e.matmul(pb[b_o][:, 128:256], lhsT=ones[:], rhs=pT[u][:, 128:256],
                                                start=False, stop=True)
                            c.op("pe", fo, reads=[r_pT[u], r_Vp[kprev], r_Vp[kcur], r_const], writes=[rpb[b_o]])
                            av = accv[:, :, bb * 128:(bb + 1) * 128, r]
                            pv = pb[b_o][:, 0:256].rearrange("p (s t) -> p s t", s=2)
                            if gi == 0:
                                c.op("dve", lambda e, av=av, pv=pv: e.tensor_copy(out=av, in_=pv),
                                     reads=[rpb[b_o]], writes=[r_acc])
                            else:
                                c.op("dve", lambda e, av=av, pv=pv: e.tensor_tensor(out=av, in0=pv, in1=av, op=ALU.add),
                                     reads=[rpb[b_o], r_acc], writes=[r_acc])
                            kprev = kcur
                c.op("dve", lambda e: e.reciprocal(out=acc[:, 1, :], in_=acc[:, 1, :]), reads=[r_acc], writes=[r_acc])
                c.op("dve", lambda e: e.tensor_tensor(out=acc[:, 0, :], in0=acc[:, 0, :], in1=acc[:, 1, :], op=ALU.mult),
                     reads=[r_acc], writes=[r_acc])
                c.op("pool", lambda e, i=i: e.tensor_tensor(out=ybT[:], in0=acc[:, 0, :], in1=gbT[i][:], op=ALU.mult),
                     reads=[r_acc, r_hd[i]], writes=[r_ybT])
                c.dma("sp", mTs[8 + h], ybT[:], reads=[r_ybT], writes=[r_mTs[8 + h]])
            c.barrier()


        PS = 128
        mTg_s = c.sb(es, "mTg_s", [128, 16, PS], BF16)
        r_mTgs = Res()
        c.op("pool", lambda e: e.memset(mTg_s[:], 0.0), writes=[r_mTgs])
        with contextlib.suppress(_SkipPhase), contextlib.ExitStack() as ess:
            if 4 not in _PH:
                raise _SkipPhase()
            make_wpool(ess, 2)
            cut(10)
            g_mix, r_gmix = load_gain(ess, "mix_s", gains["norm_mix"])
            lng, r_ln = load_gain(ess, "lng_s", sgu_ln_g, 1024)
            lnb = c.sb(ess, "g_lnb_s", [128, 1024], F32)
            c.dma("sp", lnb[:], sgu_ln_b[0:1, :].partition_broadcast(128), writes=[r_ln])
            wsf = c.sb(ess, "wsf_s", [128, 8, 128], F32)
            WsTs = c.sb(ess, "WsTs", [128, 8, 128], BF16)
            r_ws = Res()
            c.dma("sp", wsf[:], sgu_wTs.rearrange("g j i -> j g i"), writes=[r_ws])
            c.op("pool", lambda e: e.affine_select(out=wsf[:], in_=wsf[:], pattern=[[0, 8], [1, 128]],
                                                   compare_op=ALU.is_ge, fill=0.0, base=0, channel_multiplier=-1),
                 reads=[r_ws], writes=[r_ws])
            c.op("dve", lambda e: e.tensor_copy(out=WsTs[:], in_=wsf[:]), reads=[r_ws], writes=[r_ws])
            bsTs = c.sb(ess, "bsTs", [128, 8], F32)
            r_bs = Res()
            c.dma("sp", bsTs[:], sgu_bTs[:, :], writes=[r_bs])
            cut(11)

            xts = c.sb(ess, "xts", [128, D], F32)
            hbs = c.sb(ess, "hbs", [128, D], BF16)
            hTs = c.sb(ess, "hTs", [128, 16, PS], BF16)
            r_xts, r_hbs, r_hTs = Res(), Res(), Res()
            c.op("pool", lambda e: e.memset(xts[:], 0.0), writes=[r_xts])
            c.dma("sp", xts[:SP, :], xs[:, :], writes=[r_xts])
            cut(12)
            rms_h(xts, r_xts, PS, g_mix, r_gmix, hbs, r_hbs)
            cut(13)
            transpose_to(hbs, r_hbs, PS, 16, lambda c0, c1: hTs[:, c0:c1, :], r_hTs)
            cut(1)

            gv_s = c.sb(ess, "gv_s", [128, 1024], F32)
            gu_s = c.sb(ess, "gu_s", [128, 1024], BF16)
            sga_s = c.sb(ess, "sga_s", [128, 1024], BF16)
            sgb_s = c.sb(ess, "sgb_s", [128, 1024], BF16)
            q_s = c.sb(ess, "q_s", [128, 3072], BF16)
            kf_s = c.sb(ess, "kf_s", [128, 1024], F32)
            vf_s = c.sb(ess, "vf_s", [128, 1024], F32)
            k_sb = c.sb(ess, "k_sb", [128, 1024], BF16)
            vn = c.sb(ess, "vn", [128, 1024], BF16)
            r_gvs, r_gus, r_sgas, r_sgbs, r_qs, r_kfs, r_vfs, r_ksb, r_vn = (Res() for _ in range(9))
            ws = WStream([("w_in", b) for b in range(18)])
            for blk in range(18):
                cut(20 + blk)
                wt, r_w = ws.get()
                b = bank(2, 6)
                mm_tok(hTs, r_hTs, 0, PS, wt, r_w, b)
                src = pb[b][:, :]
                if blk < 2:
                    c.op("act", lambda e, blk=blk, src=src: e.activation(out=gu_s[:, blk * 512:(blk + 1) * 512], in_=src,
                                                                        func=AF.Gelu_apprx_tanh), reads=[rpb[b]], writes=[r_gus])
                elif blk < 4:
                    c.op("act", lambda e, blk=blk, src=src: e.activation(out=gv_s[:, (blk - 2) * 512:(blk - 1) * 512], in_=src,
                                                                        func=AF.Gelu_apprx_tanh), reads=[rpb[b]], writes=[r_gvs])
                elif blk < 10:
                    c.op("act", lambda e, blk=blk, src=src: e.activation(out=q_s[:, (blk - 4) * 512:(blk - 3) * 512], in_=src,
                                                                        func=AF.Copy), reads=[rpb[b]], writes=[r_qs])
                elif blk < 12:
                    c.op("act", lambda e, blk=blk, src=src: e.activation(out=kf_s[:, (blk - 10) * 512:(blk - 9) * 512], in_=src,
                                                                        func=AF.Copy), reads=[rpb[b]], writes=[r_kfs])
                    c.op("dve", lambda e, blk=blk: e.tensor_copy(out=k_sb[:, (blk - 10) * 512:(blk - 9) * 512],
                                                                 in_=kf_s[:, (blk - 10) * 512:(blk - 9) * 512]),
                         reads=[r_kfs], writes=[r_ksb])
                elif blk < 14:
                    c.op("act", lambda e, blk=blk, src=src: e.activation(out=vf_s[:, (blk - 12) * 512:(blk - 11) * 512], in_=src,
                                                                        func=AF.Copy), reads=[rpb[b]], writes=[r_vfs])
                    c.op("dve", lambda e, blk=blk: e.tensor_copy(out=vn[:, (blk - 12) * 512:(blk - 11) * 512],
                                                                 in_=vf_s[:, (blk - 12) * 512:(blk - 11) * 512]),
                         reads=[r_vfs], writes=[r_vn])
                elif blk < 16:
                    c.op("act", lambda e, blk=blk, src=src: e.activation(out=sga_s[:, (blk - 14) * 512:(blk - 13) * 512], in_=src,
                                                                        func=AF.Sigmoid), reads=[rpb[b]], writes=[r_sgas])
                else:
                    c.op("act", lambda e, blk=blk, src=src: e.activation(out=sgb_s[:, (blk - 16) * 512:(blk - 15) * 512], in_=src,
                                                                        func=AF.Sigmoid), reads=[rpb[b]], writes=[r_sgbs])
            cut(40)
            c.dma("sp", win_s[:, 0, :], kf_s[:SP, :], reads=[r_kfs])
            c.dma("sp", win_s[:, 1, :], vf_s[:SP, :], reads=[r_vfs])
            cut(2)
            gtmp_s = gmlp_tmp(ess, "s")
            vlnf_s = c.sb(ess, "vlnf_s", [128, 1024], F32)
            r_vlnf = Res()
            mA_s = c.sb(ess, "mA_s", [128, 1024], BF16)
            r_mAs = Res()
            gmlp_tile(ess, PS, gv_s, r_gvs, gu_s, r_gus, sga_s, r_sgas, WsTs, r_ws, bsTs, r_bs, lng, lnb, r_ln,
                      gtmp_s, mA_s, r_mAs, vln_out=vlnf_s, r_vln=r_vlnf)
            c.dma("sp", sgu_s[:, :], vlnf_s[:SP, :], reads=[r_vlnf])
            transpose_to(mA_s, r_mAs, PS, 8, lambda c0, c1: mTg_s[:, c0:c1, :], r_mTgs)
            cut(3)

            qTs_s = c.sb(ess, "qTs_s", [128, 24, PS], BF16)
            kTn = c.sb(ess, "kTn", [128, 8, PS], BF16)
            gbT_s = c.sb(ess, "gbT_s", [128, 8, PS], BF16)
            r_qTss, r_kTn, r_gbTs_ = Res(), Res(), Res()
            transpose_to(q_s, r_qs, PS, 24, lambda c0, c1: qTs_s[:, c0:c1, :], r_qTss)
            transpose_to(k_sb, r_ksb, PS, 8, lambda c0, c1: kTn[:, c0:c1, :], r_kTn)
            transpose_to(sgb_s, r_sgbs, PS, 8, lambda c0, c1: gbT_s[:, c0:c1, :], r_gbTs_)
            EBs = c.sb(ess, "EBs", [128, 8, 384], BF16)
            EBn = c.sb(ess, "EBn", [128, 8, 96], BF16)
            r_EBs = Res()
            c.op("pool", lambda e: e.memset(EBn[:], 0.0), writes=[r_EBs])
            smk = c.sb(ess, "smk", [128, 384], F32)
            smkn = c.sb(ess, "smkn", [128, 96], F32)
            r_smk = Res()
            c.dma("sp", smk[:], smask.rearrange("p a b -> p (a b)"), writes=[r_smk])
            c.dma("sp", smkn[:SP, :], smask_n.rearrange("p a b -> p (a b)"), writes=[r_smk])
            stmp = [c.sb(ess, "stmp%d" % i, [128, 480], F32) for i in range(2)]
            r_stmp = [Res() for _ in range(2)]
            for h in range(8):
                i = h % 2
                c.dma("sp", stmp[i][:, 0:384], sbias[h].rearrange("p a b -> p (a b)"), writes=[r_stmp[i]])
                c.dma("sp", stmp[i][:SP, 384:480], sbias_n[h].rearrange("p a b -> p (a b)"), writes=[r_stmp[i]])
                c.op("act", lambda e, i=i: e.activation(out=stmp[i][:, 0:384], in_=stmp[i][:, 0:384], func=AF.Exp),
                     reads=[r_stmp[i]], writes=[r_stmp[i]])
                c.op("act", lambda e, i=i: e.activation(out=stmp[i][:SP, 384:480], in_=stmp[i][:SP, 384:480], func=AF.Exp),
                     reads=[r_stmp[i]], writes=[r_stmp[i]])
                c.op("dve", lambda e, i=i, h=h: e.tensor_tensor(out=EBs[:, h, :], in0=stmp[i][:, 0:384], in1=smk[:],
                                                                op=ALU.mult), reads=[r_stmp[i], r_smk], writes=[r_EBs])
                c.op("dve", lambda e, i=i, h=h: e.tensor_tensor(out=EBn[:SP, h, :], in0=stmp[i][:SP, 384:480],
                                                                in1=smkn[:SP, :], op=ALU.mult),
                     reads=[r_stmp[i], r_smk], writes=[r_EBs])
            cut(4)
            Ppad = c.sb(ess, "Ppad", [128, 4, 17 * 96], BF16)
            r_Pp = Res()
            c.op("pool", lambda e: e.memset(Ppad[:], 0.0), writes=[r_Pp])
            Kc = c.sb(ess, "Kc0", [128, 16, 128], F32)
            Vc = c.sb(ess, "Vc0", [128, 16, 128], F32)
            Kcb = [c.sb(ess, "Kcb0", [128, 16, 128], BF16)] * 2
            Vcb = [c.sb(ess, "Vcb%d" % i, [128, 16, 128], BF16) for i in range(2)]
            KT = [c.sb(ess, "KT0", [128, 16, 128], BF16)] * 2
            r_Kc, r_Vc = Res(), Res()
            r_Kcb = [Res()] * 2
            r_Vcb = [Res() for _ in range(2)]
            r_KT = [Res()] * 2
            tmpE = c.sb(ess, "tmpE", [128, 408], BF16)
            r_tmpE = Res()
            qc = [c.sb(ess, "qc%d" % i, [128, 24], BF16) for i in range(2)]
            r_qc = [Res() for _ in range(2)]
            rec_s = c.sb(ess, "rec_s", [128, SP], F32)
            ot_s = c.sb(ess, "ot_s", [128, SP], F32)
            r_recs = Res()
            qv4 = qTs_s[:].rearrange("p (g h) t -> p g h t", h=8)
            tiles_g = ([15], [12, 13, 14, 15], list(range(16)))
            n = 0
            for h in range(8):
                b_o = 4 + h % 2
                b_sum = 6 + h % 2
                for b in range(4):
                    i = n % 2
                    n += 1
                    c.dma("sp", Kc[:], cwin[b, :, 0, h, :].rearrange("(rt p) d -> p rt d", p=128), writes=[r_Kc])
                    c.dma("sp", Vc[:], cwin[b, :, 1, h, :].rearrange("(rt p) d -> p rt d", p=128), writes=[r_Vc])
                    c.op("pool", lambda e, i=i: e.tensor_copy(out=Kcb[i][:], in_=Kc[:]), reads=[r_Kc], writes=[r_Kcb[i]])
                    c.op("act", lambda e, i=i: e.activation(out=Vcb[i][:], in_=Vc[:], func=AF.Copy),
                         reads=[r_Vc], writes=[r_Vcb[i]])
                    transpose_to(Kcb[i][:].rearrange("p a d -> p (a d)"), r_Kcb[i], 128, 16,
                                 lambda c0, c1, i=i: KT[i][:, c0:c1, :], r_KT[i], evac="dve")
                    c.op("dve", lambda e, i=i, h=h, b=b: e.tensor_copy(
                        out=qc[i][:].rearrange("p (g t) -> p g t", t=8), in_=qv4[:, :, h, b * 8:(b + 1) * 8]),
                        reads=[r_qTss], writes=[r_qc[i]])
                    b_s = bank(2, 4)

                    def fs(e, i=i, b_s=b_s, h=h):
                        for rt in range(16):
                            e.matmul(pb[b_s][:, rt * 24:(rt + 1) * 24], lhsT=KT[i][:, rt, :], rhs=qc[i][:],
                                     start=True, stop=True)
                        return e.matmul(pb[b_s][:, 384:408], lhsT=kTn[:, h, :], rhs=qc[i][:], start=True, stop=True)
                    c.op("pe", fs, reads=[r_KT[i], r_qc[i], r_kTn], writes=[rpb[b_s]])
                    c.op("act", lambda e, b_s=b_s: e.activation(out=tmpE[:], in_=pb[b_s][:, 0:408], func=AF.Exp,
                                                                scale=ATT_SCALE), reads=[rpb[b_s]], writes=[r_tmpE])
                    Pv = Ppad[:, b, :].rearrange("p (rt g m) -> p rt g m", g=3, m=32)
                    c.op("dve", lambda e, Pv=Pv, b=b, h=h: e.tensor_tensor(
                        out=Pv[:, 0:16, :, b * 8:(b + 1) * 8],
                        in0=tmpE[:, 0:384].rearrange("p (rt g t) -> p rt g t", g=3, t=8),
                        in1=EBs[:, h, :].rearrange("p (rt g t) -> p rt g t", g=3, t=8), op=ALU.mult),
                        reads=[r_tmpE, r_EBs], writes=[r_Pp])
                    c.op("dve", lambda e, Pv=Pv, b=b, h=h: e.tensor_tensor(
                        out=Pv[:, 16, :, b * 8:(b + 1) * 8],
                        in0=tmpE[:, 384:408].rearrange("p (g t) -> p g t", t=8),
                        in1=EBn[:, h, b * 24:(b + 1) * 24].rearrange("p (g t) -> p g t", t=8), op=ALU.mult),
                        reads=[r_tmpE, r_EBs], writes=[r_Pp])

                    def fo(e, i=i, b=b, h=h, b_o=b_o, b_sum=b_sum, Pv=Pv):
                        mms = []
                        for g in range(3):
                            for rt in tiles_g[g]:
                                mms.append((Vcb[i][:, rt, :], Pv[:, rt, g, :]))
                            mms.append((vn[:, h * 128:(h + 1) * 128], Pv[:, 16, g, :]))
                        inst = None
                        for k, (l, r) in enumerate(mms):
                            first = (b == 0 and k == 0)
                            last = (b == 3 and k == len(mms) - 1)
                            e.matmul(pb[b_o][:, 0:SP], lhsT=l, rhs=r, start=first, stop=last)
                            inst = e.matmul(pb[b_sum][:, 0:SP], lhsT=ones[:], rhs=r, start=first, stop=last)
                        return inst
                    c.op("pe", fo, reads=[r_Pp, r_Vcb[i], r_vn, r_const], writes=[rpb[b_o], rpb[b_sum]])
                c.op("dve", lambda e, b_sum=b_sum: e.reciprocal(out=rec_s[:], in_=pb[b_sum][:, 0:SP]),
                     reads=[rpb[b_sum]], writes=[r_recs])
                c.op("dve", lambda e, b_o=b_o: e.tensor_tensor(out=ot_s[:], in0=pb[b_o][:, 0:SP], in1=rec_s[:],
                                                               op=ALU.mult), reads=[rpb[b_o], r_recs], writes=[r_recs])
                c.op("dve", lambda e, h=h: e.tensor_tensor(out=mTg_s[:, 8 + h, 0:SP], in0=ot_s[:], in1=gbT_s[:, h, 0:SP],
                                                           op=ALU.mult), reads=[r_recs, r_gbTs_], writes=[r_mTgs])
            c.barrier()

        es4 = es.enter_context(contextlib.ExitStack())
        make_wpool(es4, 2)
        g_mem, r_gmem = load_gain(es4, "mem", gains["norm_mem"])
        g_peer, r_gpeer = load_gain(es4, "peer", gains["norm_peer"])
        g_fin, r_gfin = load_gain(es4, "fin", gains["norm_final"])
        keysT = c.sb(es4, "keysT", [128, 2, 8, 128], BF16)
        r_keysT = Res()
        with contextlib.ExitStack() as esk:
            kf = c.sb(esk, "kf", [128, 2, 8, 128], F32)
            kb = c.sb(esk, "kb", [128, 2, 8, 128], BF16)
            r_kf = Res()
            c.dma("sp", kf[:, 0, :, :], keys1.rearrange("h k d -> k h d"), writes=[r_kf])
            c.dma("sp", kf[:, 1, :, :], keys2.rearrange("h k d -> k h d"), writes=[r_kf])
            c.op("dve", lambda e: e.tensor_copy(out=kb[:], in_=kf[:]), reads=[r_kf], writes=[r_kf])
            transpose_to(kb[:].rearrange("p s h d -> p (s h d)"), r_kf, 128, 16,
                         lambda c0, c1: keysT[:].rearrange("p s h k -> p (s h) k")[:, c0:c1, :], r_keysT)
            c.barrier()
        iota_i = c.sb(es4, "iota_i", [128, 16], I32)
        iota16 = c.sb(es4, "iota16", [128, 16], F32)
        r_iota = Res()
        c.op("pool", lambda e: e.iota(iota_i[:], pattern=[[1, 16]], base=0, channel_multiplier=0), writes=[r_iota])
        c.op("dve", lambda e: e.tensor_copy(out=iota16[:], in_=iota_i[:]), reads=[r_iota], writes=[r_iota])

        x1 = [c.sb(es4, "x1_%d" % i, [128, D], F32) for i in range(G)]
        r_x1 = [Res() for _ in range(G)]
        h2 = [c.sb(es4, "h2_%d" % i, [128, D], BF16) for i in range(G)]
        r_h2 = [Res() for _ in range(G)]
        hb4 = c.sb(es4, "hb4", [128, D], BF16)
        r_hb4 = Res()
        bufA = c.sb(es4, "bufA", [128, 16, GT], BF16)
        bufB = c.sb(es4, "bufB", [128, 16, GT], BF16)
        r_bufA = Res()
        r_bufB = Res()
        pTm = c.sb(es4, "pTm", [128, 2, GT], BF16)
        r_pTm = Res()
        rsm = c.sb(es4, "rsm", [128, GT], F32)
        r_rsm = Res()
        sc = c.sb(es4, "sc", [128, 2048], F32)
        cand = c.sb(es4, "cand", [128, 2048], F32)
        r_sc = Res()
        r_cand = Res()
        wk = c.sb(es4, "wk", [128, 256], F32)
        r_wk = Res()
        tv = c.sb(es4, "tv", [128, 16, 16], F32)
        ti = c.sb(es4, "ti", [128, 16, 16], U32)
        tif = c.sb(es4, "tif", [128, 16, 16], F32)
        best = c.sb(es4, "best", [128, 8, 16], F32)
        pos = c.sb(es4, "pos", [128, 8, 16], U32)
        pa = c.sb(es4, "pa", [128, 128], U32)
        pbi = c.sb(es4, "pbi", [128, 128], U32)
        paf = c.sb(es4, "paf", [128, 128], F32)
        pbf = c.sb(es4, "pbf", [128, 128], F32)
        i1s = c.sb(es4, "i1s", [128, 128], F32)
        i2s = c.sb(es4, "i2s", [128, 128], F32)
        eidf = c.sb(es4, "eidf", [128, 128], F32)
        gate = c.sb(es4, "gate", [128, 8, 16], F32)
        gsm = c.sb(es4, "gsm", [128, 24], F32)
        r_pk = Res()
        idxT = c.sb(es4, "idxT", [128, 128], I32)
        GTt = c.sb(es4, "GTt", [128, 128], F32)
        apart = c.sb(es4, "apart", [128, 128, 4], F32)
        AT = c.sb(es4, "AT", [128, 128], F32)
        CT = c.sb(es4, "CT", [128, 128], BF16)
        r_idxT = Res()
        r_GTt = Res()
        r_apart = Res()
        r_CT = Res()
        NGB = 6
        gbuf = [c.sb(es4, "gbuf%d" % i, [128, D], BF16) for i in range(NGB)]
        r_gbuf = [Res() for _ in range(NGB)]
        NCB = 4
        cbuf = [c.sb(es4, "cbuf%d" % i, [128, 256], BF16) for i in range(NCB)]
        r_cbuf = [Res() for _ in range(NCB)]
        for i in range(NCB):
            c.op("pool", lambda e, i=i: e.memset(cbuf[i][:], 0.0), writes=[r_cbuf[i]])
        junk4 = c.sb(es4, "junk4", [128, 512], BF16)
        r_junk4 = Res()
        yout = cand
        r_yout = r_cand
        pctr = {"g": 0, "c": 0}

        def cross_attn(qT_, r_q, col0, n, mkT_, r_mk, mv_fn, r_mv, oT_, r_o):
            for h in range(4):
                for mt in range(2):
                    b = bank(2, 6)

                    def f(e, h=h, mt=mt, b=b):
                        inst = None
                        for dc in range(4):
                            inst = e.matmul(pb[b][:, 0:n], lhsT=mkT_[:, h * 4 + dc, mt * 128:(mt + 1) * 128],
                                            rhs=qT_[:, h * 4 + dc, col0:col0 + n], start=(dc == 0), stop=(dc == 3))
                        return inst
                    c.op("pe", f, reads=[r_q] + r_mk, writes=[rpb[b]])
                    c.op("act", lambda e, mt=mt, b=b: e.activation(out=pTm[:, mt, 0:n], in_=pb[b][:, 0:n],
                                                                  func=AF.Exp, scale=MEM_SCALE),
                         reads=[rpb[b]], writes=[r_pTm])
                b = bank(2, 6)

                def fs(e, b=b):
                    e.matmul(pb[b][:, 0:n], lhsT=ones[:], rhs=pTm[:, 0, 0:n], start=True, stop=False)
                    return e.matmul(pb[b][:, 0:n], lhsT=ones[:], rhs=pTm[:, 1, 0:n], start=False, stop=True)
                c.op("pe", fs, reads=[r_pTm, r_const], writes=[rpb[b]])
                c.op("dve", lambda e, b=b: e.reciprocal(out=rsm[:, 0:n], in_=pb[b][:, 0:n]), reads=[rpb[b]],
                     writes=[r_rsm])
                for dc in range(4):
                    b = bank(2, 6)

                    def fo(e, h=h, dc=dc, b=b):
                        c0 = h * 512 + dc * 128
                        e.matmul(pb[b][:, 0:n], lhsT=mv_fn(0, c0), rhs=pTm[:, 0, 0:n], start=True, stop=False)
                        return e.matmul(pb[b][:, 0:n], lhsT=mv_fn(1, c0), rhs=pTm[:, 1, 0:n],
                                        start=False, stop=True)
                    c.op("pe", fo, reads=[r_pTm] + r_mv, writes=[rpb[b]])
                    c.op("dve", lambda e, h=h, dc=dc, b=b: e.tensor_tensor(
                        out=oT_[:, h * 4 + dc, col0:col0 + n], in0=pb[b][:, 0:n], in1=rsm[:, 0:n], op=ALU.mult),
                        reads=[rpb[b], r_rsm], writes=[r_o])

        def top16(P, src_ap, n, dst_v, dst_i, rd, wr):
            c.op("dve", lambda e: e.max(out=dst_v[:, 0:8], in_=src_ap), reads=rd, writes=wr)
            c.op("dve", lambda e: e.max_index(out=dst_i[:, 0:8], in_max=dst_v[:, 0:8], in_values=src_ap),
                 reads=rd + wr, writes=wr)
            c.op("dve", lambda e: e.match_replace(out=wk[:P, 0:n], in_to_replace=dst_v[:, 0:8], in_values=src_ap,
                                                  imm_value=NEG), reads=rd + wr, writes=[r_wk])
            c.op("dve", lambda e: e.max(out=dst_v[:, 8:16], in_=wk[:P, 0:n]), reads=[r_wk], writes=wr)
            c.op("dve", lambda e: e.max_index(out=dst_i[:, 8:16], in_max=dst_v[:, 8:16], in_values=wk[:P, 0:n]),
                 reads=[r_wk] + wr, writes=wr)

        def peer_tile(P, nv, qpT, r_qp, col0, h2_t, r_h2t, x2_t, r_x2t, y_dst):
            def fsc(e):
                inst = None
                for hs in range(16):
                    inst = e.matmul(pb[4 + hs // 4][:P, (hs % 4) * 128:(hs % 4 + 1) * 128],
                                    lhsT=qpT[:, hs, col0:col0 + P], rhs=keysT[:, hs % 2, hs // 2, :],
                                    start=True, stop=True)
                return inst
            c.op("pe", fsc, reads=[r_qp, r_keysT], writes=[rpb[4], rpb[5], rpb[6], rpb[7]])
            for q in range(4):
                c.op("act", lambda e, q=q: e.activation(out=sc[:P, q * 512:(q + 1) * 512], in_=pb[4 + q][:P, :],
                                                        func=AF.Copy), reads=[rpb[4 + q]], writes=[r_sc])
            scv = sc[:P, :].rearrange("p (a k) -> p a k", k=128)
            for hs in range(16):
                top16(P, scv[:, hs, :], 128, tv[:P, hs, :], ti[:P, hs, :], [r_sc], [r_pk])
            tv4 = tv[:P].rearrange("p (h s) k -> p h s k", s=2)
            candv = cand[:P, :].rearrange("p (h a b) -> p h a b", a=16, b=16)
            c.op("dve", lambda e: e.tensor_tensor(out=candv, in0=tv4[:, :, 0, :].unsqueeze(3).to_broadcast([P, 8, 16, 16]),
                                                  in1=tv4[:, :, 1, :].unsqueeze(2).to_broadcast([P, 8, 16, 16]),
                                                  op=ALU.add), reads=[r_pk], writes=[r_cand])
            cand3 = cand[:P, :].rearrange("p (h c) -> p h c", c=256)
            for h in range(8):
                top16(P, cand3[:, h, :], 256, best[:P, h, :], pos[:P, h, :], [r_cand], [r_pk])
            c.op("dve", lambda e: e.tensor_scalar(out=gsm[:P, 0:8], in0=best[:P, :, 0], scalar1=-1.0, scalar2=None,
                                                  op0=ALU.mult), reads=[r_pk], writes=[r_pk])
            for h in range(8):
                c.op("act", lambda e, h=h: e.activation(out=gate[:P, h, :], in_=best[:P, h, :], func=AF.Exp,
                                                        bias=gsm[:P, h:h + 1], scale=1.0,
                                                        accum_out=gsm[:P, 8 + h:9 + h]), reads=[r_pk], writes=[r_pk])
            c.op("dve", lambda e: e.reciprocal(out=gsm[:P, 16:24], in_=gsm[:P, 8:16]), reads=[r_pk], writes=[r_pk])
            c.op("dve", lambda e: e.tensor_tensor(out=gate[:P], in0=gate[:P],
                                                  in1=gsm[:P, 16:24].unsqueeze(2).to_broadcast([P, 8, 16]),
                                                  op=ALU.mult), reads=[r_pk], writes=[r_pk])
            posf = pos[:P].rearrange("p h k -> p (h k)")
            c.op("dve", lambda e: e.tensor_copy(out=pbf[:P, :], in_=posf), reads=[r_pk], writes=[r_pk])
            c.op("dve", lambda e: e.tensor_scalar(out=paf[:P, :], in0=pbf[:P, :], scalar1=16.0, scalar2=None,
                                                  op0=ALU.is_ge), reads=[r_pk], writes=[r_pk])
            for m in range(2, 16):
                c.op("dve", lambda e, m=m: e.scalar_tensor_tensor(out=paf[:P, :], in0=pbf[:P, :], scalar=16.0 * m,
                                                                  in1=paf[:P, :], op0=ALU.is_ge, op1=ALU.add),
                     reads=[r_pk], writes=[r_pk])
            c.op("dve", lambda e: e.scalar_tensor_tensor(out=pbf[:P, :], in0=paf[:P, :], scalar=-16.0, in1=pbf[:P, :],
                                                         op0=ALU.mult, op1=ALU.add), reads=[r_pk], writes=[r_pk])
            c.op("dve", lambda e: e.tensor_copy(out=tif[:P], in_=ti[:P]), reads=[r_pk], writes=[r_pk])
            tif4 = tif[:P].rearrange("p (h s) k -> p h s k", s=2)
            ohv = sc[:P, :].rearrange("p (h a b) -> p h a b", a=16, b=16)
            io4 = iota16[:P, :].unsqueeze(1).unsqueeze(1).to_broadcast([P, 8, 16, 16])
            for side, (pf_, dsts) in enumerate(((paf, i1s), (pbf, i2s))):
                pv = pf_[:P, :].rearrange("p (h k) -> p h k", k=16).unsqueeze(3).to_broadcast([P, 8, 16, 16])
                c.op("dve", lambda e, pv=pv: e.tensor_tensor(out=ohv, in0=pv, in1=io4, op=ALU.is_equal),
                     reads=[r_pk, r_iota], writes=[r_sc])
                c.op("dve", lambda e, side=side: e.tensor_tensor(
                    out=ohv, in0=ohv, in1=tif4[:, :, side, :].unsqueeze(2).to_broadcast([P, 8, 16, 16]), op=ALU.mult),
                    reads=[r_sc, r_pk], writes=[r_sc])
                c.op("dve", lambda e, dsts=dsts: e.tensor_reduce(
                    out=dsts[:P, :].rearrange("p (h k) -> p h k", k=16), in_=ohv, axis=mybir.AxisListType.X,
                    op=ALU.add), reads=[r_sc], writes=[r_pk])
            c.op("dve", lambda e: e.scalar_tensor_tensor(out=eidf[:P, :], in0=i1s[:P, :], scalar=128.0, in1=i2s[:P, :],
                                                         op0=ALU.mult, op1=ALU.add), reads=[r_pk], writes=[r_pk])
            c.op("pe", lambda e: e.transpose(out=pb[0][:, 0:P], in_=eidf[:P, :], identity=identf[:P, :P]),
                 reads=[r_pk, r_const], writes=[rpb[0]])
            c.op("dve", lambda e: e.tensor_copy(out=idxT[:, 0:P], in_=pb[0][:, 0:P]), reads=[rpb[0]], writes=[r_idxT])
            c.op("pe", lambda e: e.transpose(out=pb[1][:, 0:P], in_=gate[:P].rearrange("p h k -> p (h k)"),
                                             identity=identf[:P, :P]), reads=[r_pk, r_const], writes=[rpb[1]])
            c.op("act", lambda e: e.activation(out=GTt[:, 0:P], in_=pb[1][:, 0:P], func=AF.Copy), reads=[rpb[1]],
                 writes=[r_GTt])
            for t in range(nv):
                k = pctr["g"] % NGB
                pctr["g"] += 1
                c.swdma(gbuf[k][:], tabu[:, :], reads=[r_idxT, r_tab], writes=[r_gbuf[k]],
                        indirect=bass.IndirectOffsetOnAxis(ap=idxT[:, t:t + 1], axis=0))
                s0 = (t % 2) * 4

                def fx(e, t=t, s0=s0):
                    inst = None
                    for q in range(4):
                        inst = e.matmul(pb[s0 + q][:, :], lhsT=ident[:P, t:t + 1].to_broadcast([P, 128]),
                                        rhs=h2_t[:P, q * 512:(q + 1) * 512], start=True, stop=True)
                    return inst
                c.op("pe", fx, reads=[r_h2t, r_const], writes=[rpb[s0 + q] for q in range(4)])
                for q in range(4):
                    c.op("dve", lambda e, t=t, q=q, k=k, s0=s0: e.scalar_tensor_tensor(
                        out=junk4[:], in0=gbuf[k][:, q * 512:(q + 1) * 512], scalar=1.0, in1=pb[s0 + q][:, :],
                        op0=ALU.mult, op1=ALU.mult, accum_out=apart[:, t, q:q + 1]),
                        reads=[r_gbuf[k], rpb[s0 + q]], writes=[r_junk4, r_apart])
            if nv < P:
                c.op("dve", lambda e: e.memset(apart[:, nv:P, :], 0.0), writes=[r_apart])
            c.op("dve", lambda e: e.tensor_reduce(out=AT[:, 0:P], in_=apart[:, 0:P, :], axis=mybir.AxisListType.X,
                                                  op=ALU.add), reads=[r_apart], writes=[r_apart])
            c.op("act", lambda e: e.activation(out=AT[:, 0:P], in_=AT[:, 0:P], func=AF.Gelu_apprx_tanh),
                 reads=[r_apart], writes=[r_apart])
            c.op("dve", lambda e: e.tensor_tensor(out=CT[:, 0:P], in0=AT[:, 0:P], in1=GTt[:, 0:P], op=ALU.mult),
                 reads=[r_apart, r_GTt], writes=[r_CT])
            for t in range(nv):
                k = pctr["g"] % NGB
                pctr["g"] += 1
                c.swdma(gbuf[k][:], tabv[:, :], reads=[r_idxT, r_tab], writes=[r_gbuf[k]],
                        indirect=bass.IndirectOffsetOnAxis(ap=idxT[:, t:t + 1], axis=0))
                kc = pctr["c"] % NCB
                pctr["c"] += 1
                c.op("act", lambda e, t=t, kc=kc: e.activation(out=cbuf[kc][:, 127:128], in_=CT[:, t:t + 1],
                                                               func=AF.Copy), reads=[r_CT], writes=[r_cbuf[kc]])

                def fy(e, t=t, k=k, kc=kc):
                    inst = None
                    for q in range(4):
                        inst = e.matmul(pb[q][:P, :], lhsT=cbuf[kc][:, 127 - t:127 - t + P],
                                        rhs=gbuf[k][:, q * 512:(q + 1) * 512], start=(t == 0), stop=(t == nv - 1))
                    return inst
                c.op("pe", fy, reads=[r_cbuf[kc], r_gbuf[k]], writes=[rpb[q] for q in range(4)])
            for q in range(4):
                c.op("dve", lambda e, q=q: e.tensor_tensor(out=x2_t[:P, q * 512:(q + 1) * 512], in0=pb[q][:P, :],
                                                           in1=x2_t[:P, q * 512:(q + 1) * 512], op=ALU.add),
                     reads=[rpb[q], r_x2t], writes=[r_x2t])
            rms_h(x2_t, r_x2t, P, g_fin, r_gfin, yout, r_yout)
            c.dma("sp", y_dst, yout[:nv, :], reads=[r_yout])

        def phase4_group(ntile, P, nv, x_src, mTg, r_mTg, cross_fn, y_dst_fn):
            ntok = ntile * P
            for j in range(ntile):
                if nv < P:
                    c.op("pool", lambda e, j=j: e.memset(x1[j][:], 0.0), writes=[r_x1[j]])
                c.dma("sp", x1[j][:nv, :], x_src(j), writes=[r_x1[j]])
            ws = WStream([(nm, b) for nm in ("w_out", "w_mq", "w_mo", "peer_wq") for b in range(4)])
            for blk in range(4):
                wt, r_w = ws.get()
                for j in range(ntile):
                    b = bank(2, 6)
                    mm_tok(mTg, r_mTg, j * P, P, wt, r_w, b)
                    c.op("dve", lambda e, j=j, blk=blk, b=b: e.tensor_tensor(
                        out=x1[j][:P, blk * 512:(blk + 1) * 512], in0=pb[b][:P, :],
                        in1=x1[j][:P, blk * 512:(blk + 1) * 512], op=ALU.add), reads=[rpb[b], r_x1[j]], writes=[r_x1[j]])
            for j in range(ntile):
                rms_h(x1[j], r_x1[j], P, g_mem, r_gmem, hb4, r_hb4)
                transpose_to(hb4, r_hb4, P, 16, lambda c0, c1, j=j: bufA[:, c0:c1, j * P:(j + 1) * P], r_bufA)
            for blk in range(4):
                wt, r_w = ws.get()
                for ct in range(4):
                    b = bank(2, 6)
                    mm_feat(bufA, r_bufA, 0, ntok, wt, r_w, ct, b)
                    c.op("act", lambda e, blk=blk, ct=ct, b=b: e.activation(
                        out=bufB[:, blk * 4 + ct, 0:ntok], in_=pb[b][:, 0:ntok], func=AF.Copy),
                        reads=[rpb[b]], writes=[r_bufB])
            cross_fn(bufB, r_bufB, bufA, r_bufA)
            for blk in range(4):
                wt, r_w = ws.get()
                for j in range(ntile):
                    b = bank(2, 6)
                    mm_tok(bufA, r_bufA, j * P, P, wt, r_w, b)
                    c.op("dve", lambda e, j=j, blk=blk, b=b: e.tensor_tensor(
                        out=x1[j][:P, blk * 512:(blk + 1) * 512], in0=pb[b][:P, :],
                        in1=x1[j][:P, blk * 512:(blk + 1) * 512], op=ALU.add), reads=[rpb[b], r_x1[j]], writes=[r_x1[j]])
            for j in range(ntile):
                rms_h(x1[j], r_x1[j], P, g_peer, r_gpeer, h2[j], r_h2[j])
                transpose_to(h2[j], r_h2[j], P, 16, lambda c0, c1, j=j: bufB[:, c0:c1, j * P:(j + 1) * P], r_bufB)
            for blk in range(4):
                wt, r_w = ws.get()
                for ct in range(4):
                    b = bank(2, 6)
                    mm_feat(bufB, r_bufB, 0, ntok, wt, r_w, ct, b)
                    c.op("act", lambda e, blk=blk, ct=ct, b=b: e.activation(
                        out=bufA[:, blk * 4 + ct, 0:ntok], in_=pb[b][:, 0:ntok], func=AF.Copy),
                        reads=[rpb[b]], writes=[r_bufA])
            for j in range(ntile):
                peer_tile(P, nv, bufA, r_bufA, j * P, h2[j], r_h2[j], x1[j], r_x1[j], y_dst_fn(j))

        mTg = c.sb(es4, "mTg", [128, 16, GT], BF16)
        r_mTg = Res()
        for g in range((NT // G) if 5 in _PH else 0)[:_LIM["groups5"]]:
            c.dma("sp", mTg[:], mTs[:, :, g * GT:(g + 1) * GT].rearrange("k p t -> p k t"), reads=r_mTs,
                  writes=[r_mTg])
            phase4_group(
                G, 128, 128, lambda j, g=g: xo[g * GT + j * 128:g * GT + (j + 1) * 128, :], mTg, r_mTg,
                lambda qT_, r_q, oT_, r_o: cross_attn(qT_, r_q, 0, GT, mkT, [r_mkT],
                                                      lambda mt, c0: mv_bf[:, mt, c0:c0 + 128], [r_mvbf], oT_, r_o),
                lambda j, g=g: y_p[g * GT + j * 128:g * GT + (j + 1) * 128, :])

        def cross_sample(qT_, r_q, oT_, r_o):
            stage = ((sc, r_sc), (cand, r_cand))
            n = 0
            for b in range(4):
                for kv in range(2):
                    for mt in range(2):
                        st, r_st = stage[n % 2]
                        n += 1
                        c.dma("sp", st[:], cmem[b, mt * 128:(mt + 1) * 128, kv, :], writes=[r_st])
                        k = kv * 2 + mt
                        c.op("pool", lambda e, st=st, k=k: e.tensor_copy(out=gbuf[k][:], in_=st[:]), reads=[r_st],
                             writes=[r_gbuf[k]])
                for mt in range(2):
                    transpose_to(gbuf[mt], r_gbuf[mt], 128, 16,
                                 lambda c0, c1, mt=mt: mTg[:, c0:c1, mt * 128:(mt + 1) * 128], r_mTg)
                cross_attn(qT_, r_q, b * 8, 8, mTg, [r_mTg], lambda mt, c0: gbuf[2 + mt][:, c0:c0 + 128],
                           [r_gbuf[2], r_gbuf[3]], oT_, r_o)

        if 6 in _PH:
            phase4_group(1, 128, SP, lambda j: xs[:, :], mTg_s, r_mTgs, cross_sample, lambda j: y_s[:, :])

        c.finish()
    return nc


def _host_tables(rel_bias):
    biasT = np.zeros((24, 128, 2, 128), np.float32)
    jl = np.arange(128)[:, None, None]
    jt = np.arange(2)[None, :, None]
    i = np.arange(128)[None, None, :]
    off = i + 128 - (jt * 128 + jl)
    band = ((off >= 0) & (off <= 128)).astype(np.float32)
    offc = np.clip(off, 0, 128)
    for g, dil in enumerate(DILS):
        bucket = _t5_bucket(dil * np.arange(129))
        for h in range(8):
            biasT[g * 8 + h] = rel_bias[bucket[offc], g * 8 + h]
    sbias = np.zeros((8, 128, 16, 24), np.float32)
    smask = np.zeros((128, 16, 24), np.float32)
    sbias_n = np.zeros((8, 32, 4, 24), np.float32)
    smask_n = np.zeros((32, 4, 24), np.float32)
    for g, dil in enumerate(DILS):
        bucket = _t5_bucket(dil * np.arange(129))
        for t in range(8):
            for j in range(129):
                row = 2048 + t - dil * j
                col = g * 8 + t
                if row < 2048:
                    smask[row % 128, row // 128, col] = 1.0
                    sbias[:, row % 128, row // 128, col] = rel_bias[bucket[j], g * 8:(g + 1) * 8]
                else:
                    tp = row - 2048
                    for b in range(4):
                        smask_n[b * 8 + tp, b, col] = 1.0
                        sbias_n[:, b * 8 + tp, b, col] = rel_bias[bucket[j], g * 8:(g + 1) * 8]
    return biasT, band, sbias, smask, sbias_n, smask_n


_NC_CACHE = {}


def _prepare(x_prompt, x_sample, mem_prompt, cache_win, cache_mem_kv, rel_bias, norm_mix, w_in,
             sgu_ln_g, sgu_ln_b, sgu_w, sgu_b, w_out, norm_mem, norm_memtok, w_mq, w_mk, w_mv, w_mo,
             norm_peer, peer_wq, peer_keys1, peer_keys2, peer_u, peer_v, norm_final):
    f = lambda a: np.ascontiguousarray(np.asarray(a, dtype=np.float32))
    xp = f(x_prompt)[0]
    xsm = f(x_sample)
    rel_bias = f(rel_bias)
    biasT, band, sbias, smask, sbias_n, smask_n = _host_tables(rel_bias)
    sgu_w0 = f(sgu_w)[0]
    sgu_wT = np.ascontiguousarray(sgu_w0.transpose(0, 2, 1))
    sgu_wTs = np.zeros((8, 128, 128), np.float32)
    for b in range(4):
        sgu_wTs[:, b * 8:(b + 1) * 8, b * 8:(b + 1) * 8] = sgu_wT[:, :8, :8]
    sgu_b0 = f(sgu_b)[0]
    sgu_bT = np.ascontiguousarray(sgu_b0.T)
    sgu_bTs = np.zeros((128, 8), np.float32)
    sgu_bTs[:32] = np.tile(sgu_b0[:, :8].T, (4, 1))
    shared = {
        "mem": f(mem_prompt)[0], "w_in": f(w_in)[0], "w_out": f(w_out)[0], "w_mq": f(w_mq)[0],
        "w_mk": f(w_mk)[0], "w_mv": f(w_mv)[0], "w_mo": f(w_mo)[0], "peer_wq": f(peer_wq)[0],
        "peer_u": f(peer_u)[0], "peer_v": f(peer_v)[0], "keys1": f(peer_keys1)[0], "keys2": f(peer_keys2)[0],
        "norm_mix": f(norm_mix), "norm_mem": f(norm_mem), "norm_memtok": f(norm_memtok),
        "norm_peer": f(norm_peer), "norm_final": f(norm_final).reshape(1, D),
        "sgu_ln_g": f(sgu_ln_g), "sgu_ln_b": f(sgu_ln_b), "sgu_wT": sgu_wT, "sgu_wTs": sgu_wTs,
        "sgu_bT": sgu_bT, "sgu_bTs": sgu_bTs, "biasT": biasT, "bandmask": band, "sbias": sbias,
        "smask": smask, "sbias_n": sbias_n, "smask_n": smask_n,
    }
    cw = f(cache_win)[0]
    cm = f(cache_mem_kv)[0].reshape(32, 256, 2, 2048)
    in_maps = []
    for cidx in range(NCORES):
        m = dict(shared)
        m["xo"] = xp[cidx * TOK:(cidx + 1) * TOK]
        m["xh"] = xp[(cidx - 1) * TOK:cidx * TOK] if cidx > 0 else np.zeros((TOK, D), np.float32)
        m["xs"] = np.ascontiguousarray(xsm[cidx * 4:(cidx + 1) * 4].reshape(SP, D))
        m["cwin"] = cw[cidx * 4:(cidx + 1) * 4]
        m["cmem"] = cm[cidx * 4:(cidx + 1) * 4]
        m["pflag"] = np.full((128, 1), 0.0 if cidx == 0 else 1.0, np.float32)
        in_maps.append(m)
    return in_maps


def kernel(**inputs):
    in_maps = _prepare(**inputs)
    if "nc" not in _NC_CACHE:
        _NC_CACHE["nc"] = build_program()
    ncr = _LIM.get("ncores") or NCORES
    res = run_bass_kernel_spmd(_NC_CACHE["nc"], in_maps[:ncr], core_ids=list(range(ncr)))
    R = list(res.results)
    while len(R) < NCORES:
        R.append(R[0])
    y_prompt = np.concatenate([R[i]["y_p"] for i in range(NCORES)], axis=0).reshape(1, NCORES * TOK, D)
    y_sample = np.concatenate([R[i]["y_s"] for i in range(NCORES)], axis=0).reshape(32, 8, D)
    win_p = R[NCORES - 1]["win_p"].reshape(1, 1, TOK, 2, 8, 128)
    memkv = R[0]["memkv"].reshape(1, 1, 256, 2, 4, 512)
    win_s = np.concatenate([R[i]["win_s"] for i in range(NCORES)], axis=0).reshape(1, 32, 8, 2, 8, 128)
    sgu_s = np.concatenate([R[i]["sgu_s"] for i in range(NCORES)], axis=0).reshape(1, 32, 8, 1024)
    return (y_prompt, y_sample, win_p, memkv, win_s, sgu_s)
```

```python
import contextlib
import math
import numpy as np
import concourse.bass as bass
import concourse.mybir as mybir
from concourse.bass_utils import run_bass_kernel_spmd

F32 = mybir.dt.float32
BF16 = mybir.dt.bfloat16
I32 = mybir.dt.int32
U32 = mybir.dt.uint32
AF = mybir.ActivationFunctionType
ALU = mybir.AluOpType

NCORES = 8
D = 2048
TOK = 2048
NT = TOK // 128
SP = 32
EPS = 1e-6
DILS = (1, 4, 16)
REL_BUCKETS = 32
REL_MAX_DIST = 2048
ATT_SCALE = 128 ** -0.5
MEM_SCALE = 512 ** -0.5
NEG = -1e30


class Res:
    __slots__ = ("w", "r")

    def __init__(self):
        self.w = None
        self.r = {}


class Ctx:
    NDMA = 32

    def __init__(self, nc, es):
        self.nc = nc
        self.es = es
        self.eng = {"pe": nc.tensor, "act": nc.scalar, "dve": nc.vector, "pool": nc.gpsimd, "sp": nc.sync}
        self.sem = {}
        self.cnt = {}
        self.known = {e: {} for e in self.eng}
        for e in self.eng:
            self.sem[e] = es.enter_context(nc.semaphore("s_" + e))
            self.cnt[e] = 0
        self.dsem = [es.enter_context(nc.semaphore("d%d" % i)) for i in range(self.NDMA)]
        self.dtgt = [0] * self.NDMA
        self.drr = 0
        self.nop = 0
        self.muted = False
        self.NSW = 8
        self.swsem = [es.enter_context(nc.semaphore("w%d" % i)) for i in range(self.NSW)]
        self.swtgt = [0] * self.NSW
        self.swrr = 0

    def sb(self, es, name, shape, dt):
        return es.enter_context(self.nc.sbuf_tensor(name, list(shape), dt))

    def _semof(self, key):
        if isinstance(key, str):
            return self.sem[key]
        if isinstance(key, tuple):
            return self.swsem[key[1]]
        return self.dsem[key]

    def swdma(self, out, in_, reads=(), writes=(), indirect=None, **kw):
        if self.muted:
            return None
        i = self.swrr % self.NSW
        self.swrr += 1
        deps = self._deps(reads, writes)
        if self.swtgt[i]:
            deps.append((("w", i), self.swtgt[i]))
        self._wait("pool", deps)
        self.swtgt[i] += 16
        if indirect is not None:
            inst = self.eng["pool"].indirect_dma_start(out=out, out_offset=None, in_=in_, in_offset=indirect, **kw)
        else:
            inst = self.eng["pool"].dma_start(out=out, in_=in_, **kw)
        inst.then_inc(self.swsem[i], 16)
        tok = (("w", i), self.swtgt[i])
        self._mark(tok, reads, writes)
        self.nop += 1
        return tok

    def _wait(self, e, deps):
        need = {}
        for tok in deps:
            if tok is None:
                continue
            k, v = tok
            if k == "pe" and e == "pe":
                continue
            if self.known[e].get(k, 0) >= v:
                continue
            if need.get(k, 0) < v:
                need[k] = v
        for k, v in need.items():
            self.eng[e].wait_ge(self._semof(k), v)
            self.known[e][k] = v

    @staticmethod
    def _deps(reads, writes):
        deps = []
        for r in reads:
            deps.append(r.w)
        for r in writes:
            deps.append(r.w)
            for k, v in r.r.items():
                deps.append((k, v))
        return deps

    @staticmethod
    def _mark(tok, reads, writes):
        k, v = tok
        for r in reads:
            if r.r.get(k, 0) < v:
                r.r[k] = v
        for r in writes:
            r.w = tok
            r.r = {}

    def op(self, e, fn, reads=(), writes=()):
        if self.muted:
            return None
        self._wait(e, self._deps(reads, writes))
        inst = fn(self.eng[e])
        self.cnt[e] += 1
        inst.then_inc(self.sem[e], 1)
        tok = (e, self.cnt[e])
        self._mark(tok, reads, writes)
        self.nop += 1
        return tok

    def dma(self, q, out, in_, reads=(), writes=(), indirect=None, **kw):
        if self.muted:
            return None
        i = self.drr % self.NDMA
        self.drr += 1
        deps = self._deps(reads, writes)
        if self.dtgt[i]:
            deps.append((i, self.dtgt[i]))
        self._wait(q, deps)
        self.dtgt[i] += 16
        if indirect is not None:
            inst = self.eng[q].indirect_dma_start(out=out, out_offset=None, in_=in_, in_offset=indirect, **kw)
        else:
            inst = self.eng[q].dma_start(out=out, in_=in_, **kw)
        inst.then_inc(self.dsem[i], 16)
        tok = (i, self.dtgt[i])
        self._mark(tok, reads, writes)
        self.nop += 1
        return tok

    def _swtoks(self):
        return [(("w", i), t) for i, t in enumerate(self.swtgt) if t]

    def barrier(self):
        self.muted = False
        deps = [(i, t) for i, t in enumerate(self.dtgt) if t] + self._swtoks()
        deps += [(e, n) for e, n in self.cnt.items() if n]
        for e in self.eng:
            self._wait(e, [d for d in deps if d[0] != e])

    def finish(self):
        deps = [(i, t) for i, t in enumerate(self.dtgt) if t] + self._swtoks()
        deps += [(e, n) for e, n in self.cnt.items() if n and e != "sp"]
        self._wait("sp", deps)


def _t5_bucket(dist):
    max_exact = REL_BUCKETS // 2
    d = np.maximum(dist, 1).astype(np.float64)
    large = max_exact + (np.log(d / max_exact) / math.log(REL_MAX_DIST / max_exact)
                         * (REL_BUCKETS - max_exact)).astype(np.int64)
    large = np.minimum(large, REL_BUCKETS - 1)
    return np.where(dist < max_exact, dist, large).astype(np.int32)


class _SkipPhase(Exception):
    pass


_PH = {0, 1, 2, 3, 4, 5, 6}
_LIM = {"groups": None, "heads": None, "groups5": None}


def build_program():
    nc = bass.Bass("TRN2", target_bir_lowering=False)

    def din(name, shape, dt=F32):
        return nc.dram_tensor(name, list(shape), dt, kind="ExternalInput").ap()

    def dout(name, shape, dt=F32):
        return nc.dram_tensor(name, list(shape), dt, kind="ExternalOutput").ap()

    def dscr(name, shape, dt=BF16):
        return nc.dram_tensor(name, list(shape), dt, kind="Internal").ap()

    xo = din("xo", [TOK, D])
    xh = din("xh", [TOK, D])
    xs = din("xs", [SP, D])
    mem = din("mem", [256, D])
    cwin = din("cwin", [4, 2048, 2, 8, 128])
    cmem = din("cmem", [4, 256, 2, 2048])
    w_in = din("w_in", [D, 9216])
    wsq = {n: din(n, [D, D]) for n in ("w_out", "w_mq", "w_mk", "w_mv", "w_mo", "peer_wq")}
    peer_u = din("peer_u", [16384, D])
    peer_v = din("peer_v", [16384, D])
    keys1 = din("keys1", [8, 128, 128])
    keys2 = din("keys2", [8, 128, 128])
    gains = {n: din(n, [1, D]) for n in ("norm_mix", "norm_mem", "norm_memtok", "norm_peer", "norm_final")}
    sgu_ln_g = din("sgu_ln_g", [1, 1024])
    sgu_ln_b = din("sgu_ln_b", [1, 1024])
    sgu_wT = din("sgu_wT", [8, 128, 128])
    sgu_wTs = din("sgu_wTs", [8, 128, 128])
    sgu_bT = din("sgu_bT", [128, 8])
    sgu_bTs = din("sgu_bTs", [128, 8])
    biasT = din("biasT", [24, 128, 2, 128])
    bandmask = din("bandmask", [128, 2, 128])
    sbias = din("sbias", [8, 128, 16, 24])
    smask = din("smask", [128, 16, 24])
    sbias_n = din("sbias_n", [8, 32, 4, 24])
    smask_n = din("smask_n", [32, 4, 24])
    pflag = din("pflag", [128, 1])

    y_p = dout("y_p", [TOK, D])
    y_s = dout("y_s", [SP, D])
    win_p = dout("win_p", [TOK, 2, 1024])
    memkv = dout("memkv", [256, 2, 2048])
    win_s = dout("win_s", [SP, 2, 1024])
    sgu_s = dout("sgu_s", [SP, 1024])

    WBLK = {"w_in": 18, "w_out": 4, "w_mq": 4, "w_mk": 4, "w_mv": 4, "w_mo": 4, "peer_wq": 4}
    wscr = {n: dscr("wb_" + n, [k, 128, 16, 512]) for n, k in WBLK.items()}
    wscr_res = {n: [Res() for _ in range(k)] for n, k in WBLK.items()}
    tabu = dscr("tabu", [16384, D])
    tabv = dscr("tabv", [16384, D])
    r_tab = Res()
    qTs = dscr("qTs", [24, 128, TOK])
    kTs = dscr("kTs", [8, 128, 2 * TOK])
    vTs = dscr("vTs", [8, 128, 2 * TOK])
    gbTs = dscr("gbTs", [8, 128, TOK])
    mTs = dscr("mTs", [16, 128, TOK])
    r_qTs = [Res() for _ in range(24)]
    r_kTs = [Res() for _ in range(8)]
    r_vTs = [Res() for _ in range(8)]
    r_gbTs = [Res() for _ in range(8)]
    r_mTs = [Res() for _ in range(16)]

    with contextlib.ExitStack() as es:
        c = Ctx(nc, es)

        pb = [es.enter_context(nc.psum_tensor("pb%d" % i, [128, 512], F32)) for i in range(8)]
        rpb = [Res() for _ in range(8)]
        pbb = [p[:].bitcast(BF16) for p in pb]

        identf = c.sb(es, "identf", [128, 128], F32)
        ident = c.sb(es, "ident", [128, 128], BF16)
        ones = c.sb(es, "ones", [128, 128], BF16)
        r_const = Res()
        c.op("pool", lambda e: e.memset(identf[:], 0.0), writes=[r_const])
        c.op("pool", lambda e: e.affine_select(out=identf[:], in_=identf[:], pattern=[[-1, 128]],
                                               compare_op=ALU.not_equal, fill=1.0, base=0,
                                               channel_multiplier=1), reads=[r_const], writes=[r_const])
        c.op("dve", lambda e: e.tensor_copy(out=ident[:], in_=identf[:]), reads=[r_const], writes=[r_const])
        c.op("dve", lambda e: e.memset(ones[:], 1.0), writes=[r_const])

        def cut(k):
            if _LIM.get("cut") == k:
                c.muted = True

        wpool = {"buf": [], "res": [], "n": 0}
        wctr = [0]

        def make_wpool(es_, n):
            tag = "%d" % len(wpool.setdefault("gen", []))
            wpool["gen"].append(n)
            wpool["buf"] = [c.sb(es_, "wbuf%s_%d" % (tag, i), [128, 16, 512], BF16) for i in range(n)]
            wpool["res"] = [Res() for _ in range(n)]
            wpool["n"] = n
            wctr[0] = 0

        class WStream:
            def __init__(self, blocks):
                self.blocks = list(blocks)
                self.issued = 0
                self.pos = 0
                self.base = wctr[0]

            def _issue(self):
                name, blk = self.blocks[self.issued]
                i = (self.base + self.issued) % wpool["n"]
                c.dma("sp", wpool["buf"][i][:], wscr[name][blk], reads=[wscr_res[name][blk]],
                      writes=[wpool["res"][i]])
                self.issued += 1

            def get(self):
                while self.issued < len(self.blocks) and self.issued < self.pos + wpool["n"]:
                    self._issue()
                i = (self.base + self.pos) % wpool["n"]
                self.pos += 1
                wctr[0] = self.base + self.pos
                return wpool["buf"][i], wpool["res"][i]

        def load_gain(es_, name, ap, width=D):
            t = c.sb(es_, "g_" + name, [128, width], F32)
            r = Res()
            c.dma("sp", t[:], ap[0:1, :].partition_broadcast(128), writes=[r])
            return t, r

        bankctr = [0]

        def bank(lo, hi):
            n = hi - lo
            i = lo + bankctr[0] % n
            bankctr[0] += 1
            return i

        NSM = 4
        sm = [c.sb(es, "sm%d" % i, [128, 8], F32) for i in range(NSM)]
        r_sm = [Res() for _ in range(NSM)]
        smctr = [0]
        junk = c.sb(es, "junk", [128, D], BF16)
        r_junk = Res()

        def rms_h(x_t, r_x, P, gain_t, r_gain, h_t, r_h):
            i = smctr[0] % NSM
            smctr[0] += 1
            s, rs = sm[i], r_sm[i]
            c.op("act", lambda e: e.activation(out=junk[:P, :], in_=x_t[:P, :], func=AF.Square,
                                               accum_out=s[:P, 0:1]), reads=[r_x], writes=[r_junk, rs])
            c.op("act", lambda e: e.activation(out=s[:P, 1:2], in_=s[:P, 0:1], func=AF.Sqrt,
                                               scale=1.0 / D, bias=epsc[:P, 0:1]), reads=[rs, r_const], writes=[rs])
            c.op("dve", lambda e: e.reciprocal(out=s[:P, 2:3], in_=s[:P, 1:2]), reads=[rs], writes=[rs])
            c.op("dve", lambda e: e.scalar_tensor_tensor(out=h_t[:P, :], in0=x_t[:P, :], scalar=s[:P, 2:3],
                                                         in1=gain_t[:P, :], op0=ALU.mult, op1=ALU.mult),
                 reads=[r_x, rs, r_gain], writes=[r_h])

        epsc = c.sb(es, "epsc", [128, 1], F32)
        c.op("dve", lambda e: e.memset(epsc[:], EPS), writes=[r_const])

        def transpose_to(h_t, r_h, P, nchunk, dst_fn, r_dst, evac="act"):
            for c0 in range(0, nchunk, 8):
                c1 = min(nchunk, c0 + 8)
                b = bank(0, 2)
                pv = pbb[b][:, 0:(c1 - c0) * 128].rearrange("p (a t) -> p a t", t=128)

                def f(e, c0=c0, c1=c1, pv=pv):
                    inst = None
                    for k in range(c0, c1):
                        inst = e.transpose(out=pv[:, k - c0, 0:P], in_=h_t[:P, k * 128:(k + 1) * 128],
                                           identity=ident[:P, :P])
                    return inst
                c.op("pe", f, reads=[r_h, r_const], writes=[rpb[b]])
                dst = dst_fn(c0, c1)
                if evac == "act":
                    c.op("act", lambda e, dst=dst, pv=pv: e.activation(out=dst, in_=pv[:, :, 0:P], func=AF.Copy),
                         reads=[rpb[b]], writes=[r_dst])
                else:
                    c.op("dve", lambda e, dst=dst, pv=pv: e.tensor_copy(out=dst, in_=pv[:, :, 0:P]),
                         reads=[rpb[b]], writes=[r_dst])

        def mm_tok(hT, r_hT, t0, P, wt, r_w, b, ncol=512, c0=0):
            def f(e):
                inst = None
                for kc in range(16):
                    inst = e.matmul(pb[b][:P, 0:ncol], lhsT=hT[:, kc, t0:t0 + P], rhs=wt[:, kc, c0:c0 + ncol],
                                    start=(kc == 0), stop=(kc == 15))
                return inst
            c.op("pe", f, reads=[r_hT, r_w], writes=[rpb[b]])

        def mm_feat(hT, r_hT, t0, ntok, wt, r_w, ct, b):
            def f(e):
                inst = None
                for kc in range(16):
                    inst = e.matmul(pb[b][:, 0:ntok], lhsT=wt[:, kc, ct * 128:(ct + 1) * 128],
                                    rhs=hT[:, kc, t0:t0 + ntok], start=(kc == 0), stop=(kc == 15))
                return inst
            c.op("pe", f, reads=[r_hT, r_w], writes=[rpb[b]])

        with contextlib.suppress(_SkipPhase), contextlib.ExitStack() as es0:
            if 0 not in _PH:
                raise _SkipPhase()
            NST = 3
            stf = [c.sb(es0, "stf%d" % i, [128, 16, 512], F32) for i in range(NST)]
            stb = [c.sb(es0, "stb%d" % i, [128, 16, 512], BF16) for i in range(NST)]
            r_stf = [Res() for _ in range(NST)]
            r_stb = [Res() for _ in range(NST)]
            n = 0
            wlist = [("w_mk", wsq["w_mk"]), ("w_mv", wsq["w_mv"]), ("w_in", w_in), ("w_out", wsq["w_out"]),
                     ("w_mq", wsq["w_mq"]), ("w_mo", wsq["w_mo"]), ("peer_wq", wsq["peer_wq"])]
            for name, W in wlist:
                Wv = W.rearrange("(kc p) n -> p kc n", p=128)
                for blk in range(WBLK[name]):
                    i = n % NST
                    c.dma("sp", stf[i][:], Wv[:, :, blk * 512:(blk + 1) * 512], writes=[r_stf[i]])
                    if n % 2 == 0:
                        c.op("dve", lambda e, i=i: e.tensor_copy(out=stb[i][:], in_=stf[i][:]),
                             reads=[r_stf[i]], writes=[r_stb[i]])
                    else:
                        c.op("pool", lambda e, i=i: e.tensor_copy(out=stb[i][:], in_=stf[i][:]),
                             reads=[r_stf[i]], writes=[r_stb[i]])
                    c.dma("act", wscr[name][blk], stb[i][:], reads=[r_stb[i]], writes=[wscr_res[name][blk]])
                    n += 1
            if 5 in _PH or 6 in _PH:
                for src, dst in ((peer_u, tabu), (peer_v, tabv)):
                    sv = src.rearrange("(c p r) d -> c p (r d)", p=128, r=4)
                    dv = dst.rearrange("(c p r) d -> c p (r d)", p=128, r=4)
                    for ci in range(32):
                        i = n % NST
                        c.dma("sp", stf[i][:].rearrange("p a b -> p (a b)"), sv[ci], writes=[r_stf[i]])
                        if n % 2 == 0:
                            c.op("dve", lambda e, i=i: e.tensor_copy(out=stb[i][:], in_=stf[i][:]),
                                 reads=[r_stf[i]], writes=[r_stb[i]])
                        else:
                            c.op("pool", lambda e, i=i: e.tensor_copy(out=stb[i][:], in_=stf[i][:]),
                                 reads=[r_stf[i]], writes=[r_stb[i]])
                        c.dma("act", dv[ci], stb[i][:].rearrange("p a b -> p (a b)"), reads=[r_stb[i]], writes=[r_tab])
                        n += 1
            c.barrier()

        mkT = c.sb(es, "mkT", [128, 16, 256], BF16)
        mv_bf = c.sb(es, "mv_bf", [128, 2, D], BF16)
        r_mkT = Res()
        r_mvbf = Res()

        with contextlib.suppress(_SkipPhase), contextlib.ExitStack() as es1:
            if 1 not in _PH:
                raise _SkipPhase()
            make_wpool(es1, 3)
            g_mt, r_gmt = load_gain(es1, "memtok", gains["norm_memtok"])
            mT = c.sb(es1, "mT", [128, 16, 256], BF16)
            r_mT = Res()
            xm = c.sb(es1, "xm", [128, D], F32)
            r_xm = Res()
            hm = c.sb(es1, "hm1", [128, D], BF16)
            r_hm = Res()
            mk_bf = c.sb(es1, "mk_bf", [128, 2, D], BF16)
            r_mkbf = Res()
            kvf = [c.sb(es1, "kvf%d" % i, [128, D], F32) for i in range(2)]
            r_kvf = [Res() for _ in range(2)]
            for mt in range(2):
                c.dma("sp", xm[:], mem[mt * 128:(mt + 1) * 128, :], writes=[r_xm])
                rms_h(xm, r_xm, 128, g_mt, r_gmt, hm, r_hm)
                transpose_to(hm, r_hm, 128, 16, lambda c0, c1, mt=mt: mT[:, c0:c1, mt * 128:(mt + 1) * 128], r_mT)
            n = 0
            for kv, name in enumerate(("w_mk", "w_mv")):
                bf = mk_bf if kv == 0 else mv_bf
                r_bf = r_mkbf if kv == 0 else r_mvbf
                for mt in range(2):
                    i = n % 2
                    n += 1
                    ws = WStream([(name, blk) for blk in range(4)])
                    for blk in range(4):
                        wt, r_w = ws.get()
                        b = bank(2, 6)
                        mm_tok(mT, r_mT, mt * 128, 128, wt, r_w, b)
                        c.op("act", lambda e, i=i, blk=blk, b=b: e.activation(
                            out=kvf[i][:, blk * 512:(blk + 1) * 512], in_=pb[b][:, :], func=AF.Copy),
                            reads=[rpb[b]], writes=[r_kvf[i]])
                    c.op("dve", lambda e, i=i, mt=mt, bf=bf: e.tensor_copy(out=bf[:, mt, :], in_=kvf[i][:]),
                         reads=[r_kvf[i]], writes=[r_bf])
                    c.dma("sp", memkv[mt * 128:(mt + 1) * 128, kv, :], kvf[i][:], reads=[r_kvf[i]])
            for mt in range(2):
                transpose_to(mk_bf[:, mt, :], r_mkbf, 128, 16,
                             lambda c0, c1, mt=mt: mkT[:, c0:c1, mt * 128:(mt + 1) * 128], r_mkT)
            c.barrier()

        G = 2
        GT = G * 128

        def gmlp_tile(es_, P, gv_j, r_gv, gu_j, r_gu, sga_j, r_sga, Wmix, r_W, bs, r_bs, lng, lnb, r_ln,
                      tmp, mA, r_mA, vln_out=None, r_vln=None):
            st, mvv, vt, vlnb, gs = tmp["st"], tmp["mvv"], tmp["vt"], tmp["vlnb"], tmp["gs"]
            r_t = tmp["r"]
            c.op("dve", lambda e: e.bn_stats(out=st[:P, 0, :], in_=gv_j[:P, 0:512]), reads=[r_gv], writes=[r_t])
            c.op("dve", lambda e: e.bn_stats(out=st[:P, 1, :], in_=gv_j[:P, 512:1024]), reads=[r_gv], writes=[r_t])
            c.op("dve", lambda e: e.bn_aggr(out=mvv[:P, 0:2], in_=st[:P].rearrange("p a b -> p (a b)")),
                 reads=[r_t], writes=[r_t])
            c.op("act", lambda e: e.activation(out=mvv[:P, 2:3], in_=mvv[:P, 1:2], func=AF.Sqrt,
                                               bias=epsc[:P, 0:1], scale=1.0), reads=[r_t, r_const], writes=[r_t])
            c.op("dve", lambda e: e.reciprocal(out=mvv[:P, 3:4], in_=mvv[:P, 2:3]), reads=[r_t], writes=[r_t])
            c.op("dve", lambda e: e.tensor_scalar(out=vt[:P, :], in0=gv_j[:P, :], scalar1=mvv[:P, 0:1],
                                                  scalar2=mvv[:P, 3:4], op0=ALU.subtract, op1=ALU.mult),
                 reads=[r_gv, r_t], writes=[r_t])
            c.op("dve", lambda e: e.tensor_tensor(out=vt[:P, :], in0=vt[:P, :], in1=lng[:P, :], op=ALU.mult),
                 reads=[r_t, r_ln], writes=[r_t])
            if vln_out is None:
                c.op("pool", lambda e: e.tensor_tensor(out=vlnb[:P, :], in0=vt[:P, :], in1=lnb[:P, :], op=ALU.add),
                     reads=[r_t, r_ln], writes=[r_t])
            else:
                c.op("pool", lambda e: e.tensor_tensor(out=vln_out[:P, :], in0=vt[:P, :], in1=lnb[:P, :],
                                                       op=ALU.add), reads=[r_t, r_ln], writes=[r_vln])
                c.op("act", lambda e: e.activation(out=vlnb[:P, :], in_=vln_out[:P, :], func=AF.Copy),
                     reads=[r_vln], writes=[r_t])

            def f(e):
                inst = None
                for g in range(8):
                    inst = e.matmul(pb[6 + g // 4][:P, (g % 4) * 128:(g % 4 + 1) * 128], lhsT=Wmix[:P, g, :P],
                                    rhs=vlnb[:P, g * 128:(g + 1) * 128], start=True, stop=True)
                return inst
            c.op("pe", f, reads=[r_t, r_W], writes=[rpb[6], rpb[7]])
            for half in range(2):
                c.op("dve", lambda e, half=half: e.tensor_tensor(
                    out=vt[:P, half * 512:(half + 1) * 512].rearrange("p (g d) -> p g d", d=128),
                    in0=pb[6 + half][:P, :].rearrange("p (g d) -> p g d", d=128),
                    in1=bs[:P, half * 4:half * 4 + 4].unsqueeze(2).to_broadcast([P, 4, 128]), op=ALU.add),
                    reads=[rpb[6 + half], r_bs], writes=[r_t])
            c.op("pool", lambda e: e.tensor_tensor(out=gs[:P, :], in0=gu_j[:P, :], in1=sga_j[:P, :], op=ALU.mult),
                 reads=[r_gu, r_sga], writes=[r_t])
            c.op("dve", lambda e: e.tensor_tensor(out=mA[:P, :], in0=vt[:P, :], in1=gs[:P, :], op=ALU.mult),
                 reads=[r_t], writes=[r_mA])

        def gmlp_tmp(es_, tag):
            return {"st": c.sb(es_, "g_st" + tag, [128, 2, 6], F32), "mvv": c.sb(es_, "g_mvv" + tag, [128, 4], F32),
                    "vt": c.sb(es_, "g_vt" + tag, [128, 1024], F32), "vlnb": c.sb(es_, "g_vlnb" + tag, [128, 1024], BF16),
                    "gs": c.sb(es_, "g_gs" + tag, [128, 1024], BF16), "r": Res()}

        with contextlib.suppress(_SkipPhase), contextlib.ExitStack() as es2:
            if 2 not in _PH:
                raise _SkipPhase()
            make_wpool(es2, 3)
            g_mix, r_gmix = load_gain(es2, "mix", gains["norm_mix"])
            lng, r_ln = load_gain(es2, "lng", sgu_ln_g, 1024)
            lnb = c.sb(es2, "g_lnb", [128, 1024], F32)
            c.dma("sp", lnb[:], sgu_ln_b[0:1, :].partition_broadcast(128), writes=[r_ln])
            wsf = c.sb(es2, "wsf", [128, 8, 128], F32)
            WsT = c.sb(es2, "WsT", [128, 8, 128], BF16)
            r_ws = Res()
            c.dma("sp", wsf[:], sgu_wT.rearrange("g j i -> j g i"), writes=[r_ws])
            c.op("pool", lambda e: e.affine_select(out=wsf[:], in_=wsf[:], pattern=[[0, 8], [1, 128]],
                                                   compare_op=ALU.is_ge, fill=0.0, base=0, channel_multiplier=-1),
                 reads=[r_ws], writes=[r_ws])
            c.op("dve", lambda e: e.tensor_copy(out=WsT[:], in_=wsf[:]), reads=[r_ws], writes=[r_ws])
            bsT = c.sb(es2, "bsT", [128, 8], F32)
            r_bs = Res()
            c.dma("sp", bsT[:], sgu_bT[:, :], writes=[r_bs])

            hT = [c.sb(es2, "hT%d" % i, [128, 16, GT], BF16) for i in range(2)]
            r_hT = [Res() for _ in range(2)]
            xt = [c.sb(es2, "xt%d" % i, [128, D], F32) for i in range(2)]
            r_xt = [Res() for _ in range(2)]
            hb = [c.sb(es2, "hb%d" % i, [128, D], BF16) for i in range(2)]
            r_hb = [Res() for _ in range(2)]
            gv = c.sb(es2, "gv", [128, G, 1024], F32)
            gu = c.sb(es2, "gu", [128, G, 1024], BF16)
            sga = c.sb(es2, "sga", [128, G, 1024], BF16)
            r_gv = [Res() for _ in range(G)]
            r_gu = [Res() for _ in range(G)]
            r_sga = [Res() for _ in range(G)]
            gtmp = gmlp_tmp(es2, "p")
            mA = c.sb(es2, "mA", [128, 1024], BF16)
            r_mA = Res()
            mst = [c.sb(es2, "mst%d" % i, [128, 8, 128], BF16) for i in range(2)]
            r_mst = [Res() for _ in range(2)]
            fst = [c.sb(es2, "fst%d" % i, [128, 4, GT], BF16) for i in range(2)]
            r_fst = [Res() for _ in range(2)]
            kvo = [c.sb(es2, "kvo%d" % i, [128, 512], F32) for i in range(2)]
            r_kvo = [Res() for _ in range(2)]
            ctr = {"x": 0, "f": 0, "k": 0, "m": 0}

            groups = [("h", g) for g in range(NT // G)][:_LIM["groups"]] + [("o", g) for g in range(NT // G)][:_LIM["groups"]]

            def prep_group(gidx):
                kind, g = groups[gidx]
                src = xh if kind == "h" else xo
                hbuf = gidx % 2
                for j in range(G):
                    i = ctr["x"] % 2
                    ctr["x"] += 1
                    t0 = g * GT + j * 128
                    c.dma("sp", xt[i][:], src[t0:t0 + 128, :], writes=[r_xt[i]])
                    rms_h(xt[i], r_xt[i], 128, g_mix, r_gmix, hb[i], r_hb[i])
                    transpose_to(hb[i], r_hb[i], 128, 16,
                                 lambda c0, c1, j=j, hbuf=hbuf: hT[hbuf][:, c0:c1, j * 128:(j + 1) * 128], r_hT[hbuf])

            def feat_block(ws, hbuf, dst, r_dst_list, tcol0, sigmoid=False):
                wt, r_w = ws.get()
                i = ctr["f"] % 2
                ctr["f"] += 1
                for ct in range(4):
                    b = bank(2, 6)
                    mm_feat(hT[hbuf], r_hT[hbuf], 0, GT, wt, r_w, ct, b)
                    if sigmoid:
                        c.op("act", lambda e, b=b, ct=ct, i=i: e.activation(out=fst[i][:, ct, :], in_=pb[b][:, 0:GT],
                                                                          func=AF.Sigmoid),
                             reads=[rpb[b]], writes=[r_fst[i]])
                    elif ct % 2 == 0:
                        c.op("act", lambda e, b=b, ct=ct, i=i: e.activation(out=fst[i][:, ct, :], in_=pb[b][:, 0:GT],
                                                                          func=AF.Copy),
                             reads=[rpb[b]], writes=[r_fst[i]])
                    else:
                        c.op("dve", lambda e, b=b, ct=ct, i=i: e.tensor_copy(out=fst[i][:, ct, :], in_=pb[b][:, 0:GT]),
                             reads=[rpb[b]], writes=[r_fst[i]])
                c.dma("act", dst[:, :, tcol0:tcol0 + GT].rearrange("g p t -> p g t"), fst[i][:],
                      reads=[r_fst[i]], writes=r_dst_list)

            prep_group(0)
            for gidx, (kind, g) in enumerate(groups):
                hbuf = gidx % 2
                if kind == "h":
                    ws = WStream([("w_in", b) for b in (10, 11, 12, 13)])
                    for bi, blk in enumerate((10, 11, 12, 13)):
                        dst = kTs if blk < 12 else vTs
                        rr = r_kTs if blk < 12 else r_vTs
                        h0 = (blk % 2) * 4
                        feat_block(ws, hbuf, dst[h0:h0 + 4], rr[h0:h0 + 4], g * GT)
                        if bi == 0 and gidx + 1 < len(groups):
                            prep_group(gidx + 1)
                    continue
                order = [2, 3, 0, 1, 14, 15, 10, 11, 12, 13, 4, 5, 6, 7, 8, 9, 10, 11, 12, 13, 16, 17]
                ws = WStream([("w_in", b) for b in order])
                for bi, blk in enumerate(order[:10]):
                    wt, r_w = ws.get()
                    for j in range(G):
                        b = bank(2, 6)
                        mm_tok(hT[hbuf], r_hT[hbuf], j * 128, 128, wt, r_w, b)
                        if blk in (2, 3):
                            c.op("act", lambda e, b=b, j=j, blk=blk: e.activation(
                                out=gv[:, j, (blk - 2) * 512:(blk - 1) * 512], in_=pb[b][:, :], func=AF.Gelu_apprx_tanh),
                                reads=[rpb[b]], writes=[r_gv[j]])
                        elif blk in (0, 1):
                            c.op("act", lambda e, b=b, j=j, blk=blk: e.activation(
                                out=gu[:, j, blk * 512:(blk + 1) * 512], in_=pb[b][:, :], func=AF.Gelu_apprx_tanh),
                                reads=[rpb[b]], writes=[r_gu[j]])
                        elif blk in (14, 15):
                            c.op("act", lambda e, b=b, j=j, blk=blk: e.activation(
                                out=sga[:, j, (blk - 14) * 512:(blk - 13) * 512], in_=pb[b][:, :], func=AF.Sigmoid),
                                reads=[rpb[b]], writes=[r_sga[j]])
                        else:
                            i = ctr["k"] % 2
                            ctr["k"] += 1
                            c.op("dve", lambda e, b=b, i=i: e.tensor_copy(out=kvo[i][:], in_=pb[b][:, :]),
                                 reads=[rpb[b]], writes=[r_kvo[i]])
                            t0 = g * GT + j * 128
                            kv = 0 if blk < 12 else 1
                            half = blk % 2
                            c.dma("act", win_p[t0:t0 + 128, kv, half * 512:(half + 1) * 512], kvo[i][:],
                                  reads=[r_kvo[i]])
                    if bi == 0 and gidx + 1 < len(groups):
                        prep_group(gidx + 1)
                    if bi == 5:
                        for j in range(G):
                            gmlp_tile(es2, 128, gv[:, j, :], r_gv[j], gu[:, j, :], r_gu[j], sga[:, j, :], r_sga[j],
                                      WsT, r_ws, bsT, r_bs, lng, lnb, r_ln, gtmp, mA, r_mA)
                            i = ctr["m"] % 2
                            ctr["m"] += 1
                            transpose_to(mA, r_mA, 128, 8, lambda c0, c1, i=i: mst[i][:, c0:c1, :], r_mst[i],
                                         evac="dve")
                            t0 = g * GT + j * 128
                            c.dma("act", mTs[0:8, :, t0:t0 + 128].rearrange("g p t -> p g t"), mst[i][:],
                                  reads=[r_mst[i]], writes=r_mTs[0:8])
                for blk in order[10:]:
                    if 4 <= blk <= 9:
                        gh0 = ((blk - 4) // 2) * 8 + ((blk - 4) % 2) * 4
                        feat_block(ws, hbuf, qTs[gh0:gh0 + 4], r_qTs[gh0:gh0 + 4], g * GT)
                    elif blk in (10, 11):
                        h0 = (blk % 2) * 4
                        feat_block(ws, hbuf, kTs[h0:h0 + 4], r_kTs[h0:h0 + 4], TOK + g * GT)
                    elif blk in (12, 13):
                        h0 = (blk % 2) * 4
                        feat_block(ws, hbuf, vTs[h0:h0 + 4], r_vTs[h0:h0 + 4], TOK + g * GT)
                    else:
                        h0 = (blk % 2) * 4
                        feat_block(ws, hbuf, gbTs[h0:h0 + 4], r_gbTs[h0:h0 + 4], g * GT, sigmoid=True)
            c.barrier()

        with contextlib.suppress(_SkipPhase), contextlib.ExitStack() as es3:
            if 3 not in _PH:
                raise _SkipPhase()
            EBT = c.sb(es3, "EBT", [128, 24, 256], BF16)
            r_EB = Res()
            bm = c.sb(es3, "bm", [128, 256], F32)
            r_bm = Res()
            c.dma("sp", bm[:], bandmask.rearrange("p a b -> p (a b)"), writes=[r_bm])
            btmp = [c.sb(es3, "btmp%d" % i, [128, 256], F32) for i in range(2)]
            r_btmp = [Res() for _ in range(2)]
            for gh in range(24):
                i = gh % 2
                c.dma("sp", btmp[i][:], biasT[gh].rearrange("p a b -> p (a b)"), writes=[r_btmp[i]])
                c.op("act", lambda e, i=i: e.activation(out=btmp[i][:], in_=btmp[i][:], func=AF.Exp),
                     reads=[r_btmp[i]], writes=[r_btmp[i]])
                c.op("dve", lambda e, i=i, gh=gh: e.tensor_tensor(out=EBT[:, gh, :], in0=btmp[i][:], in1=bm[:],
                                                                  op=ALU.mult),
                     reads=[r_btmp[i], r_bm], writes=[r_EB])
            pf = c.sb(es3, "pf", [128, 1], F32)
            r_pf = Res()
            c.dma("sp", pf[:], pflag[:, :], writes=[r_pf])

            kT = [c.sb(es3, "kT%d" % i, [128, 2 * TOK], BF16) for i in range(2)]
            vT = [c.sb(es3, "vT%d" % i, [128, 2 * TOK], BF16) for i in range(2)]
            qT3 = [c.sb(es3, "qT3%d" % i, [128, 3, TOK], BF16) for i in range(2)]
            gbT = [c.sb(es3, "gbT%d" % i, [128, TOK], BF16) for i in range(2)]
            r_hd = [Res() for _ in range(2)]
            acc = c.sb(es3, "acc", [128, 2, TOK], F32)
            r_acc = Res()
            ybT = c.sb(es3, "ybT", [128, TOK], BF16)
            r_ybT = Res()
            NVP = 4
            Vp = [c.sb(es3, "Vp%d" % i, [128, 128], BF16) for i in range(NVP)]
            r_Vp = [Res() for _ in range(NVP)]
            pTf = [c.sb(es3, "pTf%d" % i, [128, 256], BF16) for i in range(2)]
            pT = [c.sb(es3, "pT%d" % i, [128, 256], BF16) for i in range(2)]
            r_pTf = [Res() for _ in range(2)]
            r_pT = [Res() for _ in range(2)]
            qTs4 = qTs.rearrange("(g h) p t -> g h p t", h=8)
            actr = {"vp": 0, "u": 0}

            def load_head(h):
                i = h % 2
                c.dma("sp", kT[i][:], kTs[h], reads=[r_kTs[h]], writes=[r_hd[i]])
                c.dma("sp", vT[i][:], vTs[h], reads=[r_vTs[h]], writes=[r_hd[i]])
                c.dma("sp", qT3[i][:], qTs4[:, h].rearrange("g p t -> p g t"),
                      reads=[r_qTs[h], r_qTs[8 + h], r_qTs[16 + h]], writes=[r_hd[i]])
                c.dma("sp", gbT[i][:], gbTs[h], reads=[r_gbTs[h]], writes=[r_hd[i]])

            def make_vp(i, dil, a0, r):
                k = actr["vp"] % NVP
                actr["vp"] += 1
                b = bank(2, 4)
                vv = vT[i][:].rearrange("p (a b) -> p a b", b=dil)
                c.op("pe", lambda e: e.transpose(out=pbb[b][:, 0:128], in_=vv[:, a0:a0 + 128, r], identity=ident[:]),
                     reads=[r_hd[i], r_const], writes=[rpb[b]])
                c.op("dve", lambda e: e.tensor_copy(out=Vp[k][:], in_=pbb[b][:, 0:128]), reads=[rpb[b]],
                     writes=[r_Vp[k]])
                return k

            load_head(0)
            NH = 8 if _LIM["heads"] is None else _LIM["heads"]
            for h in range(NH):
                i = h % 2
                if h + 1 < NH:
                    load_head(h + 1)
                for gi, dil in enumerate(DILS):
                    span = 128 * dil
                    nbk = TOK // span
                    kv_ = kT[i][:].rearrange("p (a b) -> p a b", b=dil)
                    qv_ = qT3[i][:, gi, :].rearrange("p (a b) -> p a b", b=dil)
                    accv = acc[:].rearrange("p s (a b) -> p s a b", b=dil)
                    for r in range(dil):
                        kprev = make_vp(i, dil, (TOK - span) // dil, r)
                        for bb in range(nbk):
                            a_own = (TOK + bb * span) // dil
                            kcur = make_vp(i, dil, a_own, r)
                            u = actr["u"] % 2
                            actr["u"] += 1
                            b_s = bank(0, 2)
                            a_prev = a_own - 128

                            def fs(e, a_prev=a_prev, bb=bb, r=r, b_s=b_s, kv_=kv_, qv_=qv_):
                                inst = None
                                for jt in range(2):
                                    inst = e.matmul(pb[b_s][:, jt * 128:(jt + 1) * 128],
                                                    lhsT=kv_[:, a_prev + jt * 128:a_prev + (jt + 1) * 128, r],
                                                    rhs=qv_[:, bb * 128:(bb + 1) * 128, r], start=True, stop=True)
                                return inst
                            c.op("pe", fs, reads=[r_hd[i]], writes=[rpb[b_s]])
                            c.op("act", lambda e, u=u, b_s=b_s: e.activation(out=pTf[u][:], in_=pb[b_s][:, 0:256],
                                                                            func=AF.Exp, scale=ATT_SCALE),
                                 reads=[rpb[b_s]], writes=[r_pTf[u]])
                            c.op("pool", lambda e, u=u, gi=gi, h=h: e.tensor_tensor(
                                out=pT[u][:], in0=pTf[u][:], in1=EBT[:, gi * 8 + h, :], op=ALU.mult),
                                reads=[r_pTf[u], r_EB], writes=[r_pT[u]])
                            if bb == 0:
                                c.op("pool", lambda e, u=u: e.tensor_scalar(
                                    out=pT[u][:, 0:128], in0=pT[u][:, 0:128], scalar1=pf[:, 0:1], scalar2=None,
                                    op0=ALU.mult), reads=[r_pT[u], r_pf], writes=[r_pT[u]])
                            b_o = bank(4, 6)

                            def fo(e, u=u, b_o=b_o, kprev=kprev, kcur=kcur):
                                e.matmul(pb[b_o][:, 0:128], lhsT=Vp[kprev][:], rhs=pT[u][:, 0:128], start=True, stop=False)
                                e.matmul(pb[b_o][:, 0:128], lhsT=Vp[kcur][:], rhs=pT[u][:, 128:256], start=False, stop=True)
                                e.matmul(pb[b_o][:, 128:256], lhsT=ones[:], rhs=pT[u][:, 0:128], start=True, stop=False)
                                return e.matmul(pb[b_o][:, 128:256], lhsT=ones[:], rhs=pT[u][:, 128:256],
                                                start=False, stop=True)
                            c.op("pe", fo, reads=[r_pT[u], r_Vp[kprev], r_Vp[kcur], r_const], writes=[rpb[b_o]])
                            av = accv[:, :, bb * 128:(bb + 1) * 128, r]
                            pv = pb[b_o][:, 0:256].rearrange("p (s t) -> p s t", s=2)
                            if gi == 0:
                                c.op("dve", lambda e, av=av, pv=pv: e.tensor_copy(out=av, in_=pv),
                                     reads=[rpb[b_o]], writes=[r_acc])
                            else:
                                c.op("dve", lambda e, av=av, pv=pv: e.tensor_tensor(out=av, in0=pv, in1=av, op=ALU.add),
                                     reads=[rpb[b_o], r_acc], writes=[r_acc])
                            kprev = kcur
                c.op("dve", lambda e: e.reciprocal(out=acc[:, 1, :], in_=acc[:, 1, :]), reads=[r_acc], writes=[r_acc])
                c.op("dve", lambda e: e.tensor_tensor(out=acc[:, 0, :], in0=acc[:, 0, :], in1=acc[:, 1, :], op=ALU.mult),
                     reads=[r_acc], writes=[r_acc])
                c.op("pool", lambda e, i=i: e.tensor_tensor(out=ybT[:], in0=acc[:, 0, :], in1=gbT[i][:], op=ALU.mult),
                     reads=[r_acc, r_hd[i]], writes=[r_ybT])
                c.dma("sp", mTs[8 + h], ybT[:], reads=[r_ybT], writes=[r_mTs[8 + h]])
            c.barrier()


        PS = 128
        mTg_s = c.sb(es, "mTg_s", [128, 16, PS], BF16)
        r_mTgs = Res()
        c.op("pool", lambda e: e.memset(mTg_s[:], 0.0), writes=[r_mTgs])
        with contextlib.suppress(_SkipPhase), contextlib.ExitStack() as ess:
            if 4 not in _PH:
                raise _SkipPhase()
            make_wpool(ess, 2)
            cut(10)
            g_mix, r_gmix = load_gain(ess, "mix_s", gains["norm_mix"])
            lng, r_ln = load_gain(ess, "lng_s", sgu_ln_g, 1024)
            lnb = c.sb(ess, "g_lnb_s", [128, 1024], F32)
            c.dma("sp", lnb[:], sgu_ln_b[0:1, :].partition_broadcast(128), writes=[r_ln])
            wsf = c.sb(ess, "wsf_s", [128, 8, 128], F32)
            WsTs = c.sb(ess, "WsTs", [128, 8, 128], BF16)
            r_ws = Res()
            c.dma("sp", wsf[:], sgu_wTs.rearrange("g j i -> j g i"), writes=[r_ws])
            c.op("pool", lambda e: e.affine_select(out=wsf[:], in_=wsf[:], pattern=[[0, 8], [1, 128]],
                                                   compare_op=ALU.is_ge, fill=0.0, base=0, channel_multiplier=-1),
                 reads=[r_ws], writes=[r_ws])
            c.op("dve", lambda e: e.tensor_copy(out=WsTs[:], in_=wsf[:]), reads=[r_ws], writes=[r_ws])
            bsTs = c.sb(ess, "bsTs", [128, 8], F32)
            r_bs = Res()
            c.dma("sp", bsTs[:], sgu_bTs[:, :], writes=[r_bs])
            cut(11)

            xts = c.sb(ess, "xts", [128, D], F32)
            hbs = c.sb(ess, "hbs", [128, D], BF16)
            hTs = c.sb(ess, "hTs", [128, 16, PS], BF16)
            r_xts, r_hbs, r_hTs = Res(), Res(), Res()
            c.op("pool", lambda e: e.memset(xts[:], 0.0), writes=[r_xts])
            c.dma("sp", xts[:SP, :], xs[:, :], writes=[r_xts])
            cut(12)
            rms_h(xts, r_xts, PS, g_mix, r_gmix, hbs, r_hbs)
            cut(13)
            transpose_to(hbs, r_hbs, PS, 16, lambda c0, c1: hTs[:, c0:c1, :], r_hTs)
            cut(1)

            gv_s = c.sb(ess, "gv_s", [128, 1024], F32)
            gu_s = c.sb(ess, "gu_s", [128, 1024], BF16)
            sga_s = c.sb(ess, "sga_s", [128, 1024], BF16)
            sgb_s = c.sb(ess, "sgb_s", [128, 1024], BF16)
            q_s = c.sb(ess, "q_s", [128, 3072], BF16)
            kf_s = c.sb(ess, "kf_s", [128, 1024], F32)
            vf_s = c.sb(ess, "vf_s", [128, 1024], F32)
            k_sb = c.sb(ess, "k_sb", [128, 1024], BF16)
            vn = c.sb(ess, "vn", [128, 1024], BF16)
            r_gvs, r_gus, r_sgas, r_sgbs, r_qs, r_kfs, r_vfs, r_ksb, r_vn = (Res() for _ in range(9))
            ws = WStream([("w_in", b) for b in range(18)])
            for blk in range(18):
                cut(20 + blk)
                wt, r_w = ws.get()
                b = bank(2, 6)
                mm_tok(hTs, r_hTs, 0, PS, wt, r_w, b)
                src = pb[b][:, :]
                if blk < 2:
                    c.op("act", lambda e, blk=blk, src=src: e.activation(out=gu_s[:, blk * 512:(blk + 1) * 512], in_=src,
                                                                        func=AF.Gelu_apprx_tanh), reads=[rpb[b]], writes=[r_gus])
                elif blk < 4:
                    c.op("act", lambda e, blk=blk, src=src: e.activation(out=gv_s[:, (blk - 2) * 512:(blk - 1) * 512], in_=src,
                                                                        func=AF.Gelu_apprx_tanh), reads=[rpb[b]], writes=[r_gvs])
                elif blk < 10:
                    c.op("act", lambda e, blk=blk, src=src: e.activation(out=q_s[:, (blk - 4) * 512:(blk - 3) * 512], in_=src,
                                                                        func=AF.Copy), reads=[rpb[b]], writes=[r_qs])
                elif blk < 12:
                    c.op("act", lambda e, blk=blk, src=src: e.activation(out=kf_s[:, (blk - 10) * 512:(blk - 9) * 512], in_=src,
                                                                        func=AF.Copy), reads=[rpb[b]], writes=[r_kfs])
                    c.op("dve", lambda e, blk=blk: e.tensor_copy(out=k_sb[:, (blk - 10) * 512:(blk - 9) * 512],
                                                                 in_=kf_s[:, (blk - 10) * 512:(blk - 9) * 512]),
                         reads=[r_kfs], writes=[r_ksb])
                elif blk < 14:
                    c.op("act", lambda e, blk=blk, src=src: e.activation(out=vf_s[:, (blk - 12) * 512:(blk - 11) * 512], in_=src,
                                                                        func=AF.Copy), reads=[rpb[b]], writes=[r_vfs])
                    c.op("dve", lambda e, blk=blk: e.tensor_copy(out=vn[:, (blk - 12) * 512:(blk - 11) * 512],
                                                                 in_=vf_s[:, (blk - 12) * 512:(blk - 11) * 512]),
                         reads=[r_vfs], writes=[r_vn])
                elif blk < 16:
                    c.op("act", lambda e, blk=blk, src=src: e.activation(out=sga_s[:, (blk - 14) * 512:(blk - 13) * 512], in_=src,
                                                                        func=AF.Sigmoid), reads=[rpb[b]], writes=[r_sgas])
                else:
                    c.op("act", lambda e, blk=blk, src=src: e.activation(out=sgb_s[:, (blk - 16) * 512:(blk - 15) * 512], in_=src,
                                                                        func=AF.Sigmoid), reads=[rpb[b]], writes=[r_sgbs])
            cut(40)
            c.dma("sp", win_s[:, 0, :], kf_s[:SP, :], reads=[r_kfs])
            c.dma("sp", win_s[:, 1, :], vf_s[:SP, :], reads=[r_vfs])
            cut(2)
            gtmp_s = gmlp_tmp(ess, "s")
            vlnf_s = c.sb(ess, "vlnf_s", [128, 1024], F32)
            r_vlnf = Res()
            mA_s = c.sb(ess, "mA_s", [128, 1024], BF16)
            r_mAs = Res()
            gmlp_tile(ess, PS, gv_s, r_gvs, gu_s, r_gus, sga_s, r_sgas, WsTs, r_ws, bsTs, r_bs, lng, lnb, r_ln,
                      gtmp_s, mA_s, r_mAs, vln_out=vlnf_s, r_vln=r_vlnf)
            c.dma("sp", sgu_s[:, :], vlnf_s[:SP, :], reads=[r_vlnf])
            transpose_to(mA_s, r_mAs, PS, 8, lambda c0, c1: mTg_s[:, c0:c1, :], r_mTgs)
            cut(3)

            qTs_s = c.sb(ess, "qTs_s", [128, 24, PS], BF16)
            kTn = c.sb(ess, "kTn", [128, 8, PS], BF16)
            gbT_s = c.sb(ess, "gbT_s", [128, 8, PS], BF16)
            r_qTss, r_kTn, r_gbTs_ = Res(), Res(), Res()
            transpose_to(q_s, r_qs, PS, 24, lambda c0, c1: qTs_s[:, c0:c1, :], r_qTss)
            transpose_to(k_sb, r_ksb, PS, 8, lambda c0, c1: kTn[:, c0:c1, :], r_kTn)
            transpose_to(sgb_s, r_sgbs, PS, 8, lambda c0, c1: gbT_s[:, c0:c1, :], r_gbTs_)
            EBs = c.sb(ess, "EBs", [128, 8, 384], BF16)
            EBn = c.sb(ess, "EBn", [128, 8, 96], BF16)
            r_EBs = Res()
            c.op("pool", lambda e: e.memset(EBn[:], 0.0), writes=[r_EBs])
            smk = c.sb(ess, "smk", [128, 384], F32)
            smkn = c.sb(ess, "smkn", [128, 96], F32)
            r_smk = Res()
            c.dma("sp", smk[:], smask.rearrange("p a b -> p (a b)"), writes=[r_smk])
            c.dma("sp", smkn[:SP, :], smask_n.rearrange("p a b -> p (a b)"), writes=[r_smk])
            stmp = [c.sb(ess, "stmp%d" % i, [128, 480], F32) for i in range(2)]
            r_stmp = [Res() for _ in range(2)]
            for h in range(8):
                i = h % 2
                c.dma("sp", stmp[i][:, 0:384], sbias[h].rearrange("p a b -> p (a b)"), writes=[r_stmp[i]])
                c.dma("sp", stmp[i][:SP, 384:480], sbias_n[h].rearrange("p a b -> p (a b)"), writes=[r_stmp[i]])
                c.op("act", lambda e, i=i: e.activation(out=stmp[i][:, 0:384], in_=stmp[i][:, 0:384], func=AF.Exp),
                     reads=[r_stmp[i]], writes=[r_stmp[i]])
                c.op("act", lambda e, i=i: e.activation(out=stmp[i][:SP, 384:480], in_=stmp[i][:SP, 384:480], func=AF.Exp),
                     reads=[r_stmp[i]], writes=[r_stmp[i]])
                c.op("dve", lambda e, i=i, h=h: e.tensor_tensor(out=EBs[:, h, :], in0=stmp[i][:, 0:384], in1=smk[:],
                                                                op=ALU.mult), reads=[r_stmp[i], r_smk], writes=[r_EBs])
                c.op("dve", lambda e, i=i, h=h: e.tensor_tensor(out=EBn[:SP, h, :], in0=stmp[i][:SP, 384:480],
                                                                in1=smkn[:SP, :], op=ALU.mult),
                     reads=[r_stmp[i], r_smk], writes=[r_EBs])
            cut(4)
            Ppad = c.sb(ess, "Ppad", [128, 4, 17 * 96], BF16)
            r_Pp = Res()
            c.op("pool", lambda e: e.memset(Ppad[:], 0.0), writes=[r_Pp])
            Kc = c.sb(ess, "Kc0", [128, 16, 128], F32)
            Vc = c.sb(ess, "Vc0", [128, 16, 128], F32)
            Kcb = [c.sb(ess, "Kcb0", [128, 16, 128], BF16)] * 2
            Vcb = [c.sb(ess, "Vcb%d" % i, [128, 16, 128], BF16) for i in range(2)]
            KT = [c.sb(ess, "KT0", [128, 16, 128], BF16)] * 2
            r_Kc, r_Vc = Res(), Res()
            r_Kcb = [Res()] * 2
            r_Vcb = [Res() for _ in range(2)]
            r_KT = [Res()] * 2
            tmpE = c.sb(ess, "tmpE", [128, 408], BF16)
            r_tmpE = Res()
            qc = [c.sb(ess, "qc%d" % i, [128, 24], BF16) for i in range(2)]
            r_qc = [Res() for _ in range(2)]
            rec_s = c.sb(ess, "rec_s", [128, SP], F32)
            ot_s = c.sb(ess, "ot_s", [128, SP], F32)
            r_recs = Res()
            qv4 = qTs_s[:].rearrange("p (g h) t -> p g h t", h=8)
            tiles_g = ([15], [12, 13, 14, 15], list(range(16)))
            n = 0
            for h in range(8):
                b_o = 4 + h % 2
                b_sum = 6 + h % 2
                for b in range(4):
                    i = n % 2
                    n += 1
                    c.dma("sp", Kc[:], cwin[b, :, 0, h, :].rearrange("(rt p) d -> p rt d", p=128), writes=[r_Kc])
                    c.dma("sp", Vc[:], cwin[b, :, 1, h, :].rearrange("(rt p) d -> p rt d", p=128), writes=[r_Vc])
                    c.op("pool", lambda e, i=i: e.tensor_copy(out=Kcb[i][:], in_=Kc[:]), reads=[r_Kc], writes=[r_Kcb[i]])
                    c.op("act", lambda e, i=i: e.activation(out=Vcb[i][:], in_=Vc[:], func=AF.Copy),
                         reads=[r_Vc], writes=[r_Vcb[i]])
                    transpose_to(Kcb[i][:].rearrange("p a d -> p (a d)"), r_Kcb[i], 128, 16,
                                 lambda c0, c1, i=i: KT[i][:, c0:c1, :], r_KT[i], evac="dve")
                    c.op("dve", lambda e, i=i, h=h, b=b: e.tensor_copy(
                        out=qc[i][:].rearrange("p (g t) -> p g t", t=8), in_=qv4[:, :, h, b * 8:(b + 1) * 8]),
                        reads=[r_qTss], writes=[r_qc[i]])
                    b_s = bank(2, 4)

                    def fs(e, i=i, b_s=b_s, h=h):
                        for rt in range(16):
                            e.matmul(pb[b_s][:, rt * 24:(rt + 1) * 24], lhsT=KT[i][:, rt, :], rhs=qc[i][:],
                                     start=True, stop=True)
                        return e.matmul(pb[b_s][:, 384:408], lhsT=kTn[:, h, :], rhs=qc[i][:], start=True, stop=True)
                    c.op("pe", fs, reads=[r_KT[i], r_qc[i], r_kTn], writes=[rpb[b_s]])
                    c.op("act", lambda e, b_s=b_s: e.activation(out=tmpE[:], in_=pb[b_s][:, 0:408], func=AF.Exp,
                                                                scale=ATT_SCALE), reads=[rpb[b_s]], writes=[r_tmpE])
                    Pv = Ppad[:, b, :].rearrange("p (rt g m) -> p rt g m", g=3, m=32)
                    c.op("dve", lambda e, Pv=Pv, b=b, h=h: e.tensor_tensor(
                        out=Pv[:, 0:16, :, b * 8:(b + 1) * 8],
                        in0=tmpE[:, 0:384].rearrange("p (rt g t) -> p rt g t", g=3, t=8),
                        in1=EBs[:, h, :].rearrange("p (rt g t) -> p rt g t", g=3, t=8), op=ALU.mult),
                        reads=[r_tmpE, r_EBs], writes=[r_Pp])
                    c.op("dve", lambda e, Pv=Pv, b=b, h=h: e.tensor_tensor(
                        out=Pv[:, 16, :, b * 8:(b + 1) * 8],
                        in0=tmpE[:, 384:408].rearrange("p (g t) -> p g t", t=8),
                        in1=EBn[:, h, b * 24:(b + 1) * 24].rearrange("p (g t) -> p g t", t=8), op=ALU.mult),
                        reads=[r_tmpE, r_EBs], writes=[r_Pp])

                    def fo(e, i=i, b=b, h=h, b_o=b_o, b_sum=b_sum, Pv=Pv):
                        mms = []
                        for g in range(3):
                            for rt in tiles_g[g]:
                                mms.append((Vcb[i][:, rt, :], Pv[:, rt, g, :]))
                            mms.append((vn[:, h * 128:(h + 1) * 128], Pv[:, 16, g, :]))
                        inst = None
                        for k, (l, r) in enumerate(mms):
                            first = (b == 0 and k == 0)
                            last = (b == 3 and k == len(mms) - 1)
                            e.matmul(pb[b_o][:, 0:SP], lhsT=l, rhs=r, start=first, stop=last)
                            inst = e.matmul(pb[b_sum][:, 0:SP], lhsT=ones[:], rhs=r, start=first, stop=last)
                        return inst
                    c.op("pe", fo, reads=[r_Pp, r_Vcb[i], r_vn, r_const], writes=[rpb[b_o], rpb[b_sum]])
                c.op("dve", lambda e, b_sum=b_sum: e.reciprocal(out=rec_s[:], in_=pb[b_sum][:, 0:SP]),
                     reads=[rpb[b_sum]], writes=[r_recs])
                c.op("dve", lambda e, b_o=b_o: e.tensor_tensor(out=ot_s[:], in0=pb[b_o][:, 0:SP], in1=rec_s[:],
                                                               op=ALU.mult), reads=[rpb[b_o], r_recs], writes=[r_recs])
                c.op("dve", lambda e, h=h: e.tensor_tensor(out=mTg_s[:, 8 + h, 0:SP], in0=ot_s[:], in1=gbT_s[:, h, 0:SP],
                                                           op=ALU.mult), reads=[r_recs, r_gbTs_], writes=[r_mTgs])
            c.barrier()

        es4 = es.enter_context(contextlib.ExitStack())
        make_wpool(es4, 2)
        g_mem, r_gmem = load_gain(es4, "mem", gains["norm_mem"])
        g_peer, r_gpeer = load_gain(es4, "peer", gains["norm_peer"])
        g_fin, r_gfin = load_gain(es4, "fin", gains["norm_final"])
        keysT = c.sb(es4, "keysT", [128, 2, 8, 128], BF16)
        r_keysT = Res()
        with contextlib.ExitStack() as esk:
            kf = c.sb(esk, "kf", [128, 2, 8, 128], F32)
            kb = c.sb(esk, "kb", [128, 2, 8, 128], BF16)
            r_kf = Res()
            c.dma("sp", kf[:, 0, :, :], keys1.rearrange("h k d -> k h d"), writes=[r_kf])
            c.dma("sp", kf[:, 1, :, :], keys2.rearrange("h k d -> k h d"), writes=[r_kf])
            c.op("dve", lambda e: e.tensor_copy(out=kb[:], in_=kf[:]), reads=[r_kf], writes=[r_kf])
            transpose_to(kb[:].rearrange("p s h d -> p (s h d)"), r_kf, 128, 16,
                         lambda c0, c1: keysT[:].rearrange("p s h k -> p (s h) k")[:, c0:c1, :], r_keysT)
            c.barrier()
        iota_i = c.sb(es4, "iota_i", [128, 16], I32)
        iota16 = c.sb(es4, "iota16", [128, 16], F32)
        r_iota = Res()
        c.op("pool", lambda e: e.iota(iota_i[:], pattern=[[1, 16]], base=0, channel_multiplier=0), writes=[r_iota])
        c.op("dve", lambda e: e.tensor_copy(out=iota16[:], in_=iota_i[:]), reads=[r_iota], writes=[r_iota])

        x1 = [c.sb(es4, "x1_%d" % i, [128, D], F32) for i in range(G)]
        r_x1 = [Res() for _ in range(G)]
        h2 = [c.sb(es4, "h2_%d" % i, [128, D], BF16) for i in range(G)]
        r_h2 = [Res() for _ in range(G)]
        hb4 = c.sb(es4, "hb4", [128, D], BF16)
        r_hb4 = Res()
        bufA = c.sb(es4, "bufA", [128, 16, GT], BF16)
        bufB = c.sb(es4, "bufB", [128, 16, GT], BF16)
        r_bufA = Res()
        r_bufB = Res()
        pTm = c.sb(es4, "pTm", [128, 2, GT], BF16)
        r_pTm = Res()
        rsm = c.sb(es4, "rsm", [128, GT], F32)
        r_rsm = Res()
        sc = c.sb(es4, "sc", [128, 2048], F32)
        cand = c.sb(es4, "cand", [128, 2048], F32)
        r_sc = Res()
        r_cand = Res()
        wk = c.sb(es4, "wk", [128, 256], F32)
        r_wk = Res()
        tv = c.sb(es4, "tv", [128, 16, 16], F32)
        ti = c.sb(es4, "ti", [128, 16, 16], U32)
        tif = c.sb(es4, "tif", [128, 16, 16], F32)
        best = c.sb(es4, "best", [128, 8, 16], F32)
        pos = c.sb(es4, "pos", [128, 8, 16], U32)
        pa = c.sb(es4, "pa", [128, 128], U32)
        pbi = c.sb(es4, "pbi", [128, 128], U32)
        paf = c.sb(es4, "paf", [128, 128], F32)
        pbf = c.sb(es4, "pbf", [128, 128], F32)
        i1s = c.sb(es4, "i1s", [128, 128], F32)
        i2s = c.sb(es4, "i2s", [128, 128], F32)
        eidf = c.sb(es4, "eidf", [128, 128], F32)
        gate = c.sb(es4, "gate", [128, 8, 16], F32)
        gsm = c.sb(es4, "gsm", [128, 24], F32)
        r_pk = Res()
        idxT = c.sb(es4, "idxT", [128, 128], I32)
        GTt = c.sb(es4, "GTt", [128, 128], F32)
        apart = c.sb(es4, "apart", [128, 128, 4], F32)
        AT = c.sb(es4, "AT", [128, 128], F32)
        CT = c.sb(es4, "CT", [128, 128], BF16)
        r_idxT = Res()
        r_GTt = Res()
        r_apart = Res()
        r_CT = Res()
        NGB = 6
        gbuf = [c.sb(es4, "gbuf%d" % i, [128, D], BF16) for i in range(NGB)]
        r_gbuf = [Res() for _ in range(NGB)]
        NCB = 4
        cbuf = [c.sb(es4, "cbuf%d" % i, [128, 256], BF16) for i in range(NCB)]
        r_cbuf = [Res() for _ in range(NCB)]
        for i in range(NCB):
            c.op("pool", lambda e, i=i: e.memset(cbuf[i][:], 0.0), writes=[r_cbuf[i]])
        NJ = 4
        junk4 = [c.sb(es4, "junk4_%d" % i, [128, 512], BF16) for i in range(NJ)]
        r_junk4 = [Res() for _ in range(NJ)]
        yout = cand
        r_yout = r_cand
        pctr = {"g": 0, "c": 0, "j": 0}

        def cross_attn(qT_, r_q, col0, n, mkT_, r_mk, mv_fn, r_mv, oT_, r_o):
            for h in range(4):
                for mt in range(2):
                    b = bank(2, 6)

                    def f(e, h=h, mt=mt, b=b):
                        inst = None
                        for dc in range(4):
                            inst = e.matmul(pb[b][:, 0:n], lhsT=mkT_[:, h * 4 + dc, mt * 128:(mt + 1) * 128],
                                            rhs=qT_[:, h * 4 + dc, col0:col0 + n], start=(dc == 0), stop=(dc == 3))
                        return inst
                    c.op("pe", f, reads=[r_q] + r_mk, writes=[rpb[b]])
                    c.op("act", lambda e, mt=mt, b=b: e.activation(out=pTm[:, mt, 0:n], in_=pb[b][:, 0:n],
                                                                  func=AF.Exp, scale=MEM_SCALE),
                         reads=[rpb[b]], writes=[r_pTm])
                b = bank(2, 6)

                def fs(e, b=b):
                    e.matmul(pb[b][:, 0:n], lhsT=ones[:], rhs=pTm[:, 0, 0:n], start=True, stop=False)
                    return e.matmul(pb[b][:, 0:n], lhsT=ones[:], rhs=pTm[:, 1, 0:n], start=False, stop=True)
                c.op("pe", fs, reads=[r_pTm, r_const], writes=[rpb[b]])
                c.op("dve", lambda e, b=b: e.reciprocal(out=rsm[:, 0:n], in_=pb[b][:, 0:n]), reads=[rpb[b]],
                     writes=[r_rsm])
                for dc in range(4):
                    b = bank(2, 6)

                    def fo(e, h=h, dc=dc, b=b):
                        c0 = h * 512 + dc * 128
                        e.matmul(pb[b][:, 0:n], lhsT=mv_fn(0, c0), rhs=pTm[:, 0, 0:n], start=True, stop=False)
                        return e.matmul(pb[b][:, 0:n], lhsT=mv_fn(1, c0), rhs=pTm[:, 1, 0:n],
                                        start=False, stop=True)
                    c.op("pe", fo, reads=[r_pTm] + r_mv, writes=[rpb[b]])
                    c.op("dve", lambda e, h=h, dc=dc, b=b: e.tensor_tensor(
                        out=oT_[:, h * 4 + dc, col0:col0 + n], in0=pb[b][:, 0:n], in1=rsm[:, 0:n], op=ALU.mult),
                        reads=[rpb[b], r_rsm], writes=[r_o])

        def top16(P, src_ap, n, dst_v, dst_i, rd, wr):
            c.op("dve", lambda e: e.max(out=dst_v[:, 0:8], in_=src_ap), reads=rd, writes=wr)
            c.op("dve", lambda e: e.max_index(out=dst_i[:, 0:8], in_max=dst_v[:, 0:8], in_values=src_ap),
                 reads=rd + wr, writes=wr)
            c.op("dve", lambda e: e.match_replace(out=wk[:P, 0:n], in_to_replace=dst_v[:, 0:8], in_values=src_ap,
                                                  imm_value=NEG), reads=rd + wr, writes=[r_wk])
            c.op("dve", lambda e: e.max(out=dst_v[:, 8:16], in_=wk[:P, 0:n]), reads=[r_wk], writes=wr)
            c.op("dve", lambda e: e.max_index(out=dst_i[:, 8:16], in_max=dst_v[:, 8:16], in_values=wk[:P, 0:n]),
                 reads=[r_wk] + wr, writes=wr)

        def peer_tile(P, nv, qpT, r_qp, col0, h2_t, r_h2t, x2_t, r_x2t, y_dst):
            def fsc(e):
                inst = None
                for hs in range(16):
                    inst = e.matmul(pb[4 + hs // 4][:P, (hs % 4) * 128:(hs % 4 + 1) * 128],
                                    lhsT=qpT[:, hs, col0:col0 + P], rhs=keysT[:, hs % 2, hs // 2, :],
                                    start=True, stop=True)
                return inst
            c.op("pe", fsc, reads=[r_qp, r_keysT], writes=[rpb[4], rpb[5], rpb[6], rpb[7]])
            for q in range(4):
                c.op("act", lambda e, q=q: e.activation(out=sc[:P, q * 512:(q + 1) * 512], in_=pb[4 + q][:P, :],
                                                        func=AF.Copy), reads=[rpb[4 + q]], writes=[r_sc])
            scv = sc[:P, :].rearrange("p (a k) -> p a k", k=128)
            for hs in range(16):
                top16(P, scv[:, hs, :], 128, tv[:P, hs, :], ti[:P, hs, :], [r_sc], [r_pk])
            tv4 = tv[:P].rearrange("p (h s) k -> p h s k", s=2)
            candv = cand[:P, :].rearrange("p (h a b) -> p h a b", a=16, b=16)
            c.op("dve", lambda e: e.tensor_tensor(out=candv, in0=tv4[:, :, 0, :].unsqueeze(3).to_broadcast([P, 8, 16, 16]),
                                                  in1=tv4[:, :, 1, :].unsqueeze(2).to_broadcast([P, 8, 16, 16]),
                                                  op=ALU.add), reads=[r_pk], writes=[r_cand])
            cand3 = cand[:P, :].rearrange("p (h c) -> p h c", c=256)
            for h in range(8):
                top16(P, cand3[:, h, :], 256, best[:P, h, :], pos[:P, h, :], [r_cand], [r_pk])
            c.op("dve", lambda e: e.tensor_scalar(out=gsm[:P, 0:8], in0=best[:P, :, 0], scalar1=-1.0, scalar2=None,
                                                  op0=ALU.mult), reads=[r_pk], writes=[r_pk])
            for h in range(8):
                c.op("act", lambda e, h=h: e.activation(out=gate[:P, h, :], in_=best[:P, h, :], func=AF.Exp,
                                                        bias=gsm[:P, h:h + 1], scale=1.0,
                                                        accum_out=gsm[:P, 8 + h:9 + h]), reads=[r_pk], writes=[r_pk])
            c.op("dve", lambda e: e.reciprocal(out=gsm[:P, 16:24], in_=gsm[:P, 8:16]), reads=[r_pk], writes=[r_pk])
            c.op("dve", lambda e: e.tensor_tensor(out=gate[:P], in0=gate[:P],
                                                  in1=gsm[:P, 16:24].unsqueeze(2).to_broadcast([P, 8, 16]),
                                                  op=ALU.mult), reads=[r_pk], writes=[r_pk])
            posf = pos[:P].rearrange("p h k -> p (h k)")
            c.op("dve", lambda e: e.tensor_copy(out=pbf[:P, :], in_=posf), reads=[r_pk], writes=[r_pk])
            c.op("dve", lambda e: e.tensor_scalar(out=paf[:P, :], in0=pbf[:P, :], scalar1=16.0, scalar2=None,
                                                  op0=ALU.is_ge), reads=[r_pk], writes=[r_pk])
            for m in range(2, 16):
                c.op("dve", lambda e, m=m: e.scalar_tensor_tensor(out=paf[:P, :], in0=pbf[:P, :], scalar=16.0 * m,
                                                                  in1=paf[:P, :], op0=ALU.is_ge, op1=ALU.add),
                     reads=[r_pk], writes=[r_pk])
            c.op("dve", lambda e: e.scalar_tensor_tensor(out=pbf[:P, :], in0=paf[:P, :], scalar=-16.0, in1=pbf[:P, :],
                                                         op0=ALU.mult, op1=ALU.add), reads=[r_pk], writes=[r_pk])
            c.op("dve", lambda e: e.tensor_copy(out=tif[:P], in_=ti[:P]), reads=[r_pk], writes=[r_pk])
            tif4 = tif[:P].rearrange("p (h s) k -> p h s k", s=2)
            ohv = sc[:P, :].rearrange("p (h a b) -> p h a b", a=16, b=16)
            io4 = iota16[:P, :].unsqueeze(1).unsqueeze(1).to_broadcast([P, 8, 16, 16])
            for side, (pf_, dsts) in enumerate(((paf, i1s), (pbf, i2s))):
                pv = pf_[:P, :].rearrange("p (h k) -> p h k", k=16).unsqueeze(3).to_broadcast([P, 8, 16, 16])
                c.op("dve", lambda e, pv=pv: e.tensor_tensor(out=ohv, in0=pv, in1=io4, op=ALU.is_equal),
                     reads=[r_pk, r_iota], writes=[r_sc])
                c.op("dve", lambda e, side=side: e.tensor_tensor(
                    out=ohv, in0=ohv, in1=tif4[:, :, side, :].unsqueeze(2).to_broadcast([P, 8, 16, 16]), op=ALU.mult),
                    reads=[r_sc, r_pk], writes=[r_sc])
                c.op("dve", lambda e, dsts=dsts: e.tensor_reduce(
                    out=dsts[:P, :].rearrange("p (h k) -> p h k", k=16), in_=ohv, axis=mybir.AxisListType.X,
                    op=ALU.add), reads=[r_sc], writes=[r_pk])
            c.op("dve", lambda e: e.scalar_tensor_tensor(out=eidf[:P, :], in0=i1s[:P, :], scalar=128.0, in1=i2s[:P, :],
                                                         op0=ALU.mult, op1=ALU.add), reads=[r_pk], writes=[r_pk])
            c.op("pe", lambda e: e.transpose(out=pb[0][:, 0:P], in_=eidf[:P, :], identity=identf[:P, :P]),
                 reads=[r_pk, r_const], writes=[rpb[0]])
            c.op("dve", lambda e: e.tensor_copy(out=idxT[:, 0:P], in_=pb[0][:, 0:P]), reads=[rpb[0]], writes=[r_idxT])
            c.op("pe", lambda e: e.transpose(out=pb[1][:, 0:P], in_=gate[:P].rearrange("p h k -> p (h k)"),
                                             identity=identf[:P, :P]), reads=[r_pk, r_const], writes=[rpb[1]])
            c.op("act", lambda e: e.activation(out=GTt[:, 0:P], in_=pb[1][:, 0:P], func=AF.Copy), reads=[rpb[1]],
                 writes=[r_GTt])
            for t in range(nv):
                k = pctr["g"] % NGB
                pctr["g"] += 1
                c.swdma(gbuf[k][:], tabu[:, :], reads=[r_idxT, r_tab], writes=[r_gbuf[k]],
                        indirect=bass.IndirectOffsetOnAxis(ap=idxT[:, t:t + 1], axis=0))
                s0 = (t % 2) * 4

                def fx(e, t=t, s0=s0):
                    inst = None
                    for q in range(4):
                        inst = e.matmul(pb[s0 + q][:, :], lhsT=ident[:P, t:t + 1].to_broadcast([P, 128]),
                                        rhs=h2_t[:P, q * 512:(q + 1) * 512], start=True, stop=True)
                    return inst
                c.op("pe", fx, reads=[r_h2t, r_const], writes=[rpb[s0 + q] for q in range(4)])
                for q in range(4):
                    edge = (t == nv - 1 and q == 3) or (t == 0 and q == 0)
                    jn = pctr["j"] % NJ
                    pctr["j"] += 1
                    c.op("dve", lambda e, t=t, q=q, k=k, s0=s0, jn=jn: e.scalar_tensor_tensor(
                        out=junk4[jn][:], in0=gbuf[k][:, q * 512:(q + 1) * 512], scalar=1.0, in1=pb[s0 + q][:, :],
                        op0=ALU.mult, op1=ALU.mult, accum_out=apart[:, t, q:q + 1]),
                        reads=[r_gbuf[k], rpb[s0 + q]], writes=[r_junk4[jn]] + ([r_apart] if edge else []))
            if nv < P:
                c.op("dve", lambda e: e.memset(apart[:, nv:P, :], 0.0), writes=[r_apart])
            c.op("dve", lambda e: e.tensor_reduce(out=AT[:, 0:P], in_=apart[:, 0:P, :], axis=mybir.AxisListType.X,
                                                  op=ALU.add), reads=[r_apart], writes=[r_apart])
            c.op("act", lambda e: e.activation(out=AT[:, 0:P], in_=AT[:, 0:P], func=AF.Gelu_apprx_tanh),
                 reads=[r_apart], writes=[r_apart])
            c.op("dve", lambda e: e.tensor_tensor(out=CT[:, 0:P], in0=AT[:, 0:P], in1=GTt[:, 0:P], op=ALU.mult),
                 reads=[r_apart, r_GTt], writes=[r_CT])
            for t in range(nv):
                k = pctr["g"] % NGB
                pctr["g"] += 1
                c.swdma(gbuf[k][:], tabv[:, :], reads=[r_idxT, r_tab], writes=[r_gbuf[k]],
                        indirect=bass.IndirectOffsetOnAxis(ap=idxT[:, t:t + 1], axis=0))
                kc = pctr["c"] % NCB
                pctr["c"] += 1
                c.op("act", lambda e, t=t, kc=kc: e.activation(out=cbuf[kc][:, 127:128], in_=CT[:, t:t + 1],
                                                               func=AF.Copy), reads=[r_CT], writes=[r_cbuf[kc]])

                def fy(e, t=t, k=k, kc=kc):
                    inst = None
                    for q in range(4):
                        inst = e.matmul(pb[q][:P, :], lhsT=cbuf[kc][:, 127 - t:127 - t + P],
                                        rhs=gbuf[k][:, q * 512:(q + 1) * 512], start=(t == 0), stop=(t == nv - 1))
                    return inst
                c.op("pe", fy, reads=[r_cbuf[kc], r_gbuf[k]], writes=[rpb[q] for q in range(4)])
            for q in range(4):
                c.op("dve", lambda e, q=q: e.tensor_tensor(out=x2_t[:P, q * 512:(q + 1) * 512], in0=pb[q][:P, :],
                                                           in1=x2_t[:P, q * 512:(q + 1) * 512], op=ALU.add),
                     reads=[rpb[q], r_x2t], writes=[r_x2t])
            rms_h(x2_t, r_x2t, P, g_fin, r_gfin, yout, r_yout)
            c.dma("sp", y_dst, yout[:nv, :], reads=[r_yout])

        def phase4_group(ntile, P, nv, x_src, mTg, r_mTg, cross_fn, y_dst_fn):
            ntok = ntile * P
            for j in range(ntile):
                if nv < P:
                    c.op("pool", lambda e, j=j: e.memset(x1[j][:], 0.0), writes=[r_x1[j]])
                c.dma("sp", x1[j][:nv, :], x_src(j), writes=[r_x1[j]])
            ws = WStream([(nm, b) for nm in ("w_out", "w_mq", "w_mo", "peer_wq") for b in range(4)])
            for blk in range(4):
                wt, r_w = ws.get()
                for j in range(ntile):
                    b = bank(2, 6)
                    mm_tok(mTg, r_mTg, j * P, P, wt, r_w, b)
                    c.op("dve", lambda e, j=j, blk=blk, b=b: e.tensor_tensor(
                        out=x1[j][:P, blk * 512:(blk + 1) * 512], in0=pb[b][:P, :],
                        in1=x1[j][:P, blk * 512:(blk + 1) * 512], op=ALU.add), reads=[rpb[b], r_x1[j]], writes=[r_x1[j]])
            for j in range(ntile):
                rms_h(x1[j], r_x1[j], P, g_mem, r_gmem, hb4, r_hb4)
                transpose_to(hb4, r_hb4, P, 16, lambda c0, c1, j=j: bufA[:, c0:c1, j * P:(j + 1) * P], r_bufA)
            for blk in range(4):
                wt, r_w = ws.get()
                for ct in range(4):
                    b = bank(2, 6)
                    mm_feat(bufA, r_bufA, 0, ntok, wt, r_w, ct, b)
                    c.op("act", lambda e, blk=blk, ct=ct, b=b: e.activation(
                        out=bufB[:, blk * 4 + ct, 0:ntok], in_=pb[b][:, 0:ntok], func=AF.Copy),
                        reads=[rpb[b]], writes=[r_bufB])
            cross_fn(bufB, r_bufB, bufA, r_bufA)
            for blk in range(4):
                wt, r_w = ws.get()
                for j in range(ntile):
                    b = bank(2, 6)
                    mm_tok(bufA, r_bufA, j * P, P, wt, r_w, b)
                    c.op("dve", lambda e, j=j, blk=blk, b=b: e.tensor_tensor(
                        out=x1[j][:P, blk * 512:(blk + 1) * 512], in0=pb[b][:P, :],
                        in1=x1[j][:P, blk * 512:(blk + 1) * 512], op=ALU.add), reads=[rpb[b], r_x1[j]], writes=[r_x1[j]])
            for j in range(ntile):
                rms_h(x1[j], r_x1[j], P, g_peer, r_gpeer, h2[j], r_h2[j])
                transpose_to(h2[j], r_h2[j], P, 16, lambda c0, c1, j=j: bufB[:, c0:c1, j * P:(j + 1) * P], r_bufB)
            for blk in range(4):
                wt, r_w = ws.get()
                for ct in range(4):
                    b = bank(2, 6)
                    mm_feat(bufB, r_bufB, 0, ntok, wt, r_w, ct, b)
                    c.op("act", lambda e, blk=blk, ct=ct, b=b: e.activation(
                        out=bufA[:, blk * 4 + ct, 0:ntok], in_=pb[b][:, 0:ntok], func=AF.Copy),
                        reads=[rpb[b]], writes=[r_bufA])
            for j in range(ntile):
                peer_tile(P, nv, bufA, r_bufA, j * P, h2[j], r_h2[j], x1[j], r_x1[j], y_dst_fn(j))

        mTg = c.sb(es4, "mTg", [128, 16, GT], BF16)
        r_mTg = Res()
        for g in range((NT // G) if 5 in _PH else 0)[:_LIM["groups5"]]:
            c.dma("sp", mTg[:], mTs[:, :, g * GT:(g + 1) * GT].rearrange("k p t -> p k t"), reads=r_mTs,
                  writes=[r_mTg])
            phase4_group(
                G, 128, 128, lambda j, g=g: xo[g * GT + j * 128:g * GT + (j + 1) * 128, :], mTg, r_mTg,
                lambda qT_, r_q, oT_, r_o: cross_attn(qT_, r_q, 0, GT, mkT, [r_mkT],
                                                      lambda mt, c0: mv_bf[:, mt, c0:c0 + 128], [r_mvbf], oT_, r_o),
                lambda j, g=g: y_p[g * GT + j * 128:g * GT + (j + 1) * 128, :])

        def cross_sample(qT_, r_q, oT_, r_o):
            stage = ((sc, r_sc), (cand, r_cand))
            n = 0
            for b in range(4):
                for kv in range(2):
                    for mt in range(2):
                        st, r_st = stage[n % 2]
                        n += 1
                        c.dma("sp", st[:], cmem[b, mt * 128:(mt + 1) * 128, kv, :], writes=[r_st])
                        k = kv * 2 + mt
                        c.op("pool", lambda e, st=st, k=k: e.tensor_copy(out=gbuf[k][:], in_=st[:]), reads=[r_st],
                             writes=[r_gbuf[k]])
                for mt in range(2):
                    transpose_to(gbuf[mt], r_gbuf[mt], 128, 16,
                                 lambda c0, c1, mt=mt: mTg[:, c0:c1, mt * 128:(mt + 1) * 128], r_mTg)
                cross_attn(qT_, r_q, b * 8, 8, mTg, [r_mTg], lambda mt, c0: gbuf[2 + mt][:, c0:c0 + 128],
                           [r_gbuf[2], r_gbuf[3]], oT_, r_o)

        if 6 in _PH:
            phase4_group(1, 128, SP, lambda j: xs[:, :], mTg_s, r_mTgs, cross_sample, lambda j: y_s[:, :])

        c.finish()
    return nc


def _host_tables(rel_bias):
    biasT = np.zeros((24, 128, 2, 128), np.float32)
    jl = np.arange(128)[:, None, None]
    jt = np.arange(2)[None, :, None]
    i = np.arange(128)[None, None, :]
    off = i + 128 - (jt * 128 + jl)
    band = ((off >= 0) & (off <= 128)).astype(np.float32)
    offc = np.clip(off, 0, 128)
    for g, dil in enumerate(DILS):
        bucket = _t5_bucket(dil * np.arange(129))
        for h in range(8):
            biasT[g * 8 + h] = rel_bias[bucket[offc], g * 8 + h]
    sbias = np.zeros((8, 128, 16, 24), np.float32)
    smask = np.zeros((128, 16, 24), np.float32)
    sbias_n = np.zeros((8, 32, 4, 24), np.float32)
    smask_n = np.zeros((32, 4, 24), np.float32)
    for g, dil in enumerate(DILS):
        bucket = _t5_bucket(dil * np.arange(129))
        for t in range(8):
            for j in range(129):
                row = 2048 + t - dil * j
                col = g * 8 + t
                if row < 2048:
                    smask[row % 128, row // 128, col] = 1.0
                    sbias[:, row % 128, row // 128, col] = rel_bias[bucket[j], g * 8:(g + 1) * 8]
                else:
                    tp = row - 2048
                    for b in range(4):
                        smask_n[b * 8 + tp, b, col] = 1.0
                        sbias_n[:, b * 8 + tp, b, col] = rel_bias[bucket[j], g * 8:(g + 1) * 8]
    return biasT, band, sbias, smask, sbias_n, smask_n


_NC_CACHE = {}


def _prepare(x_prompt, x_sample, mem_prompt, cache_win, cache_mem_kv, rel_bias, norm_mix, w_in,
             sgu_ln_g, sgu_ln_b, sgu_w, sgu_b, w_out, norm_mem, norm_memtok, w_mq, w_mk, w_mv, w_mo,
             norm_peer, peer_wq, peer_keys1, peer_keys2, peer_u, peer_v, norm_final):
    f = lambda a: np.ascontiguousarray(np.asarray(a, dtype=np.float32))
    xp = f(x_prompt)[0]
    xsm = f(x_sample)
    rel_bias = f(rel_bias)
    biasT, band, sbias, smask, sbias_n, smask_n = _host_tables(rel_bias)
    sgu_w0 = f(sgu_w)[0]
    sgu_wT = np.ascontiguousarray(sgu_w0.transpose(0, 2, 1))
    sgu_wTs = np.zeros((8, 128, 128), np.float32)
    for b in range(4):
        sgu_wTs[:, b * 8:(b + 1) * 8, b * 8:(b + 1) * 8] = sgu_wT[:, :8, :8]
    sgu_b0 = f(sgu_b)[0]
    sgu_bT = np.ascontiguousarray(sgu_b0.T)
    sgu_bTs = np.zeros((128, 8), np.float32)
    sgu_bTs[:32] = np.tile(sgu_b0[:, :8].T, (4, 1))
    shared = {
        "mem": f(mem_prompt)[0], "w_in": f(w_in)[0], "w_out": f(w_out)[0], "w_mq": f(w_mq)[0],
        "w_mk": f(w_mk)[0], "w_mv": f(w_mv)[0], "w_mo": f(w_mo)[0], "peer_wq": f(peer_wq)[0],
        "peer_u": f(peer_u)[0], "peer_v": f(peer_v)[0], "keys1": f(peer_keys1)[0], "keys2": f(peer_keys2)[0],
        "norm_mix": f(norm_mix), "norm_mem": f(norm_mem), "norm_memtok": f(norm_memtok),
        "norm_peer": f(norm_peer), "norm_final": f(norm_final).reshape(1, D),
        "sgu_ln_g": f(sgu_ln_g), "sgu_ln_b": f(sgu_ln_b), "sgu_wT": sgu_wT, "sgu_wTs": sgu_wTs,
        "sgu_bT": sgu_bT, "sgu_bTs": sgu_bTs, "biasT": biasT, "bandmask": band, "sbias": sbias,
        "smask": smask, "sbias_n": sbias_n, "smask_n": smask_n,
    }
    cw = f(cache_win)[0]
    cm = f(cache_mem_kv)[0].reshape(32, 256, 2, 2048)
    in_maps = []
    for cidx in range(NCORES):
        m = dict(shared)
        m["xo"] = xp[cidx * TOK:(cidx + 1) * TOK]
        m["xh"] = xp[(cidx - 1) * TOK:cidx * TOK] if cidx > 0 else np.zeros((TOK, D), np.float32)
        m["xs"] = np.ascontiguousarray(xsm[cidx * 4:(cidx + 1) * 4].reshape(SP, D))
        m["cwin"] = cw[cidx * 4:(cidx + 1) * 4]
        m["cmem"] = cm[cidx * 4:(cidx + 1) * 4]
        m["pflag"] = np.full((128, 1), 0.0 if cidx == 0 else 1.0, np.float32)
        in_maps.append(m)
    return in_maps


def kernel(**inputs):
    in_maps = _prepare(**inputs)
    if "nc" not in _NC_CACHE:
        _NC_CACHE["nc"] = build_program()
    ncr = _LIM.get("ncores") or NCORES
    res = run_bass_kernel_spmd(_NC_CACHE["nc"], in_maps[:ncr], core_ids=list(range(ncr)))
    R = list(res.results)
    while len(R) < NCORES:
        R.append(R[0])
    y_prompt = np.concatenate([R[i]["y_p"] for i in range(NCORES)], axis=0).reshape(1, NCORES * TOK, D)
    y_sample = np.concatenate([R[i]["y_s"] for i in range(NCORES)], axis=0).reshape(32, 8, D)
    win_p = R[NCORES - 1]["win_p"].reshape(1, 1, TOK, 2, 8, 128)
    memkv = R[0]["memkv"].reshape(1, 1, 256, 2, 4, 512)
    win_s = np.concatenate([R[i]["win_s"] for i in range(NCORES)], axis=0).reshape(1, 32, 8, 2, 8, 128)
    sgu_s = np.concatenate([R[i]["sgu_s"] for i in range(NCORES)], axis=0).reshape(1, 32, 8, 1024)
    return (y_prompt, y_sample, win_p, memkv, win_s, sgu_s)
```

```python
import contextlib
import math
import numpy as np
import concourse.bass as bass
import concourse.mybir as mybir
from concourse.bass_utils import run_bass_kernel_spmd

F32 = mybir.dt.float32
BF16 = mybir.dt.bfloat16
I32 = mybir.dt.int32
U32 = mybir.dt.uint32
AF = mybir.ActivationFunctionType
ALU = mybir.AluOpType

NCORES = 8
D = 2048
TOK = 2048
NT = TOK // 128
SP = 32
EPS = 1e-6
DILS = (1, 4, 16)
REL_BUCKETS = 32
REL_MAX_DIST = 2048
ATT_SCALE = 128 ** -0.5
MEM_SCALE = 512 ** -0.5
NEG = -1e30


class Res:
    __slots__ = ("w", "r")

    def __init__(self):
        self.w = None
        self.r = {}


class Ctx:
    NDMA = 32

    def __init__(self, nc, es):
        self.nc = nc
        self.es = es
        self.eng = {"pe": nc.tensor, "act": nc.scalar, "dve": nc.vector, "pool": nc.gpsimd, "sp": nc.sync}
        self.sem = {}
        self.cnt = {}
        self.known = {e: {} for e in self.eng}
        for e in self.eng:
            self.sem[e] = es.enter_context(nc.semaphore("s_" + e))
            self.cnt[e] = 0
        self.dsem = [es.enter_context(nc.semaphore("d%d" % i)) for i in range(self.NDMA)]
        self.dtgt = [0] * self.NDMA
        self.drr = 0
        self.nop = 0
        self.muted = False
        self.NSW = 8
        self.swsem = [es.enter_context(nc.semaphore("w%d" % i)) for i in range(self.NSW)]
        self.swtgt = [0] * self.NSW
        self.swrr = 0

    def sb(self, es, name, shape, dt):
        return es.enter_context(self.nc.sbuf_tensor(name, list(shape), dt))

    def _semof(self, key):
        if isinstance(key, str):
            return self.sem[key]
        if isinstance(key, tuple):
            return self.swsem[key[1]]
        return self.dsem[key]

    def swdma(self, out, in_, reads=(), writes=(), indirect=None, **kw):
        if self.muted:
            return None
        i = self.swrr % self.NSW
        self.swrr += 1
        deps = self._deps(reads, writes)
        if self.swtgt[i]:
            deps.append((("w", i), self.swtgt[i]))
        self._wait("pool", deps)
        self.swtgt[i] += 16
        if indirect is not None:
            inst = self.eng["pool"].indirect_dma_start(out=out, out_offset=None, in_=in_, in_offset=indirect, **kw)
        else:
            inst = self.eng["pool"].dma_start(out=out, in_=in_, **kw)
        inst.then_inc(self.swsem[i], 16)
        tok = (("w", i), self.swtgt[i])
        self._mark(tok, reads, writes)
        self.nop += 1
        return tok

    def _wait(self, e, deps):
        need = {}
        for tok in deps:
            if tok is None:
                continue
            k, v = tok
            if k == "pe" and e == "pe":
                continue
            if self.known[e].get(k, 0) >= v:
                continue
            if need.get(k, 0) < v:
                need[k] = v
        for k, v in need.items():
            self.eng[e].wait_ge(self._semof(k), v)
            self.known[e][k] = v

    @staticmethod
    def _deps(reads, writes):
        deps = []
        for r in reads:
            deps.append(r.w)
        for r in writes:
            deps.append(r.w)
            for k, v in r.r.items():
                deps.append((k, v))
        return deps

    @staticmethod
    def _mark(tok, reads, writes):
        k, v = tok
        for r in reads:
            if r.r.get(k, 0) < v:
                r.r[k] = v
        for r in writes:
            r.w = tok
            r.r = {}

    def op(self, e, fn, reads=(), writes=()):
        if self.muted:
            return None
        self._wait(e, self._deps(reads, writes))
        inst = fn(self.eng[e])
        self.cnt[e] += 1
        inst.then_inc(self.sem[e], 1)
        tok = (e, self.cnt[e])
        self._mark(tok, reads, writes)
        self.nop += 1
        return tok

    def dma(self, q, out, in_, reads=(), writes=(), indirect=None, **kw):
        if self.muted:
            return None
        i = self.drr % self.NDMA
        self.drr += 1
        deps = self._deps(reads, writes)
        if self.dtgt[i]:
            deps.append((i, self.dtgt[i]))
        self._wait(q, deps)
        self.dtgt[i] += 16
        if indirect is not None:
            inst = self.eng[q].indirect_dma_start(out=out, out_offset=None, in_=in_, in_offset=indirect, **kw)
        else:
            inst = self.eng[q].dma_start(out=out, in_=in_, **kw)
        inst.then_inc(self.dsem[i], 16)
        tok = (i, self.dtgt[i])
        self._mark(tok, reads, writes)
        self.nop += 1
        return tok

    def _swtoks(self):
        return [(("w", i), t) for i, t in enumerate(self.swtgt) if t]

    def barrier(self):
        self.muted = False
        deps = [(i, t) for i, t in enumerate(self.dtgt) if t] + self._swtoks()
        deps += [(e, n) for e, n in self.cnt.items() if n]
        for e in self.eng:
            self._wait(e, [d for d in deps if d[0] != e])

    def finish(self):
        deps = [(i, t) for i, t in enumerate(self.dtgt) if t] + self._swtoks()
        deps += [(e, n) for e, n in self.cnt.items() if n and e != "sp"]
        self._wait("sp", deps)


def _t5_bucket(dist):
    max_exact = REL_BUCKETS // 2
    d = np.maximum(dist, 1).astype(np.float64)
    large = max_exact + (np.log(d / max_exact) / math.log(REL_MAX_DIST / max_exact)
                         * (REL_BUCKETS - max_exact)).astype(np.int64)
    large = np.minimum(large, REL_BUCKETS - 1)
    return np.where(dist < max_exact, dist, large).astype(np.int32)


class _SkipPhase(Exception):
    pass


_PH = {0, 1, 2, 3, 4, 5, 6}
_LIM = {"groups": None, "heads": None, "groups5": None}


def build_program():
    nc = bass.Bass("TRN2", target_bir_lowering=False)

    def din(name, shape, dt=F32):
        return nc.dram_tensor(name, list(shape), dt, kind="ExternalInput").ap()

    def dout(name, shape, dt=F32):
        return nc.dram_tensor(name, list(shape), dt, kind="ExternalOutput").ap()

    def dscr(name, shape, dt=BF16):
        return nc.dram_tensor(name, list(shape), dt, kind="Internal").ap()

    xo = din("xo", [TOK, D])
    xh = din("xh", [TOK, D])
    xs = din("xs", [SP, D])
    mem = din("mem", [256, D])
    cwin = din("cwin", [4, 2048, 2, 8, 128])
    cmem = din("cmem", [4, 256, 2, 2048])
    w_in = din("w_in", [D, 9216])
    wsq = {n: din(n, [D, D]) for n in ("w_out", "w_mq", "w_mk", "w_mv", "w_mo", "peer_wq")}
    peer_u = din("peer_u", [16384, D])
    peer_v = din("peer_v", [16384, D])
    keys1 = din("keys1", [8, 128, 128])
    keys2 = din("keys2", [8, 128, 128])
    gains = {n: din(n, [1, D]) for n in ("norm_mix", "norm_mem", "norm_memtok", "norm_peer", "norm_final")}
    sgu_ln_g = din("sgu_ln_g", [1, 1024])
    sgu_ln_b = din("sgu_ln_b", [1, 1024])
    sgu_wT = din("sgu_wT", [8, 128, 128])
    sgu_wTs = din("sgu_wTs", [8, 128, 128])
    sgu_bT = din("sgu_bT", [128, 8])
    sgu_bTs = din("sgu_bTs", [128, 8])
    biasT = din("biasT", [24, 128, 2, 128])
    bandmask = din("bandmask", [128, 2, 128])
    sbias = din("sbias", [8, 128, 16, 24])
    smask = din("smask", [128, 16, 24])
    sbias_n = din("sbias_n", [8, 32, 4, 24])
    smask_n = din("smask_n", [32, 4, 24])
    pflag = din("pflag", [128, 1])

    y_p = dout("y_p", [TOK, D])
    y_s = dout("y_s", [SP, D])
    win_p = dout("win_p", [TOK, 2, 1024])
    memkv = dout("memkv", [256, 2, 2048])
    win_s = dout("win_s", [SP, 2, 1024])
    sgu_s = dout("sgu_s", [SP, 1024])

    WBLK = {"w_in": 18, "w_out": 4, "w_mq": 4, "w_mk": 4, "w_mv": 4, "w_mo": 4, "peer_wq": 4}
    wscr = {n: dscr("wb_" + n, [k, 128, 16, 512]) for n, k in WBLK.items()}
    wscr_res = {n: [Res() for _ in range(k)] for n, k in WBLK.items()}
    tabu = dscr("tabu", [16384, D])
    tabv = dscr("tabv", [16384, D])
    r_tab = Res()
    qTs = dscr("qTs", [24, 128, TOK])
    kTs = dscr("kTs", [8, 128, 2 * TOK])
    vTs = dscr("vTs", [8, 128, 2 * TOK])
    gbTs = dscr("gbTs", [8, 128, TOK])
    mTs = dscr("mTs", [16, 128, TOK])
    r_qTs = [Res() for _ in range(24)]
    r_kTs = [Res() for _ in range(8)]
    r_vTs = [Res() for _ in range(8)]
    r_gbTs = [Res() for _ in range(8)]
    r_mTs = [Res() for _ in range(16)]

    with contextlib.ExitStack() as es:
        c = Ctx(nc, es)

        pball = es.enter_context(nc.psum_tensor("pball", [128, 4096], F32))
        pb = [pball[:, i * 512:(i + 1) * 512] for i in range(8)]
        rpb = [Res() for _ in range(8)]
        pbb = [p.bitcast(BF16) for p in pb]

        identf = c.sb(es, "identf", [128, 128], F32)
        ident = c.sb(es, "ident", [128, 128], BF16)
        ones = c.sb(es, "ones", [128, 128], BF16)
        r_const = Res()
        c.op("pool", lambda e: e.memset(identf[:], 0.0), writes=[r_const])
        c.op("pool", lambda e: e.affine_select(out=identf[:], in_=identf[:], pattern=[[-1, 128]],
                                               compare_op=ALU.not_equal, fill=1.0, base=0,
                                               channel_multiplier=1), reads=[r_const], writes=[r_const])
        c.op("dve", lambda e: e.tensor_copy(out=ident[:], in_=identf[:]), reads=[r_const], writes=[r_const])
        c.op("dve", lambda e: e.memset(ones[:], 1.0), writes=[r_const])

        def cut(k):
            if _LIM.get("cut") == k:
                c.muted = True

        wpool = {"buf": [], "res": [], "n": 0}
        wctr = [0]

        def make_wpool(es_, n):
            tag = "%d" % len(wpool.setdefault("gen", []))
            wpool["gen"].append(n)
            wpool["buf"] = [c.sb(es_, "wbuf%s_%d" % (tag, i), [128, 16, 512], BF16) for i in range(n)]
            wpool["res"] = [Res() for _ in range(n)]
            wpool["n"] = n
            wctr[0] = 0

        class WStream:
            def __init__(self, blocks):
                self.blocks = list(blocks)
                self.issued = 0
                self.pos = 0
                self.base = wctr[0]

            def _issue(self):
                name, blk = self.blocks[self.issued]
                i = (self.base + self.issued) % wpool["n"]
                c.dma("sp", wpool["buf"][i][:], wscr[name][blk], reads=[wscr_res[name][blk]],
                      writes=[wpool["res"][i]])
                self.issued += 1

            def get(self):
                while self.issued < len(self.blocks) and self.issued < self.pos + wpool["n"]:
                    self._issue()
                i = (self.base + self.pos) % wpool["n"]
                self.pos += 1
                wctr[0] = self.base + self.pos
                return wpool["buf"][i], wpool["res"][i]

        def load_gain(es_, name, ap, width=D):
            t = c.sb(es_, "g_" + name, [128, width], F32)
            r = Res()
            c.dma("sp", t[:], ap[0:1, :].partition_broadcast(128), writes=[r])
            return t, r

        bankctr = [0]

        def bank(lo, hi):
            n = hi - lo
            i = lo + bankctr[0] % n
            bankctr[0] += 1
            return i

        NSM = 4
        sm = [c.sb(es, "sm%d" % i, [128, 8], F32) for i in range(NSM)]
        r_sm = [Res() for _ in range(NSM)]
        smctr = [0]
        junk = c.sb(es, "junk", [128, D], BF16)
        r_junk = Res()

        def rms_h(x_t, r_x, P, gain_t, r_gain, h_t, r_h):
            i = smctr[0] % NSM
            smctr[0] += 1
            s, rs = sm[i], r_sm[i]
            c.op("act", lambda e: e.activation(out=junk[:P, :], in_=x_t[:P, :], func=AF.Square,
                                               accum_out=s[:P, 0:1]), reads=[r_x], writes=[r_junk, rs])
            c.op("act", lambda e: e.activation(out=s[:P, 1:2], in_=s[:P, 0:1], func=AF.Sqrt,
                                               scale=1.0 / D, bias=epsc[:P, 0:1]), reads=[rs, r_const], writes=[rs])
            c.op("dve", lambda e: e.reciprocal(out=s[:P, 2:3], in_=s[:P, 1:2]), reads=[rs], writes=[rs])
            c.op("dve", lambda e: e.scalar_tensor_tensor(out=h_t[:P, :], in0=x_t[:P, :], scalar=s[:P, 2:3],
                                                         in1=gain_t[:P, :], op0=ALU.mult, op1=ALU.mult),
                 reads=[r_x, rs, r_gain], writes=[r_h])

        epsc = c.sb(es, "epsc", [128, 1], F32)
        c.op("dve", lambda e: e.memset(epsc[:], EPS), writes=[r_const])

        def transpose_to(h_t, r_h, P, nchunk, dst_fn, r_dst, evac="act"):
            for c0 in range(0, nchunk, 8):
                c1 = min(nchunk, c0 + 8)
                b = bank(0, 2)
                pv = pbb[b][:, 0:(c1 - c0) * 128].rearrange("p (a t) -> p a t", t=128)

                def f(e, c0=c0, c1=c1, pv=pv):
                    inst = None
                    for k in range(c0, c1):
                        inst = e.transpose(out=pv[:, k - c0, 0:P], in_=h_t[:P, k * 128:(k + 1) * 128],
                                           identity=ident[:P, :P])
                    return inst
                c.op("pe", f, reads=[r_h, r_const], writes=[rpb[b]])
                dst = dst_fn(c0, c1)
                if evac == "act":
                    c.op("act", lambda e, dst=dst, pv=pv: e.activation(out=dst, in_=pv[:, :, 0:P], func=AF.Copy),
                         reads=[rpb[b]], writes=[r_dst])
                else:
                    c.op("dve", lambda e, dst=dst, pv=pv: e.tensor_copy(out=dst, in_=pv[:, :, 0:P]),
                         reads=[rpb[b]], writes=[r_dst])

        def mm_tok(hT, r_hT, t0, P, wt, r_w, b, ncol=512, c0=0):
            def f(e):
                inst = None
                for kc in range(16):
                    inst = e.matmul(pb[b][:P, 0:ncol], lhsT=hT[:, kc, t0:t0 + P], rhs=wt[:, kc, c0:c0 + ncol],
                                    start=(kc == 0), stop=(kc == 15))
                return inst
            c.op("pe", f, reads=[r_hT, r_w], writes=[rpb[b]])

        def mm_feat(hT, r_hT, t0, ntok, wt, r_w, ct, b):
            def f(e):
                inst = None
                for kc in range(16):
                    inst = e.matmul(pb[b][:, 0:ntok], lhsT=wt[:, kc, ct * 128:(ct + 1) * 128],
                                    rhs=hT[:, kc, t0:t0 + ntok], start=(kc == 0), stop=(kc == 15))
                return inst
            c.op("pe", f, reads=[r_hT, r_w], writes=[rpb[b]])

        with contextlib.suppress(_SkipPhase), contextlib.ExitStack() as es0:
            if 0 not in _PH:
                raise _SkipPhase()
            NST = 3
            stf = [c.sb(es0, "stf%d" % i, [128, 16, 512], F32) for i in range(NST)]
            stb = [c.sb(es0, "stb%d" % i, [128, 16, 512], BF16) for i in range(NST)]
            r_stf = [Res() for _ in range(NST)]
            r_stb = [Res() for _ in range(NST)]
            n = 0
            wlist = [("w_mk", wsq["w_mk"]), ("w_mv", wsq["w_mv"]), ("w_in", w_in), ("w_out", wsq["w_out"]),
                     ("w_mq", wsq["w_mq"]), ("w_mo", wsq["w_mo"]), ("peer_wq", wsq["peer_wq"])]
            for name, W in wlist:
                Wv = W.rearrange("(kc p) n -> p kc n", p=128)
                for blk in range(WBLK[name]):
                    i = n % NST
                    c.dma("sp", stf[i][:], Wv[:, :, blk * 512:(blk + 1) * 512], writes=[r_stf[i]])
                    if n % 2 == 0:
                        c.op("dve", lambda e, i=i: e.tensor_copy(out=stb[i][:], in_=stf[i][:]),
                             reads=[r_stf[i]], writes=[r_stb[i]])
                    else:
                        c.op("pool", lambda e, i=i: e.tensor_copy(out=stb[i][:], in_=stf[i][:]),
                             reads=[r_stf[i]], writes=[r_stb[i]])
                    c.dma("act", wscr[name][blk], stb[i][:], reads=[r_stb[i]], writes=[wscr_res[name][blk]])
                    n += 1
            if 5 in _PH or 6 in _PH:
                for src, dst in ((peer_u, tabu), (peer_v, tabv)):
                    sv = src.rearrange("(c p r) d -> c p (r d)", p=128, r=4)
                    dv = dst.rearrange("(c p r) d -> c p (r d)", p=128, r=4)
                    for ci in range(32):
                        i = n % NST
                        c.dma("sp", stf[i][:].rearrange("p a b -> p (a b)"), sv[ci], writes=[r_stf[i]])
                        if n % 2 == 0:
                            c.op("dve", lambda e, i=i: e.tensor_copy(out=stb[i][:], in_=stf[i][:]),
                                 reads=[r_stf[i]], writes=[r_stb[i]])
                        else:
                            c.op("pool", lambda e, i=i: e.tensor_copy(out=stb[i][:], in_=stf[i][:]),
                                 reads=[r_stf[i]], writes=[r_stb[i]])
                        c.dma("act", dv[ci], stb[i][:].rearrange("p a b -> p (a b)"), reads=[r_stb[i]], writes=[r_tab])
                        n += 1
            c.barrier()

        mkT = c.sb(es, "mkT", [128, 16, 256], BF16)
        mv_bf = c.sb(es, "mv_bf", [128, 2, D], BF16)
        r_mkT = Res()
        r_mvbf = Res()

        with contextlib.suppress(_SkipPhase), contextlib.ExitStack() as es1:
            if 1 not in _PH:
                raise _SkipPhase()
            make_wpool(es1, 3)
            g_mt, r_gmt = load_gain(es1, "memtok", gains["norm_memtok"])
            mT = c.sb(es1, "mT", [128, 16, 256], BF16)
            r_mT = Res()
            xm = c.sb(es1, "xm", [128, D], F32)
            r_xm = Res()
            hm = c.sb(es1, "hm1", [128, D], BF16)
            r_hm = Res()
            mk_bf = c.sb(es1, "mk_bf", [128, 2, D], BF16)
            r_mkbf = Res()
            kvf = [c.sb(es1, "kvf%d" % i, [128, D], F32) for i in range(2)]
            r_kvf = [Res() for _ in range(2)]
            for mt in range(2):
                c.dma("sp", xm[:], mem[mt * 128:(mt + 1) * 128, :], writes=[r_xm])
                rms_h(xm, r_xm, 128, g_mt, r_gmt, hm, r_hm)
                transpose_to(hm, r_hm, 128, 16, lambda c0, c1, mt=mt: mT[:, c0:c1, mt * 128:(mt + 1) * 128], r_mT)
            n = 0
            for kv, name in enumerate(("w_mk", "w_mv")):
                bf = mk_bf if kv == 0 else mv_bf
                r_bf = r_mkbf if kv == 0 else r_mvbf
                for mt in range(2):
                    i = n % 2
                    n += 1
                    ws = WStream([(name, blk) for blk in range(4)])
                    for blk in range(4):
                        wt, r_w = ws.get()
                        b = bank(2, 6)
                        mm_tok(mT, r_mT, mt * 128, 128, wt, r_w, b)
                        c.op("act", lambda e, i=i, blk=blk, b=b: e.activation(
                            out=kvf[i][:, blk * 512:(blk + 1) * 512], in_=pb[b][:, :], func=AF.Copy),
                            reads=[rpb[b]], writes=[r_kvf[i]])
                    c.op("dve", lambda e, i=i, mt=mt, bf=bf: e.tensor_copy(out=bf[:, mt, :], in_=kvf[i][:]),
                         reads=[r_kvf[i]], writes=[r_bf])
                    c.dma("sp", memkv[mt * 128:(mt + 1) * 128, kv, :], kvf[i][:], reads=[r_kvf[i]])
            for mt in range(2):
                transpose_to(mk_bf[:, mt, :], r_mkbf, 128, 16,
                             lambda c0, c1, mt=mt: mkT[:, c0:c1, mt * 128:(mt + 1) * 128], r_mkT)
            c.barrier()

        G = 2
        GT = G * 128

        def gmlp_tile(es_, P, gv_j, r_gv, gu_j, r_gu, sga_j, r_sga, Wmix, r_W, bs, r_bs, lng, lnb, r_ln,
                      tmp, mA, r_mA, vln_out=None, r_vln=None):
            st, mvv, vt, vlnb, gs = tmp["st"], tmp["mvv"], tmp["vt"], tmp["vlnb"], tmp["gs"]
            r_t = tmp["r"]
            c.op("dve", lambda e: e.bn_stats(out=st[:P, 0, :], in_=gv_j[:P, 0:512]), reads=[r_gv], writes=[r_t])
            c.op("dve", lambda e: e.bn_stats(out=st[:P, 1, :], in_=gv_j[:P, 512:1024]), reads=[r_gv], writes=[r_t])
            c.op("dve", lambda e: e.bn_aggr(out=mvv[:P, 0:2], in_=st[:P].rearrange("p a b -> p (a b)")),
                 reads=[r_t], writes=[r_t])
            c.op("act", lambda e: e.activation(out=mvv[:P, 2:3], in_=mvv[:P, 1:2], func=AF.Sqrt,
                                               bias=epsc[:P, 0:1], scale=1.0), reads=[r_t, r_const], writes=[r_t])
            c.op("dve", lambda e: e.reciprocal(out=mvv[:P, 3:4], in_=mvv[:P, 2:3]), reads=[r_t], writes=[r_t])
            c.op("dve", lambda e: e.tensor_scalar(out=vt[:P, :], in0=gv_j[:P, :], scalar1=mvv[:P, 0:1],
                                                  scalar2=mvv[:P, 3:4], op0=ALU.subtract, op1=ALU.mult),
                 reads=[r_gv, r_t], writes=[r_t])
            c.op("dve", lambda e: e.tensor_tensor(out=vt[:P, :], in0=vt[:P, :], in1=lng[:P, :], op=ALU.mult),
                 reads=[r_t, r_ln], writes=[r_t])
            if vln_out is None:
                c.op("pool", lambda e: e.tensor_tensor(out=vlnb[:P, :], in0=vt[:P, :], in1=lnb[:P, :], op=ALU.add),
                     reads=[r_t, r_ln], writes=[r_t])
            else:
                c.op("pool", lambda e: e.tensor_tensor(out=vln_out[:P, :], in0=vt[:P, :], in1=lnb[:P, :],
                                                       op=ALU.add), reads=[r_t, r_ln], writes=[r_vln])
                c.op("act", lambda e: e.activation(out=vlnb[:P, :], in_=vln_out[:P, :], func=AF.Copy),
                     reads=[r_vln], writes=[r_t])

            def f(e):
                inst = None
                for g in range(8):
                    inst = e.matmul(pb[6 + g // 4][:P, (g % 4) * 128:(g % 4 + 1) * 128], lhsT=Wmix[:P, g, :P],
                                    rhs=vlnb[:P, g * 128:(g + 1) * 128], start=True, stop=True)
                return inst
            c.op("pe", f, reads=[r_t, r_W], writes=[rpb[6], rpb[7]])
            for half in range(2):
                c.op("dve", lambda e, half=half: e.tensor_tensor(
                    out=vt[:P, half * 512:(half + 1) * 512].rearrange("p (g d) -> p g d", d=128),
                    in0=pb[6 + half][:P, :].rearrange("p (g d) -> p g d", d=128),
                    in1=bs[:P, half * 4:half * 4 + 4].unsqueeze(2).to_broadcast([P, 4, 128]), op=ALU.add),
                    reads=[rpb[6 + half], r_bs], writes=[r_t])
            c.op("pool", lambda e: e.tensor_tensor(out=gs[:P, :], in0=gu_j[:P, :], in1=sga_j[:P, :], op=ALU.mult),
                 reads=[r_gu, r_sga], writes=[r_t])
            c.op("dve", lambda e: e.tensor_tensor(out=mA[:P, :], in0=vt[:P, :], in1=gs[:P, :], op=ALU.mult),
                 reads=[r_t], writes=[r_mA])

        def gmlp_tmp(es_, tag):
            return {"st": c.sb(es_, "g_st" + tag, [128, 2, 6], F32), "mvv": c.sb(es_, "g_mvv" + tag, [128, 4], F32),
                    "vt": c.sb(es_, "g_vt" + tag, [128, 1024], F32), "vlnb": c.sb(es_, "g_vlnb" + tag, [128, 1024], BF16),
                    "gs": c.sb(es_, "g_gs" + tag, [128, 1024], BF16), "r": Res()}

        with contextlib.suppress(_SkipPhase), contextlib.ExitStack() as es2:
            if 2 not in _PH:
                raise _SkipPhase()
            make_wpool(es2, 3)
            g_mix, r_gmix = load_gain(es2, "mix", gains["norm_mix"])
            lng, r_ln = load_gain(es2, "lng", sgu_ln_g, 1024)
            lnb = c.sb(es2, "g_lnb", [128, 1024], F32)
            c.dma("sp", lnb[:], sgu_ln_b[0:1, :].partition_broadcast(128), writes=[r_ln])
            wsf = c.sb(es2, "wsf", [128, 8, 128], F32)
            WsT = c.sb(es2, "WsT", [128, 8, 128], BF16)
            r_ws = Res()
            c.dma("sp", wsf[:], sgu_wT.rearrange("g j i -> j g i"), writes=[r_ws])
            c.op("pool", lambda e: e.affine_select(out=wsf[:], in_=wsf[:], pattern=[[0, 8], [1, 128]],
                                                   compare_op=ALU.is_ge, fill=0.0, base=0, channel_multiplier=-1),
                 reads=[r_ws], writes=[r_ws])
            c.op("dve", lambda e: e.tensor_copy(out=WsT[:], in_=wsf[:]), reads=[r_ws], writes=[r_ws])
            bsT = c.sb(es2, "bsT", [128, 8], F32)
            r_bs = Res()
            c.dma("sp", bsT[:], sgu_bT[:, :], writes=[r_bs])

            hT = [c.sb(es2, "hT%d" % i, [128, 16, GT], BF16) for i in range(2)]
            r_hT = [Res() for _ in range(2)]
            xt = [c.sb(es2, "xt%d" % i, [128, D], F32) for i in range(2)]
            r_xt = [Res() for _ in range(2)]
            hb = [c.sb(es2, "hb%d" % i, [128, D], BF16) for i in range(2)]
            r_hb = [Res() for _ in range(2)]
            gv = c.sb(es2, "gv", [128, G, 1024], F32)
            gu = c.sb(es2, "gu", [128, G, 1024], BF16)
            sga = c.sb(es2, "sga", [128, G, 1024], BF16)
            r_gv = [Res() for _ in range(G)]
            r_gu = [Res() for _ in range(G)]
            r_sga = [Res() for _ in range(G)]
            gtmp = gmlp_tmp(es2, "p")
            mA = c.sb(es2, "mA", [128, 1024], BF16)
            r_mA = Res()
            mst = [c.sb(es2, "mst%d" % i, [128, 8, 128], BF16) for i in range(2)]
            r_mst = [Res() for _ in range(2)]
            fst = [c.sb(es2, "fst%d" % i, [128, 4, GT], BF16) for i in range(2)]
            r_fst = [Res() for _ in range(2)]
            kvo = [c.sb(es2, "kvo%d" % i, [128, 512], F32) for i in range(2)]
            r_kvo = [Res() for _ in range(2)]
            ctr = {"x": 0, "f": 0, "k": 0, "m": 0}

            groups = [("h", g) for g in range(NT // G)][:_LIM["groups"]] + [("o", g) for g in range(NT // G)][:_LIM["groups"]]

            def prep_group(gidx):
                kind, g = groups[gidx]
                src = xh if kind == "h" else xo
                hbuf = gidx % 2
                for j in range(G):
                    i = ctr["x"] % 2
                    ctr["x"] += 1
                    t0 = g * GT + j * 128
                    c.dma("sp", xt[i][:], src[t0:t0 + 128, :], writes=[r_xt[i]])
                    rms_h(xt[i], r_xt[i], 128, g_mix, r_gmix, hb[i], r_hb[i])
                    transpose_to(hb[i], r_hb[i], 128, 16,
                                 lambda c0, c1, j=j, hbuf=hbuf: hT[hbuf][:, c0:c1, j * 128:(j + 1) * 128], r_hT[hbuf])

            def feat_block(ws, hbuf, dst, r_dst_list, tcol0, sigmoid=False):
                wt, r_w = ws.get()
                i = ctr["f"] % 2
                ctr["f"] += 1
                for ct in range(4):
                    b = bank(2, 6)
                    mm_feat(hT[hbuf], r_hT[hbuf], 0, GT, wt, r_w, ct, b)
                    if sigmoid:
                        c.op("act", lambda e, b=b, ct=ct, i=i: e.activation(out=fst[i][:, ct, :], in_=pb[b][:, 0:GT],
                                                                          func=AF.Sigmoid),
                             reads=[rpb[b]], writes=[r_fst[i]])
                    elif ct % 2 == 0:
                        c.op("act", lambda e, b=b, ct=ct, i=i: e.activation(out=fst[i][:, ct, :], in_=pb[b][:, 0:GT],
                                                                          func=AF.Copy),
                             reads=[rpb[b]], writes=[r_fst[i]])
                    else:
                        c.op("dve", lambda e, b=b, ct=ct, i=i: e.tensor_copy(out=fst[i][:, ct, :], in_=pb[b][:, 0:GT]),
                             reads=[rpb[b]], writes=[r_fst[i]])
                c.dma("act", dst[:, :, tcol0:tcol0 + GT].rearrange("g p t -> p g t"), fst[i][:],
                      reads=[r_fst[i]], writes=r_dst_list)

            prep_group(0)
            for gidx, (kind, g) in enumerate(groups):
                hbuf = gidx % 2
                if kind == "h":
                    ws = WStream([("w_in", b) for b in (10, 11, 12, 13)])
                    for bi, blk in enumerate((10, 11, 12, 13)):
                        dst = kTs if blk < 12 else vTs
                        rr = r_kTs if blk < 12 else r_vTs
                        h0 = (blk % 2) * 4
                        feat_block(ws, hbuf, dst[h0:h0 + 4], rr[h0:h0 + 4], g * GT)
                        if bi == 0 and gidx + 1 < len(groups):
                            prep_group(gidx + 1)
                    continue
                order = [2, 3, 0, 1, 14, 15, 10, 11, 12, 13, 4, 5, 6, 7, 8, 9, 10, 11, 12, 13, 16, 17]
                ws = WStream([("w_in", b) for b in order])
                for bi, blk in enumerate(order[:10]):
                    wt, r_w = ws.get()
                    for j in range(G):
                        b = bank(2, 6)
                        mm_tok(hT[hbuf], r_hT[hbuf], j * 128, 128, wt, r_w, b)
                        if blk in (2, 3):
                            c.op("act", lambda e, b=b, j=j, blk=blk: e.activation(
                                out=gv[:, j, (blk - 2) * 512:(blk - 1) * 512], in_=pb[b][:, :], func=AF.Gelu_apprx_tanh),
                                reads=[rpb[b]], writes=[r_gv[j]])
                        elif blk in (0, 1):
                            c.op("act", lambda e, b=b, j=j, blk=blk: e.activation(
                                out=gu[:, j, blk * 512:(blk + 1) * 512], in_=pb[b][:, :], func=AF.Gelu_apprx_tanh),
                                reads=[rpb[b]], writes=[r_gu[j]])
                        elif blk in (14, 15):
                            c.op("act", lambda e, b=b, j=j, blk=blk: e.activation(
                                out=sga[:, j, (blk - 14) * 512:(blk - 13) * 512], in_=pb[b][:, :], func=AF.Sigmoid),
                                reads=[rpb[b]], writes=[r_sga[j]])
                        else:
                            i = ctr["k"] % 2
                            ctr["k"] += 1
                            c.op("dve", lambda e, b=b, i=i: e.tensor_copy(out=kvo[i][:], in_=pb[b][:, :]),
                                 reads=[rpb[b]], writes=[r_kvo[i]])
                            t0 = g * GT + j * 128
                            kv = 0 if blk < 12 else 1
                            half = blk % 2
                            c.dma("act", win_p[t0:t0 + 128, kv, half * 512:(half + 1) * 512], kvo[i][:],
                                  reads=[r_kvo[i]])
                    if bi == 0 and gidx + 1 < len(groups):
                        prep_group(gidx + 1)
                    if bi == 5:
                        for j in range(G):
                            gmlp_tile(es2, 128, gv[:, j, :], r_gv[j], gu[:, j, :], r_gu[j], sga[:, j, :], r_sga[j],
                                      WsT, r_ws, bsT, r_bs, lng, lnb, r_ln, gtmp, mA, r_mA)
                            i = ctr["m"] % 2
                            ctr["m"] += 1
                            transpose_to(mA, r_mA, 128, 8, lambda c0, c1, i=i: mst[i][:, c0:c1, :], r_mst[i],
                                         evac="dve")
                            t0 = g * GT + j * 128
                            c.dma("act", mTs[0:8, :, t0:t0 + 128].rearrange("g p t -> p g t"), mst[i][:],
                                  reads=[r_mst[i]], writes=r_mTs[0:8])
                for blk in order[10:]:
                    if 4 <= blk <= 9:
                        gh0 = ((blk - 4) // 2) * 8 + ((blk - 4) % 2) * 4
                        feat_block(ws, hbuf, qTs[gh0:gh0 + 4], r_qTs[gh0:gh0 + 4], g * GT)
                    elif blk in (10, 11):
                        h0 = (blk % 2) * 4
                        feat_block(ws, hbuf, kTs[h0:h0 + 4], r_kTs[h0:h0 + 4], TOK + g * GT)
                    elif blk in (12, 13):
                        h0 = (blk % 2) * 4
                        feat_block(ws, hbuf, vTs[h0:h0 + 4], r_vTs[h0:h0 + 4], TOK + g * GT)
                    else:
                        h0 = (blk % 2) * 4
                        feat_block(ws, hbuf, gbTs[h0:h0 + 4], r_gbTs[h0:h0 + 4], g * GT, sigmoid=True)
            c.barrier()

        with contextlib.suppress(_SkipPhase), contextlib.ExitStack() as es3:
            if 3 not in _PH:
                raise _SkipPhase()
            EBT = c.sb(es3, "EBT", [128, 24, 256], BF16)
            r_EB = Res()
            bm = c.sb(es3, "bm", [128, 256], F32)
            r_bm = Res()
            c.dma("sp", bm[:], bandmask.rearrange("p a b -> p (a b)"), writes=[r_bm])
            btmp = [c.sb(es3, "btmp%d" % i, [128, 256], F32) for i in range(2)]
            r_btmp = [Res() for _ in range(2)]
            for gh in range(24):
                i = gh % 2
                c.dma("sp", btmp[i][:], biasT[gh].rearrange("p a b -> p (a b)"), writes=[r_btmp[i]])
                c.op("act", lambda e, i=i: e.activation(out=btmp[i][:], in_=btmp[i][:], func=AF.Exp),
                     reads=[r_btmp[i]], writes=[r_btmp[i]])
                c.op("dve", lambda e, i=i, gh=gh: e.tensor_tensor(out=EBT[:, gh, :], in0=btmp[i][:], in1=bm[:],
                                                                  op=ALU.mult),
                     reads=[r_btmp[i], r_bm], writes=[r_EB])
            pf = c.sb(es3, "pf", [128, 1], F32)
            r_pf = Res()
            c.dma("sp", pf[:], pflag[:, :], writes=[r_pf])

            kT = [c.sb(es3, "kT%d" % i, [128, 2 * TOK], BF16) for i in range(2)]
            vT = [c.sb(es3, "vT%d" % i, [128, 2 * TOK], BF16) for i in range(2)]
            qT3 = [c.sb(es3, "qT3%d" % i, [128, 3, TOK], BF16) for i in range(2)]
            gbT = [c.sb(es3, "gbT%d" % i, [128, TOK], BF16) for i in range(2)]
            r_hd = [Res() for _ in range(2)]
            acc = c.sb(es3, "acc", [128, 2, TOK], F32)
            r_acc = Res()
            ybT = c.sb(es3, "ybT", [128, TOK], BF16)
            r_ybT = Res()
            NVP = 4
            Vp = [c.sb(es3, "Vp%d" % i, [128, 128], BF16) for i in range(NVP)]
            r_Vp = [Res() for _ in range(NVP)]
            pTf = [c.sb(es3, "pTf%d" % i, [128, 256], BF16) for i in range(2)]
            pT = [c.sb(es3, "pT%d" % i, [128, 256], BF16) for i in range(2)]
            r_pTf = [Res() for _ in range(2)]
            r_pT = [Res() for _ in range(2)]
            qTs4 = qTs.rearrange("(g h) p t -> g h p t", h=8)
            actr = {"vp": 0, "u": 0}

            def load_head(h):
                i = h % 2
                c.dma("sp", kT[i][:], kTs[h], reads=[r_kTs[h]], writes=[r_hd[i]])
                c.dma("sp", vT[i][:], vTs[h], reads=[r_vTs[h]], writes=[r_hd[i]])
                c.dma("sp", qT3[i][:], qTs4[:, h].rearrange("g p t -> p g t"),
                      reads=[r_qTs[h], r_qTs[8 + h], r_qTs[16 + h]], writes=[r_hd[i]])
                c.dma("sp", gbT[i][:], gbTs[h], reads=[r_gbTs[h]], writes=[r_hd[i]])

            def make_vp(i, dil, a0, r):
                k = actr["vp"] % NVP
                actr["vp"] += 1
                b = bank(2, 4)
                vv = vT[i][:].rearrange("p (a b) -> p a b", b=dil)
                c.op("pe", lambda e: e.transpose(out=pbb[b][:, 0:128], in_=vv[:, a0:a0 + 128, r], identity=ident[:]),
                     reads=[r_hd[i], r_const], writes=[rpb[b]])
                c.op("dve", lambda e: e.tensor_copy(out=Vp[k][:], in_=pbb[b][:, 0:128]), reads=[rpb[b]],
                     writes=[r_Vp[k]])
                return k

            load_head(0)
            NH = 8 if _LIM["heads"] is None else _LIM["heads"]
            for h in range(NH):
                i = h % 2
                if h + 1 < NH:
                    load_head(h + 1)
                for gi, dil in enumerate(DILS):
                    span = 128 * dil
                    nbk = TOK // span
                    kv_ = kT[i][:].rearrange("p (a b) -> p a b", b=dil)
                    qv_ = qT3[i][:, gi, :].rearrange("p (a b) -> p a b", b=dil)
                    accv = acc[:].rearrange("p s (a b) -> p s a b", b=dil)
                    for r in range(dil):
                        kprev = make_vp(i, dil, (TOK - span) // dil, r)
                        for bb in range(nbk):
                            a_own = (TOK + bb * span) // dil
                            kcur = make_vp(i, dil, a_own, r)
                            u = actr["u"] % 2
                            actr["u"] += 1
                            b_s = bank(0, 2)
                            a_prev = a_own - 128

                            def fs(e, a_prev=a_prev, bb=bb, r=r, b_s=b_s, kv_=kv_, qv_=qv_):
                                inst = None
                                for jt in range(2):
                                    inst = e.matmul(pb[b_s][:, jt * 128:(jt + 1) * 128],
                                                    lhsT=kv_[:, a_prev + jt * 128:a_prev + (jt + 1) * 128, r],
                                                    rhs=qv_[:, bb * 128:(bb + 1) * 128, r], start=True, stop=True)
                                return inst
                            c.op("pe", fs, reads=[r_hd[i]], writes=[rpb[b_s]])
                            c.op("act", lambda e, u=u, b_s=b_s: e.activation(out=pTf[u][:], in_=pb[b_s][:, 0:256],
                                                                            func=AF.Exp, scale=ATT_SCALE),
                                 reads=[rpb[b_s]], writes=[r_pTf[u]])
                            c.op("pool", lambda e, u=u, gi=gi, h=h: e.tensor_tensor(
                                out=pT[u][:], in0=pTf[u][:], in1=EBT[:, gi * 8 + h, :], op=ALU.mult),
                                reads=[r_pTf[u], r_EB], writes=[r_pT[u]])
                            if bb == 0:
                                c.op("pool", lambda e, u=u: e.tensor_scalar(
                                    out=pT[u][:, 0:128], in0=pT[u][:, 0:128], scalar1=pf[:, 0:1], scalar2=None,
                                    op0=ALU.mult), reads=[r_pT[u], r_pf], writes=[r_pT[u]])
                            b_o = bank(4, 6)

                            def fo(e, u=u, b_o=b_o, kprev=kprev, kcur=kcur):
                                e.matmul(pb[b_o][:, 0:128], lhsT=Vp[kprev][:], rhs=pT[u][:, 0:128], start=True, stop=False)
                                e.matmul(pb[b_o][:, 0:128], lhsT=Vp[kcur][:], rhs=pT[u][:, 128:256], start=False, stop=True)
                                e.matmul(pb[b_o][:, 128:256], lhsT=ones[:], rhs=pT[u][:, 0:128], start=True, stop=False)
                                return e.matmul(pb[b_o][:, 128:256], lhsT=ones[:], rhs=pT[u][:, 128:256],
                                                start=False, stop=True)
                            c.op("pe", fo, reads=[r_pT[u], r_Vp[kprev], r_Vp[kcur], r_const], writes=[rpb[b_o]])
                            av = accv[:, :, bb * 128:(bb + 1) * 128, r]
                            pv = pb[b_o][:, 0:256].rearrange("p (s t) -> p s t", s=2)
                            if gi == 0:
                                c.op("dve", lambda e, av=av, pv=pv: e.tensor_copy(out=av, in_=pv),
                                     reads=[rpb[b_o]], writes=[r_acc])
                            else:
                                c.op("dve", lambda e, av=av, pv=pv: e.tensor_tensor(out=av, in0=pv, in1=av, op=ALU.add),
                                     reads=[rpb[b_o], r_acc], writes=[r_acc])
                            kprev = kcur
                c.op("dve", lambda e: e.reciprocal(out=acc[:, 1, :], in_=acc[:, 1, :]), reads=[r_acc], writes=[r_acc])
                c.op("dve", lambda e: e.tensor_tensor(out=acc[:, 0, :], in0=acc[:, 0, :], in1=acc[:, 1, :], op=ALU.mult),
                     reads=[r_acc], writes=[r_acc])
                c.op("pool", lambda e, i=i: e.tensor_tensor(out=ybT[:], in0=acc[:, 0, :], in1=gbT[i][:], op=ALU.mult),
                     reads=[r_acc, r_hd[i]], writes=[r_ybT])
                c.dma("sp", mTs[8 + h], ybT[:], reads=[r_ybT], writes=[r_mTs[8 + h]])
            c.barrier()


        PS = 128
        mTg_s = c.sb(es, "mTg_s", [128, 16, PS], BF16)
        r_mTgs = Res()
        c.op("pool", lambda e: e.memset(mTg_s[:], 0.0), writes=[r_mTgs])
        with contextlib.suppress(_SkipPhase), contextlib.ExitStack() as ess:
            if 4 not in _PH:
                raise _SkipPhase()
            make_wpool(ess, 2)
            cut(10)
            g_mix, r_gmix = load_gain(ess, "mix_s", gains["norm_mix"])
            lng, r_ln = load_gain(ess, "lng_s", sgu_ln_g, 1024)
            lnb = c.sb(ess, "g_lnb_s", [128, 1024], F32)
            c.dma("sp", lnb[:], sgu_ln_b[0:1, :].partition_broadcast(128), writes=[r_ln])
            wsf = c.sb(ess, "wsf_s", [128, 8, 128], F32)
            WsTs = c.sb(ess, "WsTs", [128, 8, 128], BF16)
            r_ws = Res()
            c.dma("sp", wsf[:], sgu_wTs.rearrange("g j i -> j g i"), writes=[r_ws])
            c.op("pool", lambda e: e.affine_select(out=wsf[:], in_=wsf[:], pattern=[[0, 8], [1, 128]],
                                                   compare_op=ALU.is_ge, fill=0.0, base=0, channel_multiplier=-1),
                 reads=[r_ws], writes=[r_ws])
            c.op("dve", lambda e: e.tensor_copy(out=WsTs[:], in_=wsf[:]), reads=[r_ws], writes=[r_ws])
            bsTs = c.sb(ess, "bsTs", [128, 8], F32)
            r_bs = Res()
            c.dma("sp", bsTs[:], sgu_bTs[:, :], writes=[r_bs])
            cut(11)

            xts = c.sb(ess, "xts", [128, D], F32)
            hbs = c.sb(ess, "hbs", [128, D], BF16)
            hTs = c.sb(ess, "hTs", [128, 16, PS], BF16)
            r_xts, r_hbs, r_hTs = Res(), Res(), Res()
            c.op("pool", lambda e: e.memset(xts[:], 0.0), writes=[r_xts])
            c.dma("sp", xts[:SP, :], xs[:, :], writes=[r_xts])
            cut(12)
            rms_h(xts, r_xts, PS, g_mix, r_gmix, hbs, r_hbs)
            cut(13)
            transpose_to(hbs, r_hbs, PS, 16, lambda c0, c1: hTs[:, c0:c1, :], r_hTs)
            cut(1)

            gv_s = c.sb(ess, "gv_s", [128, 1024], F32)
            gu_s = c.sb(ess, "gu_s", [128, 1024], BF16)
            sga_s = c.sb(ess, "sga_s", [128, 1024], BF16)
            sgb_s = c.sb(ess, "sgb_s", [128, 1024], BF16)
            q_s = c.sb(ess, "q_s", [128, 3072], BF16)
            kf_s = c.sb(ess, "kf_s", [128, 1024], F32)
            vf_s = c.sb(ess, "vf_s", [128, 1024], F32)
            k_sb = c.sb(ess, "k_sb", [128, 1024], BF16)
            vn = c.sb(ess, "vn", [128, 1024], BF16)
            r_gvs, r_gus, r_sgas, r_sgbs, r_qs, r_kfs, r_vfs, r_ksb, r_vn = (Res() for _ in range(9))
            ws = WStream([("w_in", b) for b in range(18)])
            for blk in range(18):
                cut(20 + blk)
                wt, r_w = ws.get()
                b = bank(2, 6)
                mm_tok(hTs, r_hTs, 0, PS, wt, r_w, b)
                src = pb[b][:, :]
                if blk < 2:
                    c.op("act", lambda e, blk=blk, src=src: e.activation(out=gu_s[:, blk * 512:(blk + 1) * 512], in_=src,
                                                                        func=AF.Gelu_apprx_tanh), reads=[rpb[b]], writes=[r_gus])
                elif blk < 4:
                    c.op("act", lambda e, blk=blk, src=src: e.activation(out=gv_s[:, (blk - 2) * 512:(blk - 1) * 512], in_=src,
                                                                        func=AF.Gelu_apprx_tanh), reads=[rpb[b]], writes=[r_gvs])
                elif blk < 10:
                    c.op("act", lambda e, blk=blk, src=src: e.activation(out=q_s[:, (blk - 4) * 512:(blk - 3) * 512], in_=src,
                                                                        func=AF.Copy), reads=[rpb[b]], writes=[r_qs])
                elif blk < 12:
                    c.op("act", lambda e, blk=blk, src=src: e.activation(out=kf_s[:, (blk - 10) * 512:(blk - 9) * 512], in_=src,
                                                                        func=AF.Copy), reads=[rpb[b]], writes=[r_kfs])
                    c.op("dve", lambda e, blk=blk: e.tensor_copy(out=k_sb[:, (blk - 10) * 512:(blk - 9) * 512],
                                                                 in_=kf_s[:, (blk - 10) * 512:(blk - 9) * 512]),
                         reads=[r_kfs], writes=[r_ksb])
                elif blk < 14:
                    c.op("act", lambda e, blk=blk, src=src: e.activation(out=vf_s[:, (blk - 12) * 512:(blk - 11) * 512], in_=src,
                                                                        func=AF.Copy), reads=[rpb[b]], writes=[r_vfs])
                    c.op("dve", lambda e, blk=blk: e.tensor_copy(out=vn[:, (blk - 12) * 512:(blk - 11) * 512],
                                                                 in_=vf_s[:, (blk - 12) * 512:(blk - 11) * 512]),
                         reads=[r_vfs], writes=[r_vn])
                elif blk < 16:
                    c.op("act", lambda e, blk=blk, src=src: e.activation(out=sga_s[:, (blk - 14) * 512:(blk - 13) * 512], in_=src,
                                                                        func=AF.Sigmoid), reads=[rpb[b]], writes=[r_sgas])
                else:
                    c.op("act", lambda e, blk=blk, src=src: e.activation(out=sgb_s[:, (blk - 16) * 512:(blk - 15) * 512], in_=src,
                                                                        func=AF.Sigmoid), reads=[rpb[b]], writes=[r_sgbs])
            cut(40)
            c.dma("sp", win_s[:, 0, :], kf_s[:SP, :], reads=[r_kfs])
            c.dma("sp", win_s[:, 1, :], vf_s[:SP, :], reads=[r_vfs])
            cut(2)
            gtmp_s = gmlp_tmp(ess, "s")
            vlnf_s = c.sb(ess, "vlnf_s", [128, 1024], F32)
            r_vlnf = Res()
            mA_s = c.sb(ess, "mA_s", [128, 1024], BF16)
            r_mAs = Res()
            gmlp_tile(ess, PS, gv_s, r_gvs, gu_s, r_gus, sga_s, r_sgas, WsTs, r_ws, bsTs, r_bs, lng, lnb, r_ln,
                      gtmp_s, mA_s, r_mAs, vln_out=vlnf_s, r_vln=r_vlnf)
            c.dma("sp", sgu_s[:, :], vlnf_s[:SP, :], reads=[r_vlnf])
            transpose_to(mA_s, r_mAs, PS, 8, lambda c0, c1: mTg_s[:, c0:c1, :], r_mTgs)
            cut(3)

            qTs_s = c.sb(ess, "qTs_s", [128, 24, PS], BF16)
            kTn = c.sb(ess, "kTn", [128, 8, PS], BF16)
            gbT_s = c.sb(ess, "gbT_s", [128, 8, PS], BF16)
            r_qTss, r_kTn, r_gbTs_ = Res(), Res(), Res()
            transpose_to(q_s, r_qs, PS, 24, lambda c0, c1: qTs_s[:, c0:c1, :], r_qTss)
            transpose_to(k_sb, r_ksb, PS, 8, lambda c0, c1: kTn[:, c0:c1, :], r_kTn)
            transpose_to(sgb_s, r_sgbs, PS, 8, lambda c0, c1: gbT_s[:, c0:c1, :], r_gbTs_)
            EBs = c.sb(ess, "EBs", [128, 8, 384], BF16)
            EBn = c.sb(ess, "EBn", [128, 8, 96], BF16)
            r_EBs = Res()
            c.op("pool", lambda e: e.memset(EBn[:], 0.0), writes=[r_EBs])
            smk = c.sb(ess, "smk", [128, 384], F32)
            smkn = c.sb(ess, "smkn", [128, 96], F32)
            r_smk = Res()
            c.dma("sp", smk[:], smask.rearrange("p a b -> p (a b)"), writes=[r_smk])
            c.dma("sp", smkn[:SP, :], smask_n.rearrange("p a b -> p (a b)"), writes=[r_smk])
            stmp = [c.sb(ess, "stmp%d" % i, [128, 480], F32) for i in range(2)]
            r_stmp = [Res() for _ in range(2)]
            for h in range(8):
                i = h % 2
                c.dma("sp", stmp[i][:, 0:384], sbias[h].rearrange("p a b -> p (a b)"), writes=[r_stmp[i]])
                c.dma("sp", stmp[i][:SP, 384:480], sbias_n[h].rearrange("p a b -> p (a b)"), writes=[r_stmp[i]])
                c.op("act", lambda e, i=i: e.activation(out=stmp[i][:, 0:384], in_=stmp[i][:, 0:384], func=AF.Exp),
                     reads=[r_stmp[i]], writes=[r_stmp[i]])
                c.op("act", lambda e, i=i: e.activation(out=stmp[i][:SP, 384:480], in_=stmp[i][:SP, 384:480], func=AF.Exp),
                     reads=[r_stmp[i]], writes=[r_stmp[i]])
                c.op("dve", lambda e, i=i, h=h: e.tensor_tensor(out=EBs[:, h, :], in0=stmp[i][:, 0:384], in1=smk[:],
                                                                op=ALU.mult), reads=[r_stmp[i], r_smk], writes=[r_EBs])
                c.op("dve", lambda e, i=i, h=h: e.tensor_tensor(out=EBn[:SP, h, :], in0=stmp[i][:SP, 384:480],
                                                                in1=smkn[:SP, :], op=ALU.mult),
                     reads=[r_stmp[i], r_smk], writes=[r_EBs])
            cut(4)
            Ppad = c.sb(ess, "Ppad", [128, 4, 17 * 96], BF16)
            r_Pp = Res()
            c.op("pool", lambda e: e.memset(Ppad[:], 0.0), writes=[r_Pp])
            Kc = c.sb(ess, "Kc0", [128, 16, 128], F32)
            Vc = c.sb(ess, "Vc0", [128, 16, 128], F32)
            Kcb = [c.sb(ess, "Kcb0", [128, 16, 128], BF16)] * 2
            Vcb = [c.sb(ess, "Vcb%d" % i, [128, 16, 128], BF16) for i in range(2)]
            KT = [c.sb(ess, "KT0", [128, 16, 128], BF16)] * 2
            r_Kc, r_Vc = Res(), Res()
            r_Kcb = [Res()] * 2
            r_Vcb = [Res() for _ in range(2)]
            r_KT = [Res()] * 2
            tmpE = c.sb(ess, "tmpE", [128, 408], BF16)
            r_tmpE = Res()
            qc = [c.sb(ess, "qc%d" % i, [128, 24], BF16) for i in range(2)]
            r_qc = [Res() for _ in range(2)]
            rec_s = c.sb(ess, "rec_s", [128, SP], F32)
            ot_s = c.sb(ess, "ot_s", [128, SP], F32)
            r_recs = Res()
            qv4 = qTs_s[:].rearrange("p (g h) t -> p g h t", h=8)
            tiles_g = ([15], [12, 13, 14, 15], list(range(16)))
            n = 0
            for h in range(8):
                b_o = 4 + h % 2
                b_sum = 6 + h % 2
                for b in range(4):
                    i = n % 2
                    n += 1
                    c.dma("sp", Kc[:], cwin[b, :, 0, h, :].rearrange("(rt p) d -> p rt d", p=128), writes=[r_Kc])
                    c.dma("sp", Vc[:], cwin[b, :, 1, h, :].rearrange("(rt p) d -> p rt d", p=128), writes=[r_Vc])
                    c.op("pool", lambda e, i=i: e.tensor_copy(out=Kcb[i][:], in_=Kc[:]), reads=[r_Kc], writes=[r_Kcb[i]])
                    c.op("act", lambda e, i=i: e.activation(out=Vcb[i][:], in_=Vc[:], func=AF.Copy),
                         reads=[r_Vc], writes=[r_Vcb[i]])
                    transpose_to(Kcb[i][:].rearrange("p a d -> p (a d)"), r_Kcb[i], 128, 16,
                                 lambda c0, c1, i=i: KT[i][:, c0:c1, :], r_KT[i], evac="dve")
                    c.op("dve", lambda e, i=i, h=h, b=b: e.tensor_copy(
                        out=qc[i][:].rearrange("p (g t) -> p g t", t=8), in_=qv4[:, :, h, b * 8:(b + 1) * 8]),
                        reads=[r_qTss], writes=[r_qc[i]])
                    b_s = bank(2, 4)

                    def fs(e, i=i, b_s=b_s, h=h):
                        for rt in range(16):
                            e.matmul(pb[b_s][:, rt * 24:(rt + 1) * 24], lhsT=KT[i][:, rt, :], rhs=qc[i][:],
                                     start=True, stop=True)
                        return e.matmul(pb[b_s][:, 384:408], lhsT=kTn[:, h, :], rhs=qc[i][:], start=True, stop=True)
                    c.op("pe", fs, reads=[r_KT[i], r_qc[i], r_kTn], writes=[rpb[b_s]])
                    c.op("act", lambda e, b_s=b_s: e.activation(out=tmpE[:], in_=pb[b_s][:, 0:408], func=AF.Exp,
                                                                scale=ATT_SCALE), reads=[rpb[b_s]], writes=[r_tmpE])
                    Pv = Ppad[:, b, :].rearrange("p (rt g m) -> p rt g m", g=3, m=32)
                    c.op("dve", lambda e, Pv=Pv, b=b, h=h: e.tensor_tensor(
                        out=Pv[:, 0:16, :, b * 8:(b + 1) * 8],
                        in0=tmpE[:, 0:384].rearrange("p (rt g t) -> p rt g t", g=3, t=8),
                        in1=EBs[:, h, :].rearrange("p (rt g t) -> p rt g t", g=3, t=8), op=ALU.mult),
                        reads=[r_tmpE, r_EBs], writes=[r_Pp])
                    c.op("dve", lambda e, Pv=Pv, b=b, h=h: e.tensor_tensor(
                        out=Pv[:, 16, :, b * 8:(b + 1) * 8],
                        in0=tmpE[:, 384:408].rearrange("p (g t) -> p g t", t=8),
                        in1=EBn[:, h, b * 24:(b + 1) * 24].rearrange("p (g t) -> p g t", t=8), op=ALU.mult),
                        reads=[r_tmpE, r_EBs], writes=[r_Pp])

                    def fo(e, i=i, b=b, h=h, b_o=b_o, b_sum=b_sum, Pv=Pv):
                        mms = []
                        for g in range(3):
                            for rt in tiles_g[g]:
                                mms.append((Vcb[i][:, rt, :], Pv[:, rt, g, :]))
                            mms.append((vn[:, h * 128:(h + 1) * 128], Pv[:, 16, g, :]))
                        inst = None
                        for k, (l, r) in enumerate(mms):
                            first = (b == 0 and k == 0)
                            last = (b == 3 and k == len(mms) - 1)
                            e.matmul(pb[b_o][:, 0:SP], lhsT=l, rhs=r, start=first, stop=last)
                            inst = e.matmul(pb[b_sum][:, 0:SP], lhsT=ones[:], rhs=r, start=first, stop=last)
                        return inst
                    c.op("pe", fo, reads=[r_Pp, r_Vcb[i], r_vn, r_const], writes=[rpb[b_o], rpb[b_sum]])
                c.op("dve", lambda e, b_sum=b_sum: e.reciprocal(out=rec_s[:], in_=pb[b_sum][:, 0:SP]),
                     reads=[rpb[b_sum]], writes=[r_recs])
                c.op("dve", lambda e, b_o=b_o: e.tensor_tensor(out=ot_s[:], in0=pb[b_o][:, 0:SP], in1=rec_s[:],
                                                               op=ALU.mult), reads=[rpb[b_o], r_recs], writes=[r_recs])
                c.op("dve", lambda e, h=h: e.tensor_tensor(out=mTg_s[:, 8 + h, 0:SP], in0=ot_s[:], in1=gbT_s[:, h, 0:SP],
                                                           op=ALU.mult), reads=[r_recs, r_gbTs_], writes=[r_mTgs])
            c.barrier()

        es4 = es.enter_context(contextlib.ExitStack())
        make_wpool(es4, 2)
        g_mem, r_gmem = load_gain(es4, "mem", gains["norm_mem"])
        g_peer, r_gpeer = load_gain(es4, "peer", gains["norm_peer"])
        g_fin, r_gfin = load_gain(es4, "fin", gains["norm_final"])
        keysT = c.sb(es4, "keysT", [128, 2, 8, 128], BF16)
        r_keysT = Res()
        with contextlib.ExitStack() as esk:
            kf = c.sb(esk, "kf", [128, 2, 8, 128], F32)
            kb = c.sb(esk, "kb", [128, 2, 8, 128], BF16)
            r_kf = Res()
            c.dma("sp", kf[:, 0, :, :], keys1.rearrange("h k d -> k h d"), writes=[r_kf])
            c.dma("sp", kf[:, 1, :, :], keys2.rearrange("h k d -> k h d"), writes=[r_kf])
            c.op("dve", lambda e: e.tensor_copy(out=kb[:], in_=kf[:]), reads=[r_kf], writes=[r_kf])
            transpose_to(kb[:].rearrange("p s h d -> p (s h d)"), r_kf, 128, 16,
                         lambda c0, c1: keysT[:].rearrange("p s h k -> p (s h) k")[:, c0:c1, :], r_keysT)
            c.barrier()
        iota_i = c.sb(es4, "iota_i", [128, 16], I32)
        iota16 = c.sb(es4, "iota16", [128, 16], F32)
        r_iota = Res()
        c.op("pool", lambda e: e.iota(iota_i[:], pattern=[[1, 16]], base=0, channel_multiplier=0), writes=[r_iota])
        c.op("dve", lambda e: e.tensor_copy(out=iota16[:], in_=iota_i[:]), reads=[r_iota], writes=[r_iota])

        x1 = [c.sb(es4, "x1_%d" % i, [128, D], F32) for i in range(G)]
        r_x1 = [Res() for _ in range(G)]
        h2 = [c.sb(es4, "h2_%d" % i, [128, D], BF16) for i in range(G)]
        r_h2 = [Res() for _ in range(G)]
        hb4 = c.sb(es4, "hb4", [128, D], BF16)
        r_hb4 = Res()
        bufA = c.sb(es4, "bufA", [128, 16, GT], BF16)
        bufB = c.sb(es4, "bufB", [128, 16, GT], BF16)
        r_bufA = Res()
        r_bufB = Res()
        pTm = c.sb(es4, "pTm", [128, 2, GT], BF16)
        r_pTm = Res()
        rsm = c.sb(es4, "rsm", [128, GT], F32)
        r_rsm = Res()
        sc = c.sb(es4, "sc", [128, 2048], F32)
        cand = c.sb(es4, "cand", [128, 2048], F32)
        r_sc = Res()
        r_cand = Res()
        wk = c.sb(es4, "wk", [128, 256], F32)
        r_wk = Res()
        tv = c.sb(es4, "tv", [128, 16, 16], F32)
        ti = c.sb(es4, "ti", [128, 16, 16], U32)
        tif = c.sb(es4, "tif", [128, 16, 16], F32)
        best = c.sb(es4, "best", [128, 8, 16], F32)
        pos = c.sb(es4, "pos", [128, 8, 16], U32)
        pa = c.sb(es4, "pa", [128, 128], U32)
        pbi = c.sb(es4, "pbi", [128, 128], U32)
        paf = c.sb(es4, "paf", [128, 128], F32)
        pbf = c.sb(es4, "pbf", [128, 128], F32)
        i1s = c.sb(es4, "i1s", [128, 128], F32)
        i2s = c.sb(es4, "i2s", [128, 128], F32)
        eidf = c.sb(es4, "eidf", [128, 128], F32)
        gate = c.sb(es4, "gate", [128, 8, 16], F32)
        gsm = c.sb(es4, "gsm", [128, 24], F32)
        r_pk = Res()
        idxT = c.sb(es4, "idxT", [128, 128], I32)
        GTt = c.sb(es4, "GTt", [128, 128], F32)
        apart = c.sb(es4, "apart", [128, 128, 4], F32)
        AT = c.sb(es4, "AT", [128, 128], F32)
        CT = c.sb(es4, "CT", [128, 128], BF16)
        r_idxT = Res()
        r_GTt = Res()
        r_apart = Res()
        r_CT = Res()
        NGB = 6
        gbuf = [c.sb(es4, "gbuf%d" % i, [128, D], BF16) for i in range(NGB)]
        r_gbuf = [Res() for _ in range(NGB)]
        NCB = 4
        cbuf = [c.sb(es4, "cbuf%d" % i, [128, 256], BF16) for i in range(NCB)]
        r_cbuf = [Res() for _ in range(NCB)]
        for i in range(NCB):
            c.op("pool", lambda e, i=i: e.memset(cbuf[i][:], 0.0), writes=[r_cbuf[i]])
        NJ = 2
        junk4 = [c.sb(es4, "junk4_%d" % i, [128, D], BF16) for i in range(NJ)]
        r_junk4 = [Res() for _ in range(NJ)]
        yout = cand
        r_yout = r_cand
        pctr = {"g": 0, "c": 0, "j": 0}

        def cross_attn(qT_, r_q, col0, n, mkT_, r_mk, mv_fn, r_mv, oT_, r_o):
            for h in range(4):
                for mt in range(2):
                    b = bank(2, 6)

                    def f(e, h=h, mt=mt, b=b):
                        inst = None
                        for dc in range(4):
                            inst = e.matmul(pb[b][:, 0:n], lhsT=mkT_[:, h * 4 + dc, mt * 128:(mt + 1) * 128],
                                            rhs=qT_[:, h * 4 + dc, col0:col0 + n], start=(dc == 0), stop=(dc == 3))
                        return inst
                    c.op("pe", f, reads=[r_q] + r_mk, writes=[rpb[b]])
                    c.op("act", lambda e, mt=mt, b=b: e.activation(out=pTm[:, mt, 0:n], in_=pb[b][:, 0:n],
                                                                  func=AF.Exp, scale=MEM_SCALE),
                         reads=[rpb[b]], writes=[r_pTm])
                b = bank(2, 6)

                def fs(e, b=b):
                    e.matmul(pb[b][:, 0:n], lhsT=ones[:], rhs=pTm[:, 0, 0:n], start=True, stop=False)
                    return e.matmul(pb[b][:, 0:n], lhsT=ones[:], rhs=pTm[:, 1, 0:n], start=False, stop=True)
                c.op("pe", fs, reads=[r_pTm, r_const], writes=[rpb[b]])
                c.op("dve", lambda e, b=b: e.reciprocal(out=rsm[:, 0:n], in_=pb[b][:, 0:n]), reads=[rpb[b]],
                     writes=[r_rsm])
                for dc in range(4):
                    b = bank(2, 6)

                    def fo(e, h=h, dc=dc, b=b):
                        c0 = h * 512 + dc * 128
                        e.matmul(pb[b][:, 0:n], lhsT=mv_fn(0, c0), rhs=pTm[:, 0, 0:n], start=True, stop=False)
                        return e.matmul(pb[b][:, 0:n], lhsT=mv_fn(1, c0), rhs=pTm[:, 1, 0:n],
                                        start=False, stop=True)
                    c.op("pe", fo, reads=[r_pTm] + r_mv, writes=[rpb[b]])
                    c.op("dve", lambda e, h=h, dc=dc, b=b: e.tensor_tensor(
                        out=oT_[:, h * 4 + dc, col0:col0 + n], in0=pb[b][:, 0:n], in1=rsm[:, 0:n], op=ALU.mult),
                        reads=[rpb[b], r_rsm], writes=[r_o])

        def top16(P, src_ap, n, dst_v, dst_i, rd, wr):
            c.op("dve", lambda e: e.max(out=dst_v[:, 0:8], in_=src_ap), reads=rd, writes=wr)
            c.op("dve", lambda e: e.max_index(out=dst_i[:, 0:8], in_max=dst_v[:, 0:8], in_values=src_ap),
                 reads=rd + wr, writes=wr)
            c.op("dve", lambda e: e.match_replace(out=wk[:P, 0:n], in_to_replace=dst_v[:, 0:8], in_values=src_ap,
                                                  imm_value=NEG), reads=rd + wr, writes=[r_wk])
            c.op("dve", lambda e: e.max(out=dst_v[:, 8:16], in_=wk[:P, 0:n]), reads=[r_wk], writes=wr)
            c.op("dve", lambda e: e.max_index(out=dst_i[:, 8:16], in_max=dst_v[:, 8:16], in_values=wk[:P, 0:n]),
                 reads=[r_wk] + wr, writes=wr)

        def peer_tile(P, nv, qpT, r_qp, col0, h2_t, r_h2t, x2_t, r_x2t, y_dst):
            def fsc(e):
                inst = None
                for hs in range(16):
                    inst = e.matmul(pb[4 + hs // 4][:P, (hs % 4) * 128:(hs % 4 + 1) * 128],
                                    lhsT=qpT[:, hs, col0:col0 + P], rhs=keysT[:, hs % 2, hs // 2, :],
                                    start=True, stop=True)
                return inst
            c.op("pe", fsc, reads=[r_qp, r_keysT], writes=[rpb[4], rpb[5], rpb[6], rpb[7]])
            for q in range(4):
                c.op("act", lambda e, q=q: e.activation(out=sc[:P, q * 512:(q + 1) * 512], in_=pb[4 + q][:P, :],
                                                        func=AF.Copy), reads=[rpb[4 + q]], writes=[r_sc])
            scv = sc[:P, :].rearrange("p (a k) -> p a k", k=128)
            for hs in range(16):
                top16(P, scv[:, hs, :], 128, tv[:P, hs, :], ti[:P, hs, :], [r_sc], [r_pk])
            tv4 = tv[:P].rearrange("p (h s) k -> p h s k", s=2)
            candv = cand[:P, :].rearrange("p (h a b) -> p h a b", a=16, b=16)
            c.op("dve", lambda e: e.tensor_tensor(out=candv, in0=tv4[:, :, 0, :].unsqueeze(3).to_broadcast([P, 8, 16, 16]),
                                                  in1=tv4[:, :, 1, :].unsqueeze(2).to_broadcast([P, 8, 16, 16]),
                                                  op=ALU.add), reads=[r_pk], writes=[r_cand])
            cand3 = cand[:P, :].rearrange("p (h c) -> p h c", c=256)
            for h in range(8):
                top16(P, cand3[:, h, :], 256, best[:P, h, :], pos[:P, h, :], [r_cand], [r_pk])
            c.op("dve", lambda e: e.tensor_scalar(out=gsm[:P, 0:8], in0=best[:P, :, 0], scalar1=-1.0, scalar2=None,
                                                  op0=ALU.mult), reads=[r_pk], writes=[r_pk])
            for h in range(8):
                c.op("act", lambda e, h=h: e.activation(out=gate[:P, h, :], in_=best[:P, h, :], func=AF.Exp,
                                                        bias=gsm[:P, h:h + 1], scale=1.0,
                                                        accum_out=gsm[:P, 8 + h:9 + h]), reads=[r_pk], writes=[r_pk])
            c.op("dve", lambda e: e.reciprocal(out=gsm[:P, 16:24], in_=gsm[:P, 8:16]), reads=[r_pk], writes=[r_pk])
            c.op("dve", lambda e: e.tensor_tensor(out=gate[:P], in0=gate[:P],
                                                  in1=gsm[:P, 16:24].unsqueeze(2).to_broadcast([P, 8, 16]),
                                                  op=ALU.mult), reads=[r_pk], writes=[r_pk])
            posf = pos[:P].rearrange("p h k -> p (h k)")
            c.op("dve", lambda e: e.tensor_copy(out=pbf[:P, :], in_=posf), reads=[r_pk], writes=[r_pk])
            c.op("dve", lambda e: e.tensor_scalar(out=paf[:P, :], in0=pbf[:P, :], scalar1=16.0, scalar2=None,
                                                  op0=ALU.is_ge), reads=[r_pk], writes=[r_pk])
            for m in range(2, 16):
                c.op("dve", lambda e, m=m: e.scalar_tensor_tensor(out=paf[:P, :], in0=pbf[:P, :], scalar=16.0 * m,
                                                                  in1=paf[:P, :], op0=ALU.is_ge, op1=ALU.add),
                     reads=[r_pk], writes=[r_pk])
            c.op("dve", lambda e: e.scalar_tensor_tensor(out=pbf[:P, :], in0=paf[:P, :], scalar=-16.0, in1=pbf[:P, :],
                                                         op0=ALU.mult, op1=ALU.add), reads=[r_pk], writes=[r_pk])
            c.op("dve", lambda e: e.tensor_copy(out=tif[:P], in_=ti[:P]), reads=[r_pk], writes=[r_pk])
            tif4 = tif[:P].rearrange("p (h s) k -> p h s k", s=2)
            ohv = sc[:P, :].rearrange("p (h a b) -> p h a b", a=16, b=16)
            io4 = iota16[:P, :].unsqueeze(1).unsqueeze(1).to_broadcast([P, 8, 16, 16])
            for side, (pf_, dsts) in enumerate(((paf, i1s), (pbf, i2s))):
                pv = pf_[:P, :].rearrange("p (h k) -> p h k", k=16).unsqueeze(3).to_broadcast([P, 8, 16, 16])
                c.op("dve", lambda e, pv=pv: e.tensor_tensor(out=ohv, in0=pv, in1=io4, op=ALU.is_equal),
                     reads=[r_pk, r_iota], writes=[r_sc])
                c.op("dve", lambda e, side=side: e.tensor_tensor(
                    out=ohv, in0=ohv, in1=tif4[:, :, side, :].unsqueeze(2).to_broadcast([P, 8, 16, 16]), op=ALU.mult),
                    reads=[r_sc, r_pk], writes=[r_sc])
                c.op("dve", lambda e, dsts=dsts: e.tensor_reduce(
                    out=dsts[:P, :].rearrange("p (h k) -> p h k", k=16), in_=ohv, axis=mybir.AxisListType.X,
                    op=ALU.add), reads=[r_sc], writes=[r_pk])
            c.op("dve", lambda e: e.scalar_tensor_tensor(out=eidf[:P, :], in0=i1s[:P, :], scalar=128.0, in1=i2s[:P, :],
                                                         op0=ALU.mult, op1=ALU.add), reads=[r_pk], writes=[r_pk])
            c.op("pe", lambda e: e.transpose(out=pb[0][:, 0:P], in_=eidf[:P, :], identity=identf[:P, :P]),
                 reads=[r_pk, r_const], writes=[rpb[0]])
            c.op("dve", lambda e: e.tensor_copy(out=idxT[:, 0:P], in_=pb[0][:, 0:P]), reads=[rpb[0]], writes=[r_idxT])
            c.op("pe", lambda e: e.transpose(out=pb[1][:, 0:P], in_=gate[:P].rearrange("p h k -> p (h k)"),
                                             identity=identf[:P, :P]), reads=[r_pk, r_const], writes=[rpb[1]])
            c.op("act", lambda e: e.activation(out=GTt[:, 0:P], in_=pb[1][:, 0:P], func=AF.Copy), reads=[rpb[1]],
                 writes=[r_GTt])
            for t in range(nv):
                k = pctr["g"] % NGB
                pctr["g"] += 1
                c.swdma(gbuf[k][:], tabu[:, :], reads=[r_idxT, r_tab], writes=[r_gbuf[k]],
                        indirect=bass.IndirectOffsetOnAxis(ap=idxT[:, t:t + 1], axis=0))
                s0 = (t % 2) * 4

                def fx(e, t=t, s0=s0):
                    inst = None
                    for q in range(4):
                        inst = e.matmul(pb[s0 + q][:, :], lhsT=ident[:P, t:t + 1].to_broadcast([P, 128]),
                                        rhs=h2_t[:P, q * 512:(q + 1) * 512], start=True, stop=True)
                    return inst
                c.op("pe", fx, reads=[r_h2t, r_const], writes=[rpb[s0 + q] for q in range(4)])
                edge = (t == nv - 1) or (t == 0)
                jn = pctr["j"] % NJ
                pctr["j"] += 1
                c.op("dve", lambda e, t=t, k=k, s0=s0, jn=jn: e.scalar_tensor_tensor(
                    out=junk4[jn][:], in0=gbuf[k][:, :], scalar=1.0, in1=pball[:, s0 * 512:(s0 + 4) * 512],
                    op0=ALU.mult, op1=ALU.mult, accum_out=apart[:, t, 0:1]),
                    reads=[r_gbuf[k]] + [rpb[s0 + q] for q in range(4)],
                    writes=[r_junk4[jn]] + ([r_apart] if edge else []))
            if nv < P:
                c.op("dve", lambda e: e.memset(apart[:, nv:P, :], 0.0), writes=[r_apart])
            c.op("dve", lambda e: e.tensor_copy(out=AT[:, 0:P], in_=apart[:, 0:P, 0]), reads=[r_apart], writes=[r_apart])
            c.op("act", lambda e: e.activation(out=AT[:, 0:P], in_=AT[:, 0:P], func=AF.Gelu_apprx_tanh),
                 reads=[r_apart], writes=[r_apart])
            c.op("dve", lambda e: e.tensor_tensor(out=CT[:, 0:P], in0=AT[:, 0:P], in1=GTt[:, 0:P], op=ALU.mult),
                 reads=[r_apart, r_GTt], writes=[r_CT])
            for t in range(nv):
                k = pctr["g"] % NGB
                pctr["g"] += 1
                c.swdma(gbuf[k][:], tabv[:, :], reads=[r_idxT, r_tab], writes=[r_gbuf[k]],
                        indirect=bass.IndirectOffsetOnAxis(ap=idxT[:, t:t + 1], axis=0))
                kc = pctr["c"] % NCB
                pctr["c"] += 1
                c.op("act", lambda e, t=t, kc=kc: e.activation(out=cbuf[kc][:, 127:128], in_=CT[:, t:t + 1],
                                                               func=AF.Copy), reads=[r_CT], writes=[r_cbuf[kc]])

                def fy(e, t=t, k=k, kc=kc):
                    inst = None
                    for q in range(4):
                        inst = e.matmul(pb[q][:P, :], lhsT=cbuf[kc][:, 127 - t:127 - t + P],
                                        rhs=gbuf[k][:, q * 512:(q + 1) * 512], start=(t == 0), stop=(t == nv - 1))
                    return inst
                c.op("pe", fy, reads=[r_cbuf[kc], r_gbuf[k]], writes=[rpb[q] for q in range(4)])
            for q in range(4):
                c.op("dve", lambda e, q=q: e.tensor_tensor(out=x2_t[:P, q * 512:(q + 1) * 512], in0=pb[q][:P, :],
                                                           in1=x2_t[:P, q * 512:(q + 1) * 512], op=ALU.add),
                     reads=[rpb[q], r_x2t], writes=[r_x2t])
            rms_h(x2_t, r_x2t, P, g_fin, r_gfin, yout, r_yout)
            c.dma("sp", y_dst, yout[:nv, :], reads=[r_yout])

        def phase4_group(ntile, P, nv, x_src, mTg, r_mTg, cross_fn, y_dst_fn):
            ntok = ntile * P
            for j in range(ntile):
                if nv < P:
                    c.op("pool", lambda e, j=j: e.memset(x1[j][:], 0.0), writes=[r_x1[j]])
                c.dma("sp", x1[j][:nv, :], x_src(j), writes=[r_x1[j]])
            ws = WStream([(nm, b) for nm in ("w_out", "w_mq", "w_mo", "peer_wq") for b in range(4)])
            for blk in range(4):
                wt, r_w = ws.get()
                for j in range(ntile):
                    b = bank(2, 6)
                    mm_tok(mTg, r_mTg, j * P, P, wt, r_w, b)
                    c.op("dve", lambda e, j=j, blk=blk, b=b: e.tensor_tensor(
                        out=x1[j][:P, blk * 512:(blk + 1) * 512], in0=pb[b][:P, :],
                        in1=x1[j][:P, blk * 512:(blk + 1) * 512], op=ALU.add), reads=[rpb[b], r_x1[j]], writes=[r_x1[j]])
            for j in range(ntile):
                rms_h(x1[j], r_x1[j], P, g_mem, r_gmem, hb4, r_hb4)
                transpose_to(hb4, r_hb4, P, 16, lambda c0, c1, j=j: bufA[:, c0:c1, j * P:(j + 1) * P], r_bufA)
            for blk in range(4):
                wt, r_w = ws.get()
                for ct in range(4):
                    b = bank(2, 6)
                    mm_feat(bufA, r_bufA, 0, ntok, wt, r_w, ct, b)
                    c.op("act", lambda e, blk=blk, ct=ct, b=b: e.activation(
                        out=bufB[:, blk * 4 + ct, 0:ntok], in_=pb[b][:, 0:ntok], func=AF.Copy),
                        reads=[rpb[b]], writes=[r_bufB])
            cross_fn(bufB, r_bufB, bufA, r_bufA)
            for blk in range(4):
                wt, r_w = ws.get()
                for j in range(ntile):
                    b = bank(2, 6)
                    mm_tok(bufA, r_bufA, j * P, P, wt, r_w, b)
                    c.op("dve", lambda e, j=j, blk=blk, b=b: e.tensor_tensor(
                        out=x1[j][:P, blk * 512:(blk + 1) * 512], in0=pb[b][:P, :],
                        in1=x1[j][:P, blk * 512:(blk + 1) * 512], op=ALU.add), reads=[rpb[b], r_x1[j]], writes=[r_x1[j]])
            for j in range(ntile):
                rms_h(x1[j], r_x1[j], P, g_peer, r_gpeer, h2[j], r_h2[j])
                transpose_to(h2[j], r_h2[j], P, 16, lambda c0, c1, j=j: bufB[:, c0:c1, j * P:(j + 1) * P], r_bufB)
            for blk in range(4):
                wt, r_w = ws.get()
                for ct in range(4):
                    b = bank(2, 6)
                    mm_feat(bufB, r_bufB, 0, ntok, wt, r_w, ct, b)
                    c.op("act", lambda e, blk=blk, ct=ct, b=b: e.activation(
                        out=bufA[:, blk * 4 + ct, 0:ntok], in_=pb[b][:, 0:ntok], func=AF.Copy),
                        reads=[rpb[b]], writes=[r_bufA])
            for j in range(ntile):
                peer_tile(P, nv, bufA, r_bufA, j * P, h2[j], r_h2[j], x1[j], r_x1[j], y_dst_fn(j))

        mTg = c.sb(es4, "mTg", [128, 16, GT], BF16)
        r_mTg = Res()
        for g in range((NT // G) if 5 in _PH else 0)[:_LIM["groups5"]]:
            c.dma("sp", mTg[:], mTs[:, :, g * GT:(g + 1) * GT].rearrange("k p t -> p k t"), reads=r_mTs,
                  writes=[r_mTg])
            phase4_group(
                G, 128, 128, lambda j, g=g: xo[g * GT + j * 128:g * GT + (j + 1) * 128, :], mTg, r_mTg,
                lambda qT_, r_q, oT_, r_o: cross_attn(qT_, r_q, 0, GT, mkT, [r_mkT],
                                                      lambda mt, c0: mv_bf[:, mt, c0:c0 + 128], [r_mvbf], oT_, r_o),
                lambda j, g=g: y_p[g * GT + j * 128:g * GT + (j + 1) * 128, :])

        def cross_sample(qT_, r_q, oT_, r_o):
            stage = ((sc, r_sc), (cand, r_cand))
            n = 0
            for b in range(4):
                for kv in range(2):
                    for mt in range(2):
                        st, r_st = stage[n % 2]
                        n += 1
                        c.dma("sp", st[:], cmem[b, mt * 128:(mt + 1) * 128, kv, :], writes=[r_st])
                        k = kv * 2 + mt
                        c.op("pool", lambda e, st=st, k=k: e.tensor_copy(out=gbuf[k][:], in_=st[:]), reads=[r_st],
                             writes=[r_gbuf[k]])
                for mt in range(2):
                    transpose_to(gbuf[mt], r_gbuf[mt], 128, 16,
                                 lambda c0, c1, mt=mt: mTg[:, c0:c1, mt * 128:(mt + 1) * 128], r_mTg)
                cross_attn(qT_, r_q, b * 8, 8, mTg, [r_mTg], lambda mt, c0: gbuf[2 + mt][:, c0:c0 + 128],
                           [r_gbuf[2], r_gbuf[3]], oT_, r_o)

        if 6 in _PH:
            phase4_group(1, 128, SP, lambda j: xs[:, :], mTg_s, r_mTgs, cross_sample, lambda j: y_s[:, :])

        c.finish()
    return nc


def _host_tables(rel_bias):
    biasT = np.zeros((24, 128, 2, 128), np.float32)
    jl = np.arange(128)[:, None, None]
    jt = np.arange(2)[None, :, None]
    i = np.arange(128)[None, None, :]
    off = i + 128 - (jt * 128 + jl)
    band = ((off >= 0) & (off <= 128)).astype(np.float32)
    offc = np.clip(off, 0, 128)
    for g, dil in enumerate(DILS):
        bucket = _t5_bucket(dil * np.arange(129))
        for h in range(8):
            biasT[g * 8 + h] = rel_bias[bucket[offc], g * 8 + h]
    sbias = np.zeros((8, 128, 16, 24), np.float32)
    smask = np.zeros((128, 16, 24), np.float32)
    sbias_n = np.zeros((8, 32, 4, 24), np.float32)
    smask_n = np.zeros((32, 4, 24), np.float32)
    for g, dil in enumerate(DILS):
        bucket = _t5_bucket(dil * np.arange(129))
        for t in range(8):
            for j in range(129):
                row = 2048 + t - dil * j
                col = g * 8 + t
                if row < 2048:
                    smask[row % 128, row // 128, col] = 1.0
                    sbias[:, row % 128, row // 128, col] = rel_bias[bucket[j], g * 8:(g + 1) * 8]
                else:
                    tp = row - 2048
                    for b in range(4):
                        smask_n[b * 8 + tp, b, col] = 1.0
                        sbias_n[:, b * 8 + tp, b, col] = rel_bias[bucket[j], g * 8:(g + 1) * 8]
    return biasT, band, sbias, smask, sbias_n, smask_n


_NC_CACHE = {}


def _prepare(x_prompt, x_sample, mem_prompt, cache_win, cache_mem_kv, rel_bias, norm_mix, w_in,
             sgu_ln_g, sgu_ln_b, sgu_w, sgu_b, w_out, norm_mem, norm_memtok, w_mq, w_mk, w_mv, w_mo,
             norm_peer, peer_wq, peer_keys1, peer_keys2, peer_u, peer_v, norm_final):
    f = lambda a: np.ascontiguousarray(np.asarray(a, dtype=np.float32))
    xp = f(x_prompt)[0]
    xsm = f(x_sample)
    rel_bias = f(rel_bias)
    biasT, band, sbias, smask, sbias_n, smask_n = _host_tables(rel_bias)
    sgu_w0 = f(sgu_w)[0]
    sgu_wT = np.ascontiguousarray(sgu_w0.transpose(0, 2, 1))
    sgu_wTs = np.zeros((8, 128, 128), np.float32)
    for b in range(4):
        sgu_wTs[:, b * 8:(b + 1) * 8, b * 8:(b + 1) * 8] = sgu_wT[:, :8, :8]
    sgu_b0 = f(sgu_b)[0]
    sgu_bT = np.ascontiguousarray(sgu_b0.T)
    sgu_bTs = np.zeros((128, 8), np.float32)
    sgu_bTs[:32] = np.tile(sgu_b0[:, :8].T, (4, 1))
    shared = {
        "mem": f(mem_prompt)[0], "w_in": f(w_in)[0], "w_out": f(w_out)[0], "w_mq": f(w_mq)[0],
        "w_mk": f(w_mk)[0], "w_mv": f(w_mv)[0], "w_mo": f(w_mo)[0], "peer_wq": f(peer_wq)[0],
        "peer_u": f(peer_u)[0], "peer_v": f(peer_v)[0], "keys1": f(peer_keys1)[0], "keys2": f(peer_keys2)[0],
        "norm_mix": f(norm_mix), "norm_mem": f(norm_mem), "norm_memtok": f(norm_memtok),
        "norm_peer": f(norm_peer), "norm_final": f(norm_final).reshape(1, D),
        "sgu_ln_g": f(sgu_ln_g), "sgu_ln_b": f(sgu_ln_b), "sgu_wT": sgu_wT, "sgu_wTs": sgu_wTs,
        "sgu_bT": sgu_bT, "sgu_bTs": sgu_bTs, "biasT": biasT, "bandmask": band, "sbias": sbias,
        "smask": smask, "sbias_n": sbias_n, "smask_n": smask_n,
    }
    cw = f(cache_win)[0]
    cm = f(cache_mem_kv)[0].reshape(32, 256, 2, 2048)
    in_maps = []
    for cidx in range(NCORES):
        m = dict(shared)
        m["xo"] = xp[cidx * TOK:(cidx + 1) * TOK]
        m["xh"] = xp[(cidx - 1) * TOK:cidx * TOK] if cidx > 0 else np.zeros((TOK, D), np.float32)
        m["xs"] = np.ascontiguousarray(xsm[cidx * 4:(cidx + 1) * 4].reshape(SP, D))
        m["cwin"] = cw[cidx * 4:(cidx + 1) * 4]
        m["cmem"] = cm[cidx * 4:(cidx + 1) * 4]
        m["pflag"] = np.full((128, 1), 0.0 if cidx == 0 else 1.0, np.float32)
        in_maps.append(m)
    return in_maps


def kernel(**inputs):
    in_maps = _prepare(**inputs)
    if "nc" not in _NC_CACHE:
        _NC_CACHE["nc"] = build_program()
    ncr = _LIM.get("ncores") or NCORES
    res = run_bass_kernel_spmd(_NC_CACHE["nc"], in_maps[:ncr], core_ids=list(range(ncr)))
    R = list(res.results)
    while len(R) < NCORES:
        R.append(R[0])
    y_prompt = np.concatenate([R[i]["y_p"] for i in range(NCORES)], axis=0).reshape(1, NCORES * TOK, D)
    y_sample = np.concatenate([R[i]["y_s"] for i in range(NCORES)], axis=0).reshape(32, 8, D)
    win_p = R[NCORES - 1]["win_p"].reshape(1, 1, TOK, 2, 8, 128)
    memkv = R[0]["memkv"].reshape(1, 1, 256, 2, 4, 512)
    win_s = np.concatenate([R[i]["win_s"] for i in range(NCORES)], axis=0).reshape(1, 32, 8, 2, 8, 128)
    sgu_s = np.concatenate([R[i]["sgu_s"] for i in range(NCORES)], axis=0).reshape(1, 32, 8, 1024)
    return (y_prompt, y_sample, win_p, memkv, win_s, sgu_s)
```

```python
import contextlib
import math
import numpy as np
import concourse.bass as bass
import concourse.mybir as mybir
from concourse.bass_utils import run_bass_kernel_spmd

F32 = mybir.dt.float32
BF16 = mybir.dt.bfloat16
I32 = mybir.dt.int32
U32 = mybir.dt.uint32
AF = mybir.ActivationFunctionType
ALU = mybir.AluOpType

NCORES = 8
D = 2048
TOK = 2048
NT = TOK // 128
SP = 32
EPS = 1e-6
DILS = (1, 4, 16)
REL_BUCKETS = 32
REL_MAX_DIST = 2048
ATT_SCALE = 128 ** -0.5
MEM_SCALE = 512 ** -0.5
NEG = -1e30


class Res:
    __slots__ = ("w", "r")

    def __init__(self):
        self.w = None
        self.r = {}


class Ctx:
    NDMA = 32

    def __init__(self, nc, es):
        self.nc = nc
        self.es = es
        self.eng = {"pe": nc.tensor, "act": nc.scalar, "dve": nc.vector, "pool": nc.gpsimd, "sp": nc.sync}
        self.sem = {}
        self.cnt = {}
        self.known = {e: {} for e in self.eng}
        for e in self.eng:
            self.sem[e] = es.enter_context(nc.semaphore("s_" + e))
            self.cnt[e] = 0
        self.dsem = [es.enter_context(nc.semaphore("d%d" % i)) for i in range(self.NDMA)]
        self.dtgt = [0] * self.NDMA
        self.drr = 0
        self.nop = 0
        self.muted = False
        self.NSW = 8
        self.swsem = [es.enter_context(nc.semaphore("w%d" % i)) for i in range(self.NSW)]
        self.swtgt = [0] * self.NSW
        self.swrr = 0

    def sb(self, es, name, shape, dt):
        return es.enter_context(self.nc.sbuf_tensor(name, list(shape), dt))

    def _semof(self, key):
        if isinstance(key, str):
            return self.sem[key]
        if isinstance(key, tuple):
            return self.swsem[key[1]]
        return self.dsem[key]

    def swdma(self, out, in_, reads=(), writes=(), indirect=None, **kw):
        if self.muted:
            return None
        i = self.swrr % self.NSW
        self.swrr += 1
        deps = self._deps(reads, writes)
        if self.swtgt[i]:
            deps.append((("w", i), self.swtgt[i]))
        self._wait("pool", deps)
        self.swtgt[i] += 16
        if indirect is not None:
            inst = self.eng["pool"].indirect_dma_start(out=out, out_offset=None, in_=in_, in_offset=indirect, **kw)
        else:
            inst = self.eng["pool"].dma_start(out=out, in_=in_, **kw)
        inst.then_inc(self.swsem[i], 16)
        tok = (("w", i), self.swtgt[i])
        self._mark(tok, reads, writes)
        self.nop += 1
        return tok

    def _wait(self, e, deps):
        need = {}
        for tok in deps:
            if tok is None:
                continue
            k, v = tok
            if k == "pe" and e == "pe":
                continue
            if self.known[e].get(k, 0) >= v:
                continue
            if need.get(k, 0) < v:
                need[k] = v
        for k, v in need.items():
            self.eng[e].wait_ge(self._semof(k), v)
            self.known[e][k] = v

    @staticmethod
    def _deps(reads, writes):
        deps = []
        for r in reads:
            deps.append(r.w)
        for r in writes:
            deps.append(r.w)
            for k, v in r.r.items():
                deps.append((k, v))
        return deps

    @staticmethod
    def _mark(tok, reads, writes):
        k, v = tok
        for r in reads:
            if r.r.get(k, 0) < v:
                r.r[k] = v
        for r in writes:
            r.w = tok
            r.r = {}

    def op(self, e, fn, reads=(), writes=()):
        if self.muted:
            return None
        self._wait(e, self._deps(reads, writes))
        inst = fn(self.eng[e])
        self.cnt[e] += 1
        inst.then_inc(self.sem[e], 1)
        tok = (e, self.cnt[e])
        self._mark(tok, reads, writes)
        self.nop += 1
        return tok

    def dma(self, q, out, in_, reads=(), writes=(), indirect=None, **kw):
        if self.muted:
            return None
        i = self.drr % self.NDMA
        self.drr += 1
        deps = self._deps(reads, writes)
        if self.dtgt[i]:
            deps.append((i, self.dtgt[i]))
        self._wait(q, deps)
        self.dtgt[i] += 16
        if indirect is not None:
            inst = self.eng[q].indirect_dma_start(out=out, out_offset=None, in_=in_, in_offset=indirect, **kw)
        else:
            inst = self.eng[q].dma_start(out=out, in_=in_, **kw)
        inst.then_inc(self.dsem[i], 16)
        tok = (i, self.dtgt[i])
        self._mark(tok, reads, writes)
        self.nop += 1
        return tok

    def _swtoks(self):
        return [(("w", i), t) for i, t in enumerate(self.swtgt) if t]

    def barrier(self):
        self.muted = False
        deps = [(i, t) for i, t in enumerate(self.dtgt) if t] + self._swtoks()
        deps += [(e, n) for e, n in self.cnt.items() if n]
        for e in self.eng:
            self._wait(e, [d for d in deps if d[0] != e])

    def finish(self):
        deps = [(i, t) for i, t in enumerate(self.dtgt) if t] + self._swtoks()
        deps += [(e, n) for e, n in self.cnt.items() if n and e != "sp"]
        self._wait("sp", deps)


def _t5_bucket(dist):
    max_exact = REL_BUCKETS // 2
    d = np.maximum(dist, 1).astype(np.float64)
    large = max_exact + (np.log(d / max_exact) / math.log(REL_MAX_DIST / max_exact)
                         * (REL_BUCKETS - max_exact)).astype(np.int64)
    large = np.minimum(large, REL_BUCKETS - 1)
    return np.where(dist < max_exact, dist, large).astype(np.int32)


class _SkipPhase(Exception):
    pass


_PH = {0, 1, 2, 3, 4, 5, 6}
_LIM = {"groups": None, "heads": None, "groups5": None}


def build_program():
    nc = bass.Bass("TRN2", target_bir_lowering=False)

    def din(name, shape, dt=F32):
        return nc.dram_tensor(name, list(shape), dt, kind="ExternalInput").ap()

    def dout(name, shape, dt=F32):
        return nc.dram_tensor(name, list(shape), dt, kind="ExternalOutput").ap()

    def dscr(name, shape, dt=BF16):
        return nc.dram_tensor(name, list(shape), dt, kind="Internal").ap()

    xo = din("xo", [TOK, D])
    xh = din("xh", [TOK, D])
    xs = din("xs", [SP, D])
    mem = din("mem", [256, D])
    cwin = din("cwin", [4, 2048, 2, 8, 128])
    cmem = din("cmem", [4, 256, 2, 2048])
    w_in = din("w_in", [D, 9216])
    wsq = {n: din(n, [D, D]) for n in ("w_out", "w_mq", "w_mk", "w_mv", "w_mo", "peer_wq")}
    peer_u = din("peer_u", [16384, D])
    peer_v = din("peer_v", [16384, D])
    keys1 = din("keys1", [8, 128, 128])
    keys2 = din("keys2", [8, 128, 128])
    gains = {n: din(n, [1, D]) for n in ("norm_mix", "norm_mem", "norm_memtok", "norm_peer", "norm_final")}
    sgu_ln_g = din("sgu_ln_g", [1, 1024])
    sgu_ln_b = din("sgu_ln_b", [1, 1024])
    sgu_wT = din("sgu_wT", [8, 128, 128])
    sgu_wTs = din("sgu_wTs", [8, 128, 128])
    sgu_bT = din("sgu_bT", [128, 8])
    sgu_bTs = din("sgu_bTs", [128, 8])
    biasT = din("biasT", [24, 128, 2, 128])
    bandmask = din("bandmask", [128, 2, 128])
    sbias = din("sbias", [8, 128, 16, 24])
    smask = din("smask", [128, 16, 24])
    sbias_n = din("sbias_n", [8, 32, 4, 24])
    smask_n = din("smask_n", [32, 4, 24])
    pflag = din("pflag", [128, 1])

    y_p = dout("y_p", [TOK, D])
    y_s = dout("y_s", [SP, D])
    win_p = dout("win_p", [TOK, 2, 1024])
    memkv = dout("memkv", [256, 2, 2048])
    win_s = dout("win_s", [SP, 2, 1024])
    sgu_s = dout("sgu_s", [SP, 1024])

    WBLK = {"w_in": 18, "w_out": 4, "w_mq": 4, "w_mk": 4, "w_mv": 4, "w_mo": 4, "peer_wq": 4}
    wscr = {n: dscr("wb_" + n, [k, 128, 16, 512]) for n, k in WBLK.items()}
    wscr_res = {n: [Res() for _ in range(k)] for n, k in WBLK.items()}
    tabu = dscr("tabu", [16384, D])
    tabv = dscr("tabv", [16384, D])
    r_tab = Res()
    qTs = dscr("qTs", [24, 128, TOK])
    kTs = dscr("kTs", [8, 128, 2 * TOK])
    vTs = dscr("vTs", [8, 128, 2 * TOK])
    gbTs = dscr("gbTs", [8, 128, TOK])
    mTs = dscr("mTs", [16, 128, TOK])
    r_qTs = [Res() for _ in range(24)]
    r_kTs = [Res() for _ in range(8)]
    r_vTs = [Res() for _ in range(8)]
    r_gbTs = [Res() for _ in range(8)]
    r_mTs = [Res() for _ in range(16)]

    with contextlib.ExitStack() as es:
        c = Ctx(nc, es)

        pball = es.enter_context(nc.psum_tensor("pball", [128, 4096], F32))
        pb = [pball[:, i * 512:(i + 1) * 512] for i in range(8)]
        rpb = [Res() for _ in range(8)]
        pbb = [p.bitcast(BF16) for p in pb]

        identf = c.sb(es, "identf", [128, 128], F32)
        ident = c.sb(es, "ident", [128, 128], BF16)
        ones = c.sb(es, "ones", [128, 128], BF16)
        r_const = Res()
        c.op("pool", lambda e: e.memset(identf[:], 0.0), writes=[r_const])
        c.op("pool", lambda e: e.affine_select(out=identf[:], in_=identf[:], pattern=[[-1, 128]],
                                               compare_op=ALU.not_equal, fill=1.0, base=0,
                                               channel_multiplier=1), reads=[r_const], writes=[r_const])
        c.op("dve", lambda e: e.tensor_copy(out=ident[:], in_=identf[:]), reads=[r_const], writes=[r_const])
        c.op("dve", lambda e: e.memset(ones[:], 1.0), writes=[r_const])

        def cut(k):
            if _LIM.get("cut") == k:
                c.muted = True

        wpool = {"buf": [], "res": [], "n": 0}
        wctr = [0]

        def make_wpool(es_, n):
            tag = "%d" % len(wpool.setdefault("gen", []))
            wpool["gen"].append(n)
            wpool["buf"] = [c.sb(es_, "wbuf%s_%d" % (tag, i), [128, 16, 512], BF16) for i in range(n)]
            wpool["res"] = [Res() for _ in range(n)]
            wpool["n"] = n
            wctr[0] = 0

        class WStream:
            def __init__(self, blocks):
                self.blocks = list(blocks)
                self.issued = 0
                self.pos = 0
                self.base = wctr[0]

            def _issue(self):
                name, blk = self.blocks[self.issued]
                i = (self.base + self.issued) % wpool["n"]
                c.dma("sp", wpool["buf"][i][:], wscr[name][blk], reads=[wscr_res[name][blk]],
                      writes=[wpool["res"][i]])
                self.issued += 1

            def get(self):
                while self.issued < len(self.blocks) and self.issued < self.pos + wpool["n"]:
                    self._issue()
                i = (self.base + self.pos) % wpool["n"]
                self.pos += 1
                wctr[0] = self.base + self.pos
                return wpool["buf"][i], wpool["res"][i]

        def load_gain(es_, name, ap, width=D):
            t = c.sb(es_, "g_" + name, [128, width], F32)
            r = Res()
            c.dma("sp", t[:], ap[0:1, :].partition_broadcast(128), writes=[r])
            return t, r

        bankctr = [0]

        def bank(lo, hi):
            n = hi - lo
            i = lo + bankctr[0] % n
            bankctr[0] += 1
            return i

        NSM = 4
        sm = [c.sb(es, "sm%d" % i, [128, 8], F32) for i in range(NSM)]
        r_sm = [Res() for _ in range(NSM)]
        smctr = [0]
        junk = c.sb(es, "junk", [128, D], BF16)
        r_junk = Res()

        def rms_h(x_t, r_x, P, gain_t, r_gain, h_t, r_h):
            i = smctr[0] % NSM
            smctr[0] += 1
            s, rs = sm[i], r_sm[i]
            c.op("act", lambda e: e.activation(out=junk[:P, :], in_=x_t[:P, :], func=AF.Square,
                                               accum_out=s[:P, 0:1]), reads=[r_x], writes=[r_junk, rs])
            c.op("act", lambda e: e.activation(out=s[:P, 1:2], in_=s[:P, 0:1], func=AF.Sqrt,
                                               scale=1.0 / D, bias=epsc[:P, 0:1]), reads=[rs, r_const], writes=[rs])
            c.op("dve", lambda e: e.reciprocal(out=s[:P, 2:3], in_=s[:P, 1:2]), reads=[rs], writes=[rs])
            c.op("dve", lambda e: e.scalar_tensor_tensor(out=h_t[:P, :], in0=x_t[:P, :], scalar=s[:P, 2:3],
                                                         in1=gain_t[:P, :], op0=ALU.mult, op1=ALU.mult),
                 reads=[r_x, rs, r_gain], writes=[r_h])

        epsc = c.sb(es, "epsc", [128, 1], F32)
        c.op("dve", lambda e: e.memset(epsc[:], EPS), writes=[r_const])

        def transpose_to(h_t, r_h, P, nchunk, dst_fn, r_dst, evac="act"):
            for c0 in range(0, nchunk, 8):
                c1 = min(nchunk, c0 + 8)
                b = bank(0, 2)
                pv = pbb[b][:, 0:(c1 - c0) * 128].rearrange("p (a t) -> p a t", t=128)

                def f(e, c0=c0, c1=c1, pv=pv):
                    inst = None
                    for k in range(c0, c1):
                        inst = e.transpose(out=pv[:, k - c0, 0:P], in_=h_t[:P, k * 128:(k + 1) * 128],
                                           identity=ident[:P, :P])
                    return inst
                c.op("pe", f, reads=[r_h, r_const], writes=[rpb[b]])
                dst = dst_fn(c0, c1)
                if evac == "act":
                    c.op("act", lambda e, dst=dst, pv=pv: e.activation(out=dst, in_=pv[:, :, 0:P], func=AF.Copy),
                         reads=[rpb[b]], writes=[r_dst])
                else:
                    c.op("dve", lambda e, dst=dst, pv=pv: e.tensor_copy(out=dst, in_=pv[:, :, 0:P]),
                         reads=[rpb[b]], writes=[r_dst])

        def mm_tok(hT, r_hT, t0, P, wt, r_w, b, ncol=512, c0=0):
            def f(e):
                inst = None
                for kc in range(16):
                    inst = e.matmul(pb[b][:P, 0:ncol], lhsT=hT[:, kc, t0:t0 + P], rhs=wt[:, kc, c0:c0 + ncol],
                                    start=(kc == 0), stop=(kc == 15))
                return inst
            c.op("pe", f, reads=[r_hT, r_w], writes=[rpb[b]])

        def mm_feat(hT, r_hT, t0, ntok, wt, r_w, ct, b):
            def f(e):
                inst = None
                for kc in range(16):
                    inst = e.matmul(pb[b][:, 0:ntok], lhsT=wt[:, kc, ct * 128:(ct + 1) * 128],
                                    rhs=hT[:, kc, t0:t0 + ntok], start=(kc == 0), stop=(kc == 15))
                return inst
            c.op("pe", f, reads=[r_hT, r_w], writes=[rpb[b]])

        with contextlib.suppress(_SkipPhase), contextlib.ExitStack() as es0:
            if 0 not in _PH:
                raise _SkipPhase()
            NST = 3
            stf = [c.sb(es0, "stf%d" % i, [128, 16, 512], F32) for i in range(NST)]
            stb = [c.sb(es0, "stb%d" % i, [128, 16, 512], BF16) for i in range(NST)]
            r_stf = [Res() for _ in range(NST)]
            r_stb = [Res() for _ in range(NST)]
            n = 0
            wlist = [("w_mk", wsq["w_mk"]), ("w_mv", wsq["w_mv"]), ("w_in", w_in), ("w_out", wsq["w_out"]),
                     ("w_mq", wsq["w_mq"]), ("w_mo", wsq["w_mo"]), ("peer_wq", wsq["peer_wq"])]
            for name, W in wlist:
                Wv = W.rearrange("(kc p) n -> p kc n", p=128)
                for blk in range(WBLK[name]):
                    i = n % NST
                    c.dma("sp", stf[i][:], Wv[:, :, blk * 512:(blk + 1) * 512], writes=[r_stf[i]])
                    if n % 2 == 0:
                        c.op("dve", lambda e, i=i: e.tensor_copy(out=stb[i][:], in_=stf[i][:]),
                             reads=[r_stf[i]], writes=[r_stb[i]])
                    else:
                        c.op("pool", lambda e, i=i: e.tensor_copy(out=stb[i][:], in_=stf[i][:]),
                             reads=[r_stf[i]], writes=[r_stb[i]])
                    c.dma("act", wscr[name][blk], stb[i][:], reads=[r_stb[i]], writes=[wscr_res[name][blk]])
                    n += 1
            if 5 in _PH or 6 in _PH:
                for src, dst in ((peer_u, tabu), (peer_v, tabv)):
                    sv = src.rearrange("(c p r) d -> c p (r d)", p=128, r=4)
                    dv = dst.rearrange("(c p r) d -> c p (r d)", p=128, r=4)
                    for ci in range(32):
                        i = n % NST
                        c.dma("sp", stf[i][:].rearrange("p a b -> p (a b)"), sv[ci], writes=[r_stf[i]])
                        if n % 2 == 0:
                            c.op("dve", lambda e, i=i: e.tensor_copy(out=stb[i][:], in_=stf[i][:]),
                                 reads=[r_stf[i]], writes=[r_stb[i]])
                        else:
                            c.op("pool", lambda e, i=i: e.tensor_copy(out=stb[i][:], in_=stf[i][:]),
                                 reads=[r_stf[i]], writes=[r_stb[i]])
                        c.dma("act", dv[ci], stb[i][:].rearrange("p a b -> p (a b)"), reads=[r_stb[i]], writes=[r_tab])
                        n += 1
            c.barrier()

        mkT = c.sb(es, "mkT", [128, 16, 256], BF16)
        mv_bf = c.sb(es, "mv_bf", [128, 2, D], BF16)
        r_mkT = Res()
        r_mvbf = Res()

        with contextlib.suppress(_SkipPhase), contextlib.ExitStack() as es1:
            if 1 not in _PH:
                raise _SkipPhase()
            make_wpool(es1, 3)
            g_mt, r_gmt = load_gain(es1, "memtok", gains["norm_memtok"])
            mT = c.sb(es1, "mT", [128, 16, 256], BF16)
            r_mT = Res()
            xm = c.sb(es1, "xm", [128, D], F32)
            r_xm = Res()
            hm = c.sb(es1, "hm1", [128, D], BF16)
            r_hm = Res()
            mk_bf = c.sb(es1, "mk_bf", [128, 2, D], BF16)
            r_mkbf = Res()
            kvf = [c.sb(es1, "kvf%d" % i, [128, D], F32) for i in range(2)]
            r_kvf = [Res() for _ in range(2)]
            for mt in range(2):
                c.dma("sp", xm[:], mem[mt * 128:(mt + 1) * 128, :], writes=[r_xm])
                rms_h(xm, r_xm, 128, g_mt, r_gmt, hm, r_hm)
                transpose_to(hm, r_hm, 128, 16, lambda c0, c1, mt=mt: mT[:, c0:c1, mt * 128:(mt + 1) * 128], r_mT)
            n = 0
            for kv, name in enumerate(("w_mk", "w_mv")):
                bf = mk_bf if kv == 0 else mv_bf
                r_bf = r_mkbf if kv == 0 else r_mvbf
                for mt in range(2):
                    i = n % 2
                    n += 1
                    ws = WStream([(name, blk) for blk in range(4)])
                    for blk in range(4):
                        wt, r_w = ws.get()
                        b = bank(2, 6)
                        mm_tok(mT, r_mT, mt * 128, 128, wt, r_w, b)
                        c.op("act", lambda e, i=i, blk=blk, b=b: e.activation(
                            out=kvf[i][:, blk * 512:(blk + 1) * 512], in_=pb[b][:, :], func=AF.Copy),
                            reads=[rpb[b]], writes=[r_kvf[i]])
                    c.op("dve", lambda e, i=i, mt=mt, bf=bf: e.tensor_copy(out=bf[:, mt, :], in_=kvf[i][:]),
                         reads=[r_kvf[i]], writes=[r_bf])
                    c.dma("sp", memkv[mt * 128:(mt + 1) * 128, kv, :], kvf[i][:], reads=[r_kvf[i]])
            for mt in range(2):
                transpose_to(mk_bf[:, mt, :], r_mkbf, 128, 16,
                             lambda c0, c1, mt=mt: mkT[:, c0:c1, mt * 128:(mt + 1) * 128], r_mkT)
            c.barrier()

        G = 2
        GT = G * 128

        def gmlp_tile(es_, P, gv_j, r_gv, gu_j, r_gu, sga_j, r_sga, Wmix, r_W, bs, r_bs, lng, lnb, r_ln,
                      tmp, mA, r_mA, vln_out=None, r_vln=None):
            st, mvv, vt, vlnb, gs = tmp["st"], tmp["mvv"], tmp["vt"], tmp["vlnb"], tmp["gs"]
            r_t = tmp["r"]
            c.op("dve", lambda e: e.bn_stats(out=st[:P, 0, :], in_=gv_j[:P, 0:512]), reads=[r_gv], writes=[r_t])
            c.op("dve", lambda e: e.bn_stats(out=st[:P, 1, :], in_=gv_j[:P, 512:1024]), reads=[r_gv], writes=[r_t])
            c.op("dve", lambda e: e.bn_aggr(out=mvv[:P, 0:2], in_=st[:P].rearrange("p a b -> p (a b)")),
                 reads=[r_t], writes=[r_t])
            c.op("act", lambda e: e.activation(out=mvv[:P, 2:3], in_=mvv[:P, 1:2], func=AF.Sqrt,
                                               bias=epsc[:P, 0:1], scale=1.0), reads=[r_t, r_const], writes=[r_t])
            c.op("dve", lambda e: e.reciprocal(out=mvv[:P, 3:4], in_=mvv[:P, 2:3]), reads=[r_t], writes=[r_t])
            c.op("dve", lambda e: e.tensor_scalar(out=vt[:P, :], in0=gv_j[:P, :], scalar1=mvv[:P, 0:1],
                                                  scalar2=mvv[:P, 3:4], op0=ALU.subtract, op1=ALU.mult),
                 reads=[r_gv, r_t], writes=[r_t])
            c.op("dve", lambda e: e.tensor_tensor(out=vt[:P, :], in0=vt[:P, :], in1=lng[:P, :], op=ALU.mult),
                 reads=[r_t, r_ln], writes=[r_t])
            if vln_out is None:
                c.op("pool", lambda e: e.tensor_tensor(out=vlnb[:P, :], in0=vt[:P, :], in1=lnb[:P, :], op=ALU.add),
                     reads=[r_t, r_ln], writes=[r_t])
            else:
                c.op("pool", lambda e: e.tensor_tensor(out=vln_out[:P, :], in0=vt[:P, :], in1=lnb[:P, :],
                                                       op=ALU.add), reads=[r_t, r_ln], writes=[r_vln])
                c.op("act", lambda e: e.activation(out=vlnb[:P, :], in_=vln_out[:P, :], func=AF.Copy),
                     reads=[r_vln], writes=[r_t])

            def f(e):
                inst = None
                for g in range(8):
                    inst = e.matmul(pb[6 + g // 4][:P, (g % 4) * 128:(g % 4 + 1) * 128], lhsT=Wmix[:P, g, :P],
                                    rhs=vlnb[:P, g * 128:(g + 1) * 128], start=True, stop=True)
                return inst
            c.op("pe", f, reads=[r_t, r_W], writes=[rpb[6], rpb[7]])
            for half in range(2):
                c.op("dve", lambda e, half=half: e.tensor_tensor(
                    out=vt[:P, half * 512:(half + 1) * 512].rearrange("p (g d) -> p g d", d=128),
                    in0=pb[6 + half][:P, :].rearrange("p (g d) -> p g d", d=128),
                    in1=bs[:P, half * 4:half * 4 + 4].unsqueeze(2).to_broadcast([P, 4, 128]), op=ALU.add),
                    reads=[rpb[6 + half], r_bs], writes=[r_t])
            c.op("pool", lambda e: e.tensor_tensor(out=gs[:P, :], in0=gu_j[:P, :], in1=sga_j[:P, :], op=ALU.mult),
                 reads=[r_gu, r_sga], writes=[r_t])
            c.op("dve", lambda e: e.tensor_tensor(out=mA[:P, :], in0=vt[:P, :], in1=gs[:P, :], op=ALU.mult),
                 reads=[r_t], writes=[r_mA])

        def gmlp_tmp(es_, tag):
            return {"st": c.sb(es_, "g_st" + tag, [128, 2, 6], F32), "mvv": c.sb(es_, "g_mvv" + tag, [128, 4], F32),
                    "vt": c.sb(es_, "g_vt" + tag, [128, 1024], F32), "vlnb": c.sb(es_, "g_vlnb" + tag, [128, 1024], BF16),
                    "gs": c.sb(es_, "g_gs" + tag, [128, 1024], BF16), "r": Res()}

        with contextlib.suppress(_SkipPhase), contextlib.ExitStack() as es2:
            if 2 not in _PH:
                raise _SkipPhase()
            make_wpool(es2, 3)
            g_mix, r_gmix = load_gain(es2, "mix", gains["norm_mix"])
            lng, r_ln = load_gain(es2, "lng", sgu_ln_g, 1024)
            lnb = c.sb(es2, "g_lnb", [128, 1024], F32)
            c.dma("sp", lnb[:], sgu_ln_b[0:1, :].partition_broadcast(128), writes=[r_ln])
            wsf = c.sb(es2, "wsf", [128, 8, 128], F32)
            WsT = c.sb(es2, "WsT", [128, 8, 128], BF16)
            r_ws = Res()
            c.dma("sp", wsf[:], sgu_wT.rearrange("g j i -> j g i"), writes=[r_ws])
            c.op("pool", lambda e: e.affine_select(out=wsf[:], in_=wsf[:], pattern=[[0, 8], [1, 128]],
                                                   compare_op=ALU.is_ge, fill=0.0, base=0, channel_multiplier=-1),
                 reads=[r_ws], writes=[r_ws])
            c.op("dve", lambda e: e.tensor_copy(out=WsT[:], in_=wsf[:]), reads=[r_ws], writes=[r_ws])
            bsT = c.sb(es2, "bsT", [128, 8], F32)
            r_bs = Res()
            c.dma("sp", bsT[:], sgu_bT[:, :], writes=[r_bs])

            hT = [c.sb(es2, "hT%d" % i, [128, 16, GT], BF16) for i in range(2)]
            r_hT = [Res() for _ in range(2)]
            xt = [c.sb(es2, "xt%d" % i, [128, D], F32) for i in range(2)]
            r_xt = [Res() for _ in range(2)]
            hb = [c.sb(es2, "hb%d" % i, [128, D], BF16) for i in range(2)]
            r_hb = [Res() for _ in range(2)]
            gv = c.sb(es2, "gv", [128, G, 1024], F32)
            gu = c.sb(es2, "gu", [128, G, 1024], BF16)
            sga = c.sb(es2, "sga", [128, G, 1024], BF16)
            r_gv = [Res() for _ in range(G)]
            r_gu = [Res() for _ in range(G)]
            r_sga = [Res() for _ in range(G)]
            gtmp = gmlp_tmp(es2, "p")
            mA = c.sb(es2, "mA", [128, 1024], BF16)
            r_mA = Res()
            mst = [c.sb(es2, "mst%d" % i, [128, 8, 128], BF16) for i in range(2)]
            r_mst = [Res() for _ in range(2)]
            fst = [c.sb(es2, "fst%d" % i, [128, 4, GT], BF16) for i in range(2)]
            r_fst = [Res() for _ in range(2)]
            kvo = [c.sb(es2, "kvo%d" % i, [128, 512], F32) for i in range(2)]
            r_kvo = [Res() for _ in range(2)]
            ctr = {"x": 0, "f": 0, "k": 0, "m": 0}

            groups = [("h", g) for g in range(NT // G)][:_LIM["groups"]] + [("o", g) for g in range(NT // G)][:_LIM["groups"]]

            def prep_group(gidx):
                kind, g = groups[gidx]
                src = xh if kind == "h" else xo
                hbuf = gidx % 2
                for j in range(G):
                    i = ctr["x"] % 2
                    ctr["x"] += 1
                    t0 = g * GT + j * 128
                    c.dma("sp", xt[i][:], src[t0:t0 + 128, :], writes=[r_xt[i]])
                    rms_h(xt[i], r_xt[i], 128, g_mix, r_gmix, hb[i], r_hb[i])
                    transpose_to(hb[i], r_hb[i], 128, 16,
                                 lambda c0, c1, j=j, hbuf=hbuf: hT[hbuf][:, c0:c1, j * 128:(j + 1) * 128], r_hT[hbuf])

            def feat_block(ws, hbuf, dst, r_dst_list, tcol0, sigmoid=False):
                wt, r_w = ws.get()
                i = ctr["f"] % 2
                ctr["f"] += 1
                for ct in range(4):
                    b = bank(2, 6)
                    mm_feat(hT[hbuf], r_hT[hbuf], 0, GT, wt, r_w, ct, b)
                    if sigmoid:
                        c.op("act", lambda e, b=b, ct=ct, i=i: e.activation(out=fst[i][:, ct, :], in_=pb[b][:, 0:GT],
                                                                          func=AF.Sigmoid),
                             reads=[rpb[b]], writes=[r_fst[i]])
                    elif ct % 2 == 0:
                        c.op("act", lambda e, b=b, ct=ct, i=i: e.activation(out=fst[i][:, ct, :], in_=pb[b][:, 0:GT],
                                                                          func=AF.Copy),
                             reads=[rpb[b]], writes=[r_fst[i]])
                    else:
                        c.op("dve", lambda e, b=b, ct=ct, i=i: e.tensor_copy(out=fst[i][:, ct, :], in_=pb[b][:, 0:GT]),
                             reads=[rpb[b]], writes=[r_fst[i]])
                c.dma("act", dst[:, :, tcol0:tcol0 + GT].rearrange("g p t -> p g t"), fst[i][:],
                      reads=[r_fst[i]], writes=r_dst_list)

            prep_group(0)
            for gidx, (kind, g) in enumerate(groups):
                hbuf = gidx % 2
                if kind == "h":
                    ws = WStream([("w_in", b) for b in (10, 11, 12, 13)])
                    for bi, blk in enumerate((10, 11, 12, 13)):
                        dst = kTs if blk < 12 else vTs
                        rr = r_kTs if blk < 12 else r_vTs
                        h0 = (blk % 2) * 4
                        feat_block(ws, hbuf, dst[h0:h0 + 4], rr[h0:h0 + 4], g * GT)
                        if bi == 0 and gidx + 1 < len(groups):
                            prep_group(gidx + 1)
                    continue
                order = [2, 3, 0, 1, 14, 15, 10, 11, 12, 13, 4, 5, 6, 7, 8, 9, 10, 11, 12, 13, 16, 17]
                ws = WStream([("w_in", b) for b in order])
                for bi, blk in enumerate(order[:10]):
                    wt, r_w = ws.get()
                    for j in range(G):
                        b = bank(2, 6)
                        mm_tok(hT[hbuf], r_hT[hbuf], j * 128, 128, wt, r_w, b)
                        if blk in (2, 3):
                            c.op("act", lambda e, b=b, j=j, blk=blk: e.activation(
                                out=gv[:, j, (blk - 2) * 512:(blk - 1) * 512], in_=pb[b][:, :], func=AF.Gelu_apprx_tanh),
                                reads=[rpb[b]], writes=[r_gv[j]])
                        elif blk in (0, 1):
                            c.op("act", lambda e, b=b, j=j, blk=blk: e.activation(
                                out=gu[:, j, blk * 512:(blk + 1) * 512], in_=pb[b][:, :], func=AF.Gelu_apprx_tanh),
                                reads=[rpb[b]], writes=[r_gu[j]])
                        elif blk in (14, 15):
                            c.op("act", lambda e, b=b, j=j, blk=blk: e.activation(
                                out=sga[:, j, (blk - 14) * 512:(blk - 13) * 512], in_=pb[b][:, :], func=AF.Sigmoid),
                                reads=[rpb[b]], writes=[r_sga[j]])
                        else:
                            i = ctr["k"] % 2
                            ctr["k"] += 1
                            c.op("dve", lambda e, b=b, i=i: e.tensor_copy(out=kvo[i][:], in_=pb[b][:, :]),
                                 reads=[rpb[b]], writes=[r_kvo[i]])
                            t0 = g * GT + j * 128
                            kv = 0 if blk < 12 else 1
                            half = blk % 2
                            c.dma("act", win_p[t0:t0 + 128, kv, half * 512:(half + 1) * 512], kvo[i][:],
                                  reads=[r_kvo[i]])
                    if bi == 0 and gidx + 1 < len(groups):
                        prep_group(gidx + 1)
                    if bi == 5:
                        for j in range(G):
                            gmlp_tile(es2, 128, gv[:, j, :], r_gv[j], gu[:, j, :], r_gu[j], sga[:, j, :], r_sga[j],
                                      WsT, r_ws, bsT, r_bs, lng, lnb, r_ln, gtmp, mA, r_mA)
                            i = ctr["m"] % 2
                            ctr["m"] += 1
                            transpose_to(mA, r_mA, 128, 8, lambda c0, c1, i=i: mst[i][:, c0:c1, :], r_mst[i],
                                         evac="dve")
                            t0 = g * GT + j * 128
                            c.dma("act", mTs[0:8, :, t0:t0 + 128].rearrange("g p t -> p g t"), mst[i][:],
                                  reads=[r_mst[i]], writes=r_mTs[0:8])
                for blk in order[10:]:
                    if 4 <= blk <= 9:
                        gh0 = ((blk - 4) // 2) * 8 + ((blk - 4) % 2) * 4
                        feat_block(ws, hbuf, qTs[gh0:gh0 + 4], r_qTs[gh0:gh0 + 4], g * GT)
                    elif blk in (10, 11):
                        h0 = (blk % 2) * 4
                        feat_block(ws, hbuf, kTs[h0:h0 + 4], r_kTs[h0:h0 + 4], TOK + g * GT)
                    elif blk in (12, 13):
                        h0 = (blk % 2) * 4
                        feat_block(ws, hbuf, vTs[h0:h0 + 4], r_vTs[h0:h0 + 4], TOK + g * GT)
                    else:
                        h0 = (blk % 2) * 4
                        feat_block(ws, hbuf, gbTs[h0:h0 + 4], r_gbTs[h0:h0 + 4], g * GT, sigmoid=True)
            c.barrier()

        with contextlib.suppress(_SkipPhase), contextlib.ExitStack() as es3:
            if 3 not in _PH:
                raise _SkipPhase()
            EBT = c.sb(es3, "EBT", [128, 24, 256], BF16)
            r_EB = Res()
            bm = c.sb(es3, "bm", [128, 256], F32)
            r_bm = Res()
            c.dma("sp", bm[:], bandmask.rearrange("p a b -> p (a b)"), writes=[r_bm])
            btmp = [c.sb(es3, "btmp%d" % i, [128, 256], F32) for i in range(2)]
            r_btmp = [Res() for _ in range(2)]
            for gh in range(24):
                i = gh % 2
                c.dma("sp", btmp[i][:], biasT[gh].rearrange("p a b -> p (a b)"), writes=[r_btmp[i]])
                c.op("act", lambda e, i=i: e.activation(out=btmp[i][:], in_=btmp[i][:], func=AF.Exp),
                     reads=[r_btmp[i]], writes=[r_btmp[i]])
                c.op("dve", lambda e, i=i, gh=gh: e.tensor_tensor(out=EBT[:, gh, :], in0=btmp[i][:], in1=bm[:],
                                                                  op=ALU.mult),
                     reads=[r_btmp[i], r_bm], writes=[r_EB])
            pf = c.sb(es3, "pf", [128, 1], F32)
            r_pf = Res()
            c.dma("sp", pf[:], pflag[:, :], writes=[r_pf])

            kT = [c.sb(es3, "kT%d" % i, [128, 2 * TOK], BF16) for i in range(2)]
            vT = [c.sb(es3, "vT%d" % i, [128, 2 * TOK], BF16) for i in range(2)]
            qT3 = [c.sb(es3, "qT3%d" % i, [128, 3, TOK], BF16) for i in range(2)]
            gbT = [c.sb(es3, "gbT%d" % i, [128, TOK], BF16) for i in range(2)]
            r_hd = [Res() for _ in range(2)]
            acc = c.sb(es3, "acc", [128, 2, TOK], F32)
            r_acc = Res()
            ybT = c.sb(es3, "ybT", [128, TOK], BF16)
            r_ybT = Res()
            NVP = 4
            Vp = [c.sb(es3, "Vp%d" % i, [128, 128], BF16) for i in range(NVP)]
            r_Vp = [Res() for _ in range(NVP)]
            pTf = [c.sb(es3, "pTf%d" % i, [128, 256], BF16) for i in range(2)]
            pT = [c.sb(es3, "pT%d" % i, [128, 256], BF16) for i in range(2)]
            r_pTf = [Res() for _ in range(2)]
            r_pT = [Res() for _ in range(2)]
            qTs4 = qTs.rearrange("(g h) p t -> g h p t", h=8)
            actr = {"vp": 0, "u": 0}

            def load_head(h):
                i = h % 2
                c.dma("sp", kT[i][:], kTs[h], reads=[r_kTs[h]], writes=[r_hd[i]])
                c.dma("sp", vT[i][:], vTs[h], reads=[r_vTs[h]], writes=[r_hd[i]])
                c.dma("sp", qT3[i][:], qTs4[:, h].rearrange("g p t -> p g t"),
                      reads=[r_qTs[h], r_qTs[8 + h], r_qTs[16 + h]], writes=[r_hd[i]])
                c.dma("sp", gbT[i][:], gbTs[h], reads=[r_gbTs[h]], writes=[r_hd[i]])

            def make_vp(i, dil, a0, r):
                k = actr["vp"] % NVP
                actr["vp"] += 1
                b = bank(2, 4)
                vv = vT[i][:].rearrange("p (a b) -> p a b", b=dil)
                c.op("pe", lambda e: e.transpose(out=pbb[b][:, 0:128], in_=vv[:, a0:a0 + 128, r], identity=ident[:]),
                     reads=[r_hd[i], r_const], writes=[rpb[b]])
                c.op("dve", lambda e: e.tensor_copy(out=Vp[k][:], in_=pbb[b][:, 0:128]), reads=[rpb[b]],
                     writes=[r_Vp[k]])
                return k

            load_head(0)
            NH = 8 if _LIM["heads"] is None else _LIM["heads"]
            for h in range(NH):
                i = h % 2
                if h + 1 < NH:
                    load_head(h + 1)
                for gi, dil in enumerate(DILS):
                    span = 128 * dil
                    nbk = TOK // span
                    kv_ = kT[i][:].rearrange("p (a b) -> p a b", b=dil)
                    qv_ = qT3[i][:, gi, :].rearrange("p (a b) -> p a b", b=dil)
                    accv = acc[:].rearrange("p s (a b) -> p s a b", b=dil)
                    for r in range(dil):
                        kprev = make_vp(i, dil, (TOK - span) // dil, r)
                        for bb in range(nbk):
                            a_own = (TOK + bb * span) // dil
                            kcur = make_vp(i, dil, a_own, r)
                            u = actr["u"] % 2
                            actr["u"] += 1
                            b_s = bank(0, 2)
                            a_prev = a_own - 128

                            def fs(e, a_prev=a_prev, bb=bb, r=r, b_s=b_s, kv_=kv_, qv_=qv_):
                                inst = None
                                for jt in range(2):
                                    inst = e.matmul(pb[b_s][:, jt * 128:(jt + 1) * 128],
                                                    lhsT=kv_[:, a_prev + jt * 128:a_prev + (jt + 1) * 128, r],
                                                    rhs=qv_[:, bb * 128:(bb + 1) * 128, r], start=True, stop=True)
                                return inst
                            c.op("pe", fs, reads=[r_hd[i]], writes=[rpb[b_s]])
                            c.op("act", lambda e, u=u, b_s=b_s: e.activation(out=pTf[u][:], in_=pb[b_s][:, 0:256],
                                                                            func=AF.Exp, scale=ATT_SCALE),
                                 reads=[rpb[b_s]], writes=[r_pTf[u]])
                            c.op("pool", lambda e, u=u, gi=gi, h=h: e.tensor_tensor(
                                out=pT[u][:], in0=pTf[u][:], in1=EBT[:, gi * 8 + h, :], op=ALU.mult),
                                reads=[r_pTf[u], r_EB], writes=[r_pT[u]])
                            if bb == 0:
                                c.op("pool", lambda e, u=u: e.tensor_scalar(
                                    out=pT[u][:, 0:128], in0=pT[u][:, 0:128], scalar1=pf[:, 0:1], scalar2=None,
                                    op0=ALU.mult), reads=[r_pT[u], r_pf], writes=[r_pT[u]])
                            b_o = bank(4, 6)

                            def fo(e, u=u, b_o=b_o, kprev=kprev, kcur=kcur):
                                e.matmul(pb[b_o][:, 0:128], lhsT=Vp[kprev][:], rhs=pT[u][:, 0:128], start=True, stop=False)
                                e.matmul(pb[b_o][:, 0:128], lhsT=Vp[kcur][:], rhs=pT[u][:, 128:256], start=False, stop=True)
                                e.matmul(pb[b_o][:, 128:256], lhsT=ones[:], rhs=pT[u][:, 0:128], start=True, stop=False)
                                return e.matmul(pb[b_o][:, 128:256], lhsT=ones[:], rhs=pT[u][:, 128:256],
                                                start=False, stop=True)
                            c.op("pe", fo, reads=[r_pT[u], r_Vp[kprev], r_Vp[kcur], r_const], writes=[rpb[b_o]])
                            av = accv[:, :, bb * 128:(bb + 1) * 128, r]
                            pv = pb[b_o][:, 0:256].rearrange("p (s t) -> p s t", s=2)
                            if gi == 0:
                                c.op("dve", lambda e, av=av, pv=pv: e.tensor_copy(out=av, in_=pv),
                                     reads=[rpb[b_o]], writes=[r_acc])
                            else:
                                c.op("dve", lambda e, av=av, pv=pv: e.tensor_tensor(out=av, in0=pv, in1=av, op=ALU.add),
                                     reads=[rpb[b_o], r_acc], writes=[r_acc])
                            kprev = kcur
                c.op("dve", lambda e: e.reciprocal(out=acc[:, 1, :], in_=acc[:, 1, :]), reads=[r_acc], writes=[r_acc])
                c.op("dve", lambda e: e.tensor_tensor(out=acc[:, 0, :], in0=acc[:, 0, :], in1=acc[:, 1, :], op=ALU.mult),
                     reads=[r_acc], writes=[r_acc])
                c.op("pool", lambda e, i=i: e.tensor_tensor(out=ybT[:], in0=acc[:, 0, :], in1=gbT[i][:], op=ALU.mult),
                     reads=[r_acc, r_hd[i]], writes=[r_ybT])
                c.dma("sp", mTs[8 + h], ybT[:], reads=[r_ybT], writes=[r_mTs[8 + h]])
            c.barrier()


        PS = 128
        mTg_s = c.sb(es, "mTg_s", [128, 16, PS], BF16)
        r_mTgs = Res()
        c.op("pool", lambda e: e.memset(mTg_s[:], 0.0), writes=[r_mTgs])
        with contextlib.suppress(_SkipPhase), contextlib.ExitStack() as ess:
            if 4 not in _PH:
                raise _SkipPhase()
            make_wpool(ess, 2)
            cut(10)
            g_mix, r_gmix = load_gain(ess, "mix_s", gains["norm_mix"])
            lng, r_ln = load_gain(ess, "lng_s", sgu_ln_g, 1024)
            lnb = c.sb(ess, "g_lnb_s", [128, 1024], F32)
            c.dma("sp", lnb[:], sgu_ln_b[0:1, :].partition_broadcast(128), writes=[r_ln])
            wsf = c.sb(ess, "wsf_s", [128, 8, 128], F32)
            WsTs = c.sb(ess, "WsTs", [128, 8, 128], BF16)
            r_ws = Res()
            c.dma("sp", wsf[:], sgu_wTs.rearrange("g j i -> j g i"), writes=[r_ws])
            c.op("pool", lambda e: e.affine_select(out=wsf[:], in_=wsf[:], pattern=[[0, 8], [1, 128]],
                                                   compare_op=ALU.is_ge, fill=0.0, base=0, channel_multiplier=-1),
                 reads=[r_ws], writes=[r_ws])
            c.op("dve", lambda e: e.tensor_copy(out=WsTs[:], in_=wsf[:]), reads=[r_ws], writes=[r_ws])
            bsTs = c.sb(ess, "bsTs", [128, 8], F32)
            r_bs = Res()
            c.dma("sp", bsTs[:], sgu_bTs[:, :], writes=[r_bs])
            cut(11)

            xts = c.sb(ess, "xts", [128, D], F32)
            hbs = c.sb(ess, "hbs", [128, D], BF16)
            hTs = c.sb(ess, "hTs", [128, 16, PS], BF16)
            r_xts, r_hbs, r_hTs = Res(), Res(), Res()
            c.op("pool", lambda e: e.memset(xts[:], 0.0), writes=[r_xts])
            c.dma("sp", xts[:SP, :], xs[:, :], writes=[r_xts])
            cut(12)
            rms_h(xts, r_xts, PS, g_mix, r_gmix, hbs, r_hbs)
            cut(13)
            transpose_to(hbs, r_hbs, PS, 16, lambda c0, c1: hTs[:, c0:c1, :], r_hTs)
            cut(1)

            gv_s = c.sb(ess, "gv_s", [128, 1024], F32)
            gu_s = c.sb(ess, "gu_s", [128, 1024], BF16)
            sga_s = c.sb(ess, "sga_s", [128, 1024], BF16)
            sgb_s = c.sb(ess, "sgb_s", [128, 1024], BF16)
            q_s = c.sb(ess, "q_s", [128, 3072], BF16)
            kf_s = c.sb(ess, "kf_s", [128, 1024], F32)
            vf_s = c.sb(ess, "vf_s", [128, 1024], F32)
            k_sb = c.sb(ess, "k_sb", [128, 1024], BF16)
            vn = c.sb(ess, "vn", [128, 1024], BF16)
            r_gvs, r_gus, r_sgas, r_sgbs, r_qs, r_kfs, r_vfs, r_ksb, r_vn = (Res() for _ in range(9))
            ws = WStream([("w_in", b) for b in range(18)])
            for blk in range(18):
                cut(20 + blk)
                wt, r_w = ws.get()
                b = bank(2, 6)
                mm_tok(hTs, r_hTs, 0, PS, wt, r_w, b)
                src = pb[b][:, :]
                if blk < 2:
                    c.op("act", lambda e, blk=blk, src=src: e.activation(out=gu_s[:, blk * 512:(blk + 1) * 512], in_=src,
                                                                        func=AF.Gelu_apprx_tanh), reads=[rpb[b]], writes=[r_gus])
                elif blk < 4:
                    c.op("act", lambda e, blk=blk, src=src: e.activation(out=gv_s[:, (blk - 2) * 512:(blk - 1) * 512], in_=src,
                                                                        func=AF.Gelu_apprx_tanh), reads=[rpb[b]], writes=[r_gvs])
                elif blk < 10:
                    c.op("act", lambda e, blk=blk, src=src: e.activation(out=q_s[:, (blk - 4) * 512:(blk - 3) * 512], in_=src,
                                                                        func=AF.Copy), reads=[rpb[b]], writes=[r_qs])
                elif blk < 12:
                    c.op("act", lambda e, blk=blk, src=src: e.activation(out=kf_s[:, (blk - 10) * 512:(blk - 9) * 512], in_=src,
                                                                        func=AF.Copy), reads=[rpb[b]], writes=[r_kfs])
                    c.op("dve", lambda e, blk=blk: e.tensor_copy(out=k_sb[:, (blk - 10) * 512:(blk - 9) * 512],
                                                                 in_=kf_s[:, (blk - 10) * 512:(blk - 9) * 512]),
                         reads=[r_kfs], writes=[r_ksb])
                elif blk < 14:
                    c.op("act", lambda e, blk=blk, src=src: e.activation(out=vf_s[:, (blk - 12) * 512:(blk - 11) * 512], in_=src,
                                                                        func=AF.Copy), reads=[rpb[b]], writes=[r_vfs])
                    c.op("dve", lambda e, blk=blk: e.tensor_copy(out=vn[:, (blk - 12) * 512:(blk - 11) * 512],
                                                                 in_=vf_s[:, (blk - 12) * 512:(blk - 11) * 512]),
                         reads=[r_vfs], writes=[r_vn])
                elif blk < 16:
                    c.op("act", lambda e, blk=blk, src=src: e.activation(out=sga_s[:, (blk - 14) * 512:(blk - 13) * 512], in_=src,
                                                                        func=AF.Sigmoid), reads=[rpb[b]], writes=[r_sgas])
                else:
                    c.op("act", lambda e, blk=blk, src=src: e.activation(out=sgb_s[:, (blk - 16) * 512:(blk - 15) * 512], in_=src,
                                                                        func=AF.Sigmoid), reads=[rpb[b]], writes=[r_sgbs])
            cut(40)
            c.dma("sp", win_s[:, 0, :], kf_s[:SP, :], reads=[r_kfs])
            c.dma("sp", win_s[:, 1, :], vf_s[:SP, :], reads=[r_vfs])
            cut(2)
            gtmp_s = gmlp_tmp(ess, "s")
            vlnf_s = c.sb(ess, "vlnf_s", [128, 1024], F32)
            r_vlnf = Res()
            mA_s = c.sb(ess, "mA_s", [128, 1024], BF16)
            r_mAs = Res()
            gmlp_tile(ess, PS, gv_s, r_gvs, gu_s, r_gus, sga_s, r_sgas, WsTs, r_ws, bsTs, r_bs, lng, lnb, r_ln,
                      gtmp_s, mA_s, r_mAs, vln_out=vlnf_s, r_vln=r_vlnf)
            c.dma("sp", sgu_s[:, :], vlnf_s[:SP, :], reads=[r_vlnf])
            transpose_to(mA_s, r_mAs, PS, 8, lambda c0, c1: mTg_s[:, c0:c1, :], r_mTgs)
            cut(3)

            qTs_s = c.sb(ess, "qTs_s", [128, 24, PS], BF16)
            kTn = c.sb(ess, "kTn", [128, 8, PS], BF16)
            gbT_s = c.sb(ess, "gbT_s", [128, 8, PS], BF16)
            r_qTss, r_kTn, r_gbTs_ = Res(), Res(), Res()
            transpose_to(q_s, r_qs, PS, 24, lambda c0, c1: qTs_s[:, c0:c1, :], r_qTss)
            transpose_to(k_sb, r_ksb, PS, 8, lambda c0, c1: kTn[:, c0:c1, :], r_kTn)
            transpose_to(sgb_s, r_sgbs, PS, 8, lambda c0, c1: gbT_s[:, c0:c1, :], r_gbTs_)
            EBs = c.sb(ess, "EBs", [128, 8, 384], BF16)
            EBn = c.sb(ess, "EBn", [128, 8, 96], BF16)
            r_EBs = Res()
            c.op("pool", lambda e: e.memset(EBn[:], 0.0), writes=[r_EBs])
            smk = c.sb(ess, "smk", [128, 384], F32)
            smkn = c.sb(ess, "smkn", [128, 96], F32)
            r_smk = Res()
            c.dma("sp", smk[:], smask.rearrange("p a b -> p (a b)"), writes=[r_smk])
            c.dma("sp", smkn[:SP, :], smask_n.rearrange("p a b -> p (a b)"), writes=[r_smk])
            stmp = [c.sb(ess, "stmp%d" % i, [128, 480], F32) for i in range(2)]
            r_stmp = [Res() for _ in range(2)]
            for h in range(8):
                i = h % 2
                c.dma("sp", stmp[i][:, 0:384], sbias[h].rearrange("p a b -> p (a b)"), writes=[r_stmp[i]])
                c.dma("sp", stmp[i][:SP, 384:480], sbias_n[h].rearrange("p a b -> p (a b)"), writes=[r_stmp[i]])
                c.op("act", lambda e, i=i: e.activation(out=stmp[i][:, 0:384], in_=stmp[i][:, 0:384], func=AF.Exp),
                     reads=[r_stmp[i]], writes=[r_stmp[i]])
                c.op("act", lambda e, i=i: e.activation(out=stmp[i][:SP, 384:480], in_=stmp[i][:SP, 384:480], func=AF.Exp),
                     reads=[r_stmp[i]], writes=[r_stmp[i]])
                c.op("dve", lambda e, i=i, h=h: e.tensor_tensor(out=EBs[:, h, :], in0=stmp[i][:, 0:384], in1=smk[:],
                                                                op=ALU.mult), reads=[r_stmp[i], r_smk], writes=[r_EBs])
                c.op("dve", lambda e, i=i, h=h: e.tensor_tensor(out=EBn[:SP, h, :], in0=stmp[i][:SP, 384:480],
                                                                in1=smkn[:SP, :], op=ALU.mult),
                     reads=[r_stmp[i], r_smk], writes=[r_EBs])
            cut(4)
            Ppad = c.sb(ess, "Ppad", [128, 4, 17 * 96], BF16)
            r_Pp = Res()
            c.op("pool", lambda e: e.memset(Ppad[:], 0.0), writes=[r_Pp])
            Kc = c.sb(ess, "Kc0", [128, 16, 128], F32)
            Vc = c.sb(ess, "Vc0", [128, 16, 128], F32)
            Kcb = [c.sb(ess, "Kcb0", [128, 16, 128], BF16)] * 2
            Vcb = [c.sb(ess, "Vcb%d" % i, [128, 16, 128], BF16) for i in range(2)]
            KT = [c.sb(ess, "KT0", [128, 16, 128], BF16)] * 2
            r_Kc, r_Vc = Res(), Res()
            r_Kcb = [Res()] * 2
            r_Vcb = [Res() for _ in range(2)]
            r_KT = [Res()] * 2
            tmpE = c.sb(ess, "tmpE", [128, 408], BF16)
            r_tmpE = Res()
            qc = [c.sb(ess, "qc%d" % i, [128, 24], BF16) for i in range(2)]
            r_qc = [Res() for _ in range(2)]
            rec_s = c.sb(ess, "rec_s", [128, SP], F32)
            ot_s = c.sb(ess, "ot_s", [128, SP], F32)
            r_recs = Res()
            qv4 = qTs_s[:].rearrange("p (g h) t -> p g h t", h=8)
            tiles_g = ([15], [12, 13, 14, 15], list(range(16)))
            n = 0
            for h in range(8):
                b_o = 4 + h % 2
                b_sum = 6 + h % 2
                for b in range(4):
                    i = n % 2
                    n += 1
                    c.dma("sp", Kc[:], cwin[b, :, 0, h, :].rearrange("(rt p) d -> p rt d", p=128), writes=[r_Kc])
                    c.dma("sp", Vc[:], cwin[b, :, 1, h, :].rearrange("(rt p) d -> p rt d", p=128), writes=[r_Vc])
                    c.op("pool", lambda e, i=i: e.tensor_copy(out=Kcb[i][:], in_=Kc[:]), reads=[r_Kc], writes=[r_Kcb[i]])
                    c.op("act", lambda e, i=i: e.activation(out=Vcb[i][:], in_=Vc[:], func=AF.Copy),
                         reads=[r_Vc], writes=[r_Vcb[i]])
                    transpose_to(Kcb[i][:].rearrange("p a d -> p (a d)"), r_Kcb[i], 128, 16,
                                 lambda c0, c1, i=i: KT[i][:, c0:c1, :], r_KT[i], evac="dve")
                    c.op("dve", lambda e, i=i, h=h, b=b: e.tensor_copy(
                        out=qc[i][:].rearrange("p (g t) -> p g t", t=8), in_=qv4[:, :, h, b * 8:(b + 1) * 8]),
                        reads=[r_qTss], writes=[r_qc[i]])
                    b_s = bank(2, 4)

                    def fs(e, i=i, b_s=b_s, h=h):
                        for rt in range(16):
                            e.matmul(pb[b_s][:, rt * 24:(rt + 1) * 24], lhsT=KT[i][:, rt, :], rhs=qc[i][:],
                                     start=True, stop=True)
                        return e.matmul(pb[b_s][:, 384:408], lhsT=kTn[:, h, :], rhs=qc[i][:], start=True, stop=True)
                    c.op("pe", fs, reads=[r_KT[i], r_qc[i], r_kTn], writes=[rpb[b_s]])
                    c.op("act", lambda e, b_s=b_s: e.activation(out=tmpE[:], in_=pb[b_s][:, 0:408], func=AF.Exp,
                                                                scale=ATT_SCALE), reads=[rpb[b_s]], writes=[r_tmpE])
                    Pv = Ppad[:, b, :].rearrange("p (rt g m) -> p rt g m", g=3, m=32)
                    c.op("dve", lambda e, Pv=Pv, b=b, h=h: e.tensor_tensor(
                        out=Pv[:, 0:16, :, b * 8:(b + 1) * 8],
                        in0=tmpE[:, 0:384].rearrange("p (rt g t) -> p rt g t", g=3, t=8),
                        in1=EBs[:, h, :].rearrange("p (rt g t) -> p rt g t", g=3, t=8), op=ALU.mult),
                        reads=[r_tmpE, r_EBs], writes=[r_Pp])
                    c.op("dve", lambda e, Pv=Pv, b=b, h=h: e.tensor_tensor(
                        out=Pv[:, 16, :, b * 8:(b + 1) * 8],
                        in0=tmpE[:, 384:408].rearrange("p (g t) -> p g t", t=8),
                        in1=EBn[:, h, b * 24:(b + 1) * 24].rearrange("p (g t) -> p g t", t=8), op=ALU.mult),
                        reads=[r_tmpE, r_EBs], writes=[r_Pp])

                    def fo(e, i=i, b=b, h=h, b_o=b_o, b_sum=b_sum, Pv=Pv):
                        mms = []
                        for g in range(3):
                            for rt in tiles_g[g]:
                                mms.append((Vcb[i][:, rt, :], Pv[:, rt, g, :]))
                            mms.append((vn[:, h * 128:(h + 1) * 128], Pv[:, 16, g, :]))
                        inst = None
                        for k, (l, r) in enumerate(mms):
                            first = (b == 0 and k == 0)
                            last = (b == 3 and k == len(mms) - 1)
                            e.matmul(pb[b_o][:, 0:SP], lhsT=l, rhs=r, start=first, stop=last)
                            inst = e.matmul(pb[b_sum][:, 0:SP], lhsT=ones[:], rhs=r, start=first, stop=last)
                        return inst
                    c.op("pe", fo, reads=[r_Pp, r_Vcb[i], r_vn, r_const], writes=[rpb[b_o], rpb[b_sum]])
                c.op("dve", lambda e, b_sum=b_sum: e.reciprocal(out=rec_s[:], in_=pb[b_sum][:, 0:SP]),
                     reads=[rpb[b_sum]], writes=[r_recs])
                c.op("dve", lambda e, b_o=b_o: e.tensor_tensor(out=ot_s[:], in0=pb[b_o][:, 0:SP], in1=rec_s[:],
                                                               op=ALU.mult), reads=[rpb[b_o], r_recs], writes=[r_recs])
                c.op("dve", lambda e, h=h: e.tensor_tensor(out=mTg_s[:, 8 + h, 0:SP], in0=ot_s[:], in1=gbT_s[:, h, 0:SP],
                                                           op=ALU.mult), reads=[r_recs, r_gbTs_], writes=[r_mTgs])
            c.barrier()

        es4 = es.enter_context(contextlib.ExitStack())
        make_wpool(es4, 2)
        g_mem, r_gmem = load_gain(es4, "mem", gains["norm_mem"])
        g_peer, r_gpeer = load_gain(es4, "peer", gains["norm_peer"])
        g_fin, r_gfin = load_gain(es4, "fin", gains["norm_final"])
        keysT = c.sb(es4, "keysT", [128, 2, 8, 128], BF16)
        r_keysT = Res()
        with contextlib.ExitStack() as esk:
            kf = c.sb(esk, "kf", [128, 2, 8, 128], F32)
            kb = c.sb(esk, "kb", [128, 2, 8, 128], BF16)
            r_kf = Res()
            c.dma("sp", kf[:, 0, :, :], keys1.rearrange("h k d -> k h d"), writes=[r_kf])
            c.dma("sp", kf[:, 1, :, :], keys2.rearrange("h k d -> k h d"), writes=[r_kf])
            c.op("dve", lambda e: e.tensor_copy(out=kb[:], in_=kf[:]), reads=[r_kf], writes=[r_kf])
            transpose_to(kb[:].rearrange("p s h d -> p (s h d)"), r_kf, 128, 16,
                         lambda c0, c1: keysT[:].rearrange("p s h k -> p (s h) k")[:, c0:c1, :], r_keysT)
            c.barrier()
        iota_i = c.sb(es4, "iota_i", [128, 16], I32)
        iota16 = c.sb(es4, "iota16", [128, 16], F32)
        r_iota = Res()
        c.op("pool", lambda e: e.iota(iota_i[:], pattern=[[1, 16]], base=0, channel_multiplier=0), writes=[r_iota])
        c.op("dve", lambda e: e.tensor_copy(out=iota16[:], in_=iota_i[:]), reads=[r_iota], writes=[r_iota])

        x1 = [c.sb(es4, "x1_%d" % i, [128, D], F32) for i in range(G)]
        r_x1 = [Res() for _ in range(G)]
        h2 = [c.sb(es4, "h2_%d" % i, [128, D], BF16) for i in range(G)]
        r_h2 = [Res() for _ in range(G)]
        hb4 = c.sb(es4, "hb4", [128, D], BF16)
        r_hb4 = Res()
        bufA = c.sb(es4, "bufA", [128, 16, GT], BF16)
        bufB = c.sb(es4, "bufB", [128, 16, GT], BF16)
        r_bufA = Res()
        r_bufB = Res()
        pTm = c.sb(es4, "pTm", [128, 2, GT], BF16)
        r_pTm = Res()
        rsm = c.sb(es4, "rsm", [128, GT], F32)
        r_rsm = Res()
        sc = c.sb(es4, "sc", [128, 2048], F32)
        cand = c.sb(es4, "cand", [128, 2048], F32)
        r_sc = Res()
        r_cand = Res()
        wk = c.sb(es4, "wk", [128, 256], F32)
        r_wk = Res()
        tv = c.sb(es4, "tv", [128, 16, 16], F32)
        ti = c.sb(es4, "ti", [128, 16, 16], U32)
        tif = c.sb(es4, "tif", [128, 16, 16], F32)
        best = c.sb(es4, "best", [128, 8, 16], F32)
        pos = c.sb(es4, "pos", [128, 8, 16], U32)
        pa = c.sb(es4, "pa", [128, 128], U32)
        pbi = c.sb(es4, "pbi", [128, 128], U32)
        paf = c.sb(es4, "paf", [128, 128], F32)
        pbf = c.sb(es4, "pbf", [128, 128], F32)
        i1s = c.sb(es4, "i1s", [128, 128], F32)
        i2s = c.sb(es4, "i2s", [128, 128], F32)
        eidf = c.sb(es4, "eidf", [128, 128], F32)
        gate = c.sb(es4, "gate", [128, 8, 16], F32)
        gsm = c.sb(es4, "gsm", [128, 24], F32)
        r_pk = Res()
        idxT2 = [c.sb(es4, "idxT%d" % i, [128, 128], I32) for i in range(2)]
        GTt2 = [c.sb(es4, "GTt%d" % i, [128, 128], F32) for i in range(2)]
        r_idxT2 = [Res() for _ in range(2)]
        r_GTt2 = [Res() for _ in range(2)]
        apart = c.sb(es4, "apart", [128, 128, 4], F32)
        AT = c.sb(es4, "AT", [128, 128], F32)
        CT = c.sb(es4, "CT", [128, 128], BF16)
        r_apart = Res()
        r_CT = Res()
        NGB = 6
        gbuf = [c.sb(es4, "gbuf%d" % i, [128, D], BF16) for i in range(NGB)]
        r_gbuf = [Res() for _ in range(NGB)]
        NCB = 4
        cbuf = [c.sb(es4, "cbuf%d" % i, [128, 256], BF16) for i in range(NCB)]
        r_cbuf = [Res() for _ in range(NCB)]
        for i in range(NCB):
            c.op("pool", lambda e, i=i: e.memset(cbuf[i][:], 0.0), writes=[r_cbuf[i]])
        NJ = 2
        junk4 = [c.sb(es4, "junk4_%d" % i, [128, D], BF16) for i in range(NJ)]
        r_junk4 = [Res() for _ in range(NJ)]
        yout = cand
        r_yout = r_cand
        pctr = {"g": 0, "c": 0, "j": 0}

        def cross_attn(qT_, r_q, col0, n, mkT_, r_mk, mv_fn, r_mv, oT_, r_o):
            for h in range(4):
                for mt in range(2):
                    b = bank(2, 6)

                    def f(e, h=h, mt=mt, b=b):
                        inst = None
                        for dc in range(4):
                            inst = e.matmul(pb[b][:, 0:n], lhsT=mkT_[:, h * 4 + dc, mt * 128:(mt + 1) * 128],
                                            rhs=qT_[:, h * 4 + dc, col0:col0 + n], start=(dc == 0), stop=(dc == 3))
                        return inst
                    c.op("pe", f, reads=[r_q] + r_mk, writes=[rpb[b]])
                    c.op("act", lambda e, mt=mt, b=b: e.activation(out=pTm[:, mt, 0:n], in_=pb[b][:, 0:n],
                                                                  func=AF.Exp, scale=MEM_SCALE),
                         reads=[rpb[b]], writes=[r_pTm])
                b = bank(2, 6)

                def fs(e, b=b):
                    e.matmul(pb[b][:, 0:n], lhsT=ones[:], rhs=pTm[:, 0, 0:n], start=True, stop=False)
                    return e.matmul(pb[b][:, 0:n], lhsT=ones[:], rhs=pTm[:, 1, 0:n], start=False, stop=True)
                c.op("pe", fs, reads=[r_pTm, r_const], writes=[rpb[b]])
                c.op("dve", lambda e, b=b: e.reciprocal(out=rsm[:, 0:n], in_=pb[b][:, 0:n]), reads=[rpb[b]],
                     writes=[r_rsm])
                for dc in range(4):
                    b = bank(2, 6)

                    def fo(e, h=h, dc=dc, b=b):
                        c0 = h * 512 + dc * 128
                        e.matmul(pb[b][:, 0:n], lhsT=mv_fn(0, c0), rhs=pTm[:, 0, 0:n], start=True, stop=False)
                        return e.matmul(pb[b][:, 0:n], lhsT=mv_fn(1, c0), rhs=pTm[:, 1, 0:n],
                                        start=False, stop=True)
                    c.op("pe", fo, reads=[r_pTm] + r_mv, writes=[rpb[b]])
                    c.op("dve", lambda e, h=h, dc=dc, b=b: e.tensor_tensor(
                        out=oT_[:, h * 4 + dc, col0:col0 + n], in0=pb[b][:, 0:n], in1=rsm[:, 0:n], op=ALU.mult),
                        reads=[rpb[b], r_rsm], writes=[r_o])

        def top16(P, src_ap, n, dst_v, dst_i, rd, wr):
            c.op("dve", lambda e: e.max(out=dst_v[:, 0:8], in_=src_ap), reads=rd, writes=wr)
            c.op("dve", lambda e: e.max_index(out=dst_i[:, 0:8], in_max=dst_v[:, 0:8], in_values=src_ap),
                 reads=rd + wr, writes=wr)
            c.op("dve", lambda e: e.match_replace(out=wk[:P, 0:n], in_to_replace=dst_v[:, 0:8], in_values=src_ap,
                                                  imm_value=NEG), reads=rd + wr, writes=[r_wk])
            c.op("dve", lambda e: e.max(out=dst_v[:, 8:16], in_=wk[:P, 0:n]), reads=[r_wk], writes=wr)
            c.op("dve", lambda e: e.max_index(out=dst_i[:, 8:16], in_max=dst_v[:, 8:16], in_values=wk[:P, 0:n]),
                 reads=[r_wk] + wr, writes=wr)

        def peer_parts(P, nv, qpT, r_qp, col0, h2_t, r_h2t, x2_t, r_x2t, y_dst, slot):
            idxT, r_idxT, GTt, r_GTt = idxT2[slot], r_idxT2[slot], GTt2[slot], r_GTt2[slot]
            def sel_a():
                def fsc(e):
                    inst = None
                    for hs in range(16):
                        inst = e.matmul(pb[4 + hs // 4][:P, (hs % 4) * 128:(hs % 4 + 1) * 128],
                                        lhsT=qpT[:, hs, col0:col0 + P], rhs=keysT[:, hs % 2, hs // 2, :],
                                        start=True, stop=True)
                    return inst
                c.op("pe", fsc, reads=[r_qp, r_keysT], writes=[rpb[4], rpb[5], rpb[6], rpb[7]])
                for q in range(4):
                    c.op("act", lambda e, q=q: e.activation(out=sc[:P, q * 512:(q + 1) * 512], in_=pb[4 + q][:P, :],
                                                            func=AF.Copy), reads=[rpb[4 + q]], writes=[r_sc])
                scv = sc[:P, :].rearrange("p (a k) -> p a k", k=128)
                for hs in range(16):
                    top16(P, scv[:, hs, :], 128, tv[:P, hs, :], ti[:P, hs, :], [r_sc], [r_pk])
                tv4 = tv[:P].rearrange("p (h s) k -> p h s k", s=2)
                candv = cand[:P, :].rearrange("p (h a b) -> p h a b", a=16, b=16)
                c.op("dve", lambda e: e.tensor_tensor(out=candv, in0=tv4[:, :, 0, :].unsqueeze(3).to_broadcast([P, 8, 16, 16]),
                                                      in1=tv4[:, :, 1, :].unsqueeze(2).to_broadcast([P, 8, 16, 16]),
                                                      op=ALU.add), reads=[r_pk], writes=[r_cand])
                cand3 = cand[:P, :].rearrange("p (h c) -> p h c", c=256)
                for h in range(8):
                    top16(P, cand3[:, h, :], 256, best[:P, h, :], pos[:P, h, :], [r_cand], [r_pk])
                c.op("dve", lambda e: e.tensor_scalar(out=gsm[:P, 0:8], in0=best[:P, :, 0], scalar1=-1.0, scalar2=None,
                                                      op0=ALU.mult), reads=[r_pk], writes=[r_pk])
                for h in range(8):
                    c.op("act", lambda e, h=h: e.activation(out=gate[:P, h, :], in_=best[:P, h, :], func=AF.Exp,
                                                            bias=gsm[:P, h:h + 1], scale=1.0,
                                                            accum_out=gsm[:P, 8 + h:9 + h]), reads=[r_pk], writes=[r_pk])
                c.op("dve", lambda e: e.reciprocal(out=gsm[:P, 16:24], in_=gsm[:P, 8:16]), reads=[r_pk], writes=[r_pk])
                c.op("dve", lambda e: e.tensor_tensor(out=gate[:P], in0=gate[:P],
                                                      in1=gsm[:P, 16:24].unsqueeze(2).to_broadcast([P, 8, 16]),
                                                      op=ALU.mult), reads=[r_pk], writes=[r_pk])
                posf = pos[:P].rearrange("p h k -> p (h k)")
                c.op("dve", lambda e: e.tensor_copy(out=pbf[:P, :], in_=posf), reads=[r_pk], writes=[r_pk])
                c.op("dve", lambda e: e.tensor_scalar(out=paf[:P, :], in0=pbf[:P, :], scalar1=16.0, scalar2=None,
                                                      op0=ALU.is_ge), reads=[r_pk], writes=[r_pk])
                for m in range(2, 16):
                    c.op("dve", lambda e, m=m: e.scalar_tensor_tensor(out=paf[:P, :], in0=pbf[:P, :], scalar=16.0 * m,
                                                                      in1=paf[:P, :], op0=ALU.is_ge, op1=ALU.add),
                         reads=[r_pk], writes=[r_pk])
                c.op("dve", lambda e: e.scalar_tensor_tensor(out=pbf[:P, :], in0=paf[:P, :], scalar=-16.0, in1=pbf[:P, :],
                                                             op0=ALU.mult, op1=ALU.add), reads=[r_pk], writes=[r_pk])
                c.op("dve", lambda e: e.tensor_copy(out=tif[:P], in_=ti[:P]), reads=[r_pk], writes=[r_pk])
                tif4 = tif[:P].rearrange("p (h s) k -> p h s k", s=2)
                ohv = sc[:P, :].rearrange("p (h a b) -> p h a b", a=16, b=16)
                io4 = iota16[:P, :].unsqueeze(1).unsqueeze(1).to_broadcast([P, 8, 16, 16])
                for side, (pf_, dsts) in enumerate(((paf, i1s), (pbf, i2s))):
                    pv = pf_[:P, :].rearrange("p (h k) -> p h k", k=16).unsqueeze(3).to_broadcast([P, 8, 16, 16])
                    c.op("dve", lambda e, pv=pv: e.tensor_tensor(out=ohv, in0=pv, in1=io4, op=ALU.is_equal),
                         reads=[r_pk, r_iota], writes=[r_sc])
                    c.op("dve", lambda e, side=side: e.tensor_tensor(
                        out=ohv, in0=ohv, in1=tif4[:, :, side, :].unsqueeze(2).to_broadcast([P, 8, 16, 16]), op=ALU.mult),
                        reads=[r_sc, r_pk], writes=[r_sc])
                    c.op("dve", lambda e, dsts=dsts: e.tensor_reduce(
                        out=dsts[:P, :].rearrange("p (h k) -> p h k", k=16), in_=ohv, axis=mybir.AxisListType.X,
                        op=ALU.add), reads=[r_sc], writes=[r_pk])
                c.op("dve", lambda e: e.scalar_tensor_tensor(out=eidf[:P, :], in0=i1s[:P, :], scalar=128.0, in1=i2s[:P, :],
                                                             op0=ALU.mult, op1=ALU.add), reads=[r_pk], writes=[r_pk])

            def sel_b():
                c.op("pe", lambda e: e.transpose(out=pb[4][:, 0:P], in_=eidf[:P, :], identity=identf[:P, :P]),
                     reads=[r_pk, r_const], writes=[rpb[4]])
                c.op("dve", lambda e: e.tensor_copy(out=idxT[:, 0:P], in_=pb[4][:, 0:P]), reads=[rpb[4]], writes=[r_idxT])
                c.op("pe", lambda e: e.transpose(out=pb[5][:, 0:P], in_=gate[:P].rearrange("p h k -> p (h k)"),
                                                 identity=identf[:P, :P]), reads=[r_pk, r_const], writes=[rpb[5]])
                c.op("act", lambda e: e.activation(out=GTt[:, 0:P], in_=pb[5][:, 0:P], func=AF.Copy), reads=[rpb[5]],
                     writes=[r_GTt])

            def pass1():
                for t in range(nv):
                    k = pctr["g"] % NGB
                    pctr["g"] += 1
                    c.swdma(gbuf[k][:], tabu[:, :], reads=[r_idxT, r_tab], writes=[r_gbuf[k]],
                            indirect=bass.IndirectOffsetOnAxis(ap=idxT[:, t:t + 1], axis=0))
                    s0 = (t % 2) * 4

                    def fx(e, t=t, s0=s0):
                        inst = None
                        for q in range(4):
                            inst = e.matmul(pb[s0 + q][:, :], lhsT=ident[:P, t:t + 1].to_broadcast([P, 128]),
                                            rhs=h2_t[:P, q * 512:(q + 1) * 512], start=True, stop=True)
                        return inst
                    c.op("pe", fx, reads=[r_h2t, r_const], writes=[rpb[s0 + q] for q in range(4)])
                    edge = (t == nv - 1) or (t == 0)
                    jn = pctr["j"] % NJ
                    pctr["j"] += 1
                    c.op("dve", lambda e, t=t, k=k, s0=s0, jn=jn: e.scalar_tensor_tensor(
                        out=junk4[jn][:], in0=gbuf[k][:, :], scalar=1.0, in1=pball[:, s0 * 512:(s0 + 4) * 512],
                        op0=ALU.mult, op1=ALU.mult, accum_out=apart[:, t, 0:1]),
                        reads=[r_gbuf[k]] + [rpb[s0 + q] for q in range(4)],
                        writes=[r_junk4[jn]] + ([r_apart] if edge else []))
                if nv < P:
                    c.op("dve", lambda e: e.memset(apart[:, nv:P, :], 0.0), writes=[r_apart])
                c.op("dve", lambda e: e.tensor_copy(out=AT[:, 0:P], in_=apart[:, 0:P, 0]), reads=[r_apart], writes=[r_apart])
                c.op("act", lambda e: e.activation(out=AT[:, 0:P], in_=AT[:, 0:P], func=AF.Gelu_apprx_tanh),
                     reads=[r_apart], writes=[r_apart])
                c.op("dve", lambda e: e.tensor_tensor(out=CT[:, 0:P], in0=AT[:, 0:P], in1=GTt[:, 0:P], op=ALU.mult),
                     reads=[r_apart, r_GTt], writes=[r_CT])

            def pass2():
                for t in range(nv):
                    k = pctr["g"] % NGB
                    pctr["g"] += 1
                    c.swdma(gbuf[k][:], tabv[:, :], reads=[r_idxT, r_tab], writes=[r_gbuf[k]],
                            indirect=bass.IndirectOffsetOnAxis(ap=idxT[:, t:t + 1], axis=0))
                    kc = pctr["c"] % NCB
                    pctr["c"] += 1
                    c.op("act", lambda e, t=t, kc=kc: e.activation(out=cbuf[kc][:, 127:128], in_=CT[:, t:t + 1],
                                                                   func=AF.Copy), reads=[r_CT], writes=[r_cbuf[kc]])

                    def fy(e, t=t, k=k, kc=kc):
                        inst = None
                        for q in range(4):
                            inst = e.matmul(pb[q][:P, :], lhsT=cbuf[kc][:, 127 - t:127 - t + P],
                                            rhs=gbuf[k][:, q * 512:(q + 1) * 512], start=(t == 0), stop=(t == nv - 1))
                        return inst
                    c.op("pe", fy, reads=[r_cbuf[kc], r_gbuf[k]], writes=[rpb[q] for q in range(4)])

            def epi():
                for q in range(4):
                    c.op("dve", lambda e, q=q: e.tensor_tensor(out=x2_t[:P, q * 512:(q + 1) * 512], in0=pb[q][:P, :],
                                                               in1=x2_t[:P, q * 512:(q + 1) * 512], op=ALU.add),
                         reads=[rpb[q], r_x2t], writes=[r_x2t])
                rms_h(x2_t, r_x2t, P, g_fin, r_gfin, yout, r_yout)
                c.dma("sp", y_dst, yout[:nv, :], reads=[r_yout])

            return sel_a, sel_b, pass1, pass2, epi

        def phase4_group(ntile, P, nv, x_src, mTg, r_mTg, cross_fn, y_dst_fn):
            ntok = ntile * P
            for j in range(ntile):
                if nv < P:
                    c.op("pool", lambda e, j=j: e.memset(x1[j][:], 0.0), writes=[r_x1[j]])
                c.dma("sp", x1[j][:nv, :], x_src(j), writes=[r_x1[j]])
            ws = WStream([(nm, b) for nm in ("w_out", "w_mq", "w_mo", "peer_wq") for b in range(4)])
            for blk in range(4):
                wt, r_w = ws.get()
                for j in range(ntile):
                    b = bank(2, 6)
                    mm_tok(mTg, r_mTg, j * P, P, wt, r_w, b)
                    c.op("dve", lambda e, j=j, blk=blk, b=b: e.tensor_tensor(
                        out=x1[j][:P, blk * 512:(blk + 1) * 512], in0=pb[b][:P, :],
                        in1=x1[j][:P, blk * 512:(blk + 1) * 512], op=ALU.add), reads=[rpb[b], r_x1[j]], writes=[r_x1[j]])
            for j in range(ntile):
                rms_h(x1[j], r_x1[j], P, g_mem, r_gmem, hb4, r_hb4)
                transpose_to(hb4, r_hb4, P, 16, lambda c0, c1, j=j: bufA[:, c0:c1, j * P:(j + 1) * P], r_bufA)
            for blk in range(4):
                wt, r_w = ws.get()
                for ct in range(4):
                    b = bank(2, 6)
                    mm_feat(bufA, r_bufA, 0, ntok, wt, r_w, ct, b)
                    c.op("act", lambda e, blk=blk, ct=ct, b=b: e.activation(
                        out=bufB[:, blk * 4 + ct, 0:ntok], in_=pb[b][:, 0:ntok], func=AF.Copy),
                        reads=[rpb[b]], writes=[r_bufB])
            cross_fn(bufB, r_bufB, bufA, r_bufA)
            for blk in range(4):
                wt, r_w = ws.get()
                for j in range(ntile):
                    b = bank(2, 6)
                    mm_tok(bufA, r_bufA, j * P, P, wt, r_w, b)
                    c.op("dve", lambda e, j=j, blk=blk, b=b: e.tensor_tensor(
                        out=x1[j][:P, blk * 512:(blk + 1) * 512], in0=pb[b][:P, :],
                        in1=x1[j][:P, blk * 512:(blk + 1) * 512], op=ALU.add), reads=[rpb[b], r_x1[j]], writes=[r_x1[j]])
            for j in range(ntile):
                rms_h(x1[j], r_x1[j], P, g_peer, r_gpeer, h2[j], r_h2[j])
                transpose_to(h2[j], r_h2[j], P, 16, lambda c0, c1, j=j: bufB[:, c0:c1, j * P:(j + 1) * P], r_bufB)
            for blk in range(4):
                wt, r_w = ws.get()
                for ct in range(4):
                    b = bank(2, 6)
                    mm_feat(bufB, r_bufB, 0, ntok, wt, r_w, ct, b)
                    c.op("act", lambda e, blk=blk, ct=ct, b=b: e.activation(
                        out=bufA[:, blk * 4 + ct, 0:ntok], in_=pb[b][:, 0:ntok], func=AF.Copy),
                        reads=[rpb[b]], writes=[r_bufA])
            parts = [peer_parts(P, nv, bufA, r_bufA, j * P, h2[j], r_h2[j], x1[j], r_x1[j], y_dst_fn(j), j % 2)
                     for j in range(ntile)]
            parts[0][0]()
            parts[0][1]()
            parts[0][2]()
            for j in range(ntile):
                if j + 1 < ntile:
                    parts[j + 1][0]()
                parts[j][3]()
                if j + 1 < ntile:
                    parts[j + 1][1]()
                parts[j][4]()
                if j + 1 < ntile:
                    parts[j + 1][2]()

        mTg = c.sb(es4, "mTg", [128, 16, GT], BF16)
        r_mTg = Res()
        for g in range((NT // G) if 5 in _PH else 0)[:_LIM["groups5"]]:
            c.dma("sp", mTg[:], mTs[:, :, g * GT:(g + 1) * GT].rearrange("k p t -> p k t"), reads=r_mTs,
                  writes=[r_mTg])
            phase4_group(
                G, 128, 128, lambda j, g=g: xo[g * GT + j * 128:g * GT + (j + 1) * 128, :], mTg, r_mTg,
                lambda qT_, r_q, oT_, r_o: cross_attn(qT_, r_q, 0, GT, mkT, [r_mkT],
                                                      lambda mt, c0: mv_bf[:, mt, c0:c0 + 128], [r_mvbf], oT_, r_o),
                lambda j, g=g: y_p[g * GT + j * 128:g * GT + (j + 1) * 128, :])

        def cross_sample(qT_, r_q, oT_, r_o):
            stage = ((sc, r_sc), (cand, r_cand))
            n = 0
            for b in range(4):
                for kv in range(2):
                    for mt in range(2):
                        st, r_st = stage[n % 2]
                        n += 1
                        c.dma("sp", st[:], cmem[b, mt * 128:(mt + 1) * 128, kv, :], writes=[r_st])
                        k = kv * 2 + mt
                        c.op("pool", lambda e, st=st, k=k: e.tensor_copy(out=gbuf[k][:], in_=st[:]), reads=[r_st],
                             writes=[r_gbuf[k]])
                for mt in range(2):
                    transpose_to(gbuf[mt], r_gbuf[mt], 128, 16,
                                 lambda c0, c1, mt=mt: mTg[:, c0:c1, mt * 128:(mt + 1) * 128], r_mTg)
                cross_attn(qT_, r_q, b * 8, 8, mTg, [r_mTg], lambda mt, c0: gbuf[2 + mt][:, c0:c0 + 128],
                           [r_gbuf[2], r_gbuf[3]], oT_, r_o)

        if 6 in _PH:
            phase4_group(1, 128, SP, lambda j: xs[:, :], mTg_s, r_mTgs, cross_sample, lambda j: y_s[:, :])

        c.finish()
    return nc


def _host_tables(rel_bias):
    biasT = np.zeros((24, 128, 2, 128), np.float32)
    jl = np.arange(128)[:, None, None]
    jt = np.arange(2)[None, :, None]
    i = np.arange(128)[None, None, :]
    off = i + 128 - (jt * 128 + jl)
    band = ((off >= 0) & (off <= 128)).astype(np.float32)
    offc = np.clip(off, 0, 128)
    for g, dil in enumerate(DILS):
        bucket = _t5_bucket(dil * np.arange(129))
        for h in range(8):
            biasT[g * 8 + h] = rel_bias[bucket[offc], g * 8 + h]
    sbias = np.zeros((8, 128, 16, 24), np.float32)
    smask = np.zeros((128, 16, 24), np.float32)
    sbias_n = np.zeros((8, 32, 4, 24), np.float32)
    smask_n = np.zeros((32, 4, 24), np.float32)
    for g, dil in enumerate(DILS):
        bucket = _t5_bucket(dil * np.arange(129))
        for t in range(8):
            for j in range(129):
                row = 2048 + t - dil * j
                col = g * 8 + t
                if row < 2048:
                    smask[row % 128, row // 128, col] = 1.0
                    sbias[:, row % 128, row // 128, col] = rel_bias[bucket[j], g * 8:(g + 1) * 8]
                else:
                    tp = row - 2048
                    for b in range(4):
                        smask_n[b * 8 + tp, b, col] = 1.0
                        sbias_n[:, b * 8 + tp, b, col] = rel_bias[bucket[j], g * 8:(g + 1) * 8]
    return biasT, band, sbias, smask, sbias_n, smask_n


_NC_CACHE = {}


def _prepare(x_prompt, x_sample, mem_prompt, cache_win, cache_mem_kv, rel_bias, norm_mix, w_in,
             sgu_ln_g, sgu_ln_b, sgu_w, sgu_b, w_out, norm_mem, norm_memtok, w_mq, w_mk, w_mv, w_mo,
             norm_peer, peer_wq, peer_keys1, peer_keys2, peer_u, peer_v, norm_final):
    f = lambda a: np.ascontiguousarray(np.asarray(a, dtype=np.float32))
    xp = f(x_prompt)[0]
    xsm = f(x_sample)
    rel_bias = f(rel_bias)
    biasT, band, sbias, smask, sbias_n, smask_n = _host_tables(rel_bias)
    sgu_w0 = f(sgu_w)[0]
    sgu_wT = np.ascontiguousarray(sgu_w0.transpose(0, 2, 1))
    sgu_wTs = np.zeros((8, 128, 128), np.float32)
    for b in range(4):
        sgu_wTs[:, b * 8:(b + 1) * 8, b * 8:(b + 1) * 8] = sgu_wT[:, :8, :8]
    sgu_b0 = f(sgu_b)[0]
    sgu_bT = np.ascontiguousarray(sgu_b0.T)
    sgu_bTs = np.zeros((128, 8), np.float32)
    sgu_bTs[:32] = np.tile(sgu_b0[:, :8].T, (4, 1))
    shared = {
        "mem": f(mem_prompt)[0], "w_in": f(w_in)[0], "w_out": f(w_out)[0], "w_mq": f(w_mq)[0],
        "w_mk": f(w_mk)[0], "w_mv": f(w_mv)[0], "w_mo": f(w_mo)[0], "peer_wq": f(peer_wq)[0],
        "peer_u": f(peer_u)[0], "peer_v": f(peer_v)[0], "keys1": f(peer_keys1)[0], "keys2": f(peer_keys2)[0],
        "norm_mix": f(norm_mix), "norm_mem": f(norm_mem), "norm_memtok": f(norm_memtok),
        "norm_peer": f(norm_peer), "norm_final": f(norm_final).reshape(1, D),
        "sgu_ln_g": f(sgu_ln_g), "sgu_ln_b": f(sgu_ln_b), "sgu_wT": sgu_wT, "sgu_wTs": sgu_wTs,
        "sgu_bT": sgu_bT, "sgu_bTs": sgu_bTs, "biasT": biasT, "bandmask": band, "sbias": sbias,
        "smask": smask, "sbias_n": sbias_n, "smask_n": smask_n,
    }
    cw = f(cache_win)[0]
    cm = f(cache_mem_kv)[0].reshape(32, 256, 2, 2048)
    in_maps = []
    for cidx in range(NCORES):
        m = dict(shared)
        m["xo"] = xp[cidx * TOK:(cidx + 1) * TOK]
        m["xh"] = xp[(cidx - 1) * TOK:cidx * TOK] if cidx > 0 else np.zeros((TOK, D), np.float32)
        m["xs"] = np.ascontiguousarray(xsm[cidx * 4:(cidx + 1) * 4].reshape(SP, D))
        m["cwin"] = cw[cidx * 4:(cidx + 1) * 4]
        m["cmem"] = cm[cidx * 4:(cidx + 1) * 4]
        m["pflag"] = np.full((128, 1), 0.0 if cidx == 0 else 1.0, np.float32)
        in_maps.append(m)
    return in_maps


def kernel(**inputs):
    in_maps = _prepare(**inputs)
    if "nc" not in _NC_CACHE:
        _NC_CACHE["nc"] = build_program()
    ncr = _LIM.get("ncores") or NCORES
    res = run_bass_kernel_spmd(_NC_CACHE["nc"], in_maps[:ncr], core_ids=list(range(ncr)))
    R = list(res.results)
    while len(R) < NCORES:
        R.append(R[0])
    y_prompt = np.concatenate([R[i]["y_p"] for i in range(NCORES)], axis=0).reshape(1, NCORES * TOK, D)
    y_sample = np.concatenate([R[i]["y_s"] for i in range(NCORES)], axis=0).reshape(32, 8, D)
    win_p = R[NCORES - 1]["win_p"].reshape(1, 1, TOK, 2, 8, 128)
    memkv = R[0]["memkv"].reshape(1, 1, 256, 2, 4, 512)
    win_s = np.concatenate([R[i]["win_s"] for i in range(NCORES)], axis=0).reshape(1, 32, 8, 2, 8, 128)
    sgu_s = np.concatenate([R[i]["sgu_s"] for i in range(NCORES)], axis=0).reshape(1, 32, 8, 1024)
    return (y_prompt, y_sample, win_p, memkv, win_s, sgu_s)
```

```python
import contextlib
import math
import numpy as np
import concourse.bass as bass
import concourse.mybir as mybir
from concourse.bass_utils import run_bass_kernel_spmd

F32 = mybir.dt.float32
BF16 = mybir.dt.bfloat16
I32 = mybir.dt.int32
U32 = mybir.dt.uint32
AF = mybir.ActivationFunctionType
ALU = mybir.AluOpType

NCORES = 8
D = 2048
TOK = 2048
NT = TOK // 128
SP = 32
EPS = 1e-6
DILS = (1, 4, 16)
REL_BUCKETS = 32
REL_MAX_DIST = 2048
ATT_SCALE = 128 ** -0.5
MEM_SCALE = 512 ** -0.5
NEG = -1e30


class Res:
    __slots__ = ("w", "r")

    def __init__(self):
        self.w = None
        self.r = {}


class Ctx:
    NDMA = 32

    def __init__(self, nc, es):
        self.nc = nc
        self.es = es
        self.eng = {"pe": nc.tensor, "act": nc.scalar, "dve": nc.vector, "pool": nc.gpsimd, "sp": nc.sync}
        self.sem = {}
        self.cnt = {}
        self.known = {e: {} for e in self.eng}
        for e in self.eng:
            self.sem[e] = es.enter_context(nc.semaphore("s_" + e))
            self.cnt[e] = 0
        self.dsem = [es.enter_context(nc.semaphore("d%d" % i)) for i in range(self.NDMA)]
        self.dtgt = [0] * self.NDMA
        self.drr = 0
        self.nop = 0
        self.muted = False
        self.NSW = 12
        self.swsem = [es.enter_context(nc.semaphore("w%d" % i)) for i in range(self.NSW)]
        self.swtgt = [0] * self.NSW
        self.swrr = 0

    def sb(self, es, name, shape, dt):
        return es.enter_context(self.nc.sbuf_tensor(name, list(shape), dt))

    def _semof(self, key):
        if isinstance(key, str):
            return self.sem[key]
        if isinstance(key, tuple):
            return self.swsem[key[1]]
        return self.dsem[key]

    def swdma(self, out, in_, reads=(), writes=(), indirect=None, **kw):
        if self.muted:
            return None
        i = self.swrr % self.NSW
        self.swrr += 1
        deps = self._deps(reads, writes)
        if self.swtgt[i]:
            deps.append((("w", i), self.swtgt[i]))
        self._wait("pool", deps)
        self.swtgt[i] += 16
        if indirect is not None:
            inst = self.eng["pool"].indirect_dma_start(out=out, out_offset=None, in_=in_, in_offset=indirect, **kw)
        else:
            inst = self.eng["pool"].dma_start(out=out, in_=in_, **kw)
        inst.then_inc(self.swsem[i], 16)
        tok = (("w", i), self.swtgt[i])
        self._mark(tok, reads, writes)
        self.nop += 1
        return tok

    def _wait(self, e, deps):
        need = {}
        for tok in deps:
            if tok is None:
                continue
            k, v = tok
            if k == "pe" and e == "pe":
                continue
            if self.known[e].get(k, 0) >= v:
                continue
            if need.get(k, 0) < v:
                need[k] = v
        for k, v in need.items():
            self.eng[e].wait_ge(self._semof(k), v)
            self.known[e][k] = v

    @staticmethod
    def _deps(reads, writes):
        deps = []
        for r in reads:
            deps.append(r.w)
        for r in writes:
            deps.append(r.w)
            for k, v in r.r.items():
                deps.append((k, v))
        return deps

    @staticmethod
    def _mark(tok, reads, writes):
        k, v = tok
        for r in reads:
            if r.r.get(k, 0) < v:
                r.r[k] = v
        for r in writes:
            r.w = tok
            r.r = {}

    def op(self, e, fn, reads=(), writes=()):
        if self.muted:
            return None
        self._wait(e, self._deps(reads, writes))
        inst = fn(self.eng[e])
        self.cnt[e] += 1
        inst.then_inc(self.sem[e], 1)
        tok = (e, self.cnt[e])
        self._mark(tok, reads, writes)
        self.nop += 1
        return tok

    def dma(self, q, out, in_, reads=(), writes=(), indirect=None, **kw):
        if self.muted:
            return None
        i = self.drr % self.NDMA
        self.drr += 1
        deps = self._deps(reads, writes)
        if self.dtgt[i]:
            deps.append((i, self.dtgt[i]))
        self._wait(q, deps)
        self.dtgt[i] += 16
        if indirect is not None:
            inst = self.eng[q].indirect_dma_start(out=out, out_offset=None, in_=in_, in_offset=indirect, **kw)
        else:
            inst = self.eng[q].dma_start(out=out, in_=in_, **kw)
        inst.then_inc(self.dsem[i], 16)
        tok = (i, self.dtgt[i])
        self._mark(tok, reads, writes)
        self.nop += 1
        return tok

    def _swtoks(self):
        return [(("w", i), t) for i, t in enumerate(self.swtgt) if t]

    def barrier(self):
        self.muted = False
        deps = [(i, t) for i, t in enumerate(self.dtgt) if t] + self._swtoks()
        deps += [(e, n) for e, n in self.cnt.items() if n]
        for e in self.eng:
            self._wait(e, [d for d in deps if d[0] != e])

    def finish(self):
        deps = [(i, t) for i, t in enumerate(self.dtgt) if t] + self._swtoks()
        deps += [(e, n) for e, n in self.cnt.items() if n and e != "sp"]
        self._wait("sp", deps)


def _t5_bucket(dist):
    max_exact = REL_BUCKETS // 2
    d = np.maximum(dist, 1).astype(np.float64)
    large = max_exact + (np.log(d / max_exact) / math.log(REL_MAX_DIST / max_exact)
                         * (REL_BUCKETS - max_exact)).astype(np.int64)
    large = np.minimum(large, REL_BUCKETS - 1)
    return np.where(dist < max_exact, dist, large).astype(np.int32)


class _SkipPhase(Exception):
    pass


_PH = {0, 1, 2, 3, 4, 5, 6}
_LIM = {"groups": None, "heads": None, "groups5": None}


def build_program():
    nc = bass.Bass("TRN2", target_bir_lowering=False)

    def din(name, shape, dt=F32):
        return nc.dram_tensor(name, list(shape), dt, kind="ExternalInput").ap()

    def dout(name, shape, dt=F32):
        return nc.dram_tensor(name, list(shape), dt, kind="ExternalOutput").ap()

    def dscr(name, shape, dt=BF16):
        return nc.dram_tensor(name, list(shape), dt, kind="Internal").ap()

    xo = din("xo", [TOK, D])
    xh = din("xh", [TOK, D])
    xs = din("xs", [SP, D])
    mem = din("mem", [256, D])
    cwin = din("cwin", [4, 2048, 2, 8, 128])
    cmem = din("cmem", [4, 256, 2, 2048])
    w_in = din("w_in", [D, 9216])
    wsq = {n: din(n, [D, D]) for n in ("w_out", "w_mq", "w_mk", "w_mv", "w_mo", "peer_wq")}
    peer_u = din("peer_u", [16384, D])
    peer_v = din("peer_v", [16384, D])
    keys1 = din("keys1", [8, 128, 128])
    keys2 = din("keys2", [8, 128, 128])
    gains = {n: din(n, [1, D]) for n in ("norm_mix", "norm_mem", "norm_memtok", "norm_peer", "norm_final")}
    sgu_ln_g = din("sgu_ln_g", [1, 1024])
    sgu_ln_b = din("sgu_ln_b", [1, 1024])
    sgu_wT = din("sgu_wT", [8, 128, 128])
    sgu_wTs = din("sgu_wTs", [8, 128, 128])
    sgu_bT = din("sgu_bT", [128, 8])
    sgu_bTs = din("sgu_bTs", [128, 8])
    biasT = din("biasT", [24, 128, 2, 128])
    bandmask = din("bandmask", [128, 2, 128])
    sbias = din("sbias", [8, 128, 16, 24])
    smask = din("smask", [128, 16, 24])
    sbias_n = din("sbias_n", [8, 32, 4, 24])
    smask_n = din("smask_n", [32, 4, 24])
    pflag = din("pflag", [128, 1])

    y_p = dout("y_p", [TOK, D])
    y_s = dout("y_s", [SP, D])
    win_p = dout("win_p", [TOK, 2, 1024])
    memkv = dout("memkv", [256, 2, 2048])
    win_s = dout("win_s", [SP, 2, 1024])
    sgu_s = dout("sgu_s", [SP, 1024])

    WBLK = {"w_in": 18, "w_out": 4, "w_mq": 4, "w_mk": 4, "w_mv": 4, "w_mo": 4, "peer_wq": 4}
    wscr = {n: dscr("wb_" + n, [k, 128, 16, 512]) for n, k in WBLK.items()}
    wscr_res = {n: [Res() for _ in range(k)] for n, k in WBLK.items()}
    tabu = dscr("tabu", [16384, D])
    tabv = dscr("tabv", [16384, D])
    r_tab = Res()
    qTs = dscr("qTs", [24, 128, TOK])
    kTs = dscr("kTs", [8, 128, 2 * TOK])
    vTs = dscr("vTs", [8, 128, 2 * TOK])
    gbTs = dscr("gbTs", [8, 128, TOK])
    mTs = dscr("mTs", [16, 128, TOK])
    r_qTs = [Res() for _ in range(24)]
    r_kTs = [Res() for _ in range(8)]
    r_vTs = [Res() for _ in range(8)]
    r_gbTs = [Res() for _ in range(8)]
    r_mTs = [Res() for _ in range(16)]

    with contextlib.ExitStack() as es:
        c = Ctx(nc, es)

        pball = es.enter_context(nc.psum_tensor("pball", [128, 4096], F32))
        pb = [pball[:, i * 512:(i + 1) * 512] for i in range(8)]
        rpb = [Res() for _ in range(8)]
        pbb = [p.bitcast(BF16) for p in pb]

        identf = c.sb(es, "identf", [128, 128], F32)
        ident = c.sb(es, "ident", [128, 128], BF16)
        ones = c.sb(es, "ones", [128, 128], BF16)
        r_const = Res()
        c.op("pool", lambda e: e.memset(identf[:], 0.0), writes=[r_const])
        c.op("pool", lambda e: e.affine_select(out=identf[:], in_=identf[:], pattern=[[-1, 128]],
                                               compare_op=ALU.not_equal, fill=1.0, base=0,
                                               channel_multiplier=1), reads=[r_const], writes=[r_const])
        c.op("dve", lambda e: e.tensor_copy(out=ident[:], in_=identf[:]), reads=[r_const], writes=[r_const])
        c.op("dve", lambda e: e.memset(ones[:], 1.0), writes=[r_const])

        def cut(k):
            if _LIM.get("cut") == k:
                c.muted = True

        wpool = {"buf": [], "res": [], "n": 0}
        wctr = [0]

        def make_wpool(es_, n):
            tag = "%d" % len(wpool.setdefault("gen", []))
            wpool["gen"].append(n)
            wpool["buf"] = [c.sb(es_, "wbuf%s_%d" % (tag, i), [128, 16, 512], BF16) for i in range(n)]
            wpool["res"] = [Res() for _ in range(n)]
            wpool["n"] = n
            wctr[0] = 0

        class WStream:
            def __init__(self, blocks):
                self.blocks = list(blocks)
                self.issued = 0
                self.pos = 0
                self.base = wctr[0]

            def _issue(self):
                name, blk = self.blocks[self.issued]
                i = (self.base + self.issued) % wpool["n"]
                c.dma("sp", wpool["buf"][i][:], wscr[name][blk], reads=[wscr_res[name][blk]],
                      writes=[wpool["res"][i]])
                self.issued += 1

            def get(self):
                while self.issued < len(self.blocks) and self.issued < self.pos + wpool["n"]:
                    self._issue()
                i = (self.base + self.pos) % wpool["n"]
                self.pos += 1
                wctr[0] = self.base + self.pos
                return wpool["buf"][i], wpool["res"][i]

        def load_gain(es_, name, ap, width=D):
            t = c.sb(es_, "g_" + name, [128, width], F32)
            r = Res()
            c.dma("sp", t[:], ap[0:1, :].partition_broadcast(128), writes=[r])
            return t, r

        bankctr = [0]

        def bank(lo, hi):
            n = hi - lo
            i = lo + bankctr[0] % n
            bankctr[0] += 1
            return i

        NSM = 4
        sm = [c.sb(es, "sm%d" % i, [128, 8], F32) for i in range(NSM)]
        r_sm = [Res() for _ in range(NSM)]
        smctr = [0]
        junk = c.sb(es, "junk", [128, D], BF16)
        r_junk = Res()

        def rms_h(x_t, r_x, P, gain_t, r_gain, h_t, r_h):
            i = smctr[0] % NSM
            smctr[0] += 1
            s, rs = sm[i], r_sm[i]
            c.op("act", lambda e: e.activation(out=junk[:P, :], in_=x_t[:P, :], func=AF.Square,
                                               accum_out=s[:P, 0:1]), reads=[r_x], writes=[r_junk, rs])
            c.op("act", lambda e: e.activation(out=s[:P, 1:2], in_=s[:P, 0:1], func=AF.Sqrt,
                                               scale=1.0 / D, bias=epsc[:P, 0:1]), reads=[rs, r_const], writes=[rs])
            c.op("dve", lambda e: e.reciprocal(out=s[:P, 2:3], in_=s[:P, 1:2]), reads=[rs], writes=[rs])
            c.op("dve", lambda e: e.scalar_tensor_tensor(out=h_t[:P, :], in0=x_t[:P, :], scalar=s[:P, 2:3],
                                                         in1=gain_t[:P, :], op0=ALU.mult, op1=ALU.mult),
                 reads=[r_x, rs, r_gain], writes=[r_h])

        epsc = c.sb(es, "epsc", [128, 1], F32)
        c.op("dve", lambda e: e.memset(epsc[:], EPS), writes=[r_const])

        def transpose_to(h_t, r_h, P, nchunk, dst_fn, r_dst, evac="act"):
            for c0 in range(0, nchunk, 8):
                c1 = min(nchunk, c0 + 8)
                b = bank(0, 2)
                pv = pbb[b][:, 0:(c1 - c0) * 128].rearrange("p (a t) -> p a t", t=128)

                def f(e, c0=c0, c1=c1, pv=pv):
                    inst = None
                    for k in range(c0, c1):
                        inst = e.transpose(out=pv[:, k - c0, 0:P], in_=h_t[:P, k * 128:(k + 1) * 128],
                                           identity=ident[:P, :P])
                    return inst
                c.op("pe", f, reads=[r_h, r_const], writes=[rpb[b]])
                dst = dst_fn(c0, c1)
                if evac == "act":
                    c.op("act", lambda e, dst=dst, pv=pv: e.activation(out=dst, in_=pv[:, :, 0:P], func=AF.Copy),
                         reads=[rpb[b]], writes=[r_dst])
                else:
                    c.op("dve", lambda e, dst=dst, pv=pv: e.tensor_copy(out=dst, in_=pv[:, :, 0:P]),
                         reads=[rpb[b]], writes=[r_dst])

        def mm_tok(hT, r_hT, t0, P, wt, r_w, b, ncol=512, c0=0):
            def f(e):
                inst = None
                for kc in range(16):
                    inst = e.matmul(pb[b][:P, 0:ncol], lhsT=hT[:, kc, t0:t0 + P], rhs=wt[:, kc, c0:c0 + ncol],
                                    start=(kc == 0), stop=(kc == 15))
                return inst
            c.op("pe", f, reads=[r_hT, r_w], writes=[rpb[b]])

        def mm_feat(hT, r_hT, t0, ntok, wt, r_w, ct, b):
            def f(e):
                inst = None
                for kc in range(16):
                    inst = e.matmul(pb[b][:, 0:ntok], lhsT=wt[:, kc, ct * 128:(ct + 1) * 128],
                                    rhs=hT[:, kc, t0:t0 + ntok], start=(kc == 0), stop=(kc == 15))
                return inst
            c.op("pe", f, reads=[r_hT, r_w], writes=[rpb[b]])

        with contextlib.suppress(_SkipPhase), contextlib.ExitStack() as es0:
            if 0 not in _PH:
                raise _SkipPhase()
            NST = 3
            stf = [c.sb(es0, "stf%d" % i, [128, 16, 512], F32) for i in range(NST)]
            stb = [c.sb(es0, "stb%d" % i, [128, 16, 512], BF16) for i in range(NST)]
            r_stf = [Res() for _ in range(NST)]
            r_stb = [Res() for _ in range(NST)]
            n = 0
            wlist = [("w_mk", wsq["w_mk"]), ("w_mv", wsq["w_mv"]), ("w_in", w_in), ("w_out", wsq["w_out"]),
                     ("w_mq", wsq["w_mq"]), ("w_mo", wsq["w_mo"]), ("peer_wq", wsq["peer_wq"])]
            for name, W in wlist:
                Wv = W.rearrange("(kc p) n -> p kc n", p=128)
                for blk in range(WBLK[name]):
                    i = n % NST
                    c.dma("sp", stf[i][:], Wv[:, :, blk * 512:(blk + 1) * 512], writes=[r_stf[i]])
                    if n % 2 == 0:
                        c.op("dve", lambda e, i=i: e.tensor_copy(out=stb[i][:], in_=stf[i][:]),
                             reads=[r_stf[i]], writes=[r_stb[i]])
                    else:
                        c.op("pool", lambda e, i=i: e.tensor_copy(out=stb[i][:], in_=stf[i][:]),
                             reads=[r_stf[i]], writes=[r_stb[i]])
                    c.dma("act", wscr[name][blk], stb[i][:], reads=[r_stb[i]], writes=[wscr_res[name][blk]])
                    n += 1
            if 5 in _PH or 6 in _PH:
                for src, dst in ((peer_u, tabu), (peer_v, tabv)):
                    sv = src.rearrange("(c p r) d -> c p (r d)", p=128, r=4)
                    dv = dst.rearrange("(c p r) d -> c p (r d)", p=128, r=4)
                    for ci in range(32):
                        i = n % NST
                        c.dma("sp", stf[i][:].rearrange("p a b -> p (a b)"), sv[ci], writes=[r_stf[i]])
                        if n % 2 == 0:
                            c.op("dve", lambda e, i=i: e.tensor_copy(out=stb[i][:], in_=stf[i][:]),
                                 reads=[r_stf[i]], writes=[r_stb[i]])
                        else:
                            c.op("pool", lambda e, i=i: e.tensor_copy(out=stb[i][:], in_=stf[i][:]),
                                 reads=[r_stf[i]], writes=[r_stb[i]])
                        c.dma("act", dv[ci], stb[i][:].rearrange("p a b -> p (a b)"), reads=[r_stb[i]], writes=[r_tab])
                        n += 1
            c.barrier()

        mkT = c.sb(es, "mkT", [128, 16, 256], BF16)
        mv_bf = c.sb(es, "mv_bf", [128, 2, D], BF16)
        r_mkT = Res()
        r_mvbf = Res()

        with contextlib.suppress(_SkipPhase), contextlib.ExitStack() as es1:
            if 1 not in _PH:
                raise _SkipPhase()
            make_wpool(es1, 3)
            g_mt, r_gmt = load_gain(es1, "memtok", gains["norm_memtok"])
            mT = c.sb(es1, "mT", [128, 16, 256], BF16)
            r_mT = Res()
            xm = c.sb(es1, "xm", [128, D], F32)
            r_xm = Res()
            hm = c.sb(es1, "hm1", [128, D], BF16)
            r_hm = Res()
            mk_bf = c.sb(es1, "mk_bf", [128, 2, D], BF16)
            r_mkbf = Res()
            kvf = [c.sb(es1, "kvf%d" % i, [128, D], F32) for i in range(2)]
            r_kvf = [Res() for _ in range(2)]
            for mt in range(2):
                c.dma("sp", xm[:], mem[mt * 128:(mt + 1) * 128, :], writes=[r_xm])
                rms_h(xm, r_xm, 128, g_mt, r_gmt, hm, r_hm)
                transpose_to(hm, r_hm, 128, 16, lambda c0, c1, mt=mt: mT[:, c0:c1, mt * 128:(mt + 1) * 128], r_mT)
            n = 0
            for kv, name in enumerate(("w_mk", "w_mv")):
                bf = mk_bf if kv == 0 else mv_bf
                r_bf = r_mkbf if kv == 0 else r_mvbf
                for mt in range(2):
                    i = n % 2
                    n += 1
                    ws = WStream([(name, blk) for blk in range(4)])
                    for blk in range(4):
                        wt, r_w = ws.get()
                        b = bank(2, 6)
                        mm_tok(mT, r_mT, mt * 128, 128, wt, r_w, b)
                        c.op("act", lambda e, i=i, blk=blk, b=b: e.activation(
                            out=kvf[i][:, blk * 512:(blk + 1) * 512], in_=pb[b][:, :], func=AF.Copy),
                            reads=[rpb[b]], writes=[r_kvf[i]])
                    c.op("dve", lambda e, i=i, mt=mt, bf=bf: e.tensor_copy(out=bf[:, mt, :], in_=kvf[i][:]),
                         reads=[r_kvf[i]], writes=[r_bf])
                    c.dma("sp", memkv[mt * 128:(mt + 1) * 128, kv, :], kvf[i][:], reads=[r_kvf[i]])
            for mt in range(2):
                transpose_to(mk_bf[:, mt, :], r_mkbf, 128, 16,
                             lambda c0, c1, mt=mt: mkT[:, c0:c1, mt * 128:(mt + 1) * 128], r_mkT)
            c.barrier()

        G = 2
        GT = G * 128

        def gmlp_tile(es_, P, gv_j, r_gv, gu_j, r_gu, sga_j, r_sga, Wmix, r_W, bs, r_bs, lng, lnb, r_ln,
                      tmp, mA, r_mA, vln_out=None, r_vln=None):
            st, mvv, vt, vlnb, gs = tmp["st"], tmp["mvv"], tmp["vt"], tmp["vlnb"], tmp["gs"]
            r_t = tmp["r"]
            c.op("dve", lambda e: e.bn_stats(out=st[:P, 0, :], in_=gv_j[:P, 0:512]), reads=[r_gv], writes=[r_t])
            c.op("dve", lambda e: e.bn_stats(out=st[:P, 1, :], in_=gv_j[:P, 512:1024]), reads=[r_gv], writes=[r_t])
            c.op("dve", lambda e: e.bn_aggr(out=mvv[:P, 0:2], in_=st[:P].rearrange("p a b -> p (a b)")),
                 reads=[r_t], writes=[r_t])
            c.op("act", lambda e: e.activation(out=mvv[:P, 2:3], in_=mvv[:P, 1:2], func=AF.Sqrt,
                                               bias=epsc[:P, 0:1], scale=1.0), reads=[r_t, r_const], writes=[r_t])
            c.op("dve", lambda e: e.reciprocal(out=mvv[:P, 3:4], in_=mvv[:P, 2:3]), reads=[r_t], writes=[r_t])
            c.op("dve", lambda e: e.tensor_scalar(out=vt[:P, :], in0=gv_j[:P, :], scalar1=mvv[:P, 0:1],
                                                  scalar2=mvv[:P, 3:4], op0=ALU.subtract, op1=ALU.mult),
                 reads=[r_gv, r_t], writes=[r_t])
            c.op("dve", lambda e: e.tensor_tensor(out=vt[:P, :], in0=vt[:P, :], in1=lng[:P, :], op=ALU.mult),
                 reads=[r_t, r_ln], writes=[r_t])
            if vln_out is None:
                c.op("pool", lambda e: e.tensor_tensor(out=vlnb[:P, :], in0=vt[:P, :], in1=lnb[:P, :], op=ALU.add),
                     reads=[r_t, r_ln], writes=[r_t])
            else:
                c.op("pool", lambda e: e.tensor_tensor(out=vln_out[:P, :], in0=vt[:P, :], in1=lnb[:P, :],
                                                       op=ALU.add), reads=[r_t, r_ln], writes=[r_vln])
                c.op("act", lambda e: e.activation(out=vlnb[:P, :], in_=vln_out[:P, :], func=AF.Copy),
                     reads=[r_vln], writes=[r_t])

            def f(e):
                inst = None
                for g in range(8):
                    inst = e.matmul(pb[6 + g // 4][:P, (g % 4) * 128:(g % 4 + 1) * 128], lhsT=Wmix[:P, g, :P],
                                    rhs=vlnb[:P, g * 128:(g + 1) * 128], start=True, stop=True)
                return inst
            c.op("pe", f, reads=[r_t, r_W], writes=[rpb[6], rpb[7]])
            for half in range(2):
                c.op("dve", lambda e, half=half: e.tensor_tensor(
                    out=vt[:P, half * 512:(half + 1) * 512].rearrange("p (g d) -> p g d", d=128),
                    in0=pb[6 + half][:P, :].rearrange("p (g d) -> p g d", d=128),
                    in1=bs[:P, half * 4:half * 4 + 4].unsqueeze(2).to_broadcast([P, 4, 128]), op=ALU.add),
                    reads=[rpb[6 + half], r_bs], writes=[r_t])
            c.op("pool", lambda e: e.tensor_tensor(out=gs[:P, :], in0=gu_j[:P, :], in1=sga_j[:P, :], op=ALU.mult),
                 reads=[r_gu, r_sga], writes=[r_t])
            c.op("dve", lambda e: e.tensor_tensor(out=mA[:P, :], in0=vt[:P, :], in1=gs[:P, :], op=ALU.mult),
                 reads=[r_t], writes=[r_mA])

        def gmlp_tmp(es_, tag):
            return {"st": c.sb(es_, "g_st" + tag, [128, 2, 6], F32), "mvv": c.sb(es_, "g_mvv" + tag, [128, 4], F32),
                    "vt": c.sb(es_, "g_vt" + tag, [128, 1024], F32), "vlnb": c.sb(es_, "g_vlnb" + tag, [128, 1024], BF16),
                    "gs": c.sb(es_, "g_gs" + tag, [128, 1024], BF16), "r": Res()}

        with contextlib.suppress(_SkipPhase), contextlib.ExitStack() as es2:
            if 2 not in _PH:
                raise _SkipPhase()
            make_wpool(es2, 3)
            g_mix, r_gmix = load_gain(es2, "mix", gains["norm_mix"])
            lng, r_ln = load_gain(es2, "lng", sgu_ln_g, 1024)
            lnb = c.sb(es2, "g_lnb", [128, 1024], F32)
            c.dma("sp", lnb[:], sgu_ln_b[0:1, :].partition_broadcast(128), writes=[r_ln])
            wsf = c.sb(es2, "wsf", [128, 8, 128], F32)
            WsT = c.sb(es2, "WsT", [128, 8, 128], BF16)
            r_ws = Res()
            c.dma("sp", wsf[:], sgu_wT.rearrange("g j i -> j g i"), writes=[r_ws])
            c.op("pool", lambda e: e.affine_select(out=wsf[:], in_=wsf[:], pattern=[[0, 8], [1, 128]],
                                                   compare_op=ALU.is_ge, fill=0.0, base=0, channel_multiplier=-1),
                 reads=[r_ws], writes=[r_ws])
            c.op("dve", lambda e: e.tensor_copy(out=WsT[:], in_=wsf[:]), reads=[r_ws], writes=[r_ws])
            bsT = c.sb(es2, "bsT", [128, 8], F32)
            r_bs = Res()
            c.dma("sp", bsT[:], sgu_bT[:, :], writes=[r_bs])

            hT = [c.sb(es2, "hT%d" % i, [128, 16, GT], BF16) for i in range(2)]
            r_hT = [Res() for _ in range(2)]
            xt = [c.sb(es2, "xt%d" % i, [128, D], F32) for i in range(2)]
            r_xt = [Res() for _ in range(2)]
            hb = [c.sb(es2, "hb%d" % i, [128, D], BF16) for i in range(2)]
            r_hb = [Res() for _ in range(2)]
            gv = c.sb(es2, "gv", [128, G, 1024], F32)
            gu = c.sb(es2, "gu", [128, G, 1024], BF16)
            sga = c.sb(es2, "sga", [128, G, 1024], BF16)
            r_gv = [Res() for _ in range(G)]
            r_gu = [Res() for _ in range(G)]
            r_sga = [Res() for _ in range(G)]
            gtmp = gmlp_tmp(es2, "p")
            mA = c.sb(es2, "mA", [128, 1024], BF16)
            r_mA = Res()
            mst = [c.sb(es2, "mst%d" % i, [128, 8, 128], BF16) for i in range(2)]
            r_mst = [Res() for _ in range(2)]
            fst = [c.sb(es2, "fst%d" % i, [128, 4, GT], BF16) for i in range(2)]
            r_fst = [Res() for _ in range(2)]
            kvo = [c.sb(es2, "kvo%d" % i, [128, 512], F32) for i in range(2)]
            r_kvo = [Res() for _ in range(2)]
            ctr = {"x": 0, "f": 0, "k": 0, "m": 0}

            groups = [("h", g) for g in range(NT // G)][:_LIM["groups"]] + [("o", g) for g in range(NT // G)][:_LIM["groups"]]

            def prep_group(gidx):
                kind, g = groups[gidx]
                src = xh if kind == "h" else xo
                hbuf = gidx % 2
                for j in range(G):
                    i = ctr["x"] % 2
                    ctr["x"] += 1
                    t0 = g * GT + j * 128
                    c.dma("sp", xt[i][:], src[t0:t0 + 128, :], writes=[r_xt[i]])
                    rms_h(xt[i], r_xt[i], 128, g_mix, r_gmix, hb[i], r_hb[i])
                    transpose_to(hb[i], r_hb[i], 128, 16,
                                 lambda c0, c1, j=j, hbuf=hbuf: hT[hbuf][:, c0:c1, j * 128:(j + 1) * 128], r_hT[hbuf])

            def feat_block(ws, hbuf, dst, r_dst_list, tcol0, sigmoid=False):
                wt, r_w = ws.get()
                i = ctr["f"] % 2
                ctr["f"] += 1
                for ct in range(4):
                    b = bank(2, 6)
                    mm_feat(hT[hbuf], r_hT[hbuf], 0, GT, wt, r_w, ct, b)
                    if sigmoid:
                        c.op("act", lambda e, b=b, ct=ct, i=i: e.activation(out=fst[i][:, ct, :], in_=pb[b][:, 0:GT],
                                                                          func=AF.Sigmoid),
                             reads=[rpb[b]], writes=[r_fst[i]])
                    elif ct % 2 == 0:
                        c.op("act", lambda e, b=b, ct=ct, i=i: e.activation(out=fst[i][:, ct, :], in_=pb[b][:, 0:GT],
                                                                          func=AF.Copy),
                             reads=[rpb[b]], writes=[r_fst[i]])
                    else:
                        c.op("dve", lambda e, b=b, ct=ct, i=i: e.tensor_copy(out=fst[i][:, ct, :], in_=pb[b][:, 0:GT]),
                             reads=[rpb[b]], writes=[r_fst[i]])
                c.dma("act", dst[:, :, tcol0:tcol0 + GT].rearrange("g p t -> p g t"), fst[i][:],
                      reads=[r_fst[i]], writes=r_dst_list)

            prep_group(0)
            for gidx, (kind, g) in enumerate(groups):
                hbuf = gidx % 2
                if kind == "h":
                    ws = WStream([("w_in", b) for b in (10, 11, 12, 13)])
                    for bi, blk in enumerate((10, 11, 12, 13)):
                        dst = kTs if blk < 12 else vTs
                        rr = r_kTs if blk < 12 else r_vTs
                        h0 = (blk % 2) * 4
                        feat_block(ws, hbuf, dst[h0:h0 + 4], rr[h0:h0 + 4], g * GT)
                        if bi == 0 and gidx + 1 < len(groups):
                            prep_group(gidx + 1)
                    continue
                order = [2, 3, 0, 1, 14, 15, 10, 11, 12, 13, 4, 5, 6, 7, 8, 9, 10, 11, 12, 13, 16, 17]
                ws = WStream([("w_in", b) for b in order])
                for bi, blk in enumerate(order[:10]):
                    wt, r_w = ws.get()
                    for j in range(G):
                        b = bank(2, 6)
                        mm_tok(hT[hbuf], r_hT[hbuf], j * 128, 128, wt, r_w, b)
                        if blk in (2, 3):
                            c.op("act", lambda e, b=b, j=j, blk=blk: e.activation(
                                out=gv[:, j, (blk - 2) * 512:(blk - 1) * 512], in_=pb[b][:, :], func=AF.Gelu_apprx_tanh),
                                reads=[rpb[b]], writes=[r_gv[j]])
                        elif blk in (0, 1):
                            c.op("act", lambda e, b=b, j=j, blk=blk: e.activation(
                                out=gu[:, j, blk * 512:(blk + 1) * 512], in_=pb[b][:, :], func=AF.Gelu_apprx_tanh),
                                reads=[rpb[b]], writes=[r_gu[j]])
                        elif blk in (14, 15):
                            c.op("act", lambda e, b=b, j=j, blk=blk: e.activation(
                                out=sga[:, j, (blk - 14) * 512:(blk - 13) * 512], in_=pb[b][:, :], func=AF.Sigmoid),
                                reads=[rpb[b]], writes=[r_sga[j]])
                        else:
                            i = ctr["k"] % 2
                            ctr["k"] += 1
                            c.op("dve", lambda e, b=b, i=i: e.tensor_copy(out=kvo[i][:], in_=pb[b][:, :]),
                                 reads=[rpb[b]], writes=[r_kvo[i]])
                            t0 = g * GT + j * 128
                            kv = 0 if blk < 12 else 1
                            half = blk % 2
                            c.dma("act", win_p[t0:t0 + 128, kv, half * 512:(half + 1) * 512], kvo[i][:],
                                  reads=[r_kvo[i]])
                    if bi == 0 and gidx + 1 < len(groups):
                        prep_group(gidx + 1)
                    if bi == 5:
                        for j in range(G):
                            gmlp_tile(es2, 128, gv[:, j, :], r_gv[j], gu[:, j, :], r_gu[j], sga[:, j, :], r_sga[j],
                                      WsT, r_ws, bsT, r_bs, lng, lnb, r_ln, gtmp, mA, r_mA)
                            i = ctr["m"] % 2
                            ctr["m"] += 1
                            transpose_to(mA, r_mA, 128, 8, lambda c0, c1, i=i: mst[i][:, c0:c1, :], r_mst[i],
                                         evac="dve")
                            t0 = g * GT + j * 128
                            c.dma("act", mTs[0:8, :, t0:t0 + 128].rearrange("g p t -> p g t"), mst[i][:],
                                  reads=[r_mst[i]], writes=r_mTs[0:8])
                for blk in order[10:]:
                    if 4 <= blk <= 9:
                        gh0 = ((blk - 4) // 2) * 8 + ((blk - 4) % 2) * 4
                        feat_block(ws, hbuf, qTs[gh0:gh0 + 4], r_qTs[gh0:gh0 + 4], g * GT)
                    elif blk in (10, 11):
                        h0 = (blk % 2) * 4
                        feat_block(ws, hbuf, kTs[h0:h0 + 4], r_kTs[h0:h0 + 4], TOK + g * GT)
                    elif blk in (12, 13):
                        h0 = (blk % 2) * 4
                        feat_block(ws, hbuf, vTs[h0:h0 + 4], r_vTs[h0:h0 + 4], TOK + g * GT)
                    else:
                        h0 = (blk % 2) * 4
                        feat_block(ws, hbuf, gbTs[h0:h0 + 4], r_gbTs[h0:h0 + 4], g * GT, sigmoid=True)
            c.barrier()

        with contextlib.suppress(_SkipPhase), contextlib.ExitStack() as es3:
            if 3 not in _PH:
                raise _SkipPhase()
            EBT = c.sb(es3, "EBT", [128, 24, 256], BF16)
            r_EB = Res()
            bm = c.sb(es3, "bm", [128, 256], F32)
            r_bm = Res()
            c.dma("sp", bm[:], bandmask.rearrange("p a b -> p (a b)"), writes=[r_bm])
            btmp = [c.sb(es3, "btmp%d" % i, [128, 256], F32) for i in range(2)]
            r_btmp = [Res() for _ in range(2)]
            for gh in range(24):
                i = gh % 2
                c.dma("sp", btmp[i][:], biasT[gh].rearrange("p a b -> p (a b)"), writes=[r_btmp[i]])
                c.op("act", lambda e, i=i: e.activation(out=btmp[i][:], in_=btmp[i][:], func=AF.Exp),
                     reads=[r_btmp[i]], writes=[r_btmp[i]])
                c.op("dve", lambda e, i=i, gh=gh: e.tensor_tensor(out=EBT[:, gh, :], in0=btmp[i][:], in1=bm[:],
                                                                  op=ALU.mult),
                     reads=[r_btmp[i], r_bm], writes=[r_EB])
            pf = c.sb(es3, "pf", [128, 1], F32)
            r_pf = Res()
            c.dma("sp", pf[:], pflag[:, :], writes=[r_pf])

            kT = [c.sb(es3, "kT%d" % i, [128, 2 * TOK], BF16) for i in range(2)]
            vT = [c.sb(es3, "vT%d" % i, [128, 2 * TOK], BF16) for i in range(2)]
            qT3 = [c.sb(es3, "qT3%d" % i, [128, 3, TOK], BF16) for i in range(2)]
            gbT = [c.sb(es3, "gbT%d" % i, [128, TOK], BF16) for i in range(2)]
            r_hd = [Res() for _ in range(2)]
            acc = c.sb(es3, "acc", [128, 2, TOK], F32)
            r_acc = Res()
            ybT = c.sb(es3, "ybT", [128, TOK], BF16)
            r_ybT = Res()
            NVP = 4
            Vp = [c.sb(es3, "Vp%d" % i, [128, 128], BF16) for i in range(NVP)]
            r_Vp = [Res() for _ in range(NVP)]
            pTf = [c.sb(es3, "pTf%d" % i, [128, 256], BF16) for i in range(2)]
            pT = [c.sb(es3, "pT%d" % i, [128, 256], BF16) for i in range(2)]
            r_pTf = [Res() for _ in range(2)]
            r_pT = [Res() for _ in range(2)]
            qTs4 = qTs.rearrange("(g h) p t -> g h p t", h=8)
            actr = {"vp": 0, "u": 0}

            def load_head(h):
                i = h % 2
                c.dma("sp", kT[i][:], kTs[h], reads=[r_kTs[h]], writes=[r_hd[i]])
                c.dma("sp", vT[i][:], vTs[h], reads=[r_vTs[h]], writes=[r_hd[i]])
                c.dma("sp", qT3[i][:], qTs4[:, h].rearrange("g p t -> p g t"),
                      reads=[r_qTs[h], r_qTs[8 + h], r_qTs[16 + h]], writes=[r_hd[i]])
                c.dma("sp", gbT[i][:], gbTs[h], reads=[r_gbTs[h]], writes=[r_hd[i]])

            def make_vp(i, dil, a0, r):
                k = actr["vp"] % NVP
                actr["vp"] += 1
                b = bank(2, 4)
                vv = vT[i][:].rearrange("p (a b) -> p a b", b=dil)
                c.op("pe", lambda e: e.transpose(out=pbb[b][:, 0:128], in_=vv[:, a0:a0 + 128, r], identity=ident[:]),
                     reads=[r_hd[i], r_const], writes=[rpb[b]])
                c.op("dve", lambda e: e.tensor_copy(out=Vp[k][:], in_=pbb[b][:, 0:128]), reads=[rpb[b]],
                     writes=[r_Vp[k]])
                return k

            load_head(0)
            NH = 8 if _LIM["heads"] is None else _LIM["heads"]
            for h in range(NH):
                i = h % 2
                if h + 1 < NH:
                    load_head(h + 1)
                for gi, dil in enumerate(DILS):
                    span = 128 * dil
                    nbk = TOK // span
                    kv_ = kT[i][:].rearrange("p (a b) -> p a b", b=dil)
                    qv_ = qT3[i][:, gi, :].rearrange("p (a b) -> p a b", b=dil)
                    accv = acc[:].rearrange("p s (a b) -> p s a b", b=dil)
                    for r in range(dil):
                        kprev = make_vp(i, dil, (TOK - span) // dil, r)
                        for bb in range(nbk):
                            a_own = (TOK + bb * span) // dil
                            kcur = make_vp(i, dil, a_own, r)
                            u = actr["u"] % 2
                            actr["u"] += 1
                            b_s = bank(0, 2)
                            a_prev = a_own - 128

                            def fs(e, a_prev=a_prev, bb=bb, r=r, b_s=b_s, kv_=kv_, qv_=qv_):
                                inst = None
                                for jt in range(2):
                                    inst = e.matmul(pb[b_s][:, jt * 128:(jt + 1) * 128],
                                                    lhsT=kv_[:, a_prev + jt * 128:a_prev + (jt + 1) * 128, r],
                                                    rhs=qv_[:, bb * 128:(bb + 1) * 128, r], start=True, stop=True)
                                return inst
                            c.op("pe", fs, reads=[r_hd[i]], writes=[rpb[b_s]])
                            c.op("act", lambda e, u=u, b_s=b_s: e.activation(out=pTf[u][:], in_=pb[b_s][:, 0:256],
                                                                            func=AF.Exp, scale=ATT_SCALE),
                                 reads=[rpb[b_s]], writes=[r_pTf[u]])
                            c.op("pool", lambda e, u=u, gi=gi, h=h: e.tensor_tensor(
                                out=pT[u][:], in0=pTf[u][:], in1=EBT[:, gi * 8 + h, :], op=ALU.mult),
                                reads=[r_pTf[u], r_EB], writes=[r_pT[u]])
                            if bb == 0:
                                c.op("pool", lambda e, u=u: e.tensor_scalar(
                                    out=pT[u][:, 0:128], in0=pT[u][:, 0:128], scalar1=pf[:, 0:1], scalar2=None,
                                    op0=ALU.mult), reads=[r_pT[u], r_pf], writes=[r_pT[u]])
                            b_o = bank(4, 6)

                            def fo(e, u=u, b_o=b_o, kprev=kprev, kcur=kcur):
                                e.matmul(pb[b_o][:, 0:128], lhsT=Vp[kprev][:], rhs=pT[u][:, 0:128], start=True, stop=False)
                                e.matmul(pb[b_o][:, 0:128], lhsT=Vp[kcur][:], rhs=pT[u][:, 128:256], start=False, stop=True)
                                e.matmul(pb[b_o][:, 128:256], lhsT=ones[:], rhs=pT[u][:, 0:128], start=True, stop=False)
                                return e.matmul(pb[b_o][:, 128:256], lhsT=ones[:], rhs=pT[u][:, 128:256],
                                                start=False, stop=True)
                            c.op("pe", fo, reads=[r_pT[u], r_Vp[kprev], r_Vp[kcur], r_const], writes=[rpb[b_o]])
                            av = accv[:, :, bb * 128:(bb + 1) * 128, r]
                            pv = pb[b_o][:, 0:256].rearrange("p (s t) -> p s t", s=2)
                            if gi == 0:
                                c.op("dve", lambda e, av=av, pv=pv: e.tensor_copy(out=av, in_=pv),
                                     reads=[rpb[b_o]], writes=[r_acc])
                            else:
                                c.op("dve", lambda e, av=av, pv=pv: e.tensor_tensor(out=av, in0=pv, in1=av, op=ALU.add),
                                     reads=[rpb[b_o], r_acc], writes=[r_acc])
                            kprev = kcur
                c.op("dve", lambda e: e.reciprocal(out=acc[:, 1, :], in_=acc[:, 1, :]), reads=[r_acc], writes=[r_acc])
                c.op("dve", lambda e: e.tensor_tensor(out=acc[:, 0, :], in0=acc[:, 0, :], in1=acc[:, 1, :], op=ALU.mult),
                     reads=[r_acc], writes=[r_acc])
                c.op("pool", lambda e, i=i: e.tensor_tensor(out=ybT[:], in0=acc[:, 0, :], in1=gbT[i][:], op=ALU.mult),
                     reads=[r_acc, r_hd[i]], writes=[r_ybT])
                c.dma("sp", mTs[8 + h], ybT[:], reads=[r_ybT], writes=[r_mTs[8 + h]])
            c.barrier()


        PS = 128
        mTg_s = c.sb(es, "mTg_s", [128, 16, PS], BF16)
        r_mTgs = Res()
        c.op("pool", lambda e: e.memset(mTg_s[:], 0.0), writes=[r_mTgs])
        with contextlib.suppress(_SkipPhase), contextlib.ExitStack() as ess:
            if 4 not in _PH:
                raise _SkipPhase()
            make_wpool(ess, 2)
            cut(10)
            g_mix, r_gmix = load_gain(ess, "mix_s", gains["norm_mix"])
            lng, r_ln = load_gain(ess, "lng_s", sgu_ln_g, 1024)
            lnb = c.sb(ess, "g_lnb_s", [128, 1024], F32)
            c.dma("sp", lnb[:], sgu_ln_b[0:1, :].partition_broadcast(128), writes=[r_ln])
            wsf = c.sb(ess, "wsf_s", [128, 8, 128], F32)
            WsTs = c.sb(ess, "WsTs", [128, 8, 128], BF16)
            r_ws = Res()
            c.dma("sp", wsf[:], sgu_wTs.rearrange("g j i -> j g i"), writes=[r_ws])
            c.op("pool", lambda e: e.affine_select(out=wsf[:], in_=wsf[:], pattern=[[0, 8], [1, 128]],
                                                   compare_op=ALU.is_ge, fill=0.0, base=0, channel_multiplier=-1),
                 reads=[r_ws], writes=[r_ws])
            c.op("dve", lambda e: e.tensor_copy(out=WsTs[:], in_=wsf[:]), reads=[r_ws], writes=[r_ws])
            bsTs = c.sb(ess, "bsTs", [128, 8], F32)
            r_bs = Res()
            c.dma("sp", bsTs[:], sgu_bTs[:, :], writes=[r_bs])
            cut(11)

            xts = c.sb(ess, "xts", [128, D], F32)
            hbs = c.sb(ess, "hbs", [128, D], BF16)
            hTs = c.sb(ess, "hTs", [128, 16, PS], BF16)
            r_xts, r_hbs, r_hTs = Res(), Res(), Res()
            c.op("pool", lambda e: e.memset(xts[:], 0.0), writes=[r_xts])
            c.dma("sp", xts[:SP, :], xs[:, :], writes=[r_xts])
            cut(12)
            rms_h(xts, r_xts, PS, g_mix, r_gmix, hbs, r_hbs)
            cut(13)
            transpose_to(hbs, r_hbs, PS, 16, lambda c0, c1: hTs[:, c0:c1, :], r_hTs)
            cut(1)

            gv_s = c.sb(ess, "gv_s", [128, 1024], F32)
            gu_s = c.sb(ess, "gu_s", [128, 1024], BF16)
            sga_s = c.sb(ess, "sga_s", [128, 1024], BF16)
            sgb_s = c.sb(ess, "sgb_s", [128, 1024], BF16)
            q_s = c.sb(ess, "q_s", [128, 3072], BF16)
            kf_s = c.sb(ess, "kf_s", [128, 1024], F32)
            vf_s = c.sb(ess, "vf_s", [128, 1024], F32)
            k_sb = c.sb(ess, "k_sb", [128, 1024], BF16)
            vn = c.sb(ess, "vn", [128, 1024], BF16)
            r_gvs, r_gus, r_sgas, r_sgbs, r_qs, r_kfs, r_vfs, r_ksb, r_vn = (Res() for _ in range(9))
            ws = WStream([("w_in", b) for b in range(18)])
            for blk in range(18):
                cut(20 + blk)
                wt, r_w = ws.get()
                b = bank(2, 6)
                mm_tok(hTs, r_hTs, 0, PS, wt, r_w, b)
                src = pb[b][:, :]
                if blk < 2:
                    c.op("act", lambda e, blk=blk, src=src: e.activation(out=gu_s[:, blk * 512:(blk + 1) * 512], in_=src,
                                                                        func=AF.Gelu_apprx_tanh), reads=[rpb[b]], writes=[r_gus])
                elif blk < 4:
                    c.op("act", lambda e, blk=blk, src=src: e.activation(out=gv_s[:, (blk - 2) * 512:(blk - 1) * 512], in_=src,
                                                                        func=AF.Gelu_apprx_tanh), reads=[rpb[b]], writes=[r_gvs])
                elif blk < 10:
                    c.op("act", lambda e, blk=blk, src=src: e.activation(out=q_s[:, (blk - 4) * 512:(blk - 3) * 512], in_=src,
                                                                        func=AF.Copy), reads=[rpb[b]], writes=[r_qs])
                elif blk < 12:
                    c.op("act", lambda e, blk=blk, src=src: e.activation(out=kf_s[:, (blk - 10) * 512:(blk - 9) * 512], in_=src,
                                                                        func=AF.Copy), reads=[rpb[b]], writes=[r_kfs])
                    c.op("dve", lambda e, blk=blk: e.tensor_copy(out=k_sb[:, (blk - 10) * 512:(blk - 9) * 512],
                                                                 in_=kf_s[:, (blk - 10) * 512:(blk - 9) * 512]),
                         reads=[r_kfs], writes=[r_ksb])
                elif blk < 14:
                    c.op("act", lambda e, blk=blk, src=src: e.activation(out=vf_s[:, (blk - 12) * 512:(blk - 11) * 512], in_=src,
                                                                        func=AF.Copy), reads=[rpb[b]], writes=[r_vfs])
                    c.op("dve", lambda e, blk=blk: e.tensor_copy(out=vn[:, (blk - 12) * 512:(blk - 11) * 512],
                                                                 in_=vf_s[:, (blk - 12) * 512:(blk - 11) * 512]),
                         reads=[r_vfs], writes=[r_vn])
                elif blk < 16:
                    c.op("act", lambda e, blk=blk, src=src: e.activation(out=sga_s[:, (blk - 14) * 512:(blk - 13) * 512], in_=src,
                                                                        func=AF.Sigmoid), reads=[rpb[b]], writes=[r_sgas])
                else:
                    c.op("act", lambda e, blk=blk, src=src: e.activation(out=sgb_s[:, (blk - 16) * 512:(blk - 15) * 512], in_=src,
                                                                        func=AF.Sigmoid), reads=[rpb[b]], writes=[r_sgbs])
            cut(40)
            c.dma("sp", win_s[:, 0, :], kf_s[:SP, :], reads=[r_kfs])
            c.dma("sp", win_s[:, 1, :], vf_s[:SP, :], reads=[r_vfs])
            cut(2)
            gtmp_s = gmlp_tmp(ess, "s")
            vlnf_s = c.sb(ess, "vlnf_s", [128, 1024], F32)
            r_vlnf = Res()
            mA_s = c.sb(ess, "mA_s", [128, 1024], BF16)
            r_mAs = Res()
            gmlp_tile(ess, PS, gv_s, r_gvs, gu_s, r_gus, sga_s, r_sgas, WsTs, r_ws, bsTs, r_bs, lng, lnb, r_ln,
                      gtmp_s, mA_s, r_mAs, vln_out=vlnf_s, r_vln=r_vlnf)
            c.dma("sp", sgu_s[:, :], vlnf_s[:SP, :], reads=[r_vlnf])
            transpose_to(mA_s, r_mAs, PS, 8, lambda c0, c1: mTg_s[:, c0:c1, :], r_mTgs)
            cut(3)

            qTs_s = c.sb(ess, "qTs_s", [128, 24, PS], BF16)
            kTn = c.sb(ess, "kTn", [128, 8, PS], BF16)
            gbT_s = c.sb(ess, "gbT_s", [128, 8, PS], BF16)
            r_qTss, r_kTn, r_gbTs_ = Res(), Res(), Res()
            transpose_to(q_s, r_qs, PS, 24, lambda c0, c1: qTs_s[:, c0:c1, :], r_qTss)
            transpose_to(k_sb, r_ksb, PS, 8, lambda c0, c1: kTn[:, c0:c1, :], r_kTn)
            transpose_to(sgb_s, r_sgbs, PS, 8, lambda c0, c1: gbT_s[:, c0:c1, :], r_gbTs_)
            EBs = c.sb(ess, "EBs", [128, 8, 384], BF16)
            EBn = c.sb(ess, "EBn", [128, 8, 96], BF16)
            r_EBs = Res()
            c.op("pool", lambda e: e.memset(EBn[:], 0.0), writes=[r_EBs])
            smk = c.sb(ess, "smk", [128, 384], F32)
            smkn = c.sb(ess, "smkn", [128, 96], F32)
            r_smk = Res()
            c.dma("sp", smk[:], smask.rearrange("p a b -> p (a b)"), writes=[r_smk])
            c.dma("sp", smkn[:SP, :], smask_n.rearrange("p a b -> p (a b)"), writes=[r_smk])
            stmp = [c.sb(ess, "stmp%d" % i, [128, 480], F32) for i in range(2)]
            r_stmp = [Res() for _ in range(2)]
            for h in range(8):
                i = h % 2
                c.dma("sp", stmp[i][:, 0:384], sbias[h].rearrange("p a b -> p (a b)"), writes=[r_stmp[i]])
                c.dma("sp", stmp[i][:SP, 384:480], sbias_n[h].rearrange("p a b -> p (a b)"), writes=[r_stmp[i]])
                c.op("act", lambda e, i=i: e.activation(out=stmp[i][:, 0:384], in_=stmp[i][:, 0:384], func=AF.Exp),
                     reads=[r_stmp[i]], writes=[r_stmp[i]])
                c.op("act", lambda e, i=i: e.activation(out=stmp[i][:SP, 384:480], in_=stmp[i][:SP, 384:480], func=AF.Exp),
                     reads=[r_stmp[i]], writes=[r_stmp[i]])
                c.op("dve", lambda e, i=i, h=h: e.tensor_tensor(out=EBs[:, h, :], in0=stmp[i][:, 0:384], in1=smk[:],
                                                                op=ALU.mult), reads=[r_stmp[i], r_smk], writes=[r_EBs])
                c.op("dve", lambda e, i=i, h=h: e.tensor_tensor(out=EBn[:SP, h, :], in0=stmp[i][:SP, 384:480],
                                                                in1=smkn[:SP, :], op=ALU.mult),
                     reads=[r_stmp[i], r_smk], writes=[r_EBs])
            cut(4)
            Ppad = c.sb(ess, "Ppad", [128, 4, 17 * 96], BF16)
            r_Pp = Res()
            c.op("pool", lambda e: e.memset(Ppad[:], 0.0), writes=[r_Pp])
            Kc = c.sb(ess, "Kc0", [128, 16, 128], F32)
            Vc = c.sb(ess, "Vc0", [128, 16, 128], F32)
            Kcb = [c.sb(ess, "Kcb0", [128, 16, 128], BF16)] * 2
            Vcb = [c.sb(ess, "Vcb%d" % i, [128, 16, 128], BF16) for i in range(2)]
            KT = [c.sb(ess, "KT0", [128, 16, 128], BF16)] * 2
            r_Kc, r_Vc = Res(), Res()
            r_Kcb = [Res()] * 2
            r_Vcb = [Res() for _ in range(2)]
            r_KT = [Res()] * 2
            tmpE = c.sb(ess, "tmpE", [128, 408], BF16)
            r_tmpE = Res()
            qc = [c.sb(ess, "qc%d" % i, [128, 24], BF16) for i in range(2)]
            r_qc = [Res() for _ in range(2)]
            rec_s = c.sb(ess, "rec_s", [128, SP], F32)
            ot_s = c.sb(ess, "ot_s", [128, SP], F32)
            r_recs = Res()
            qv4 = qTs_s[:].rearrange("p (g h) t -> p g h t", h=8)
            tiles_g = ([15], [12, 13, 14, 15], list(range(16)))
            n = 0
            for h in range(8):
                b_o = 4 + h % 2
                b_sum = 6 + h % 2
                for b in range(4):
                    i = n % 2
                    n += 1
                    c.dma("sp", Kc[:], cwin[b, :, 0, h, :].rearrange("(rt p) d -> p rt d", p=128), writes=[r_Kc])
                    c.dma("sp", Vc[:], cwin[b, :, 1, h, :].rearrange("(rt p) d -> p rt d", p=128), writes=[r_Vc])
                    c.op("pool", lambda e, i=i: e.tensor_copy(out=Kcb[i][:], in_=Kc[:]), reads=[r_Kc], writes=[r_Kcb[i]])
                    c.op("act", lambda e, i=i: e.activation(out=Vcb[i][:], in_=Vc[:], func=AF.Copy),
                         reads=[r_Vc], writes=[r_Vcb[i]])
                    transpose_to(Kcb[i][:].rearrange("p a d -> p (a d)"), r_Kcb[i], 128, 16,
                                 lambda c0, c1, i=i: KT[i][:, c0:c1, :], r_KT[i], evac="dve")
                    c.op("dve", lambda e, i=i, h=h, b=b: e.tensor_copy(
                        out=qc[i][:].rearrange("p (g t) -> p g t", t=8), in_=qv4[:, :, h, b * 8:(b + 1) * 8]),
                        reads=[r_qTss], writes=[r_qc[i]])
                    b_s = bank(2, 4)

                    def fs(e, i=i, b_s=b_s, h=h):
                        for rt in range(16):
                            e.matmul(pb[b_s][:, rt * 24:(rt + 1) * 24], lhsT=KT[i][:, rt, :], rhs=qc[i][:],
                                     start=True, stop=True)
                        return e.matmul(pb[b_s][:, 384:408], lhsT=kTn[:, h, :], rhs=qc[i][:], start=True, stop=True)
                    c.op("pe", fs, reads=[r_KT[i], r_qc[i], r_kTn], writes=[rpb[b_s]])
                    c.op("act", lambda e, b_s=b_s: e.activation(out=tmpE[:], in_=pb[b_s][:, 0:408], func=AF.Exp,
                                                                scale=ATT_SCALE), reads=[rpb[b_s]], writes=[r_tmpE])
                    Pv = Ppad[:, b, :].rearrange("p (rt g m) -> p rt g m", g=3, m=32)
                    c.op("dve", lambda e, Pv=Pv, b=b, h=h: e.tensor_tensor(
                        out=Pv[:, 0:16, :, b * 8:(b + 1) * 8],
                        in0=tmpE[:, 0:384].rearrange("p (rt g t) -> p rt g t", g=3, t=8),
                        in1=EBs[:, h, :].rearrange("p (rt g t) -> p rt g t", g=3, t=8), op=ALU.mult),
                        reads=[r_tmpE, r_EBs], writes=[r_Pp])
                    c.op("dve", lambda e, Pv=Pv, b=b, h=h: e.tensor_tensor(
                        out=Pv[:, 16, :, b * 8:(b + 1) * 8],
                        in0=tmpE[:, 384:408].rearrange("p (g t) -> p g t", t=8),
                        in1=EBn[:, h, b * 24:(b + 1) * 24].rearrange("p (g t) -> p g t", t=8), op=ALU.mult),
                        reads=[r_tmpE, r_EBs], writes=[r_Pp])

                    def fo(e, i=i, b=b, h=h, b_o=b_o, b_sum=b_sum, Pv=Pv):
                        mms = []
                        for g in range(3):
                            for rt in tiles_g[g]:
                                mms.append((Vcb[i][:, rt, :], Pv[:, rt, g, :]))
                            mms.append((vn[:, h * 128:(h + 1) * 128], Pv[:, 16, g, :]))
                        inst = None
                        for k, (l, r) in enumerate(mms):
                            first = (b == 0 and k == 0)
                            last = (b == 3 and k == len(mms) - 1)
                            e.matmul(pb[b_o][:, 0:SP], lhsT=l, rhs=r, start=first, stop=last)
                            inst = e.matmul(pb[b_sum][:, 0:SP], lhsT=ones[:], rhs=r, start=first, stop=last)
                        return inst
                    c.op("pe", fo, reads=[r_Pp, r_Vcb[i], r_vn, r_const], writes=[rpb[b_o], rpb[b_sum]])
                c.op("dve", lambda e, b_sum=b_sum: e.reciprocal(out=rec_s[:], in_=pb[b_sum][:, 0:SP]),
                     reads=[rpb[b_sum]], writes=[r_recs])
                c.op("dve", lambda e, b_o=b_o: e.tensor_tensor(out=ot_s[:], in0=pb[b_o][:, 0:SP], in1=rec_s[:],
                                                               op=ALU.mult), reads=[rpb[b_o], r_recs], writes=[r_recs])
                c.op("dve", lambda e, h=h: e.tensor_tensor(out=mTg_s[:, 8 + h, 0:SP], in0=ot_s[:], in1=gbT_s[:, h, 0:SP],
                                                           op=ALU.mult), reads=[r_recs, r_gbTs_], writes=[r_mTgs])
            c.barrier()

        es4 = es.enter_context(contextlib.ExitStack())
        make_wpool(es4, 2)
        g_mem, r_gmem = load_gain(es4, "mem", gains["norm_mem"])
        g_peer, r_gpeer = load_gain(es4, "peer", gains["norm_peer"])
        g_fin, r_gfin = load_gain(es4, "fin", gains["norm_final"])
        keysT = c.sb(es4, "keysT", [128, 2, 8, 128], BF16)
        r_keysT = Res()
        with contextlib.ExitStack() as esk:
            kf = c.sb(esk, "kf", [128, 2, 8, 128], F32)
            kb = c.sb(esk, "kb", [128, 2, 8, 128], BF16)
            r_kf = Res()
            c.dma("sp", kf[:, 0, :, :], keys1.rearrange("h k d -> k h d"), writes=[r_kf])
            c.dma("sp", kf[:, 1, :, :], keys2.rearrange("h k d -> k h d"), writes=[r_kf])
            c.op("dve", lambda e: e.tensor_copy(out=kb[:], in_=kf[:]), reads=[r_kf], writes=[r_kf])
            transpose_to(kb[:].rearrange("p s h d -> p (s h d)"), r_kf, 128, 16,
                         lambda c0, c1: keysT[:].rearrange("p s h k -> p (s h) k")[:, c0:c1, :], r_keysT)
            c.barrier()
        iota_i = c.sb(es4, "iota_i", [128, 16], I32)
        iota16 = c.sb(es4, "iota16", [128, 16], F32)
        r_iota = Res()
        c.op("pool", lambda e: e.iota(iota_i[:], pattern=[[1, 16]], base=0, channel_multiplier=0), writes=[r_iota])
        c.op("dve", lambda e: e.tensor_copy(out=iota16[:], in_=iota_i[:]), reads=[r_iota], writes=[r_iota])

        x1 = [c.sb(es4, "x1_%d" % i, [128, D], F32) for i in range(G)]
        r_x1 = [Res() for _ in range(G)]
        h2 = [c.sb(es4, "h2_%d" % i, [128, D], BF16) for i in range(G)]
        r_h2 = [Res() for _ in range(G)]
        hb4 = c.sb(es4, "hb4", [128, D], BF16)
        r_hb4 = Res()
        bufA = c.sb(es4, "bufA", [128, 16, GT], BF16)
        bufB = c.sb(es4, "bufB", [128, 16, GT], BF16)
        r_bufA = Res()
        r_bufB = Res()
        pTm = c.sb(es4, "pTm", [128, 2, GT], BF16)
        r_pTm = Res()
        rsm = c.sb(es4, "rsm", [128, GT], F32)
        r_rsm = Res()
        sc = c.sb(es4, "sc", [128, 2048], F32)
        cand = c.sb(es4, "cand", [128, 2048], F32)
        r_sc = Res()
        r_cand = Res()
        wk = c.sb(es4, "wk", [128, 256], F32)
        r_wk = Res()
        tv = c.sb(es4, "tv", [128, 16, 16], F32)
        ti = c.sb(es4, "ti", [128, 16, 16], U32)
        tif = c.sb(es4, "tif", [128, 16, 16], F32)
        best = c.sb(es4, "best", [128, 8, 16], F32)
        pos = c.sb(es4, "pos", [128, 8, 16], U32)
        pa = c.sb(es4, "pa", [128, 128], U32)
        pbi = c.sb(es4, "pbi", [128, 128], U32)
        paf = c.sb(es4, "paf", [128, 128], F32)
        pbf = c.sb(es4, "pbf", [128, 128], F32)
        i1s = c.sb(es4, "i1s", [128, 128], F32)
        i2s = c.sb(es4, "i2s", [128, 128], F32)
        eidf = c.sb(es4, "eidf", [128, 128], F32)
        gate = c.sb(es4, "gate", [128, 8, 16], F32)
        gsm = c.sb(es4, "gsm", [128, 24], F32)
        r_pk = Res()
        idxT2 = [c.sb(es4, "idxT%d" % i, [128, 128], I32) for i in range(2)]
        GTt2 = [c.sb(es4, "GTt%d" % i, [128, 128], F32) for i in range(2)]
        r_idxT2 = [Res() for _ in range(2)]
        r_GTt2 = [Res() for _ in range(2)]
        apart = c.sb(es4, "apart", [128, 128, 4], F32)
        AT = c.sb(es4, "AT", [128, 128], F32)
        CT = c.sb(es4, "CT", [128, 128], BF16)
        r_apart = Res()
        r_CT = Res()
        NGB = 7
        gbuf = [c.sb(es4, "gbuf%d" % i, [128, D], BF16) for i in range(NGB)]
        r_gbuf = [Res() for _ in range(NGB)]
        NCB = 4
        cbuf = [c.sb(es4, "cbuf%d" % i, [128, 256], BF16) for i in range(NCB)]
        r_cbuf = [Res() for _ in range(NCB)]
        for i in range(NCB):
            c.op("pool", lambda e, i=i: e.memset(cbuf[i][:], 0.0), writes=[r_cbuf[i]])
        NJ = 2
        junk4 = [c.sb(es4, "junk4_%d" % i, [128, D], BF16) for i in range(NJ)]
        r_junk4 = [Res() for _ in range(NJ)]
        yout = cand
        r_yout = r_cand
        pctr = {"g": 0, "c": 0, "j": 0}

        def cross_attn(qT_, r_q, col0, n, mkT_, r_mk, mv_fn, r_mv, oT_, r_o):
            for h in range(4):
                for mt in range(2):
                    b = bank(2, 6)

                    def f(e, h=h, mt=mt, b=b):
                        inst = None
                        for dc in range(4):
                            inst = e.matmul(pb[b][:, 0:n], lhsT=mkT_[:, h * 4 + dc, mt * 128:(mt + 1) * 128],
                                            rhs=qT_[:, h * 4 + dc, col0:col0 + n], start=(dc == 0), stop=(dc == 3))
                        return inst
                    c.op("pe", f, reads=[r_q] + r_mk, writes=[rpb[b]])
                    c.op("act", lambda e, mt=mt, b=b: e.activation(out=pTm[:, mt, 0:n], in_=pb[b][:, 0:n],
                                                                  func=AF.Exp, scale=MEM_SCALE),
                         reads=[rpb[b]], writes=[r_pTm])
                b = bank(2, 6)

                def fs(e, b=b):
                    e.matmul(pb[b][:, 0:n], lhsT=ones[:], rhs=pTm[:, 0, 0:n], start=True, stop=False)
                    return e.matmul(pb[b][:, 0:n], lhsT=ones[:], rhs=pTm[:, 1, 0:n], start=False, stop=True)
                c.op("pe", fs, reads=[r_pTm, r_const], writes=[rpb[b]])
                c.op("dve", lambda e, b=b: e.reciprocal(out=rsm[:, 0:n], in_=pb[b][:, 0:n]), reads=[rpb[b]],
                     writes=[r_rsm])
                for dc in range(4):
                    b = bank(2, 6)

                    def fo(e, h=h, dc=dc, b=b):
                        c0 = h * 512 + dc * 128
                        e.matmul(pb[b][:, 0:n], lhsT=mv_fn(0, c0), rhs=pTm[:, 0, 0:n], start=True, stop=False)
                        return e.matmul(pb[b][:, 0:n], lhsT=mv_fn(1, c0), rhs=pTm[:, 1, 0:n],
                                        start=False, stop=True)
                    c.op("pe", fo, reads=[r_pTm] + r_mv, writes=[rpb[b]])
                    c.op("dve", lambda e, h=h, dc=dc, b=b: e.tensor_tensor(
                        out=oT_[:, h * 4 + dc, col0:col0 + n], in0=pb[b][:, 0:n], in1=rsm[:, 0:n], op=ALU.mult),
                        reads=[rpb[b], r_rsm], writes=[r_o])

        def top16(P, src_ap, n, dst_v, dst_i, rd, wr):
            c.op("dve", lambda e: e.max(out=dst_v[:, 0:8], in_=src_ap), reads=rd, writes=wr)
            c.op("dve", lambda e: e.max_index(out=dst_i[:, 0:8], in_max=dst_v[:, 0:8], in_values=src_ap),
                 reads=rd + wr, writes=wr)
            c.op("dve", lambda e: e.match_replace(out=wk[:P, 0:n], in_to_replace=dst_v[:, 0:8], in_values=src_ap,
                                                  imm_value=NEG), reads=rd + wr, writes=[r_wk])
            c.op("dve", lambda e: e.max(out=dst_v[:, 8:16], in_=wk[:P, 0:n]), reads=[r_wk], writes=wr)
            c.op("dve", lambda e: e.max_index(out=dst_i[:, 8:16], in_max=dst_v[:, 8:16], in_values=wk[:P, 0:n]),
                 reads=[r_wk] + wr, writes=wr)

        def peer_parts(P, nv, qpT, r_qp, col0, h2_t, r_h2t, x2_t, r_x2t, y_dst, slot):
            idxT, r_idxT, GTt, r_GTt = idxT2[slot], r_idxT2[slot], GTt2[slot], r_GTt2[slot]
            def sel_a():
                def fsc(e):
                    inst = None
                    for hs in range(16):
                        inst = e.matmul(pb[4 + hs // 4][:P, (hs % 4) * 128:(hs % 4 + 1) * 128],
                                        lhsT=qpT[:, hs, col0:col0 + P], rhs=keysT[:, hs % 2, hs // 2, :],
                                        start=True, stop=True)
                    return inst
                c.op("pe", fsc, reads=[r_qp, r_keysT], writes=[rpb[4], rpb[5], rpb[6], rpb[7]])
                for q in range(4):
                    c.op("act", lambda e, q=q: e.activation(out=sc[:P, q * 512:(q + 1) * 512], in_=pb[4 + q][:P, :],
                                                            func=AF.Copy), reads=[rpb[4 + q]], writes=[r_sc])
                scv = sc[:P, :].rearrange("p (a k) -> p a k", k=128)
                for hs in range(16):
                    top16(P, scv[:, hs, :], 128, tv[:P, hs, :], ti[:P, hs, :], [r_sc], [r_pk])
                tv4 = tv[:P].rearrange("p (h s) k -> p h s k", s=2)
                candv = cand[:P, :].rearrange("p (h a b) -> p h a b", a=16, b=16)
                c.op("dve", lambda e: e.tensor_tensor(out=candv, in0=tv4[:, :, 0, :].unsqueeze(3).to_broadcast([P, 8, 16, 16]),
                                                      in1=tv4[:, :, 1, :].unsqueeze(2).to_broadcast([P, 8, 16, 16]),
                                                      op=ALU.add), reads=[r_pk], writes=[r_cand])
                cand3 = cand[:P, :].rearrange("p (h c) -> p h c", c=256)
                for h in range(8):
                    top16(P, cand3[:, h, :], 256, best[:P, h, :], pos[:P, h, :], [r_cand], [r_pk])
                c.op("dve", lambda e: e.tensor_scalar(out=gsm[:P, 0:8], in0=best[:P, :, 0], scalar1=-1.0, scalar2=None,
                                                      op0=ALU.mult), reads=[r_pk], writes=[r_pk])
                for h in range(8):
                    c.op("act", lambda e, h=h: e.activation(out=gate[:P, h, :], in_=best[:P, h, :], func=AF.Exp,
                                                            bias=gsm[:P, h:h + 1], scale=1.0,
                                                            accum_out=gsm[:P, 8 + h:9 + h]), reads=[r_pk], writes=[r_pk])
                c.op("dve", lambda e: e.reciprocal(out=gsm[:P, 16:24], in_=gsm[:P, 8:16]), reads=[r_pk], writes=[r_pk])
                c.op("dve", lambda e: e.tensor_tensor(out=gate[:P], in0=gate[:P],
                                                      in1=gsm[:P, 16:24].unsqueeze(2).to_broadcast([P, 8, 16]),
                                                      op=ALU.mult), reads=[r_pk], writes=[r_pk])
                posf = pos[:P].rearrange("p h k -> p (h k)")
                c.op("dve", lambda e: e.tensor_copy(out=pbf[:P, :], in_=posf), reads=[r_pk], writes=[r_pk])
                c.op("dve", lambda e: e.tensor_scalar(out=paf[:P, :], in0=pbf[:P, :], scalar1=16.0, scalar2=None,
                                                      op0=ALU.is_ge), reads=[r_pk], writes=[r_pk])
                for m in range(2, 16):
                    c.op("dve", lambda e, m=m: e.scalar_tensor_tensor(out=paf[:P, :], in0=pbf[:P, :], scalar=16.0 * m,
                                                                      in1=paf[:P, :], op0=ALU.is_ge, op1=ALU.add),
                         reads=[r_pk], writes=[r_pk])
                c.op("dve", lambda e: e.scalar_tensor_tensor(out=pbf[:P, :], in0=paf[:P, :], scalar=-16.0, in1=pbf[:P, :],
                                                             op0=ALU.mult, op1=ALU.add), reads=[r_pk], writes=[r_pk])
                c.op("dve", lambda e: e.tensor_copy(out=tif[:P], in_=ti[:P]), reads=[r_pk], writes=[r_pk])
                tif4 = tif[:P].rearrange("p (h s) k -> p h s k", s=2)
                ohv = sc[:P, :].rearrange("p (h a b) -> p h a b", a=16, b=16)
                io4 = iota16[:P, :].unsqueeze(1).unsqueeze(1).to_broadcast([P, 8, 16, 16])
                for side, (pf_, dsts) in enumerate(((paf, i1s), (pbf, i2s))):
                    pv = pf_[:P, :].rearrange("p (h k) -> p h k", k=16).unsqueeze(3).to_broadcast([P, 8, 16, 16])
                    c.op("dve", lambda e, pv=pv: e.tensor_tensor(out=ohv, in0=pv, in1=io4, op=ALU.is_equal),
                         reads=[r_pk, r_iota], writes=[r_sc])
                    c.op("dve", lambda e, side=side: e.tensor_tensor(
                        out=ohv, in0=ohv, in1=tif4[:, :, side, :].unsqueeze(2).to_broadcast([P, 8, 16, 16]), op=ALU.mult),
                        reads=[r_sc, r_pk], writes=[r_sc])
                    c.op("dve", lambda e, dsts=dsts: e.tensor_reduce(
                        out=dsts[:P, :].rearrange("p (h k) -> p h k", k=16), in_=ohv, axis=mybir.AxisListType.X,
                        op=ALU.add), reads=[r_sc], writes=[r_pk])
                c.op("dve", lambda e: e.scalar_tensor_tensor(out=eidf[:P, :], in0=i1s[:P, :], scalar=128.0, in1=i2s[:P, :],
                                                             op0=ALU.mult, op1=ALU.add), reads=[r_pk], writes=[r_pk])

            def sel_b():
                c.op("pe", lambda e: e.transpose(out=pb[4][:, 0:P], in_=eidf[:P, :], identity=identf[:P, :P]),
                     reads=[r_pk, r_const], writes=[rpb[4]])
                c.op("dve", lambda e: e.tensor_copy(out=idxT[:, 0:P], in_=pb[4][:, 0:P]), reads=[rpb[4]], writes=[r_idxT])
                c.op("pe", lambda e: e.transpose(out=pb[5][:, 0:P], in_=gate[:P].rearrange("p h k -> p (h k)"),
                                                 identity=identf[:P, :P]), reads=[r_pk, r_const], writes=[rpb[5]])
                c.op("act", lambda e: e.activation(out=GTt[:, 0:P], in_=pb[5][:, 0:P], func=AF.Copy), reads=[rpb[5]],
                     writes=[r_GTt])

            def pass1():
                for t in range(nv):
                    k = pctr["g"] % NGB
                    pctr["g"] += 1
                    c.swdma(gbuf[k][:], tabu[:, :], reads=[r_idxT, r_tab], writes=[r_gbuf[k]],
                            indirect=bass.IndirectOffsetOnAxis(ap=idxT[:, t:t + 1], axis=0))
                    s0 = (t % 2) * 4

                    def fx(e, t=t, s0=s0):
                        inst = None
                        for q in range(4):
                            inst = e.matmul(pb[s0 + q][:, :], lhsT=ident[:P, t:t + 1].to_broadcast([P, 128]),
                                            rhs=h2_t[:P, q * 512:(q + 1) * 512], start=True, stop=True)
                        return inst
                    c.op("pe", fx, reads=[r_h2t, r_const], writes=[rpb[s0 + q] for q in range(4)])
                    edge = (t == nv - 1) or (t == 0)
                    jn = pctr["j"] % NJ
                    pctr["j"] += 1
                    c.op("dve", lambda e, t=t, k=k, s0=s0, jn=jn: e.scalar_tensor_tensor(
                        out=junk4[jn][:], in0=gbuf[k][:, :], scalar=1.0, in1=pball[:, s0 * 512:(s0 + 4) * 512],
                        op0=ALU.mult, op1=ALU.mult, accum_out=apart[:, t, 0:1]),
                        reads=[r_gbuf[k]] + [rpb[s0 + q] for q in range(4)],
                        writes=[r_junk4[jn]] + ([r_apart] if edge else []))
                if nv < P:
                    c.op("dve", lambda e: e.memset(apart[:, nv:P, :], 0.0), writes=[r_apart])
                c.op("dve", lambda e: e.tensor_copy(out=AT[:, 0:P], in_=apart[:, 0:P, 0]), reads=[r_apart], writes=[r_apart])
                c.op("act", lambda e: e.activation(out=AT[:, 0:P], in_=AT[:, 0:P], func=AF.Gelu_apprx_tanh),
                     reads=[r_apart], writes=[r_apart])
                c.op("dve", lambda e: e.tensor_tensor(out=CT[:, 0:P], in0=AT[:, 0:P], in1=GTt[:, 0:P], op=ALU.mult),
                     reads=[r_apart, r_GTt], writes=[r_CT])

            def pass2():
                for t in range(nv):
                    k = pctr["g"] % NGB
                    pctr["g"] += 1
                    c.swdma(gbuf[k][:], tabv[:, :], reads=[r_idxT, r_tab], writes=[r_gbuf[k]],
                            indirect=bass.IndirectOffsetOnAxis(ap=idxT[:, t:t + 1], axis=0))
                    kc = pctr["c"] % NCB
                    pctr["c"] += 1
                    c.op("act", lambda e, t=t, kc=kc: e.activation(out=cbuf[kc][:, 127:128], in_=CT[:, t:t + 1],
                                                                   func=AF.Copy), reads=[r_CT], writes=[r_cbuf[kc]])

                    def fy(e, t=t, k=k, kc=kc):
                        inst = None
                        for q in range(4):
                            inst = e.matmul(pb[q][:P, :], lhsT=cbuf[kc][:, 127 - t:127 - t + P],
                                            rhs=gbuf[k][:, q * 512:(q + 1) * 512], start=(t == 0), stop=(t == nv - 1))
                        return inst
                    c.op("pe", fy, reads=[r_cbuf[kc], r_gbuf[k]], writes=[rpb[q] for q in range(4)])

            def epi():
                for q in range(4):
                    c.op("dve", lambda e, q=q: e.tensor_tensor(out=x2_t[:P, q * 512:(q + 1) * 512], in0=pb[q][:P, :],
                                                               in1=x2_t[:P, q * 512:(q + 1) * 512], op=ALU.add),
                         reads=[rpb[q], r_x2t], writes=[r_x2t])
                rms_h(x2_t, r_x2t, P, g_fin, r_gfin, yout, r_yout)
                c.dma("sp", y_dst, yout[:nv, :], reads=[r_yout])

            return sel_a, sel_b, pass1, pass2, epi

        def phase4_group(ntile, P, nv, x_src, mTg, r_mTg, cross_fn, y_dst_fn):
            ntok = ntile * P
            for j in range(ntile):
                if nv < P:
                    c.op("pool", lambda e, j=j: e.memset(x1[j][:], 0.0), writes=[r_x1[j]])
                c.dma("sp", x1[j][:nv, :], x_src(j), writes=[r_x1[j]])
            ws = WStream([(nm, b) for nm in ("w_out", "w_mq", "w_mo", "peer_wq") for b in range(4)])
            for blk in range(4):
                wt, r_w = ws.get()
                for j in range(ntile):
                    b = bank(2, 6)
                    mm_tok(mTg, r_mTg, j * P, P, wt, r_w, b)
                    c.op("dve", lambda e, j=j, blk=blk, b=b: e.tensor_tensor(
                        out=x1[j][:P, blk * 512:(blk + 1) * 512], in0=pb[b][:P, :],
                        in1=x1[j][:P, blk * 512:(blk + 1) * 512], op=ALU.add), reads=[rpb[b], r_x1[j]], writes=[r_x1[j]])
            for j in range(ntile):
                rms_h(x1[j], r_x1[j], P, g_mem, r_gmem, hb4, r_hb4)
                transpose_to(hb4, r_hb4, P, 16, lambda c0, c1, j=j: bufA[:, c0:c1, j * P:(j + 1) * P], r_bufA)
            for blk in range(4):
                wt, r_w = ws.get()
                for ct in range(4):
                    b = bank(2, 6)
                    mm_feat(bufA, r_bufA, 0, ntok, wt, r_w, ct, b)
                    c.op("act", lambda e, blk=blk, ct=ct, b=b: e.activation(
                        out=bufB[:, blk * 4 + ct, 0:ntok], in_=pb[b][:, 0:ntok], func=AF.Copy),
                        reads=[rpb[b]], writes=[r_bufB])
            cross_fn(bufB, r_bufB, bufA, r_bufA)
            for blk in range(4):
                wt, r_w = ws.get()
                for j in range(ntile):
                    b = bank(2, 6)
                    mm_tok(bufA, r_bufA, j * P, P, wt, r_w, b)
                    c.op("dve", lambda e, j=j, blk=blk, b=b: e.tensor_tensor(
                        out=x1[j][:P, blk * 512:(blk + 1) * 512], in0=pb[b][:P, :],
                        in1=x1[j][:P, blk * 512:(blk + 1) * 512], op=ALU.add), reads=[rpb[b], r_x1[j]], writes=[r_x1[j]])
            for j in range(ntile):
                rms_h(x1[j], r_x1[j], P, g_peer, r_gpeer, h2[j], r_h2[j])
                transpose_to(h2[j], r_h2[j], P, 16, lambda c0, c1, j=j: bufB[:, c0:c1, j * P:(j + 1) * P], r_bufB)
            for blk in range(4):
                wt, r_w = ws.get()
                for ct in range(4):
                    b = bank(2, 6)
                    mm_feat(bufB, r_bufB, 0, ntok, wt, r_w, ct, b)
                    c.op("act", lambda e, blk=blk, ct=ct, b=b: e.activation(
                        out=bufA[:, blk * 4 + ct, 0:ntok], in_=pb[b][:, 0:ntok], func=AF.Copy),
                        reads=[rpb[b]], writes=[r_bufA])
            parts = [peer_parts(P, nv, bufA, r_bufA, j * P, h2[j], r_h2[j], x1[j], r_x1[j], y_dst_fn(j), j % 2)
                     for j in range(ntile)]
            parts[0][0]()
            parts[0][1]()
            parts[0][2]()
            for j in range(ntile):
                if j + 1 < ntile:
                    parts[j + 1][0]()
                parts[j][3]()
                if j + 1 < ntile:
                    parts[j + 1][1]()
                parts[j][4]()
                if j + 1 < ntile:
                    parts[j + 1][2]()

        mTg = c.sb(es4, "mTg", [128, 16, GT], BF16)
        r_mTg = Res()
        for g in range((NT // G) if 5 in _PH else 0)[:_LIM["groups5"]]:
            c.dma("sp", mTg[:], mTs[:, :, g * GT:(g + 1) * GT].rearrange("k p t -> p k t"), reads=r_mTs,
                  writes=[r_mTg])
            phase4_group(
                G, 128, 128, lambda j, g=g: xo[g * GT + j * 128:g * GT + (j + 1) * 128, :], mTg, r_mTg,
                lambda qT_, r_q, oT_, r_o: cross_attn(qT_, r_q, 0, GT, mkT, [r_mkT],
                                                      lambda mt, c0: mv_bf[:, mt, c0:c0 + 128], [r_mvbf], oT_, r_o),
                lambda j, g=g: y_p[g * GT + j * 128:g * GT + (j + 1) * 128, :])

        def cross_sample(qT_, r_q, oT_, r_o):
            stage = ((sc, r_sc), (cand, r_cand))
            n = 0
            for b in range(4):
                for kv in range(2):
                    for mt in range(2):
                        st, r_st = stage[n % 2]
                        n += 1
                        c.dma("sp", st[:], cmem[b, mt * 128:(mt + 1) * 128, kv, :], writes=[r_st])
                        k = kv * 2 + mt
                        c.op("pool", lambda e, st=st, k=k: e.tensor_copy(out=gbuf[k][:], in_=st[:]), reads=[r_st],
                             writes=[r_gbuf[k]])
                for mt in range(2):
                    transpose_to(gbuf[mt], r_gbuf[mt], 128, 16,
                                 lambda c0, c1, mt=mt: mTg[:, c0:c1, mt * 128:(mt + 1) * 128], r_mTg)
                cross_attn(qT_, r_q, b * 8, 8, mTg, [r_mTg], lambda mt, c0: gbuf[2 + mt][:, c0:c0 + 128],
                           [r_gbuf[2], r_gbuf[3]], oT_, r_o)

        if 6 in _PH:
            phase4_group(1, 128, SP, lambda j: xs[:, :], mTg_s, r_mTgs, cross_sample, lambda j: y_s[:, :])

        c.finish()
    return nc


def _host_tables(rel_bias):
    biasT = np.zeros((24, 128, 2, 128), np.float32)
    jl = np.arange(128)[:, None, None]
    jt = np.arange(2)[None, :, None]
    i = np.arange(128)[None, None, :]
    off = i + 128 - (jt * 128 + jl)
    band = ((off >= 0) & (off <= 128)).astype(np.float32)
    offc = np.clip(off, 0, 128)
    for g, dil in enumerate(DILS):
        bucket = _t5_bucket(dil * np.arange(129))
        for h in range(8):
            biasT[g * 8 + h] = rel_bias[bucket[offc], g * 8 + h]
    sbias = np.zeros((8, 128, 16, 24), np.float32)
    smask = np.zeros((128, 16, 24), np.float32)
    sbias_n = np.zeros((8, 32, 4, 24), np.float32)
    smask_n = np.zeros((32, 4, 24), np.float32)
    for g, dil in enumerate(DILS):
        bucket = _t5_bucket(dil * np.arange(129))
        for t in range(8):
            for j in range(129):
                row = 2048 + t - dil * j
                col = g * 8 + t
                if row < 2048:
                    smask[row % 128, row // 128, col] = 1.0
                    sbias[:, row % 128, row // 128, col] = rel_bias[bucket[j], g * 8:(g + 1) * 8]
                else:
                    tp = row - 2048
                    for b in range(4):
                        smask_n[b * 8 + tp, b, col] = 1.0
                        sbias_n[:, b * 8 + tp, b, col] = rel_bias[bucket[j], g * 8:(g + 1) * 8]
    return biasT, band, sbias, smask, sbias_n, smask_n


_NC_CACHE = {}


def _prepare(x_prompt, x_sample, mem_prompt, cache_win, cache_mem_kv, rel_bias, norm_mix, w_in,
             sgu_ln_g, sgu_ln_b, sgu_w, sgu_b, w_out, norm_mem, norm_memtok, w_mq, w_mk, w_mv, w_mo,
             norm_peer, peer_wq, peer_keys1, peer_keys2, peer_u, peer_v, norm_final):
    f = lambda a: np.ascontiguousarray(np.asarray(a, dtype=np.float32))
    xp = f(x_prompt)[0]
    xsm = f(x_sample)
    rel_bias = f(rel_bias)
    biasT, band, sbias, smask, sbias_n, smask_n = _host_tables(rel_bias)
    sgu_w0 = f(sgu_w)[0]
    sgu_wT = np.ascontiguousarray(sgu_w0.transpose(0, 2, 1))
    sgu_wTs = np.zeros((8, 128, 128), np.float32)
    for b in range(4):
        sgu_wTs[:, b * 8:(b + 1) * 8, b * 8:(b + 1) * 8] = sgu_wT[:, :8, :8]
    sgu_b0 = f(sgu_b)[0]
    sgu_bT = np.ascontiguousarray(sgu_b0.T)
    sgu_bTs = np.zeros((128, 8), np.float32)
    sgu_bTs[:32] = np.tile(sgu_b0[:, :8].T, (4, 1))
    shared = {
        "mem": f(mem_prompt)[0], "w_in": f(w_in)[0], "w_out": f(w_out)[0], "w_mq": f(w_mq)[0],
        "w_mk": f(w_mk)[0], "w_mv": f(w_mv)[0], "w_mo": f(w_mo)[0], "peer_wq": f(peer_wq)[0],
        "peer_u": f(peer_u)[0], "peer_v": f(peer_v)[0], "keys1": f(peer_keys1)[0], "keys2": f(peer_keys2)[0],
        "norm_mix": f(norm_mix), "norm_mem": f(norm_mem), "norm_memtok": f(norm_memtok),
        "norm_peer": f(norm_peer), "norm_final": f(norm_final).reshape(1, D),
        "sgu_ln_g": f(sgu_ln_g), "sgu_ln_b": f(sgu_ln_b), "sgu_wT": sgu_wT, "sgu_wTs": sgu_wTs,
        "sgu_bT": sgu_bT, "sgu_bTs": sgu_bTs, "biasT": biasT, "bandmask": band, "sbias": sbias,
        "smask": smask, "sbias_n": sbias_n, "smask_n": smask_n,
    }
    cw = f(cache_win)[0]
    cm = f(cache_mem_kv)[0].reshape(32, 256, 2, 2048)
    in_maps = []
    for cidx in range(NCORES):
        m = dict(shared)
        m["xo"] = xp[cidx * TOK:(cidx + 1) * TOK]
        m["xh"] = xp[(cidx - 1) * TOK:cidx * TOK] if cidx > 0 else np.zeros((TOK, D), np.float32)
        m["xs"] = np.ascontiguousarray(xsm[cidx * 4:(cidx + 1) * 4].reshape(SP, D))
        m["cwin"] = cw[cidx * 4:(cidx + 1) * 4]
        m["cmem"] = cm[cidx * 4:(cidx + 1) * 4]
        m["pflag"] = np.full((128, 1), 0.0 if cidx == 0 else 1.0, np.float32)
        in_maps.append(m)
    return in_maps


def kernel(**inputs):
    in_maps = _prepare(**inputs)
    if "nc" not in _NC_CACHE:
        _NC_CACHE["nc"] = build_program()
    ncr = _LIM.get("ncores") or NCORES
    res = run_bass_kernel_spmd(_NC_CACHE["nc"], in_maps[:ncr], core_ids=list(range(ncr)))
    R = list(res.results)
    while len(R) < NCORES:
        R.append(R[0])
    y_prompt = np.concatenate([R[i]["y_p"] for i in range(NCORES)], axis=0).reshape(1, NCORES * TOK, D)
    y_sample = np.concatenate([R[i]["y_s"] for i in range(NCORES)], axis=0).reshape(32, 8, D)
    win_p = R[NCORES - 1]["win_p"].reshape(1, 1, TOK, 2, 8, 128)
    memkv = R[0]["memkv"].reshape(1, 1, 256, 2, 4, 512)
    win_s = np.concatenate([R[i]["win_s"] for i in range(NCORES)], axis=0).reshape(1, 32, 8, 2, 8, 128)
    sgu_s = np.concatenate([R[i]["sgu_s"] for i in range(NCORES)], axis=0).reshape(1, 32, 8, 1024)
    return (y_prompt, y_sample, win_p, memkv, win_s, sgu_s)
```
